# Optimizing a Trainium2 kernel written in Bass

```python
import math
import jax, jax.numpy as jnp
from jax import lax
import numpy as np

D_MODEL = 1024
BATCH = 8
SEQ = 4096
DEPTH = 2

HEAD_DIM = 64
N_MLSTM_HEADS = 4
N_DIL_HEADS = 6
N_SB_HEADS = 6
N_HEADS = N_MLSTM_HEADS + N_DIL_HEADS + N_SB_HEADS
D_MIX = N_HEADS * HEAD_DIM
D_ML = N_MLSTM_HEADS * HEAD_DIM
D_DIL = N_DIL_HEADS * HEAD_DIM
D_SB = N_SB_HEADS * HEAD_DIM
CONV_WIDTH = 4
MLSTM_CHUNK = 128
FGATE_BIAS_RANGE = (3.0, 6.0)
DIL_PATTERNS = ((128, 1), (512, 4), (2048, 16))
DIL_BLOCK = 128
ALIBI_MAX_BIAS = 8.0
SB_BLOCK = 128
N_EXPERTS = 16
N_GROUPS = 4
EXPERTS_PER_GROUP = N_EXPERTS // N_GROUPS
TOP_K = 2
D_EXPERT = 512
N_MOD = 6
EPS = 1e-6

OFF_ML_X = 0
OFF_ML_O = OFF_ML_X + D_ML
OFF_ML_I = OFF_ML_O + D_ML
OFF_ML_F = OFF_ML_I + N_MLSTM_HEADS
OFF_DIL = OFF_ML_F + N_MLSTM_HEADS
OFF_SB = OFF_DIL + 3 * D_DIL
D_IN_PROJ = OFF_SB + 3 * D_SB

kernel_name = 'hybrid_mlstm_dilated_stickbreaking_moe'


def rms_norm(x):
    xf = x.astype(jnp.float32)
    return xf * lax.rsqrt(jnp.mean(xf * xf, axis=-1, keepdims=True) + EPS)


def split_heads(t, n_heads):
    b, s, _ = t.shape
    return t.reshape(b, s, n_heads, HEAD_DIM).transpose(0, 2, 1, 3).astype(jnp.float32)


def mlstm_chunkwise(q, k, v, ig, lf):
    b, h, s, dh = q.shape
    nc = s // MLSTM_CHUNK

    def chunks(t):
        return jnp.moveaxis(t.reshape(b, h, nc, MLSTM_CHUNK, *t.shape[3:]), 2, 0)

    causal = jnp.tril(jnp.ones((MLSTM_CHUNK, MLSTM_CHUNK), dtype=bool))

    def step(carry, xs):
        c_st, n_st, m_st = carry
        qc, kc, vc, igc, lfc = xs
        cum = jnp.cumsum(lfc, axis=-1)
        log_d = jnp.where(causal, cum[..., :, None] - cum[..., None, :] + igc[..., None, :], -jnp.inf)
        log_inter = cum + m_st[..., None]
        m_t = jnp.maximum(log_inter, jnp.max(log_d, axis=-1))
        dmat = jnp.exp(log_d - m_t[..., None])
        inter = jnp.exp(log_inter - m_t)
        sw = jnp.einsum('bhtd,bhsd->bhts', qc, kc) * dmat
        num = inter[..., None] * jnp.einsum('bhtd,bhde->bhte', qc, c_st) + jnp.einsum('bhts,bhse->bhte', sw, vc)
        den = inter * jnp.einsum('bhtd,bhd->bht', qc, n_st) + jnp.sum(sw, axis=-1)
        h_out = num / jnp.maximum(jnp.abs(den), jnp.exp(-m_t))[..., None]
        last = cum[..., -1]
        log_w = last[..., None] - cum + igc
        m_new = jnp.maximum(last + m_st, jnp.max(log_w, axis=-1))
        w = jnp.exp(log_w - m_new[..., None])
        decay = jnp.exp(last + m_st - m_new)
        c_new = decay[..., None, None] * c_st + jnp.einsum('bhs,bhsd,bhse->bhde', w, kc, vc)
        n_new = decay[..., None] * n_st + jnp.einsum('bhs,bhsd->bhd', w, kc)
        return (c_new, n_new, m_new), h_out

    init = (jnp.zeros((b, h, dh, dh), jnp.float32), jnp.zeros((b, h, dh), jnp.float32),
            jnp.zeros((b, h), jnp.float32))
    _, hs = lax.scan(step, init, (chunks(q), chunks(k), chunks(v), chunks(ig), chunks(lf)))
    return jnp.moveaxis(hs, 0, 2).reshape(b, h, s, dh)


def mlstm_mixer(u, conv_w, conv_b, w_mq, w_mk, w_mv, gate_bias):
    xm = u[..., OFF_ML_X:OFF_ML_X + D_ML]
    xc = lax.conv_general_dilated(xm, conv_w[:, None, :].astype(xm.dtype), (1,), [(CONV_WIDTH - 1, 0)],
                                  dimension_numbers=('NWC', 'WIO', 'NWC'), feature_group_count=D_ML)
    xc = jax.nn.silu(xc + conv_b)
    xc_h = split_heads(xc, N_MLSTM_HEADS)
    q = jnp.einsum('bhsd,hde->bhse', xc_h, w_mq).astype(jnp.float32)
    k = (jnp.einsum('bhsd,hde->bhse', xc_h, w_mk) * HEAD_DIM ** -0.5).astype(jnp.float32)
    v = jnp.einsum('bhsd,hde->bhse', split_heads(xm, N_MLSTM_HEADS), w_mv).astype(jnp.float32)
    ig = (u[..., OFF_ML_I:OFF_ML_I + N_MLSTM_HEADS] + gate_bias[0]).astype(jnp.float32).transpose(0, 2, 1)
    lf = jax.nn.log_sigmoid((u[..., OFF_ML_F:OFF_ML_F + N_MLSTM_HEADS] + gate_bias[1]).astype(jnp.float32)).transpose(0, 2, 1)
    o = jax.nn.sigmoid(split_heads(u[..., OFF_ML_O:OFF_ML_O + D_ML], N_MLSTM_HEADS))
    return o * mlstm_chunkwise(q, k, v, ig, lf)


def dilated_pattern(q, k, v, slopes, window, dilation):
    b, h, s, dh = q.shape
    length = s // dilation
    span = window // dilation
    bq = math.gcd(length, DIL_BLOCK)
    nblk = length // bq

    def by_residue(t):
        return t.reshape(b, h, length, dilation, dh).transpose(0, 1, 3, 2, 4)

    pad = ((0, 0), (0, 0), (0, 0), (span, 0), (0, 0))
    qb = by_residue(q).reshape(b, h, dilation, nblk, bq, dh)
    kp = jnp.pad(by_residue(k), pad)
    vp = jnp.pad(by_residue(v), pad)
    key_idx = jnp.arange(nblk)[:, None] * bq + jnp.arange(bq + span)[None, :]
    kb = kp[:, :, :, key_idx, :]
    vb = vp[:, :, :, key_idx, :]
    steps = jnp.arange(bq)[:, None] - jnp.arange(bq + span)[None, :] + span
    key_pos = key_idx[:, None, :] - span
    valid = (steps >= 0) & (steps <= span) & (key_pos >= 0)
    scores = jnp.einsum('bhrnqd,bhrnkd->bhrnqk', qb, kb) * HEAD_DIM ** -0.5
    scores = scores - slopes[None, :, None, None, None, None] * (steps * dilation).astype(jnp.float32)
    scores = jnp.where(valid, scores, -jnp.inf)
    mx = jnp.max(scores, axis=-1, keepdims=True)
    p = jnp.exp(scores - mx)
    den = jnp.sum(p, axis=-1)
    out = jnp.einsum('bhrnqk,bhrnkd->bhrnqd', p, vb) / den[..., None]
    lse = mx[..., 0] + jnp.log(den)
    out = out.reshape(b, h, dilation, length, dh).transpose(0, 1, 3, 2, 4).reshape(b, h, s, dh)
    lse = lse.reshape(b, h, dilation, length).transpose(0, 1, 3, 2).reshape(b, h, s)
    return out, lse


def dilated_mixer(u):
    q = split_heads(u[..., OFF_DIL:OFF_DIL + D_DIL], N_DIL_HEADS)
    k = split_heads(u[..., OFF_DIL + D_DIL:OFF_DIL + 2 * D_DIL], N_DIL_HEADS)
    v = split_heads(u[..., OFF_DIL + 2 * D_DIL:OFF_DIL + 3 * D_DIL], N_DIL_HEADS)
    slopes = jnp.exp2(-ALIBI_MAX_BIAS * jnp.arange(1, N_DIL_HEADS + 1, dtype=jnp.float32) / N_DIL_HEADS)
    outs, lses = [], []
    for window, dilation in DIL_PATTERNS:
        o, l = dilated_pattern(q, k, v, slopes, window, dilation)
        outs.append(o)
        lses.append(l)
    weights = jax.nn.softmax(jnp.stack(lses), axis=0)
    return jnp.einsum('pbhs,pbhsd->bhsd', weights, jnp.stack(outs))


def stick_breaking_mixer(u):
    q = split_heads(u[..., OFF_SB:OFF_SB + D_SB], N_SB_HEADS)
    k = split_heads(u[..., OFF_SB + D_SB:OFF_SB + 2 * D_SB], N_SB_HEADS)
    v = split_heads(u[..., OFF_SB + 2 * D_SB:OFF_SB + 3 * D_SB], N_SB_HEADS)
    b, h, s, dh = q.shape
    nblk = s // SB_BLOCK
    qb = jnp.moveaxis(q.reshape(b, h, nblk, SB_BLOCK, dh), 2, 0)
    key_pos = jnp.arange(s)

    def block(args):
        qblk, bi = args
        z = jnp.einsum('bhqd,bhkd->bhqk', qblk, k) * HEAD_DIM ** -0.5
        qpos = bi * SB_BLOCK + jnp.arange(SB_BLOCK)
        causal = key_pos[None, :] < qpos[:, None]
        log_keep = jnp.where(causal, jax.nn.log_sigmoid(-z), 0.0)
        log_rest = lax.cumsum(log_keep, axis=3, reverse=True) - log_keep
        a = jnp.where(causal, jnp.exp(jax.nn.log_sigmoid(z) + log_rest), 0.0)
        return jnp.einsum('bhqk,bhkd->bhqd', a, v)

    ob = lax.map(block, (qb, jnp.arange(nblk)))
    return jnp.moveaxis(ob, 0, 2).reshape(b, h, s, dh)


def routed_moe(h, w_router, router_bias, w_gate_e, w_up_e, w_down_e):
    b, s, d = h.shape
    t = h.reshape(b * s, d)
    scores = jax.nn.sigmoid((t @ w_router).astype(jnp.float32))
    grouped = (scores + router_bias).reshape(-1, N_GROUPS, EXPERTS_PER_GROUP)
    group_score = jnp.sum(lax.top_k(grouped, TOP_K)[0], axis=-1)
    _, g_sel = lax.top_k(group_score, 1)
    in_group = jnp.take_along_axis(grouped, g_sel[:, :, None], axis=1)[:, 0]
    _, e_loc = lax.top_k(in_group, TOP_K)
    e_idx = g_sel * EXPERTS_PER_GROUP + e_loc
    w = jnp.take_along_axis(scores, e_idx, axis=1)
    w = w / jnp.sum(w, axis=-1, keepdims=True)
    combine = jnp.einsum('tk,tke->te', w, jax.nn.one_hot(e_idx, N_EXPERTS, dtype=jnp.float32))
    y = jnp.zeros((b * s, d), jnp.float32)
    for e in range(N_EXPERTS):
        he = jax.nn.silu(t @ w_gate_e[e]) * (t @ w_up_e[e])
        y = y + combine[:, e:e + 1] * (he @ w_down_e[e])
    return y.reshape(b, s, d)


def setup_inputs(seed: int = 0) -> dict:
    key = jax.random.key(seed)
    ks = jax.random.split(key, 20)
    f32 = jnp.float32

    def nrm(k, shape, scale):
        return jax.random.normal(k, shape, f32) * scale

    fgate = jnp.linspace(FGATE_BIAS_RANGE[0], FGATE_BIAS_RANGE[1], N_MLSTM_HEADS, dtype=f32)
    gate_bias = jnp.stack([nrm(ks[8], (DEPTH, N_MLSTM_HEADS), 0.1),
                           fgate[None, :] + nrm(ks[9], (DEPTH, N_MLSTM_HEADS), 0.1)], axis=1)
    return {
        'x': nrm(ks[0], (BATCH, SEQ, D_MODEL), 1.0),
        'c': nrm(ks[1], (BATCH, D_MODEL), 1.0),
        'w_in': nrm(ks[2], (DEPTH, D_MODEL, D_IN_PROJ), D_MODEL ** -0.5),
        'conv_w': nrm(ks[3], (DEPTH, CONV_WIDTH, D_ML), CONV_WIDTH ** -0.5),
        'conv_b': nrm(ks[4], (DEPTH, D_ML), 0.02),
        'w_mq': nrm(ks[5], (DEPTH, N_MLSTM_HEADS, HEAD_DIM, HEAD_DIM), HEAD_DIM ** -0.5),
        'w_mk': nrm(ks[6], (DEPTH, N_MLSTM_HEADS, HEAD_DIM, HEAD_DIM), HEAD_DIM ** -0.5),
        'w_mv': nrm(ks[7], (DEPTH, N_MLSTM_HEADS, HEAD_DIM, HEAD_DIM), HEAD_DIM ** -0.5),
        'gate_bias': gate_bias,
        'g_head': 1.0 + nrm(ks[10], (DEPTH, D_MIX), 0.02),
        'w_out': nrm(ks[11], (DEPTH, D_MIX, D_MODEL), D_MIX ** -0.5),
        'w_ada': nrm(ks[12], (DEPTH, D_MODEL, N_MOD * D_MODEL), 0.5 * D_MODEL ** -0.5),
        'b_ada': nrm(ks[13], (DEPTH, N_MOD * D_MODEL), 0.02),
        'w_router': nrm(ks[14], (D_MODEL, N_EXPERTS), D_MODEL ** -0.5),
        'router_bias': nrm(ks[15], (N_EXPERTS,), 0.01),
        'w_gate_e': nrm(ks[16], (DEPTH, N_EXPERTS, D_MODEL, D_EXPERT), D_MODEL ** -0.5),
        'w_up_e': nrm(ks[17], (DEPTH, N_EXPERTS, D_MODEL, D_EXPERT), D_MODEL ** -0.5),
        'w_down_e': nrm(ks[18], (DEPTH, N_EXPERTS, D_EXPERT, D_MODEL), D_EXPERT ** -0.5),
        'g_final': 1.0 + nrm(ks[19], (D_MODEL,), 0.02),
    }


def reference(x, c, w_in, conv_w, conv_b, w_mq, w_mk, w_mv, gate_bias, g_head, w_out,
              w_ada, b_ada, w_router, router_bias, w_gate_e, w_up_e, w_down_e, g_final):
    b, s, _ = x.shape
    c_act = jax.nn.silu(c.astype(jnp.float32))
    for l in range(DEPTH):
        mod = c_act @ w_ada[l] + b_ada[l]
        shift1, scale1, gate1, shift2, scale2, gate2 = [m[:, None, :] for m in jnp.split(mod, N_MOD, axis=-1)]
        h = rms_norm(x) * (1.0 + scale1) + shift1
        u = h @ w_in[l]
        y_ml = mlstm_mixer(u, conv_w[l], conv_b[l], w_mq[l], w_mk[l], w_mv[l], gate_bias[l])
        y_dil = dilated_mixer(u)
        y_sb = stick_breaking_mixer(u)
        y = jnp.concatenate([y_ml, y_dil, y_sb], axis=1)
        y = rms_norm(y) * g_head[l].reshape(N_HEADS, 1, HEAD_DIM)
        y = y.transpose(0, 2, 1, 3).reshape(b, s, D_MIX) @ w_out[l]
        x = x + gate1 * y
        h = rms_norm(x) * (1.0 + scale2) + shift2
        x = x + gate2 * routed_moe(h, w_router, router_bias, w_gate_e[l], w_up_e[l], w_down_e[l])
    return rms_norm(x) * g_final
```

```python
import math
import numpy as np
from contextlib import ExitStack
import concourse.bass as bass
import concourse.mybir as mybir
from concourse.bass_utils import run_bass_kernel_spmd

F32 = mybir.dt.float32
BF16 = mybir.dt.bfloat16
AF = mybir.ActivationFunctionType
ALU = mybir.AluOpType
AX = mybir.AxisListType

SEQ = 4096
D = 1024
DEPTH = 2
NT = SEQ // 128
NSB = SEQ // 512
DIN = 2824
NEXP = 16
DEXP = 512
EPS = 1e-6
OFF_O = 256
OFF_DIL = 520
OFF_SB = 520 + 1152
DIL_PATTERNS = ((128, 1), (512, 4), (2048, 16))


class Buf:
    __slots__ = ("name", "w", "r", "excl")

    def __init__(self, name, excl=False):
        self.name = name
        self.w = None
        self.r = []
        self.excl = excl


class _Eng:
    def __init__(self, name, e, sem):
        self.name = name
        self.e = e
        self.sem = sem
        self.cnt = 0
        self.known = {}


class Sched:
    def __init__(self, nc, n_dma_sems=32):
        self.nc = nc
        self.sems = []
        self.eng = {}
        for name, e in (("pe", nc.tensor), ("act", nc.scalar), ("dve", nc.vector),
                        ("pool", nc.gpsimd), ("sp", nc.sync)):
            sem = nc.semaphore("s_" + name).__enter__()
            self.sems.append(sem)
            self.eng[name] = _Eng(name, e, len(self.sems) - 1)
        self.dma_sems = []
        for i in range(n_dma_sems):
            sem = nc.semaphore("s_dma%d" % i).__enter__()
            self.sems.append(sem)
            self.dma_sems.append([len(self.sems) - 1, 0])
        self.dma_rr = 0
        self.ninstr = 0

    def _deps(self, R, W):
        deps = {}

        def add(t):
            if t is None:
                return
            s, v = t
            if deps.get(s, 0) < v:
                deps[s] = v
        for b in R:
            add(b.w)
            if b.excl:
                for t in b.r:
                    add(t)
        for b in W:
            add(b.w)
            for t in b.r:
                add(t)
        return deps

    def _wait(self, E, deps):
        for s, v in deps.items():
            if E.known.get(s, 0) >= v:
                continue
            E.e.wait_ge(self.sems[s], v)
            E.known[s] = v
            self.ninstr += 1

    def _mark(self, R, W, ticket):
        for b in R:
            if b.excl:
                b.w = ticket
                b.r = []
            else:
                b.r.append(ticket)
                if len(b.r) > 32:
                    m = {}
                    for s, v in b.r:
                        if m.get(s, 0) < v:
                            m[s] = v
                    b.r = list(m.items())
        for b in W:
            b.w = ticket
            b.r = []

    def op(self, eng, fns, R=(), W=()):
        E = self.eng[eng]
        if not isinstance(fns, (list, tuple)):
            fns = [fns]
        self._wait(E, self._deps(R, W))
        ins = None
        for f in fns:
            ins = f(E.e)
            self.ninstr += 1
        E.cnt += 1
        ins.then_inc(self.sems[E.sem], 1)
        t = (E.sem, E.cnt)
        self._mark(R, W, t)
        return t

    def dma(self, eng, out, in_, R=(), W=()):
        E = self.eng[eng]
        slot = self.dma_sems[self.dma_rr]
        self.dma_rr = (self.dma_rr + 1) % len(self.dma_sems)
        s, v = slot
        deps = self._deps(R, W)
        if v > 0 and deps.get(s, 0) < v:
            deps[s] = v
        self._wait(E, deps)
        ins = E.e.dma_start(out=out, in_=in_)
        slot[1] = v + 16
        ins.then_inc(self.sems[s], 16)
        self.ninstr += 1
        t = (s, v + 16)
        self._mark(R, W, t)
        return t

    def wait_all(self, eng, bufs):
        E = self.eng[eng]
        self._wait(E, self._deps(bufs, bufs))

    def barrier(self):
        tot = {}
        for E in self.eng.values():
            if E.cnt:
                tot[E.sem] = E.cnt
        for s, v in self.dma_sems:
            if v:
                tot[s] = v
        for E in self.eng.values():
            self._wait(E, dict(tot))


CB = {}
CF = {}


def _layout(table, items):
    off = 0
    for name, w in items:
        table[name] = (off, w)
        off += w
    return off


NCB = _layout(CB, [("ident", 128), ("ones", 128), ("negtri", 128), ("negones", 128),
                   ("mstrict", 128), ("triu", 128), ("onesA", 128), ("onesB", 128),
                   ("blk64", 128), ("zeros", 128), ("dilE", 18 * 256)])
NCF = _layout(CF, [("ident", 128), ("ones", 128), ("triu", 128), ("blk64", 128), ("selA", 128), ("selB", 128)])


def make_consts():
    p = np.arange(128)[:, None].astype(np.float64)
    f = np.arange(128)[None, :].astype(np.float64)
    cb = np.zeros((128, NCB), np.float32)
    cf = np.zeros((128, NCF), np.float32)

    def put(tab, lay, name, val):
        o, w = lay[name]
        tab[:, o:o + w] = val
    ident = (p == f).astype(np.float32)
    ones = np.ones((128, 128), np.float32)
    triu = (p <= f).astype(np.float32)
    blk64 = ((p // 64) == (f // 64)).astype(np.float32)
    put(cb, CB, "ident", ident)
    put(cb, CB, "ones", ones)
    put(cb, CB, "negtri", -(p >= f).astype(np.float32))
    put(cb, CB, "negones", -ones)
    put(cb, CB, "mstrict", (p < f).astype(np.float32))
    put(cb, CB, "triu", triu)
    put(cb, CB, "onesA", (f < 64).astype(np.float32) * ones)
    put(cb, CB, "onesB", (f >= 64).astype(np.float32) * ones)
    put(cb, CB, "blk64", blk64)
    mk = np.arange(128)[:, None].astype(np.float64)
    mq = np.arange(256)[None, :].astype(np.float64)
    dlt = mq - mk
    valid = (dlt >= 0) & (dlt <= 128)
    o, _ = CB["dilE"]
    for h in range(6):
        slope = 2.0 ** (-8.0 * (h + 1) / 6.0)
        for pi, (win, dil) in enumerate(DIL_PATTERNS):
            e = np.where(valid, np.exp(-slope * dil * dlt), 0.0)
            ix = ((h // 2) * 3 + pi) * 2 + (h % 2)
            cb[:, o + ix * 256: o + (ix + 1) * 256] = e
    put(cf, CF, "ident", ident)
    put(cf, CF, "ones", ones)
    put(cf, CF, "triu", triu)
    put(cf, CF, "blk64", blk64)
    selA = np.zeros((128, 128), np.float32); selA[64, 0:64] = 1.0
    selB = np.zeros((128, 128), np.float32); selB[0, 64:128] = 1.0
    put(cf, CF, "selA", selA)
    put(cf, CF, "selB", selB)
    return cb, cf


ML_CUT = [99]
MOE_DBG = [0]
CO_EVERY = [1]
CO_SKIP = [10 ** 9]
SB_DEPTH = [2, 3]


def build_program(stop_after=None, debug=()):
    nc = bass.Bass("TRN2", target_bir_lowering=False)
    S = Sched(nc)
    dbg = {}

    def din(name, shape, dt=F32):
        return nc.dram_tensor(name, list(shape), dt, kind="ExternalInput").ap()

    def dscr(name, shape, dt):
        return nc.dram_tensor(name, list(shape), dt, kind="Internal").ap()

    x_in = din("x", [SEQ, D])
    c_lay = din("c_lay", [128, 8])
    w_in = din("w_in", [DEPTH, D, DIN])
    conv_w = din("conv_w", [DEPTH, 128, 2, 4])
    conv_b = din("conv_b", [DEPTH, 128, 2])
    w_mq = din("w_mq", [DEPTH, 4, 64, 64])
    w_mk = din("w_mk", [DEPTH, 4, 64, 64])
    w_mv = din("w_mv", [DEPTH, 4, 64, 64])
    gbias = din("gbias", [DEPTH, 128, 8])
    g_head_p = din("g_head_p", [DEPTH, 128, 8])
    g_head_r = din("g_head_r", [DEPTH, 128, 256])
    w_out = din("w_out", [DEPTH, D, D])
    w_ada = din("w_ada", [DEPTH, D, 6 * D])
    b_ada = din("b_ada", [DEPTH, 1, 6 * D])
    w_router = din("w_router", [D, NEXP])
    rbias = din("rbias", [128, NEXP])
    w_gate = din("w_gate_e", [DEPTH, NEXP, D, DEXP])
    w_up = din("w_up_e", [DEPTH, NEXP, D, DEXP])
    w_down = din("w_down_e", [DEPTH, NEXP, DEXP, D])
    g_final = din("g_final_r", [128, D])
    cb_in = din("cb", [128, NCB])
    cf_in = din("cf", [128, NCF])
    y_out = nc.dram_tensor("y", [SEQ, D], F32, kind="ExternalOutput").ap()

    xA = dscr("xA", [SEQ, D], F32)
    xB = dscr("xB", [SEQ, D], F32)
    xmT_d = dscr("xmT", [256, SEQ], BF16)
    og_d = dscr("og", [SEQ, 256], BF16)
    gates_d = dscr("gates", [SEQ, 8], F32)
    dqT_d = dscr("dqT", [384, SEQ], BF16)
    dkT_d = dscr("dkT", [384, SEQ], BF16)
    dv_d = dscr("dv", [SEQ, 384], BF16)
    sqT_d = dscr("sqT", [384, SEQ], BF16)
    skT_d = dscr("skT", [384, SEQ], BF16)
    sv_d = dscr("sv", [SEQ, 384], BF16)
    B_xA, B_xB = Buf("xA"), Buf("xB")
    B_scr = {n: Buf(n) for n in ("xmT", "og", "gates", "dqT", "dkT", "dv", "sqT", "skT", "sv")}

    for name in debug:
        pass

    def sbt(name, shape, dt):
        return nc.alloc_sbuf_tensor(name, list(shape), dt)

    cb = sbt("cb_sb", [128, NCB], BF16)
    cf = sbt("cf_sb", [128, NCF], F32)
    B_cb, B_cf = Buf("cb"), Buf("cf")
    S.dma("pool", cb[:], cb_in[:, :], W=[B_cb])
    S.dma("sp", cf[:], cf_in[:, :], W=[B_cf])

    def cbs(name, rows=slice(0, 128)):
        o, w = CB[name]
        return cb[rows, o:o + w]

    def cfs(name, rows=slice(0, 128)):
        o, w = CF[name]
        return cf[rows, o:o + w]

    pp = [nc.alloc_psum_tensor("pp%d" % i, [128, 1024], F32) for i in range(4)]
    ps = [pp[i // 2][:, (i % 2) * 512:(i % 2 + 1) * 512] for i in range(8)]
    PP = pp
    B_ps = [Buf("ps%d" % i, excl=True) for i in range(8)]

    mod = sbt("mod", [128, 6, D], F32)
    B_mod = Buf("mod")
    B_crep = Buf("c_rep")
    c_sb = sbt("c_sb", [128, 8], F32)
    B_c = Buf("c_sb")
    S.dma("sp", c_sb[:], c_lay[:, :], W=[B_c])
    S.op("act", lambda e: e.activation(out=c_sb[:], in_=c_sb[:], func=AF.Silu), R=[B_c], W=[B_c])

    out_tensors = {}

    def dbg_out(name, shape, dt=F32):
        t = nc.dram_tensor("dbg_" + name, list(shape), dt, kind="ExternalOutput").ap()
        out_tensors[name] = t
        return t

    fin_bufs = []

    def phase_mod(l):
        with ExitStack() as st:
            c_rep = st.enter_context(nc.sbuf_tensor("c_rep_L%d" % l, [128, 8, 128], F32))
            S.op("dve", lambda e: e.tensor_copy(out=c_rep[:], in_=c_sb[:].unsqueeze(2).to_broadcast([128, 8, 128])),
                 R=[B_c], W=[B_crep])
            wt = [st.enter_context(nc.sbuf_tensor("wada%d_L%d" % (i, l), [128, 8, 512], F32)) for i in range(2)]
            br = [st.enter_context(nc.sbuf_tensor("brow%d_L%d" % (i, l), [1, 512], F32)) for i in range(2)]
            B_wt = [Buf("wada%d" % i) for i in range(2)]
            B_br = [Buf("brow%d" % i) for i in range(2)]
            for blk in range(12):
                i = blk % 2
                S.dma("sp", wt[i][:], w_ada[l, :, blk * 512:(blk + 1) * 512].rearrange("(k p) n -> p k n", p=128),
                      W=[B_wt[i]])
                S.dma("sp", br[i][:], b_ada[l, :, blk * 512:(blk + 1) * 512], W=[B_br[i]])
                pb = blk % 2
                fns = []
                for k in range(8):
                    fns.append(lambda e, k=k, i=i, pb=pb: e.matmul(ps[pb][:], lhsT=c_rep[:, k, :], rhs=wt[i][:, k, :],
                                                                   start=(k == 0), stop=False))
                fns.append(lambda e, i=i, pb=pb: e.matmul(ps[pb][:], lhsT=cfs("ones", slice(0, 1)), rhs=br[i][:],
                                                          start=False, stop=True))
                S.op("pe", fns, R=[B_wt[i], B_br[i], B_crep, B_cf], W=[B_ps[pb]])
                m = blk // 2
                dst = mod[:, m, (blk % 2) * 512:(blk % 2 + 1) * 512]
                if m in (1, 4):
                    S.op("dve", lambda e, dst=dst, pb=pb: e.tensor_scalar(out=dst, in0=ps[pb][:], scalar1=1.0, scalar2=None,
                                                                         op0=ALU.add), R=[B_ps[pb]], W=[B_mod])
                else:
                    S.op("dve", lambda e, dst=dst, pb=pb: e.tensor_copy(out=dst, in_=ps[pb][:]), R=[B_ps[pb]], W=[B_mod])
            S.barrier()

    def norm_tile(xt, B_xt, sidx, tmp, B_tmp, hout, B_hout, ss, B_ss):
        S.op("act", lambda e: e.activation(out=tmp[:], in_=xt[:], func=AF.Square, accum_out=ss[:, 0:1]),
             R=[B_xt], W=[B_tmp, B_ss])
        S.op("act", lambda e: e.activation(out=ss[:, 1:2], in_=ss[:, 0:1], func=AF.Ln, scale=1.0 / D, bias=EPS),
             R=[B_ss], W=[B_ss])
        S.op("act", lambda e: e.activation(out=ss[:, 2:3], in_=ss[:, 1:2], func=AF.Exp, scale=-0.5),
             R=[B_ss], W=[B_ss])
        S.op("dve", lambda e: e.scalar_tensor_tensor(out=tmp[:], in0=xt[:], scalar=ss[:, 2:3], in1=mod[:, sidx + 1, :],
                                                     op0=ALU.mult, op1=ALU.mult), R=[B_xt, B_ss, B_mod], W=[B_tmp])
        S.op("dve", lambda e: e.tensor_tensor(out=hout[:], in0=tmp[:], in1=mod[:, sidx, :], op=ALU.add),
             R=[B_tmp, B_mod], W=[B_hout])

    def load_win(l, st):
        win = st.enter_context(nc.sbuf_tensor("win_L%d" % l, [128, 8, DIN], BF16))
        B_win = Buf("win")
        for k in range(8):
            S.dma("pool", win[:, k, :], w_in[l, k * 128:(k + 1) * 128, :], W=[B_win])
        return win, B_win

    def phase_inproj(l, x_src, B_xsrc, win, B_win):
        with ExitStack() as st:
            def T(name, shape, dt):
                return st.enter_context(nc.sbuf_tensor(name + "_L%d" % l, list(shape), dt))
            xt = [T("xt%d" % i, [128, D], F32) for i in range(2)]
            B_xt = [Buf("xt%d" % i) for i in range(2)]
            tmp = [T("ntmp%d" % i, [128, D], F32) for i in range(2)]
            B_tmp = [Buf("ntmp%d" % i) for i in range(2)]
            hb = [T("hb%d" % i, [128, D], BF16) for i in range(2)]
            B_hb = [Buf("hb%d" % i) for i in range(2)]
            ss = [T("ss%d" % i, [128, 4], F32) for i in range(2)]
            B_ss = [Buf("ss%d" % i) for i in range(2)]
            hT = [T("hT%d" % i, [128, 8, 512], BF16) for i in range(2)]
            B_hT = [Buf("hT%d" % i) for i in range(2)]
            stF = {n: [T("st_%s%d" % (n, i), [128, w, 512], BF16) for i in range(2)]
                   for n, w in (("xmT", 2), ("dqT", 3), ("dkT", 3), ("sqT", 3), ("skT", 3))}
            B_stF = {n: [Buf("st_%s%d" % (n, i)) for i in range(2)] for n in stF}
            st_og = [T("st_og%d" % i, [128, 4, 256], BF16) for i in range(2)]
            st_g = [T("st_g%d" % i, [128, 4, 8], F32) for i in range(2)]
            st_dv = [T("st_dv%d" % i, [128, 4, 384], BF16) for i in range(2)]
            st_sv = [T("st_sv%d" % i, [128, 4, 384], BF16) for i in range(2)]
            B_og = [Buf("st_og%d" % i) for i in range(2)]
            B_g = [Buf("st_g%d" % i) for i in range(2)]
            B_dv = [Buf("st_dv%d" % i) for i in range(2)]
            B_sv = [Buf("st_sv%d" % i) for i in range(2)]
            fm_specs = [("xmT", 0, 2, 1.0, xmT_d), ("dqT", OFF_DIL, 3, 1.0, dqT_d), ("dkT", OFF_DIL + 384, 3, 0.125, dkT_d),
                        ("sqT", OFF_SB, 3, 1.0, sqT_d), ("skT", OFF_SB + 384, 3, 0.125, skT_d)]
            pcount = [0]

            def next_ps():
                pcount[0] += 1
                return 2 + (pcount[0] % 6)

            def NA(sb, tt):
                ti = sb * 4 + tt
                i = ti % 2
                S.dma("sp", xt[i][:], x_src[ti * 128:(ti + 1) * 128, :], R=[B_xsrc], W=[B_xt[i]])
                norm_tile(xt[i], B_xt[i], 0, tmp[i], B_tmp[i], hb[i], B_hb[i], ss[i], B_ss[i])

            def NB(sb, tt):
                ti = sb * 4 + tt
                i = ti % 2
                sl = sb % 2
                pb = ti % 2
                pT = ps[pb][:].bitcast(BF16)
                S.op("pe", [lambda e, c=c: e.transpose(out=pT[:, c * 128:(c + 1) * 128], in_=hb[i][:, c * 128:(c + 1) * 128],
                                                       identity=cbs("ident")) for c in range(8)],
                     R=[B_hb[i], B_cb], W=[B_ps[pb]])
                S.op("act", lambda e: e.copy(out=hT[sl][:, :, tt * 128:(tt + 1) * 128], in_=pT.rearrange("p (c t) -> p c t", c=8)),
                     R=[B_ps[pb]], W=[B_hT[sl]])

            def M_items(sb):
                sl = sb % 2
                items = []
                for (n, off, nch, scale, dst) in fm_specs:
                    for c in range(nch):
                        def it(n=n, off=off, nch=nch, scale=scale, dst=dst, c=c):
                            pb = next_ps()
                            S.op("pe", [lambda e, k=k: e.matmul(ps[pb][:], lhsT=win[:, k, off + c * 128: off + (c + 1) * 128],
                                                                rhs=hT[sl][:, k, :], start=(k == 0), stop=(k == 7)) for k in range(8)],
                                 R=[B_win, B_hT[sl]], W=[B_ps[pb]])
                            S.op("act", lambda e: e.activation(out=stF[n][sl][:, c, :], in_=ps[pb][:], func=AF.Copy, scale=scale),
                                 R=[B_ps[pb]], W=[B_stF[n][sl]])
                            if c == nch - 1:
                                S.dma("sp", dst[:, sb * 512:(sb + 1) * 512].rearrange("(c p) t -> p c t", p=128), stF[n][sl][:],
                                      R=[B_stF[n][sl]], W=[B_scr[n]])
                        items.append(it)
                for tt in range(4):
                    def it(tt=tt):
                        pb = next_ps()
                        S.op("pe", [lambda e, k=k: e.matmul(ps[pb][:, 0:264], lhsT=hT[sl][:, k, tt * 128:(tt + 1) * 128],
                                                            rhs=win[:, k, OFF_O:OFF_O + 264], start=(k == 0), stop=(k == 7))
                                    for k in range(8)], R=[B_win, B_hT[sl]], W=[B_ps[pb]])
                        S.op("act", lambda e: e.activation(out=st_og[sl][:, tt, :], in_=ps[pb][:, 0:256], func=AF.Copy),
                             R=[B_ps[pb]], W=[B_og[sl]])
                        S.op("dve", lambda e: e.tensor_copy(out=st_g[sl][:, tt, :], in_=ps[pb][:, 256:264]), R=[B_ps[pb]], W=[B_g[sl]])
                    items.append(it)
                    for (stt, Bst, off) in ((st_dv, B_dv, OFF_DIL + 768), (st_sv, B_sv, OFF_SB + 768)):
                        def it(tt=tt, stt=stt, Bst=Bst, off=off):
                            pb = next_ps()
                            S.op("pe", [lambda e, k=k: e.matmul(ps[pb][:, 0:384], lhsT=hT[sl][:, k, tt * 128:(tt + 1) * 128],
                                                                rhs=win[:, k, off:off + 384], start=(k == 0), stop=(k == 7))
                                        for k in range(8)], R=[B_win, B_hT[sl]], W=[B_ps[pb]])
                            S.op("dve", lambda e: e.tensor_copy(out=stt[sl][:, tt, :], in_=ps[pb][:, 0:384]), R=[B_ps[pb]], W=[Bst[sl]])
                        items.append(it)

                def fin():
                    r0, r1 = sb * 512, (sb + 1) * 512
                    S.dma("sp", og_d[r0:r1, :].rearrange("(t p) c -> p t c", p=128), st_og[sl][:], R=[B_og[sl]], W=[B_scr["og"]])
                    S.dma("sp", gates_d[r0:r1, :].rearrange("(t p) c -> p t c", p=128), st_g[sl][:], R=[B_g[sl]], W=[B_scr["gates"]])
                    S.dma("sp", dv_d[r0:r1, :].rearrange("(t p) c -> p t c", p=128), st_dv[sl][:], R=[B_dv[sl]], W=[B_scr["dv"]])
                    S.dma("sp", sv_d[r0:r1, :].rearrange("(t p) c -> p t c", p=128), st_sv[sl][:], R=[B_sv[sl]], W=[B_scr["sv"]])
                return items, fin

            for tt in range(4):
                NA(0, tt)
                NB(0, tt)
            for sb in range(NSB):
                items, fin = M_items(sb)
                nper = (len(items) + 3) // 4
                for part in range(4):
                    if sb + 1 < NSB:
                        NA(sb + 1, part)
                    for it in items[part * nper:(part + 1) * nper]:
                        it()
                    if sb + 1 < NSB:
                        NB(sb + 1, part)
                fin()
            S.barrier()


    Y = {}
    B_ymix = Buf("ymixT")
    ghp = sbt("ghp", [128, DEPTH, 8], F32)
    B_ghp = Buf("ghp")
    for l_ in range(DEPTH):
        S.dma("sp", ghp[:, l_, :], g_head_p[l_, :, :], W=[B_ghp])

    def head_norm_fm(l, src_ap, B_src, chunk, col0, T, tag):
        sq, B_sq, rs, B_rs, pstat, B_pstat = T
        S.op("act", lambda e: e.activation(out=sq[:], in_=src_ap, func=AF.Square), R=[B_src], W=[B_sq])
        S.op("pe", lambda e: e.matmul(pstat[:], lhsT=cfs("blk64"), rhs=sq[:], start=True, stop=True),
             R=[B_sq, B_cf], W=[B_pstat])
        S.op("act", lambda e: e.activation(out=rs[:], in_=pstat[:], func=AF.Ln, scale=1.0 / 64, bias=EPS),
             R=[B_pstat], W=[B_rs])
        S.op("act", lambda e: e.activation(out=rs[:], in_=rs[:], func=AF.Exp, scale=-0.5), R=[B_rs], W=[B_rs])
        S.op("dve", lambda e: e.scalar_tensor_tensor(out=Y["t"][:, chunk, col0:col0 + 512], in0=src_ap,
                                                     scalar=ghp[:, l, chunk:chunk + 1], in1=rs[:],
                                                     op0=ALU.mult, op1=ALU.mult),
             R=[B_src, B_rs, B_ghp], W=[B_ymix])

    def phase_sb(l, co=None):
        with ExitStack() as st:
            def T(name, shape, dt):
                return st.enter_context(nc.sbuf_tensor(name + "_L%d" % l, list(shape), dt))
            qT = T("sb_qT", [128, SEQ], BF16)
            kT = T("sb_kT", [128, SEQ], BF16)
            V = [T("sb_V%d" % i, [128, NT, 128], BF16) for i in range(2)]
            B_qT, B_kT, B_V = Buf("sb_qT"), Buf("sb_kT"), [Buf("sb_V0"), Buf("sb_V1")]
            C32 = [T("sb_C32_%d" % p, [128, 2, 512], F32) for p in range(2)]
            Cb = [T("sb_Cb_%d" % p, [128, 2, 512], BF16) for p in range(2)]
            B_C32 = [Buf("c32_%d" % p) for p in range(2)]
            B_Cb = [Buf("cb_%d" % p) for p in range(2)]
            e_sb = T("sb_e", [128, 2, 512], F32)
            B_e = Buf("sb_e")
            NSP, NA = 3, 2
            sp_sb = [T("sb_sp%d" % i, [128, 2, 512], BF16) for i in range(NSP)]
            B_sp = [Buf("sb_sp%d" % i) for i in range(NSP)]
            A_sb = [T("sb_A%d" % i, [128, 2, 512], BF16) for i in range(NA)]
            B_A = [Buf("sb_A%d" % i) for i in range(NA)]
            sq = T("sb_sq", [128, 512], F32)
            rs = T("sb_rs", [128, 512], F32)
            normT = (sq, Buf("sb_sq"), rs, Buf("sb_rs"), ps[0], B_ps[0])
            zP = pp[0][:].rearrange("p (h q) -> p h q", h=2)
            xP = pp[1][:].rearrange("p (h q) -> p h q", h=2)
            mstr2 = cbs("mstrict").unsqueeze(1).to_broadcast([128, 2, 128])
            for i in range(2):
                S.op("dve", lambda e, i=i: e.memset(V[i][:], 0.0), W=[B_V[i]])
            for c in range(3):
                S.dma("sp", qT[:], sqT_d[c * 128:(c + 1) * 128, :], R=[B_scr["sqT"]], W=[B_qT])
                S.dma("sp", kT[:], skT_d[c * 128:(c + 1) * 128, :], R=[B_scr["skT"]], W=[B_kT])
                for h in range(2):
                    S.dma("sp", V[h][:, :, h * 64:(h + 1) * 64],
                          sv_d[:, c * 128 + h * 64: c * 128 + (h + 1) * 64].rearrange("(t p) e -> p t e", p=128),
                          R=[B_scr["sv"]], W=[B_V[h]])
                tiles = []
                for sb in range(NSB):
                    for j in range(4 * sb + 3, -1, -1):
                        tiles.append((sb, j))
                n = len(tiles)

                def geom(t):
                    sb, j = tiles[t]
                    c0 = (j - 4 * sb) * 128 if j >= 4 * sb else 0
                    return sb, j, c0, (j >= 4 * sb), (j == 4 * sb + 3), (j == 0)

                def S1(t):
                    sb, j, c0, diag, first, last = geom(t)
                    par = sb % 2
                    if first:
                        S.op("dve", lambda e: e.memset(C32[par][:], 0.0), W=[B_C32[par]])
                        ob = 6 + par
                        S.op("pe", lambda e: e.matmul(ps[ob][:], lhsT=cbs("zeros"), rhs=qT[:, 0:512], start=True, stop=False),
                             R=[B_cb, B_qT], W=[B_ps[ob]])
                    S.op("pe", [lambda e, h=h: e.matmul(ps[h][:, c0:512], lhsT=kT[h * 64:(h + 1) * 64, j * 128:(j + 1) * 128],
                                                        rhs=qT[h * 64:(h + 1) * 64, sb * 512 + c0:(sb + 1) * 512], start=True, stop=True)
                                for h in range(2)], R=[B_kT, B_qT], W=[B_ps[0], B_ps[1]])

                def S2a(t):
                    sb, j, c0, diag, first, last = geom(t)
                    S.op("act", lambda e: e.activation(out=e_sb[:, :, c0:512], in_=zP[:, :, c0:512], func=AF.Exp),
                         R=[B_ps[0], B_ps[1]], W=[B_e])

                def S2(t):
                    sb, j, c0, diag, first, last = geom(t)
                    k = t % NSP
                    S.op("act", lambda e: e.activation(out=sp_sb[k][:, :, c0:512], in_=e_sb[:, :, c0:512], func=AF.Ln, bias=1.0),
                         R=[B_e], W=[B_sp[k]])
                    if diag:
                        S.op("dve", lambda e: e.tensor_tensor(out=sp_sb[k][:, :, c0:c0 + 128], in0=sp_sb[k][:, :, c0:c0 + 128],
                                                              in1=mstr2, op=ALU.mult), R=[B_sp[k], B_cb], W=[B_sp[k]])

                def S3(t):
                    sb, j, c0, diag, first, last = geom(t)
                    k = t % NSP
                    par = sb % 2
                    fns = []
                    for h in range(2):
                        r = slice(h * 64, (h + 1) * 64)
                        pb = 2 + h
                        fns.append(lambda e, r=r, pb=pb: e.matmul(ps[pb][:, c0:512], lhsT=kT[r, j * 128:(j + 1) * 128],
                                                                  rhs=qT[r, sb * 512 + c0:(sb + 1) * 512], start=True, stop=False))
                        fns.append(lambda e, h=h, pb=pb: e.matmul(ps[pb][:, c0:512], lhsT=cbs("negtri"), rhs=sp_sb[k][:, h, c0:512],
                                                                  start=False, stop=first))
                        if not first:
                            chi = C32[par][:, h, :].bitcast(BF16)[:, 2 * c0 + 1:1024:2]
                            fns.append(lambda e, chi=chi, pb=pb: e.matmul(ps[pb][:, c0:512], lhsT=cbs("negones"), rhs=chi,
                                                                          start=False, stop=True))
                    S.op("pe", fns, R=[B_kT, B_qT, B_cb, B_sp[k], B_C32[par]], W=[B_ps[2], B_ps[3]])

                def S4(t):
                    sb, j, c0, diag, first, last = geom(t)
                    a = t % NA
                    S.op("act", lambda e: e.activation(out=A_sb[a][:, :, c0:512], in_=xP[:, :, c0:512], func=AF.Exp),
                         R=[B_ps[2], B_ps[3]], W=[B_A[a]])
                    if diag:
                        S.op("dve", lambda e: e.tensor_tensor(out=A_sb[a][:, :, c0:c0 + 128], in0=A_sb[a][:, :, c0:c0 + 128],
                                                              in1=mstr2, op=ALU.mult), R=[B_A[a], B_cb], W=[B_A[a]])

                def S6(t):
                    sb, j, c0, diag, first, last = geom(t)
                    if last:
                        return
                    k = t % NSP
                    par = sb % 2
                    S.op("dve", lambda e: e.tensor_tensor(out=C32[par][:, :, c0:512], in0=C32[par][:, :, c0:512],
                                                          in1=sp_sb[k][:, :, c0:512], op=ALU.add),
                         R=[B_sp[k]], W=[B_C32[par]])


                def S5(t):
                    sb, j, c0, diag, first, last = geom(t)
                    a = t % NA
                    ob = 6 + (sb % 2)
                    S.op("pe", [lambda e, h=h: e.matmul(ps[ob][:, c0:512], lhsT=V[h][:, j, :], rhs=A_sb[a][:, h, c0:512],
                                                        start=False, stop=False) for h in range(2)],
                         R=[B_V[0], B_V[1], B_A[a]], W=[B_ps[ob]])
                    if last:
                        head_norm_fm(l, ps[ob][:], B_ps[ob], 5 + c, sb * 512, normT, "sb")

                for tau in range(n + 2):
                    if tau < n:
                        S1(tau)
                        S2a(tau)
                        S2(tau)
                    if 0 <= tau - 1 < n:
                        S3(tau - 1)
                        S4(tau - 1)
                        S6(tau - 1)
                    if 0 <= tau - 2 < n:
                        S5(tau - 2)
                    if co is not None and co[0] is not None and (tau % CO_SKIP[0] != CO_SKIP[0] - 1):
                        if next(co[0], "end") in ("hold", "end"):
                            co[0] = None
            if co is not None:
                while co[0] is not None:
                    if next(co[0], "end") in ("hold", "end"):
                        co[0] = None
            S.barrier()

    def phase_dil(l):
        with ExitStack() as st:
            def T(name, shape, dt):
                return st.enter_context(nc.sbuf_tensor(name + "_L%d" % l, list(shape), dt))
            qT = T("dl_qT", [128, SEQ], BF16)
            kT = T("dl_kT", [128, SEQ], BF16)
            Vs = [[T("dl_V%d_%d" % (i, pp), [128, NT, 128], BF16) for i in range(2)] for pp in range(2)]
            B_Vs = [[Buf("dl_V%d_%d" % (i, pp)) for i in range(2)] for pp in range(2)]
            B_qT, B_kT = Buf("dl_qT"), Buf("dl_kT")
            accn = T("dl_accn", [128, SEQ], F32)
            accd = T("dl_accd", [128, SEQ], F32)
            B_accn, B_accd = Buf("accn"), Buf("accd")
            NP_ = 8
            P_sb = [T("dl_P%d" % i, [128, 2, 128], BF16) for i in range(NP_)]
            B_P = [Buf("dl_P%d" % i) for i in range(NP_)]
            rsA = [T("dl_rsA%d" % i, [128, 512], F32) for i in range(4)]
            B_rsA = [Buf("dl_rsA%d" % i) for i in range(4)]
            sq2 = [T("dl_sq%d" % i, [128, 512], F32) for i in range(2)]
            B_sq2 = [Buf("dl_sq%d" % i) for i in range(2)]
            rsB = [T("dl_rsB%d" % i, [128, 512], F32) for i in range(2)]
            B_rsB = [Buf("dl_rsB%d" % i) for i in range(2)]
            for pp in range(2):
                for i in range(2):
                    S.op("dve", lambda e, i=i, pp=pp: e.memset(Vs[pp][i][:], 0.0), W=[B_Vs[pp][i]])
                S.op("dve", lambda e, pp=pp: e.memset(Vs[pp][0][:, :, 64:65], 1.0), W=[B_Vs[pp][0]])
                S.op("dve", lambda e, pp=pp: e.memset(Vs[pp][1][:, :, 0:1], 1.0), W=[B_Vs[pp][1]])
            eo, _ = CB["dilE"]
            vcnt = [0]

            def load_V(c, pi):
                win_, d_ = DIL_PATTERNS[pi]
                nb_ = (SEQ // d_) // 128
                pp = vcnt[0] % 2
                vcnt[0] += 1
                for h in range(2):
                    src = dv_d[:, c * 128 + h * 64: c * 128 + (h + 1) * 64].rearrange("(j i r) e -> i r j e", i=128, r=d_)
                    dst = Vs[pp][h][:, :, h * 64:(h + 1) * 64].rearrange("p (r j) e -> p r j e", r=d_)
                    if d_ <= nb_:
                        for r0 in range(d_):
                            S.dma("sp", dst[:, r0, :, :], src[:, r0, :, :], R=[B_scr["dv"]], W=[B_Vs[pp][h]])
                    else:
                        for j0 in range(nb_):
                            S.dma("sp", dst[:, :, j0, :], src[:, :, j0, :], R=[B_scr["dv"]], W=[B_Vs[pp][h]])
                return pp
            pending = {}
            pending[(0, 0)] = load_V(0, 0)
            for c in range(3):
                S.dma("sp", qT[:], dqT_d[c * 128:(c + 1) * 128, :], R=[B_scr["dqT"]], W=[B_qT])
                S.dma("sp", kT[:], dkT_d[c * 128:(c + 1) * 128, :], R=[B_scr["dkT"]], W=[B_kT])
                grp = [0]
                for pi, (win, d) in enumerate(DIL_PATTERNS):
                    L = SEQ // d
                    nblk = L // 128
                    pp_cur = pending.pop((c, pi))
                    V = Vs[pp_cur]
                    B_V = B_Vs[pp_cur]
                    nxt = (c, pi + 1) if pi + 1 < 3 else ((c + 1, 0) if c + 1 < 3 else None)
                    if nxt is not None:
                        pending[nxt] = load_V(*nxt)
                    gsz = min(4, nblk)
                    tiles = []
                    for r in range(d):
                        for g in range(nblk // gsz):
                            for nloc in range(gsz):
                                nq = g * gsz + nloc
                                for j in (nq - 1, nq):
                                    if j >= 0:
                                        tiles.append((r, g, nloc, nq, j))
                    n = len(tiles)
                    gpar = {}

                    def tok(r, blk):
                        a = r + d * 128 * blk
                        return slice(a, a + d * 127 + 1, d) if d > 1 else slice(a, a + 128)

                    def S1(t):
                        r, g, nloc, nq, j = tiles[t]
                        first = (nloc == 0 and j == max(nq - 1, 0))
                        if first:
                            gp = (grp[0] % 2)
                            pn, pd = 4 + 2 * gp, 5 + 2 * gp
                            grp[0] += 1
                            N = gsz * 128
                            S.op("dve", lambda e: e.memset(ps[pn][:, 0:N], 0.0), W=[B_ps[pn]])
                            S.op("dve", lambda e: e.memset(ps[pd][:, 0:N], 0.0), W=[B_ps[pd]])
                        gpar[t] = (grp[0] - 1) % 2
                        zb = 2 * (t % 2)
                        S.op("pe", [lambda e, h=h: e.matmul(ps[zb + h][:, 0:128], lhsT=kT[h * 64:(h + 1) * 64, tok(r, j)],
                                                            rhs=qT[h * 64:(h + 1) * 64, tok(r, nq)], start=True, stop=True)
                                    for h in range(2)], R=[B_kT, B_qT], W=[B_ps[zb], B_ps[zb + 1]])

                    def S2(t):
                        r, g, nloc, nq, j = tiles[t]
                        zb = 2 * (t % 2)
                        pq = t % NP_
                        zv = PP[t % 2][:].rearrange("p (h q) -> p h q", h=2)[:, :, 0:128]
                        S.op("act", lambda e: e.activation(out=P_sb[pq][:], in_=zv, func=AF.Exp),
                             R=[B_ps[zb], B_ps[zb + 1]], W=[B_P[pq]])
                        e0 = eo + ((c * 3 + pi) * 2) * 256
                        off = 0 if j == nq else 128
                        ev = cb[:, e0:e0 + 512].rearrange("p (h x) -> p h x", h=2)[:, :, off:off + 128]
                        S.op("dve", lambda e: e.tensor_tensor(out=P_sb[pq][:], in0=P_sb[pq][:], in1=ev, op=ALU.mult),
                             R=[B_P[pq], B_cb], W=[B_P[pq]])

                    def S3(t):
                        r, g, nloc, nq, j = tiles[t]
                        gp = gpar[t]
                        pa = t % NP_
                        pn, pd = 4 + 2 * gp, 5 + 2 * gp
                        cs = slice(nloc * 128, (nloc + 1) * 128)
                        S.op("pe", [lambda e, h=h: e.matmul(ps[(pn, pd)[h]][:, cs], lhsT=V[h][:, r * nblk + j, :], rhs=P_sb[pa][:, h, :],
                                                            start=False, stop=False) for h in range(2)],
                             R=[B_V[0], B_V[1], B_P[pa]], W=[B_ps[pn], B_ps[pd]])
                        last = (nloc == gsz - 1 and j == nq)
                        if last:
                            N = gsz * 128
                            a = r + d * 128 * gsz * g
                            dst = slice(a, a + d * (N - 1) + 1, d) if d > 1 else slice(a, a + N)
                            if pi == 0:
                                S.op("dve", lambda e: e.tensor_copy(out=accn[:, dst], in_=ps[pn][:, 0:N]), R=[B_ps[pn]], W=[B_accn])
                                S.op("dve", lambda e: e.tensor_copy(out=accd[:, dst], in_=ps[pd][:, 0:N]), R=[B_ps[pd]], W=[B_accd])
                            else:
                                S.op("dve", lambda e: e.tensor_tensor(out=accn[:, dst], in0=accn[:, dst], in1=ps[pn][:, 0:N], op=ALU.add),
                                     R=[B_ps[pn]], W=[B_accn])
                                S.op("dve", lambda e: e.tensor_tensor(out=accd[:, dst], in0=accd[:, dst], in1=ps[pd][:, 0:N], op=ALU.add),
                                     R=[B_ps[pd]], W=[B_accd])

                    DEP = 3
                    for tau in range(n + DEP):
                        if tau < n:
                            S1(tau)
                            S2(tau)
                        if 0 <= tau - DEP < n:
                            S3(tau - DEP)
                def NA(sb):
                    cs = slice(sb * 512, (sb + 1) * 512)
                    pb = sb % 4
                    S.op("pe", [lambda e: e.matmul(ps[pb][:], lhsT=cfs("selA"), rhs=accn[:, cs], start=True, stop=False),
                                lambda e: e.matmul(ps[pb][:], lhsT=cfs("selB"), rhs=accd[:, cs], start=False, stop=True)],
                         R=[B_accn, B_accd, B_cf], W=[B_ps[pb]])
                    S.op("act", lambda e: e.activation(out=rsA[pb][:], in_=ps[pb][:], func=AF.Ln), R=[B_ps[pb]], W=[B_rsA[pb]])
                    S.op("act", lambda e: e.activation(out=rsA[pb][:], in_=rsA[pb][:], func=AF.Exp, scale=-1.0), R=[B_rsA[pb]], W=[B_rsA[pb]])

                def NB_(sb):
                    cs = slice(sb * 512, (sb + 1) * 512)
                    pb = sb % 4
                    S.op("dve", lambda e: e.tensor_tensor(out=accn[0:64, cs], in0=accn[0:64, cs], in1=rsA[pb][0:64, :], op=ALU.mult),
                         R=[B_rsA[pb]], W=[B_accn])
                    S.op("dve", lambda e: e.tensor_tensor(out=accn[64:128, cs], in0=accd[64:128, cs], in1=rsA[pb][64:128, :], op=ALU.mult),
                         R=[B_rsA[pb], B_accd], W=[B_accn])

                def NC(sb):
                    cs = slice(sb * 512, (sb + 1) * 512)
                    i_ = sb % 2
                    pb = 4 + i_
                    S.op("act", lambda e: e.activation(out=sq2[i_][:], in_=accn[:, cs], func=AF.Square), R=[B_accn], W=[B_sq2[i_]])
                    S.op("pe", lambda e: e.matmul(ps[pb][:], lhsT=cfs("blk64"), rhs=sq2[i_][:], start=True, stop=True),
                         R=[B_sq2[i_], B_cf], W=[B_ps[pb]])
                    S.op("act", lambda e: e.activation(out=rsB[i_][:], in_=ps[pb][:], func=AF.Ln, scale=1.0 / 64, bias=EPS),
                         R=[B_ps[pb]], W=[B_rsB[i_]])
                    S.op("act", lambda e: e.activation(out=rsB[i_][:], in_=rsB[i_][:], func=AF.Exp, scale=-0.5), R=[B_rsB[i_]], W=[B_rsB[i_]])

                def ND(sb):
                    cs = slice(sb * 512, (sb + 1) * 512)
                    i_ = sb % 2
                    S.op("dve", lambda e: e.scalar_tensor_tensor(out=Y["t"][:, 2 + c, cs], in0=accn[:, cs], scalar=ghp[:, l, 2 + c:3 + c],
                                                                 in1=rsB[i_][:], op0=ALU.mult, op1=ALU.mult),
                         R=[B_accn, B_rsB[i_], B_ghp], W=[B_ymix])

                for st_ in range(NSB + 3):
                    if st_ < NSB:
                        NA(st_)
                    if 0 <= st_ - 1 < NSB:
                        NB_(st_ - 1)
                    if 0 <= st_ - 2 < NSB:
                        NC(st_ - 2)
                    if 0 <= st_ - 3 < NSB:
                        ND(st_ - 3)
            S.barrier()


    def gen_mlstm(l, bk):
        with ExitStack() as st:
            def T(name, shape, dt):
                return st.enter_context(nc.sbuf_tensor(name + "_L%d" % l, list(shape), dt))
            cw = T("ml_cw", [128, 2, 4], F32)
            cbi = T("ml_cb", [128, 2], F32)
            ncbi = T("ml_ncb", [128, 2], F32)
            gb = T("ml_gb", [128, 8], F32)
            ghr = T("ml_ghr", [128, 256], F32)
            B_small = Buf("ml_small")
            S.dma("sp", cw[:], conv_w[l, :, :, :], W=[B_small])
            S.dma("sp", cbi[:], conv_b[l, :, :], W=[B_small])
            S.dma("sp", gb[:], gbias[l, :, :], W=[B_small])
            S.dma("sp", ghr[:], g_head_r[l, :, :], W=[B_small])
            S.op("dve", lambda e: e.tensor_scalar(out=ncbi[:], in0=cbi[:], scalar1=-1.0, scalar2=None, op0=ALU.mult),
                 R=[B_small], W=[B_small])
            Wbd = {}
            B_W = Buf("ml_W")
            for nm, src in (("q", w_mq), ("k", w_mk), ("v", w_mv)):
                Wbd[nm] = T("ml_W" + nm, [128, 2, 128], BF16)
                S.op("dve", lambda e, nm=nm: e.memset(Wbd[nm][:], 0.0), W=[B_W])
                for c in range(2):
                    for hh in range(2):
                        S.dma("pool", Wbd[nm][hh * 64:(hh + 1) * 64, c, hh * 64:(hh + 1) * 64], src[l, 2 * c + hh, :, :], W=[B_W])
            graw = T("ml_graw", [128, NT, 8], F32)
            B_g = Buf("ml_graw")
            S.dma("sp", graw[:], gates_d[:, :].rearrange("(t p) c -> p t c", p=128), R=[B_scr["gates"]], W=[B_g])
            S.op("dve", lambda e: e.tensor_tensor(out=graw[:], in0=graw[:], in1=gb[:].unsqueeze(1).to_broadcast([128, NT, 8]),
                                                  op=ALU.add), R=[B_small], W=[B_g])
            nl = T("ml_nl", [128, NT, 4], F32)
            a_t = T("ml_a", [128, NT, 4], F32)
            b_t = T("ml_b", [128, NT, 4], F32)
            bl_t = T("ml_bl", [128, NT, 4], F32)
            B_nl, B_a, B_b, B_bl = Buf("nl"), Buf("a"), Buf("b"), Buf("bl")
            S.op("act", lambda e: e.activation(out=nl[:], in_=graw[:, :, 4:8], func=AF.Exp, scale=-1.0), R=[B_g], W=[B_nl])
            S.op("act", lambda e: e.activation(out=nl[:], in_=nl[:], func=AF.Ln, bias=1.0), R=[B_nl], W=[B_nl])
            nlf = nl[:].rearrange("p t h -> p (t h)")
            S.op("pe", lambda e: e.matmul(ps[bk["c0"]][:, 0:128], lhsT=cfs("triu"), rhs=nlf, start=True, stop=True),
                 R=[B_nl, B_cf], W=[B_ps[bk["c0"]]])
            S.op("pe", lambda e: e.matmul(ps[bk["c1"]][:, 0:128], lhsT=cfs("ones"), rhs=nlf, start=True, stop=True),
                 R=[B_nl, B_cf], W=[B_ps[bk["c1"]]])
            pc = ps[bk["c0"]][:, 0:128].rearrange("p (t h) -> p t h", h=4)
            S.op("dve", lambda e: e.tensor_tensor(out=a_t[:], in0=graw[:, :, 0:4], in1=pc, op=ALU.add), R=[B_g, B_ps[bk["c0"]]], W=[B_a])
            S.op("act", lambda e: e.activation(out=a_t[:], in_=a_t[:], func=AF.Exp), R=[B_a], W=[B_a])
            S.op("act", lambda e: e.activation(out=b_t[:], in_=pc, func=AF.Exp, scale=-1.0), R=[B_ps[bk["c0"]]], W=[B_b])
            S.op("act", lambda e: e.activation(out=bl_t[:], in_=ps[bk["c1"]][:, 0:128].rearrange("p (t h) -> p t h", h=4), func=AF.Exp,
                                               scale=-1.0), R=[B_ps[bk["c1"]]], W=[B_bl])
            C32 = T("ml_C32", [128, 2, 65], F32)
            Cbf = T("ml_Cbf", [128, 2, 65], BF16)
            B_C32, B_Cbf = Buf("ml_C32"), Buf("ml_Cbf")
            S.op("dve", lambda e: e.memset(C32[:], 0.0), W=[B_C32])
            S.op("dve", lambda e: e.memset(Cbf[:], 0.0), W=[B_Cbf])
            xmp = [T("ml_xmp%d" % i, [128, 2, 515], BF16) for i in range(2)]
            B_xmp = [Buf("ml_xmp%d" % i) for i in range(2)]
            ogt = [T("ml_og%d" % i, [128, 4, 256], BF16) for i in range(2)]
            B_ogt = [Buf("ml_og%d" % i) for i in range(2)]
            acc = T("ml_acc", [128, 2, 512], F32)
            ez = T("ml_ez", [128, 2, 512], F32)
            xc = T("ml_xc", [128, 2, 512], BF16)
            B_acc, B_ez, B_xc = Buf("ml_acc"), Buf("ml_ez"), Buf("ml_xc")
            qTs = T("ml_qT", [128, 2, 512], BF16)
            kTs = T("ml_kT", [128, 2, 512], BF16)
            B_qTs, B_kTs = Buf("ml_qT"), Buf("ml_kT")
            ktok = T("ml_ktok", [128, 256], BF16)
            Vaug = T("ml_Vaug", [128, 4, 65], BF16)
            swm = T("ml_swm", [128, 4, 128], BF16)
            B_ktok, B_Vaug, B_swm = Buf("ml_ktok"), Buf("ml_Vaug"), Buf("ml_swm")
            sm = T("ml_sm", [128, 16], F32)
            B_sm = Buf("ml_sm")
            eo = T("ml_eo", [128, 256], F32)
            t1 = T("ml_t1", [128, 256], F32)
            ysq = T("ml_ysq", [128, 256], F32)
            yn = T("ml_yn", [128, 256], BF16)
            B_eo, B_t1, B_ysq, B_yn = Buf("ml_eo"), Buf("ml_t1"), Buf("ml_ysq"), Buf("ml_yn")
            ctmp = T("ml_ctmp", [128, 2, 65], F32)
            B_ctmp = Buf("ml_ctmp")

            yield
            for sb in range(NSB if ML_CUT[0] > 1 else 0):
                i2 = sb % 2
                xm = xmp[i2]
                if sb == 0:
                    S.op("dve", lambda e: e.memset(xm[:, :, 0:3], 0.0), W=[B_xmp[i2]])
                    S.dma("sp", xm[:, :, 3:515], xmT_d[:, 0:512].rearrange("(c p) t -> p c t", p=128), R=[B_scr["xmT"]], W=[B_xmp[i2]])
                else:
                    S.dma("sp", xm[:, :, 0:515], xmT_d[:, sb * 512 - 3:(sb + 1) * 512].rearrange("(c p) t -> p c t", p=128),
                          R=[B_scr["xmT"]], W=[B_xmp[i2]])
                S.dma("sp", ogt[i2][:], og_d[sb * 512:(sb + 1) * 512, :].rearrange("(t p) c -> p t c", p=128), R=[B_scr["og"]],
                      W=[B_ogt[i2]])
                yield
                for c in range(2):
                    S.op("dve", lambda e, c=c: e.tensor_scalar(out=acc[:, c, :], in0=xm[:, c, 0:512], scalar1=cw[:, c, 0:1], scalar2=None,
                                                               op0=ALU.mult), R=[B_xmp[i2], B_small], W=[B_acc])
                    for j in range(1, 4):
                        S.op("dve", lambda e, c=c, j=j: e.scalar_tensor_tensor(out=acc[:, c, :], in0=xm[:, c, j:j + 512],
                                                                               scalar=cw[:, c, j:j + 1], in1=acc[:, c, :],
                                                                               op0=ALU.mult, op1=ALU.add),
                             R=[B_xmp[i2], B_small], W=[B_acc])
                    S.op("act", lambda e, c=c: e.activation(out=ez[:, c, :], in_=acc[:, c, :], func=AF.Exp, scale=-1.0,
                                                            bias=ncbi[:, c:c + 1]), R=[B_acc, B_small], W=[B_ez])
                    S.op("dve", lambda e, c=c: e.tensor_scalar(out=ez[:, c, :], in0=ez[:, c, :], scalar1=1.0, scalar2=None, op0=ALU.add),
                         R=[B_ez], W=[B_ez])
                    S.op("dve", lambda e, c=c: e.reciprocal(out=ez[:, c, :], in_=ez[:, c, :]), R=[B_ez], W=[B_ez])
                    S.op("dve", lambda e, c=c: e.scalar_tensor_tensor(out=xc[:, c, :], in0=acc[:, c, :], scalar=cbi[:, c:c + 1],
                                                                      in1=ez[:, c, :], op0=ALU.add, op1=ALU.mult),
                         R=[B_acc, B_ez, B_small], W=[B_xc])
                yield
                for c in range(2):
                    S.op("pe", lambda e, c=c: e.matmul(ps[bk["q"]][:], lhsT=Wbd["q"][:, c, :], rhs=xc[:, c, :], start=True, stop=True),
                         R=[B_W, B_xc], W=[B_ps[bk["q"]]])
                    S.op("act", lambda e, c=c: e.copy(out=qTs[:, c, :], in_=ps[bk["q"]][:]), R=[B_ps[bk["q"]]], W=[B_qTs])
                    S.op("pe", lambda e, c=c: e.matmul(ps[bk["k"]][:], lhsT=Wbd["k"][:, c, :], rhs=xc[:, c, :], start=True, stop=True),
                         R=[B_W, B_xc], W=[B_ps[bk["k"]]])
                    S.op("act", lambda e, c=c: e.activation(out=kTs[:, c, :], in_=ps[bk["k"]][:], func=AF.Copy, scale=0.125),
                         R=[B_ps[bk["k"]]], W=[B_kTs])
                for i in range(4 if ML_CUT[0] > 2 else 0):
                    cut = ML_CUT[0]
                    ci = sb * 4 + i
                    ts = slice(i * 128, (i + 1) * 128)
                    yield
                    S.op("pe", [lambda e, c=c: e.matmul(ps[bk["kt"]][:, c * 128:(c + 1) * 128], lhsT=xc[:, c, ts], rhs=Wbd["k"][:, c, :],
                                                        start=True, stop=True) for c in range(2)],
                         R=[B_xc, B_W], W=[B_ps[bk["kt"]]])
                    S.op("act", lambda e: e.activation(out=ktok[:], in_=ps[bk["kt"]][:, 0:256], func=AF.Copy, scale=0.125),
                         R=[B_ps[bk["kt"]]], W=[B_ktok])
                    S.op("pe", [lambda e, c=c: e.matmul(ps[bk["vt"]][:, c * 128:(c + 1) * 128], lhsT=xm[:, c, 3 + i * 128:3 + (i + 1) * 128],
                                                        rhs=Wbd["v"][:, c, :], start=True, stop=True) for c in range(2)],
                         R=[B_xmp[i2], B_W], W=[B_ps[bk["vt"]]])
                    S.op("dve", lambda e: e.tensor_tensor(out=Vaug[:, :, 0:64], in0=ps[bk["vt"]][:, 0:256].rearrange("p (h e) -> p h e", h=4),
                                                          in1=a_t[:, ci, :].unsqueeze(2).to_broadcast([128, 4, 64]), op=ALU.mult),
                         R=[B_ps[bk["vt"]], B_a], W=[B_Vaug])
                    S.op("dve", lambda e: e.tensor_copy(out=Vaug[:, :, 64], in_=a_t[:, ci, :]), R=[B_a], W=[B_Vaug])
                    if cut <= 3:
                        continue
                    yield
                    sbank = (bk["S0"], bk["S1"])
                    S.op("pe", [lambda e, h=h: e.matmul(ps[sbank[h % 2]][:, (h // 2) * 128:(h // 2 + 1) * 128],
                                                        lhsT=kTs[(h % 2) * 64:(h % 2 + 1) * 64, h // 2, ts],
                                                        rhs=qTs[(h % 2) * 64:(h % 2 + 1) * 64, h // 2, ts], start=True, stop=True)
                                for h in range(4)], R=[B_kTs, B_qTs], W=[B_ps[bk["S0"]], B_ps[bk["S1"]]])
                    for hh in range(2):
                        S.op("dve", lambda e, hh=hh: e.tensor_tensor(
                            out=swm[:, hh::2, :], in0=ps[sbank[hh]][:, 0:256].rearrange("p (c t) -> p c t", c=2),
                            in1=cbs("triu").unsqueeze(1).to_broadcast([128, 2, 128]), op=ALU.mult),
                            R=[B_ps[sbank[hh]], B_cb], W=[B_swm])
                    if cut <= 4:
                        continue
                    yield
                    fns = []
                    for h in range(4):
                        rows = slice((h % 2) * 64, (h % 2 + 1) * 64)
                        fns.append(lambda e, h=h, rows=rows: e.matmul(ps[bk["H"]][:, h * 65:(h + 1) * 65], lhsT=qTs[rows, h // 2, ts],
                                                                      rhs=Cbf[rows, h // 2, :], start=True, stop=False))
                        fns.append(lambda e, h=h: e.matmul(ps[bk["H"]][:, h * 65:(h + 1) * 65], lhsT=swm[:, h, :], rhs=Vaug[:, h, :],
                                                           start=False, stop=True))
                    S.op("pe", fns, R=[B_qTs, B_Cbf, B_swm, B_Vaug], W=[B_ps[bk["H"]]])
                    if cut <= 5:
                        continue
                    yield
                    S.op("pe", [lambda e, c=c: e.matmul(ps[bk["dC"]][:, c * 130:(c + 1) * 130], lhsT=ktok[:, c * 128:(c + 1) * 128],
                                                        rhs=Vaug[:, 2 * c:2 * c + 2, :].rearrange("p h e -> p (h e)"),
                                                        start=True, stop=True) for c in range(2)],
                         R=[B_ktok, B_Vaug], W=[B_ps[bk["dC"]]])
                    for c in range(2):
                        for hh in range(2):
                            rows = slice(hh * 64, (hh + 1) * 64)
                            S.op("dve", lambda e, c=c, hh=hh, rows=rows: e.tensor_tensor(
                                out=ctmp[rows, c, :], in0=C32[rows, c, :], in1=ps[bk["dC"]][rows, c * 130 + hh * 65:c * 130 + (hh + 1) * 65],
                                op=ALU.add), R=[B_C32, B_ps[bk["dC"]]], W=[B_ctmp])
                            S.op("dve", lambda e, c=c, hh=hh, rows=rows: e.tensor_scalar(
                                out=C32[rows, c, :], in0=ctmp[rows, c, :], scalar1=bl_t[rows, ci, 2 * c + hh:2 * c + hh + 1], scalar2=None,
                                op0=ALU.mult), R=[B_ctmp, B_bl], W=[B_C32])
                    S.op("dve", lambda e: e.tensor_copy(out=Cbf[:], in_=C32[:]), R=[B_C32], W=[B_Cbf])
                    if cut <= 6:
                        continue
                    yield
                    pH = ps[bk["H"]][:, 0:260].rearrange("p (h e) -> p h e", h=4)
                    S.op("dve", lambda e: e.tensor_tensor(out=sm[:, 0:4], in0=pH[:, :, 64], in1=b_t[:, ci, :], op=ALU.mult),
                         R=[B_ps[bk["H"]], B_b], W=[B_sm])
                    S.op("dve", lambda e: e.tensor_scalar(out=sm[:, 4:8], in0=sm[:, 0:4], scalar1=-1.0, scalar2=None, op0=ALU.mult),
                         R=[B_sm], W=[B_sm])
                    S.op("dve", lambda e: e.tensor_tensor(out=sm[:, 4:8], in0=sm[:, 4:8], in1=sm[:, 0:4], op=ALU.max),
                         R=[B_sm], W=[B_sm])
                    S.op("dve", lambda e: e.tensor_scalar(out=sm[:, 4:8], in0=sm[:, 4:8], scalar1=1.0, scalar2=None, op0=ALU.max),
                         R=[B_sm], W=[B_sm])
                    S.op("dve", lambda e: e.reciprocal(out=sm[:, 4:8], in_=sm[:, 4:8]), R=[B_sm], W=[B_sm])
                    S.op("dve", lambda e: e.tensor_tensor(out=sm[:, 8:12], in0=b_t[:, ci, :], in1=sm[:, 4:8], op=ALU.mult),
                         R=[B_sm, B_b], W=[B_sm])
                    yield
                    S.op("act", lambda e: e.activation(out=eo[:], in_=ogt[i2][:, i, :], func=AF.Exp, scale=-1.0), R=[B_ogt[i2]], W=[B_eo])
                    S.op("dve", lambda e: e.tensor_scalar(out=eo[:], in0=eo[:], scalar1=1.0, scalar2=None, op0=ALU.add), R=[B_eo], W=[B_eo])
                    S.op("dve", lambda e: e.tensor_tensor(out=t1[:].rearrange("p (h e) -> p h e", h=4), in0=pH[:, :, 0:64],
                                                          in1=sm[:, 8:12].unsqueeze(2).to_broadcast([128, 4, 64]), op=ALU.mult),
                         R=[B_ps[bk["H"]], B_sm], W=[B_t1])
                    S.op("dve", lambda e: e.reciprocal(out=eo[:], in_=eo[:]), R=[B_eo], W=[B_eo])
                    S.op("dve", lambda e: e.tensor_tensor(out=t1[:], in0=t1[:], in1=eo[:], op=ALU.mult), R=[B_t1, B_eo], W=[B_t1])
                    yield
                    S.op("dve", lambda e: e.tensor_tensor(out=ysq[:], in0=t1[:], in1=t1[:], op=ALU.mult), R=[B_t1], W=[B_ysq])
                    S.op("dve", lambda e: e.tensor_reduce(out=sm[:, 12:16], in_=ysq[:].rearrange("p (h e) -> p h e", h=4), axis=AX.X,
                                                          op=ALU.add), R=[B_ysq], W=[B_sm])
                    S.op("act", lambda e: e.activation(out=sm[:, 12:16], in_=sm[:, 12:16], func=AF.Ln, scale=1.0 / 64, bias=EPS),
                         R=[B_sm], W=[B_sm])
                    S.op("act", lambda e: e.activation(out=sm[:, 12:16], in_=sm[:, 12:16], func=AF.Exp, scale=-0.5), R=[B_sm], W=[B_sm])
                    S.op("dve", lambda e: e.tensor_tensor(out=t1[:].rearrange("p (h e) -> p h e", h=4),
                                                          in0=t1[:].rearrange("p (h e) -> p h e", h=4),
                                                          in1=sm[:, 12:16].unsqueeze(2).to_broadcast([128, 4, 64]), op=ALU.mult),
                         R=[B_sm], W=[B_t1])
                    S.op("dve", lambda e: e.tensor_tensor(out=yn[:], in0=t1[:], in1=ghr[:], op=ALU.mult), R=[B_t1, B_small], W=[B_yn])
                    if cut <= 7:
                        continue
                    yield
                    pT = ps[bk["T"]][:].bitcast(BF16)
                    S.op("pe", [lambda e, c=c: e.transpose(out=pT[:, c * 128:(c + 1) * 128], in_=yn[:, c * 128:(c + 1) * 128],
                                                           identity=cbs("ident")) for c in range(2)], R=[B_yn, B_cb], W=[B_ps[bk["T"]]])
                    S.op("act", lambda e: e.copy(out=Y["t"][:, 0:2, ci * 128:(ci + 1) * 128],
                                                 in_=pT[:, 0:256].rearrange("p (c t) -> p c t", c=2)), R=[B_ps[bk["T"]]], W=[B_ymix])
            yield "hold"


    ML_BANKS_ALONE = {"c0": 0, "c1": 1, "q": 0, "k": 1, "kt": 2, "vt": 3, "S0": 4, "S1": 0, "H": 5, "dC": 6, "T": 7}
    ML_BANKS_CO = {"c0": 4, "c1": 5, "q": 4, "k": 4, "kt": 4, "vt": 4, "S0": 4, "S1": 5, "H": 5, "dC": 4, "T": 4}

    def phase_mlstm(l):
        g = gen_mlstm(l, ML_BANKS_ALONE)
        for _ in g:
            pass
        S.barrier()

    def phase_outproj(l, x_src, B_xsrc, x_dst, B_xdst):
        with ExitStack() as st:
            def T(name, shape, dt):
                return st.enter_context(nc.sbuf_tensor(name + "_L%d" % l, list(shape), dt))
            wo = T("op_w", [128, 8, D], BF16)
            B_wo = Buf("op_w")
            for k in range(8):
                S.dma("pool", wo[:, k, :], w_out[l, k * 128:(k + 1) * 128, :], W=[B_wo])
            xt = [T("op_xt%d" % i, [128, D], F32) for i in range(4)]
            xo = [T("op_xo%d" % i, [128, D], F32) for i in range(4)]
            B_xt = [Buf("op_xt%d" % i) for i in range(4)]
            B_xo = [Buf("op_xo%d" % i) for i in range(4)]
            for t0 in range(2):
                S.dma("sp", xt[t0][:], x_src[t0 * 128:(t0 + 1) * 128, :], R=[B_xsrc], W=[B_xt[t0]])
            for ti in range(NT):
                i = ti % 4
                if ti + 2 < NT:
                    S.dma("sp", xt[(ti + 2) % 4][:], x_src[(ti + 2) * 128:(ti + 3) * 128, :], R=[B_xsrc], W=[B_xt[(ti + 2) % 4]])
                for hf in range(2):
                    pb = (ti * 2 + hf) % 8
                    cs = slice(hf * 512, (hf + 1) * 512)
                    S.op("pe", [lambda e, k=k: e.matmul(ps[pb][:], lhsT=Y["t"][:, k, ti * 128:(ti + 1) * 128], rhs=wo[:, k, cs],
                                                        start=(k == 0), stop=(k == 7)) for k in range(8)],
                         R=[B_ymix, B_wo], W=[B_ps[pb]])
                    S.op("dve", lambda e: e.tensor_tensor(out=xo[i][:, cs], in0=ps[pb][:], in1=mod[:, 2, cs], op=ALU.mult),
                         R=[B_ps[pb], B_mod], W=[B_xo[i]])
                    S.op("dve", lambda e: e.tensor_tensor(out=xo[i][:, cs], in0=xo[i][:, cs], in1=xt[i][:, cs], op=ALU.add),
                         R=[B_xt[i]], W=[B_xo[i]])
                S.dma("sp", x_dst[ti * 128:(ti + 1) * 128, :], xo[i][:], R=[B_xo[i]], W=[B_xdst])
            S.barrier()

    def phase_moe(l, x_src, B_xsrc, x_dst, B_xdst, final):
        with ExitStack() as st:
            def T(name, shape, dt):
                return st.enter_context(nc.sbuf_tensor(name + "_L%d" % l, list(shape), dt))
            wr = T("mo_wr", [128, 8, NEXP], F32)
            rb = T("mo_rb", [128, NEXP], F32)
            B_wr = Buf("mo_wr")
            S.dma("sp", wr[:], w_router.rearrange("(k p) e -> p k e", p=128), W=[B_wr])
            S.dma("sp", rb[:], rbias[:, :], W=[B_wr])
            gfin = None
            if final:
                gfin = mod[:, 0, :]
                S.dma("sp", gfin, g_final[:, :], W=[B_mod])
            h2T = [T("mo_h2T%d" % i, [128, 8, 1024], BF16) for i in range(2)]
            B_h2T = [Buf("mo_h2T%d" % i) for i in range(2)]
            yaccA = T("mo_yacc", [128, 8, D], F32)
            yaccB = T("mo_yaccB", [128, 4, D], F32)
            B_yaccA = [Buf("mo_yacc%d" % i) for i in range(8)]
            B_yaccB = [Buf("mo_yaccB%d" % i) for i in range(4)]

            def ysel(qt, tl):
                if tl < 4 and qt % 2 == 1:
                    return yaccB[:, tl, :], B_yaccB[tl]
                return yaccA[:, tl, :], B_yaccA[tl]
            combTok = [T("mo_combTok%d" % i, [128, 8, NEXP], F32) for i in range(2)]
            B_combT = [Buf("mo_combTok%d" % i) for i in range(2)]
            Wg = [T("mo_Wg%d" % i, [128, 8, DEXP], BF16) for i in range(2)]
            Wu = [T("mo_Wu%d" % i, [128, 8, DEXP], BF16) for i in range(2)]
            Wd = [T("mo_Wd%d" % i, [128, 4, D], BF16) for i in range(2)]
            B_Wg = [Buf("mo_Wg%d" % i) for i in range(2)]
            B_Wu = [Buf("mo_Wu%d" % i) for i in range(2)]
            B_Wd = [Buf("mo_Wd%d" % i) for i in range(2)]
            he = [T("mo_he%d" % i, [128, 4, 512], BF16) for i in range(2)]
            B_he = [Buf("mo_he%d" % i) for i in range(2)]
            sg = [T("mo_sg%d" % i, [128, 512], BF16) for i in range(2)]
            B_sg = [Buf("mo_sg%d" % i) for i in range(2)]
            xt = [T("mo_xt%d" % i, [128, D], F32) for i in range(2)]
            B_xt = [Buf("mo_xt%d" % i) for i in range(2)]
            tmp = T("mo_tmp", [128, D], F32)
            B_tmp = Buf("mo_tmp")
            h2f = [T("mo_h2f%d" % i, [128, D], F32) for i in range(2)]
            B_h2f = [Buf("mo_h2f%d" % i) for i in range(2)]
            h2Tf1 = T("mo_h2Tf", [128, 8, 128], F32)
            h2Tf = [h2Tf1, h2Tf1]
            B_h2Tf1 = Buf("mo_h2Tf")
            B_h2Tf = [B_h2Tf1, B_h2Tf1]
            ss = [T("mo_ss%d" % i, [128, 4], F32) for i in range(2)]
            B_ss = [Buf("mo_ss%d" % i) for i in range(2)]
            ssf = T("mo_ssf", [128, 8, 4], F32)
            B_ssf = [Buf("mo_ssf%d" % i) for i in range(8)]
            rt = [T("mo_rt%d" % i, [128, 8, 64], F32) for i in range(2)]
            B_rt = [Buf("mo_rt%d" % i) for i in range(2)]
            PR = 7

            def load_gu(e):
                i = e % 2
                S.dma("pool", Wg[i][:], w_gate[l, e, :, :].rearrange("(k p) f -> p k f", p=128), W=[B_Wg[i]])
                S.dma("pool", Wu[i][:], w_up[l, e, :, :].rearrange("(k p) f -> p k f", p=128), W=[B_Wu[i]])

            def load_d(e):
                i = e % 2
                S.dma("pool", Wd[i][:], w_down[l, e, :, :].rearrange("(k p) d -> p k d", p=128), W=[B_Wd[i]])

            def load_w(e):
                load_gu(e)
                load_d(e)

            def Ra(qt, tt):
                ti = qt * 8 + tt
                i = ti % 2
                S.dma("sp", xt[i][:], x_src[ti * 128:(ti + 1) * 128, :], R=[B_xsrc], W=[B_xt[i]])
                norm_tile(xt[i], B_xt[i], 3, tmp, B_tmp, h2f[i], B_h2f[i], ss[i], B_ss[i])

            def Rb(qt, tt):
                ti = qt * 8 + tt
                i = ti % 2
                qb = qt % 2
                for half in range(2):
                    S.op("pe", [lambda e, c=c: e.transpose(out=ps[PR][:, (c % 4) * 128:(c % 4 + 1) * 128],
                                                           in_=h2f[i][:, c * 128:(c + 1) * 128], identity=cfs("ident"))
                                for c in range(half * 4, half * 4 + 4)], R=[B_h2f[i], B_cf], W=[B_ps[PR]])
                    S.op("act", lambda e: e.copy(out=h2T[qb][:, half * 4:half * 4 + 4, tt * 128:(tt + 1) * 128],
                                                 in_=ps[PR][:].rearrange("p (c t) -> p c t", c=4)), R=[B_ps[PR]], W=[B_h2T[qb]])
                    S.op("dve", lambda e: e.tensor_copy(out=h2Tf[i][:, half * 4:half * 4 + 4, :],
                                                        in_=ps[PR][:].rearrange("p (c t) -> p c t", c=4)), R=[B_ps[PR]], W=[B_h2Tf[i]])

            def Rb2(qt, tt):
                ti = qt * 8 + tt
                i = ti % 2
                bi = (ti // 4) % 2
                S.op("pe", [lambda e, k=k: e.matmul(ps[PR][:, 0:16], lhsT=h2Tf[i][:, k, :], rhs=wr[:, k, :], start=(k == 0), stop=(k == 7))
                            for k in range(8)], R=[B_h2Tf[i], B_wr], W=[B_ps[PR]])
                S.op("act", lambda e: e.activation(out=rt[bi][:, 0, (tt % 4) * 16:(tt % 4 + 1) * 16], in_=ps[PR][:, 0:16], func=AF.Exp,
                                                   scale=-1.0), R=[B_ps[PR]], W=[B_rt[bi]])
                if tt % 4 == 3:
                    RT(qt, tt // 4, bi)

            def RT(qt, b, bi):
                qb = qt % 2
                r_ = rt[bi]
                sc, g, eq, g2, sel, w_ = [r_[:, k_, :] for k_ in range(6)]
                m1, m2, gs, gmk = r_[:, 6, 0:16], r_[:, 6, 16:32], r_[:, 6, 32:48], r_[:, 6, 48:64]
                gmx, wsum = r_[:, 7, 0:4], r_[:, 7, 4:8]

                def vg(a):
                    return a.rearrange("p (g e) -> p g e", e=4)

                def vt(a):
                    return a.rearrange("p (t e) -> p t e", e=16)

                def bg(a):
                    return a.unsqueeze(2).to_broadcast([128, 16, 4])

                def t4(a):
                    return a.rearrange("p (t g) -> p t g", g=4)
                ops = [
                    lambda e: e.tensor_scalar(out=sc, in0=sc, scalar1=1.0, scalar2=None, op0=ALU.add),
                    lambda e: e.reciprocal(out=sc, in_=sc),
                    lambda e: e.tensor_tensor(out=vt(g), in0=vt(sc), in1=rb[:].unsqueeze(1).to_broadcast([128, 4, 16]), op=ALU.add),
                    lambda e: e.tensor_reduce(out=m1, in_=vg(g), axis=AX.X, op=ALU.max),
                    lambda e: e.tensor_tensor(out=vg(eq), in0=vg(g), in1=bg(m1), op=ALU.is_equal),
                    lambda e: e.scalar_tensor_tensor(out=g2, in0=eq, scalar=-1.0e9, in1=g, op0=ALU.mult, op1=ALU.add),
                    lambda e: e.tensor_reduce(out=m2, in_=vg(g2), axis=AX.X, op=ALU.max),
                    lambda e: e.tensor_tensor(out=gs, in0=m1, in1=m2, op=ALU.add),
                    lambda e: e.tensor_reduce(out=gmx, in_=t4(gs), axis=AX.X, op=ALU.max),
                    lambda e: e.tensor_tensor(out=t4(gmk), in0=t4(gs), in1=gmx.unsqueeze(2).to_broadcast([128, 4, 4]), op=ALU.is_ge),
                    lambda e: e.tensor_tensor(out=vg(sel), in0=vg(g), in1=bg(m2), op=ALU.is_ge),
                    lambda e: e.tensor_tensor(out=vg(sel), in0=vg(sel), in1=bg(gmk), op=ALU.mult),
                    lambda e: e.tensor_tensor(out=w_, in0=sc, in1=sel, op=ALU.mult),
                    lambda e: e.tensor_reduce(out=wsum, in_=vt(w_), axis=AX.X, op=ALU.add),
                    lambda e: e.reciprocal(out=wsum, in_=wsum),
                ]
                for f_ in ops:
                    S.op("dve", f_, R=[B_rt[bi], B_wr], W=[B_rt[bi]])
                S.op("dve", lambda e: e.tensor_tensor(out=combTok[qb][:, 4 * b:4 * b + 4, :], in0=vt(w_),
                                                      in1=wsum.unsqueeze(2).to_broadcast([128, 4, 16]), op=ALU.mult),
                     R=[B_rt[bi]], W=[B_combT[qb]])

            units = [(e_, s2) for e_ in range(NEXP) for s2 in range(2)]

            def GU(qt, u):
                e_, s2 = units[u]
                wi = e_ % 2
                qb = qt % 2
                ci_ = u % 2
                for f in range(4):
                    pg, pu = (0, 1) if f % 2 == 0 else (2, 3)
                    fs = slice(f * 128, (f + 1) * 128)
                    S.op("pe", [lambda e, k=k: e.matmul(ps[pg][:], lhsT=Wg[wi][:, k, fs], rhs=h2T[qb][:, k, s2 * 512:(s2 + 1) * 512],
                                                        start=(k == 0), stop=(k == 7)) for k in range(8)],
                         R=[B_Wg[wi], B_h2T[qb]], W=[B_ps[pg]])
                    S.op("pe", [lambda e, k=k: e.matmul(ps[pu][:], lhsT=Wu[wi][:, k, fs], rhs=h2T[qb][:, k, s2 * 512:(s2 + 1) * 512],
                                                        start=(k == 0), stop=(k == 7)) for k in range(8)],
                         R=[B_Wu[wi], B_h2T[qb]], W=[B_ps[pu]])
                    j = f % 2
                    S.op("act", lambda e: e.activation(out=sg[j][:], in_=ps[pg][:], func=AF.Silu), R=[B_ps[pg]], W=[B_sg[j]])
                    S.op("dve", lambda e: e.tensor_tensor(out=he[ci_][:, f, :], in0=ps[pu][:], in1=sg[j][:], op=ALU.mult),
                         R=[B_ps[pu], B_sg[j]], W=[B_he[ci_]])

            def DOWN(qt, u):
                e_, s2 = units[u]
                wi = e_ % 2
                ci_ = u % 2
                qb = qt % 2
                for t4 in range(4):
                    tl = s2 * 4 + t4
                    for dh in range(2):
                        py = 4 + ((t4 * 2 + dh) % 3)
                        ds_ = slice(dh * 512, (dh + 1) * 512)
                        S.op("pe", [lambda e, f=f: e.matmul(ps[py][:], lhsT=he[ci_][:, f, t4 * 128:(t4 + 1) * 128], rhs=Wd[wi][:, f, ds_],
                                                            start=(f == 0), stop=(f == 3)) for f in range(4)],
                             R=[B_he[ci_], B_Wd[wi]], W=[B_ps[py]])
                        cw_ = combTok[qb][:, tl, e_:e_ + 1]
                        ya_, B_ya = ysel(qt, tl)
                        if e_ == 0:
                            S.op("dve", lambda e: e.tensor_scalar(out=ya_[:, ds_], in0=ps[py][:], scalar1=cw_, scalar2=None, op0=ALU.mult),
                                 R=[B_ps[py], B_combT[qb]], W=[B_ya])
                        else:
                            S.op("dve", lambda e: e.scalar_tensor_tensor(out=ya_[:, ds_], in0=ps[py][:], scalar=cw_, in1=ya_[:, ds_],
                                                                         op0=ALU.mult, op1=ALU.add),
                                 R=[B_ps[py], B_combT[qb]], W=[B_ya])

            def EPIa(qt, tt):
                ti = qt * 8 + tt
                ya_, B_ya = ysel(qt, tt)
                xe, B_xe = xt[tt % 2], B_xt[tt % 2]
                S.dma("sp", xe[:], x_src[ti * 128:(ti + 1) * 128, :], R=[B_xsrc], W=[B_xe])
                S.op("pool", lambda e: e.tensor_tensor(out=ya_, in0=ya_, in1=mod[:, 5, :], op=ALU.mult), R=[B_mod], W=[B_ya])
                S.op("pool", lambda e: e.tensor_tensor(out=ya_, in0=ya_, in1=xe[:], op=ALU.add), R=[B_xe], W=[B_ya])
                if final:
                    s_ = ssf[:, tt, :]
                    S.op("act", lambda e: e.activation(out=xe[:], in_=ya_, func=AF.Square, accum_out=s_[:, 0:1]),
                         R=[B_ya], W=[B_xe, B_ssf[tt]])
                    S.op("act", lambda e: e.activation(out=s_[:, 1:2], in_=s_[:, 0:1], func=AF.Ln, scale=1.0 / D, bias=EPS),
                         R=[B_ssf[tt]], W=[B_ssf[tt]])
                    S.op("act", lambda e: e.activation(out=s_[:, 2:3], in_=s_[:, 1:2], func=AF.Exp, scale=-0.5), R=[B_ssf[tt]], W=[B_ssf[tt]])
                else:
                    S.dma("sp", x_dst[ti * 128:(ti + 1) * 128, :], ya_, R=[B_ya], W=[B_xdst])

            def EPIb(qt, tt):
                if not final:
                    return
                ti = qt * 8 + tt
                ya_, B_ya = ysel(qt, tt)
                s_ = ssf[:, tt, :]
                S.op("dve", lambda e: e.scalar_tensor_tensor(out=ya_, in0=ya_, scalar=s_[:, 2:3], in1=gfin, op0=ALU.mult, op1=ALU.mult),
                     R=[B_ssf[tt], B_mod], W=[B_ya])
                S.dma("sp", x_dst[ti * 128:(ti + 1) * 128, :], ya_, R=[B_ya], W=[B_xdst])

            load_w(0)
            load_w(1)
            for tt in range(9):
                if tt < 8:
                    Ra(0, tt)
                if tt >= 1:
                    Rb2(0, tt - 1)
                if tt < 8:
                    Rb(0, tt)
            for qt in range(4):
                sched = {}
                if qt + 1 < 4:
                    for tt in range(8):
                        sched.setdefault(3 + 3 * tt, []).append(("a", tt))
                        sched.setdefault(4 + 3 * tt, []).append(("b", tt))
                        sched.setdefault(5 + 3 * tt, []).append(("b2", tt))
                GU(qt, 0)
                if qt > 0:
                    for tt in range(4, 8):
                        EPIa(qt - 1, tt)
                for u in range(len(units)):
                    if u + 1 < len(units):
                        GU(qt, u + 1)
                    e_u, s_u = units[u]
                    if s_u == 0 and (e_u + 2 < NEXP or qt + 1 < 4):
                        load_gu((e_u + 2) % NEXP)
                    if qt > 0 and u == 1:
                        for tt in range(4, 8):
                            EPIb(qt - 1, tt)
                    if qt > 0 and 1 <= u <= 4:
                        EPIa(qt - 1, u - 1)
                    if qt > 0 and 2 <= u <= 5:
                        EPIb(qt - 1, u - 2)
                    for kind, tt in sched.get(u, []):
                        {"a": Ra, "b": Rb, "b2": Rb2}[kind](qt + 1, tt)
                    DOWN(qt, u)
                    if s_u == 1 and (e_u + 2 < NEXP or qt + 1 < 4):
                        load_d((e_u + 2) % NEXP)
                    if qt == 3 and e_u == NEXP - 1 and s_u == 0:
                        for tt in range(4):
                            EPIa(qt, tt)
                if qt == 3:
                    for tt in range(4):
                        EPIb(qt, tt)
                    for tt in range(4, 8):
                        EPIa(qt, tt)
                    for tt in range(4, 8):
                        EPIb(qt, tt)
            S.barrier()

    def dump_dram(name, src, B_src, shape, dt):
        t = dbg_out(name, shape, dt)
        b = Buf("dbg_" + name)
        S.dma("sp", t, src, R=[B_src], W=[b])
        fin_bufs.append(b)

    def dump_sbuf(name, src_ap, B_src, shape, dt):
        t = dbg_out(name, shape, dt)
        b = Buf("dbg_" + name)
        S.dma("sp", t, src_ap, R=[B_src], W=[b])
        fin_bufs.append(b)

    B_xin, B_y = Buf("x_in"), Buf("y_out")
    x_cur, B_xcur = x_in, B_xin
    if isinstance(stop_after, str) and stop_after.startswith("only:"):
        ph = stop_after[5:]
        l = 0
        if ph == "mod":
            phase_mod(0)
        elif ph == "inproj":
            with ExitStack() as wst:
                win_, B_win_ = load_win(0, wst)
                phase_inproj(0, x_in, B_xin, win_, B_win_)
        elif ph == "modin":
            with ExitStack() as wst:
                win_, B_win_ = load_win(0, wst)
                phase_mod(0)
                phase_inproj(0, x_in, B_xin, win_, B_win_)
        elif ph == "sbml":
            with ExitStack() as lay:
                Y["t"] = lay.enter_context(nc.sbuf_tensor("ymixT_L%d" % l, [128, 8, SEQ], BF16))
                g_ml = gen_mlstm(0, ML_BANKS_CO)
                next(g_ml)
                phase_sb(0, co=[g_ml])
                for _ in g_ml:
                    pass
        elif ph in ("ml", "dil", "sb", "outproj"):
            with ExitStack() as lay:
                Y["t"] = lay.enter_context(nc.sbuf_tensor("ymixT_L%d" % l, [128, 8, SEQ], BF16))
                {"ml": phase_mlstm, "dil": phase_dil, "sb": phase_sb}.get(ph, lambda l_: phase_outproj(0, x_in, B_xin, xA, B_xA))(0)
        elif ph == "moe":
            phase_moe(0, x_in, B_xin, xB, B_xB, False)
        S.barrier()
        return nc, out_tensors
    for l in range(DEPTH):
        if stop_after == "mlonly%d" % l:
            with ExitStack() as lay:
                Y["t"] = lay.enter_context(nc.sbuf_tensor("ymixT_L%d" % l, [128, 8, SEQ], BF16))
                phase_mlstm(l)
            break
        with ExitStack() as wst:
            win_, B_win_ = load_win(l, wst)
            phase_mod(l)
            phase_inproj(l, x_cur, B_xcur, win_, B_win_)
        with ExitStack() as lay:
            Y["t"] = lay.enter_context(nc.sbuf_tensor("ymixT_L%d" % l, [128, 8, SEQ], BF16))
            phase_dil(l)
            g_ml = gen_mlstm(l, ML_BANKS_CO)
            next(g_ml)
            phase_sb(l, co=[g_ml])
            for _ in g_ml:
                pass
            if stop_after == "mix%d" % l:
                dump_sbuf("ymixT", Y["t"][:].rearrange("p c t -> p (c t)"), B_ymix, [128, 8 * SEQ], BF16)
                S.wait_all("sp", fin_bufs)
                S.barrier()
                break
            phase_outproj(l, x_cur, B_xcur, xA, B_xA)
        if stop_after == "x1_%d" % l:
            dump_dram("x1", xA, B_xA, [SEQ, D], F32)
            break
        last = (l == DEPTH - 1)
        if last:
            phase_moe(l, xA, B_xA, y_out, B_y, True)
        else:
            phase_moe(l, xA, B_xA, xB, B_xB, False)
            x_cur, B_xcur = xB, B_xB
        if stop_after == "x2_%d" % l:
            dump_dram("x2", xB, B_xB, [SEQ, D], F32)
            break

    S.wait_all("sp", fin_bufs + [B_y])
    S.barrier()
    return nc, out_tensors


def prep_inputs(b, inp, consts):
    cbn, cfn = consts
    f = np.float32
    d = {
        "x": np.ascontiguousarray(inp["x"][b]),
        "c_lay": np.ascontiguousarray(inp["c"][b].reshape(8, 128).T),
        "w_in": inp["w_in"],
        "conv_w": np.ascontiguousarray(inp["conv_w"].reshape(DEPTH, 4, 2, 128).transpose(0, 3, 2, 1)),
        "conv_b": np.ascontiguousarray(inp["conv_b"].reshape(DEPTH, 2, 128).transpose(0, 2, 1)),
        "w_mq": inp["w_mq"], "w_mk": inp["w_mk"], "w_mv": inp["w_mv"],
        "gbias": np.ascontiguousarray(np.broadcast_to(inp["gate_bias"].reshape(DEPTH, 1, 8), (DEPTH, 128, 8))),
        "g_head_p": np.ascontiguousarray(inp["g_head"].reshape(DEPTH, 8, 128).transpose(0, 2, 1)),
        "g_head_r": np.ascontiguousarray(np.broadcast_to(inp["g_head"][:, None, :256], (DEPTH, 128, 256))),
        "w_out": inp["w_out"],
        "w_ada": inp["w_ada"],
        "b_ada": np.ascontiguousarray(inp["b_ada"].reshape(DEPTH, 1, 6 * D)),
        "w_router": inp["w_router"],
        "rbias": np.ascontiguousarray(np.broadcast_to(inp["router_bias"][None, :], (128, NEXP))),
        "w_gate_e": inp["w_gate_e"], "w_up_e": inp["w_up_e"], "w_down_e": inp["w_down_e"],
        "g_final_r": np.ascontiguousarray(np.broadcast_to(inp["g_final"][None, :], (128, D))),
        "cb": cbn, "cf": cfn,
    }
    return {k: np.ascontiguousarray(v, dtype=f) for k, v in d.items()}


def kernel(**inputs):
    inp = {k: np.asarray(v) for k, v in inputs.items()}
    nc, _ = build_program()
    consts = make_consts()
    in_maps = [prep_inputs(b, inp, consts) for b in range(8)]
    res = run_bass_kernel_spmd(nc, in_maps, core_ids=list(range(8)))
    return np.stack([np.asarray(r["y"]) for r in res.results], axis=0).astype(np.float32)
```

```python
import math
import numpy as np
from contextlib import ExitStack
import concourse.bass as bass
import concourse.mybir as mybir
from concourse.bass_utils import run_bass_kernel_spmd

F32 = mybir.dt.float32
BF16 = mybir.dt.bfloat16
AF = mybir.ActivationFunctionType
ALU = mybir.AluOpType
AX = mybir.AxisListType

SEQ = 4096
D = 1024
DEPTH = 2
NT = SEQ // 128
NSB = SEQ // 512
DIN = 2824
NEXP = 16
DEXP = 512
EPS = 1e-6
OFF_O = 256
OFF_DIL = 520
OFF_SB = 520 + 1152
DIL_PATTERNS = ((128, 1), (512, 4), (2048, 16))


class Buf:
    __slots__ = ("name", "w", "r", "excl")

    def __init__(self, name, excl=False):
        self.name = name
        self.w = None
        self.r = []
        self.excl = excl


class _Eng:
    def __init__(self, name, e, sem):
        self.name = name
        self.e = e
        self.sem = sem
        self.cnt = 0
        self.known = {}


class Sched:
    def __init__(self, nc, n_dma_sems=32):
        self.nc = nc
        self.sems = []
        self.eng = {}
        for name, e in (("pe", nc.tensor), ("act", nc.scalar), ("dve", nc.vector),
                        ("pool", nc.gpsimd), ("sp", nc.sync)):
            sem = nc.semaphore("s_" + name).__enter__()
            self.sems.append(sem)
            self.eng[name] = _Eng(name, e, len(self.sems) - 1)
        self.dma_sems = []
        for i in range(n_dma_sems):
            sem = nc.semaphore("s_dma%d" % i).__enter__()
            self.sems.append(sem)
            self.dma_sems.append([len(self.sems) - 1, 0])
        self.dma_rr = 0
        self.ninstr = 0

    def _deps(self, R, W):
        deps = {}

        def add(t):
            if t is None:
                return
            s, v = t
            if deps.get(s, 0) < v:
                deps[s] = v
        for b in R:
            add(b.w)
            if b.excl:
                for t in b.r:
                    add(t)
        for b in W:
            add(b.w)
            for t in b.r:
                add(t)
        return deps

    def _wait(self, E, deps):
        for s, v in deps.items():
            if E.known.get(s, 0) >= v:
                continue
            E.e.wait_ge(self.sems[s], v)
            E.known[s] = v
            self.ninstr += 1

    def _mark(self, R, W, ticket):
        for b in R:
            if b.excl:
                b.w = ticket
                b.r = []
            else:
                b.r.append(ticket)
                if len(b.r) > 32:
                    m = {}
                    for s, v in b.r:
                        if m.get(s, 0) < v:
                            m[s] = v
                    b.r = list(m.items())
        for b in W:
            b.w = ticket
            b.r = []

    def op(self, eng, fns, R=(), W=()):
        E = self.eng[eng]
        if not isinstance(fns, (list, tuple)):
            fns = [fns]
        self._wait(E, self._deps(R, W))
        ins = None
        for f in fns:
            ins = f(E.e)
            self.ninstr += 1
        E.cnt += 1
        ins.then_inc(self.sems[E.sem], 1)
        t = (E.sem, E.cnt)
        self._mark(R, W, t)
        return t

    def dma(self, eng, out, in_, R=(), W=()):
        E = self.eng[eng]
        slot = self.dma_sems[self.dma_rr]
        self.dma_rr = (self.dma_rr + 1) % len(self.dma_sems)
        s, v = slot
        deps = self._deps(R, W)
        if v > 0 and deps.get(s, 0) < v:
            deps[s] = v
        self._wait(E, deps)
        ins = E.e.dma_start(out=out, in_=in_)
        slot[1] = v + 16
        ins.then_inc(self.sems[s], 16)
        self.ninstr += 1
        t = (s, v + 16)
        self._mark(R, W, t)
        return t

    def wait_all(self, eng, bufs):
        E = self.eng[eng]
        self._wait(E, self._deps(bufs, bufs))

    def barrier(self):
        tot = {}
        for E in self.eng.values():
            if E.cnt:
                tot[E.sem] = E.cnt
        for s, v in self.dma_sems:
            if v:
                tot[s] = v
        for E in self.eng.values():
            self._wait(E, dict(tot))


CB = {}
CF = {}


def _layout(table, items):
    off = 0
    for name, w in items:
        table[name] = (off, w)
        off += w
    return off


NCB = _layout(CB, [("ident", 128), ("ones", 128), ("negtri", 128), ("negones", 128),
                   ("mstrict", 128), ("triu", 128), ("onesA", 128), ("onesB", 128),
                   ("blk64", 128), ("zeros", 128), ("dilE", 18 * 256)])
NCF = _layout(CF, [("ident", 128), ("ones", 128), ("triu", 128), ("blk64", 128), ("selA", 128), ("selB", 128)])


def make_consts():
    p = np.arange(128)[:, None].astype(np.float64)
    f = np.arange(128)[None, :].astype(np.float64)
    cb = np.zeros((128, NCB), np.float32)
    cf = np.zeros((128, NCF), np.float32)

    def put(tab, lay, name, val):
        o, w = lay[name]
        tab[:, o:o + w] = val
    ident = (p == f).astype(np.float32)
    ones = np.ones((128, 128), np.float32)
    triu = (p <= f).astype(np.float32)
    blk64 = ((p // 64) == (f // 64)).astype(np.float32)
    put(cb, CB, "ident", ident)
    put(cb, CB, "ones", ones)
    put(cb, CB, "negtri", -(p >= f).astype(np.float32))
    put(cb, CB, "negones", -ones)
    put(cb, CB, "mstrict", (p < f).astype(np.float32))
    put(cb, CB, "triu", triu)
    put(cb, CB, "onesA", (f < 64).astype(np.float32) * ones)
    put(cb, CB, "onesB", (f >= 64).astype(np.float32) * ones)
    put(cb, CB, "blk64", blk64)
    mk = np.arange(128)[:, None].astype(np.float64)
    mq = np.arange(256)[None, :].astype(np.float64)
    dlt = mq - mk
    valid = (dlt >= 0) & (dlt <= 128)
    o, _ = CB["dilE"]
    for h in range(6):
        slope = 2.0 ** (-8.0 * (h + 1) / 6.0)
        for pi, (win, dil) in enumerate(DIL_PATTERNS):
            e = np.where(valid, np.exp(-slope * dil * dlt), 0.0)
            ix = ((h // 2) * 3 + pi) * 2 + (h % 2)
            cb[:, o + ix * 256: o + (ix + 1) * 256] = e
    put(cf, CF, "ident", ident)
    put(cf, CF, "ones", ones)
    put(cf, CF, "triu", triu)
    put(cf, CF, "blk64", blk64)
    selA = np.zeros((128, 128), np.float32); selA[64, 0:64] = 1.0
    selB = np.zeros((128, 128), np.float32); selB[0, 64:128] = 1.0
    put(cf, CF, "selA", selA)
    put(cf, CF, "selB", selB)
    return cb, cf


ML_CUT = [99]
MOE_DBG = [0]
CO_EVERY = [1]
CO_SKIP = [10 ** 9]
SB_DEPTH = [2, 3]


def build_program(stop_after=None, debug=()):
    nc = bass.Bass("TRN2", target_bir_lowering=False)
    S = Sched(nc)
    dbg = {}

    def din(name, shape, dt=F32):
        return nc.dram_tensor(name, list(shape), dt, kind="ExternalInput").ap()

    def dscr(name, shape, dt):
        return nc.dram_tensor(name, list(shape), dt, kind="Internal").ap()

    x_in = din("x", [SEQ, D])
    c_lay = din("c_lay", [128, 8])
    w_in = din("w_in", [DEPTH, D, DIN])
    conv_w = din("conv_w", [DEPTH, 128, 2, 4])
    conv_b = din("conv_b", [DEPTH, 128, 2])
    w_mq = din("w_mq", [DEPTH, 4, 64, 64])
    w_mk = din("w_mk", [DEPTH, 4, 64, 64])
    w_mv = din("w_mv", [DEPTH, 4, 64, 64])
    gbias = din("gbias", [DEPTH, 128, 8])
    g_head_p = din("g_head_p", [DEPTH, 128, 8])
    g_head_r = din("g_head_r", [DEPTH, 128, 256])
    w_out = din("w_out", [DEPTH, D, D])
    w_ada = din("w_ada", [DEPTH, D, 6 * D])
    b_ada = din("b_ada", [DEPTH, 1, 6 * D])
    w_router = din("w_router", [D, NEXP])
    rbias = din("rbias", [128, NEXP])
    w_gate = din("w_gate_e", [DEPTH, NEXP, D, DEXP])
    w_up = din("w_up_e", [DEPTH, NEXP, D, DEXP])
    w_down = din("w_down_e", [DEPTH, NEXP, DEXP, D])
    g_final = din("g_final_r", [128, D])
    cb_in = din("cb", [128, NCB])
    cf_in = din("cf", [128, NCF])
    y_out = nc.dram_tensor("y", [SEQ, D], F32, kind="ExternalOutput").ap()

    xA = dscr("xA", [SEQ, D], F32)
    xB = dscr("xB", [SEQ, D], F32)
    xmT_d = dscr("xmT", [256, SEQ], BF16)
    og_d = dscr("og", [SEQ, 256], BF16)
    gates_d = dscr("gates", [SEQ, 8], F32)
    dqT_d = dscr("dqT", [384, SEQ], BF16)
    dkT_d = dscr("dkT", [384, SEQ], BF16)
    dv_d = dscr("dv", [SEQ, 384], BF16)
    sqT_d = dscr("sqT", [384, SEQ], BF16)
    skT_d = dscr("skT", [384, SEQ], BF16)
    sv_d = dscr("sv", [SEQ, 384], BF16)
    B_xA, B_xB = Buf("xA"), Buf("xB")
    B_scr = {n: Buf(n) for n in ("xmT", "og", "gates", "dqT", "dkT", "dv", "sqT", "skT", "sv")}

    for name in debug:
        pass

    def sbt(name, shape, dt):
        return nc.alloc_sbuf_tensor(name, list(shape), dt)

    cb = sbt("cb_sb", [128, NCB], BF16)
    cf = sbt("cf_sb", [128, NCF], F32)
    B_cb, B_cf = Buf("cb"), Buf("cf")
    S.dma("pool", cb[:], cb_in[:, :], W=[B_cb])
    S.dma("sp", cf[:], cf_in[:, :], W=[B_cf])

    def cbs(name, rows=slice(0, 128)):
        o, w = CB[name]
        return cb[rows, o:o + w]

    def cfs(name, rows=slice(0, 128)):
        o, w = CF[name]
        return cf[rows, o:o + w]

    pp = [nc.alloc_psum_tensor("pp%d" % i, [128, 1024], F32) for i in range(4)]
    ps = [pp[i // 2][:, (i % 2) * 512:(i % 2 + 1) * 512] for i in range(8)]
    PP = pp
    B_ps = [Buf("ps%d" % i, excl=True) for i in range(8)]

    mod = sbt("mod", [128, 6, D], F32)
    B_mod = Buf("mod")
    B_crep = Buf("c_rep")
    c_sb = sbt("c_sb", [128, 8], F32)
    B_c = Buf("c_sb")
    S.dma("sp", c_sb[:], c_lay[:, :], W=[B_c])
    S.op("act", lambda e: e.activation(out=c_sb[:], in_=c_sb[:], func=AF.Silu), R=[B_c], W=[B_c])

    out_tensors = {}

    def dbg_out(name, shape, dt=F32):
        t = nc.dram_tensor("dbg_" + name, list(shape), dt, kind="ExternalOutput").ap()
        out_tensors[name] = t
        return t

    fin_bufs = []

    def phase_mod(l):
        with ExitStack() as st:
            c_rep = st.enter_context(nc.sbuf_tensor("c_rep_L%d" % l, [128, 8, 128], F32))
            S.op("dve", lambda e: e.tensor_copy(out=c_rep[:], in_=c_sb[:].unsqueeze(2).to_broadcast([128, 8, 128])),
                 R=[B_c], W=[B_crep])
            wt = [st.enter_context(nc.sbuf_tensor("wada%d_L%d" % (i, l), [128, 8, 512], F32)) for i in range(2)]
            br = [st.enter_context(nc.sbuf_tensor("brow%d_L%d" % (i, l), [1, 512], F32)) for i in range(2)]
            B_wt = [Buf("wada%d" % i) for i in range(2)]
            B_br = [Buf("brow%d" % i) for i in range(2)]
            for blk in range(12):
                i = blk % 2
                S.dma("sp", wt[i][:], w_ada[l, :, blk * 512:(blk + 1) * 512].rearrange("(k p) n -> p k n", p=128),
                      W=[B_wt[i]])
                S.dma("sp", br[i][:], b_ada[l, :, blk * 512:(blk + 1) * 512], W=[B_br[i]])
                pb = blk % 2
                fns = []
                for k in range(8):
                    fns.append(lambda e, k=k, i=i, pb=pb: e.matmul(ps[pb][:], lhsT=c_rep[:, k, :], rhs=wt[i][:, k, :],
                                                                   start=(k == 0), stop=False))
                fns.append(lambda e, i=i, pb=pb: e.matmul(ps[pb][:], lhsT=cfs("ones", slice(0, 1)), rhs=br[i][:],
                                                          start=False, stop=True))
                S.op("pe", fns, R=[B_wt[i], B_br[i], B_crep, B_cf], W=[B_ps[pb]])
                m = blk // 2
                dst = mod[:, m, (blk % 2) * 512:(blk % 2 + 1) * 512]
                if m in (1, 4):
                    S.op("dve", lambda e, dst=dst, pb=pb: e.tensor_scalar(out=dst, in0=ps[pb][:], scalar1=1.0, scalar2=None,
                                                                         op0=ALU.add), R=[B_ps[pb]], W=[B_mod])
                else:
                    S.op("dve", lambda e, dst=dst, pb=pb: e.tensor_copy(out=dst, in_=ps[pb][:]), R=[B_ps[pb]], W=[B_mod])
            S.barrier()

    def norm_tile(xt, B_xt, sidx, tmp, B_tmp, hout, B_hout, ss, B_ss):
        S.op("act", lambda e: e.activation(out=tmp[:], in_=xt[:], func=AF.Square, accum_out=ss[:, 0:1]),
             R=[B_xt], W=[B_tmp, B_ss])
        S.op("act", lambda e: e.activation(out=ss[:, 1:2], in_=ss[:, 0:1], func=AF.Ln, scale=1.0 / D, bias=EPS),
             R=[B_ss], W=[B_ss])
        S.op("act", lambda e: e.activation(out=ss[:, 2:3], in_=ss[:, 1:2], func=AF.Exp, scale=-0.5),
             R=[B_ss], W=[B_ss])
        S.op("dve", lambda e: e.scalar_tensor_tensor(out=tmp[:], in0=xt[:], scalar=ss[:, 2:3], in1=mod[:, sidx + 1, :],
                                                     op0=ALU.mult, op1=ALU.mult), R=[B_xt, B_ss, B_mod], W=[B_tmp])
        S.op("dve", lambda e: e.tensor_tensor(out=hout[:], in0=tmp[:], in1=mod[:, sidx, :], op=ALU.add),
             R=[B_tmp, B_mod], W=[B_hout])

    def load_win(l, st):
        win = st.enter_context(nc.sbuf_tensor("win_L%d" % l, [128, 8, DIN], BF16))
        B_win = Buf("win")
        for k in range(8):
            S.dma("pool", win[:, k, :], w_in[l, k * 128:(k + 1) * 128, :], W=[B_win])
        return win, B_win

    def phase_inproj(l, x_src, B_xsrc, win, B_win):
        with ExitStack() as st:
            def T(name, shape, dt):
                return st.enter_context(nc.sbuf_tensor(name + "_L%d" % l, list(shape), dt))
            xt = [T("xt%d" % i, [128, D], F32) for i in range(2)]
            B_xt = [Buf("xt%d" % i) for i in range(2)]
            tmp = [T("ntmp%d" % i, [128, D], F32) for i in range(2)]
            B_tmp = [Buf("ntmp%d" % i) for i in range(2)]
            hb = [T("hb%d" % i, [128, D], BF16) for i in range(2)]
            B_hb = [Buf("hb%d" % i) for i in range(2)]
            ss = [T("ss%d" % i, [128, 4], F32) for i in range(2)]
            B_ss = [Buf("ss%d" % i) for i in range(2)]
            hT = [T("hT%d" % i, [128, 8, 512], BF16) for i in range(2)]
            B_hT = [Buf("hT%d" % i) for i in range(2)]
            stF = {n: [T("st_%s%d" % (n, i), [128, w, 512], BF16) for i in range(2)]
                   for n, w in (("xmT", 2), ("dqT", 3), ("dkT", 3), ("sqT", 3), ("skT", 3))}
            B_stF = {n: [Buf("st_%s%d" % (n, i)) for i in range(2)] for n in stF}
            st_og = [T("st_og%d" % i, [128, 4, 256], BF16) for i in range(2)]
            st_g = [T("st_g%d" % i, [128, 4, 8], F32) for i in range(2)]
            st_dv = [T("st_dv%d" % i, [128, 4, 384], BF16) for i in range(2)]
            st_sv = [T("st_sv%d" % i, [128, 4, 384], BF16) for i in range(2)]
            B_og = [Buf("st_og%d" % i) for i in range(2)]
            B_g = [Buf("st_g%d" % i) for i in range(2)]
            B_dv = [Buf("st_dv%d" % i) for i in range(2)]
            B_sv = [Buf("st_sv%d" % i) for i in range(2)]
            fm_specs = [("xmT", 0, 2, 1.0, xmT_d), ("dqT", OFF_DIL, 3, 1.0, dqT_d), ("dkT", OFF_DIL + 384, 3, 0.125, dkT_d),
                        ("sqT", OFF_SB, 3, 1.0, sqT_d), ("skT", OFF_SB + 384, 3, 0.125, skT_d)]
            pcount = [0]

            def next_ps():
                pcount[0] += 1
                return 2 + (pcount[0] % 6)

            def NA(sb, tt):
                ti = sb * 4 + tt
                i = ti % 2
                S.dma("sp", xt[i][:], x_src[ti * 128:(ti + 1) * 128, :], R=[B_xsrc], W=[B_xt[i]])
                norm_tile(xt[i], B_xt[i], 0, tmp[i], B_tmp[i], hb[i], B_hb[i], ss[i], B_ss[i])

            def NB(sb, tt):
                ti = sb * 4 + tt
                i = ti % 2
                sl = sb % 2
                pb = ti % 2
                pT = ps[pb][:].bitcast(BF16)
                S.op("pe", [lambda e, c=c: e.transpose(out=pT[:, c * 128:(c + 1) * 128], in_=hb[i][:, c * 128:(c + 1) * 128],
                                                       identity=cbs("ident")) for c in range(8)],
                     R=[B_hb[i], B_cb], W=[B_ps[pb]])
                S.op("act", lambda e: e.copy(out=hT[sl][:, :, tt * 128:(tt + 1) * 128], in_=pT.rearrange("p (c t) -> p c t", c=8)),
                     R=[B_ps[pb]], W=[B_hT[sl]])

            def M_items(sb):
                sl = sb % 2
                items = []
                for (n, off, nch, scale, dst) in fm_specs:
                    for c in range(nch):
                        def it(n=n, off=off, nch=nch, scale=scale, dst=dst, c=c):
                            pb = next_ps()
                            S.op("pe", [lambda e, k=k: e.matmul(ps[pb][:], lhsT=win[:, k, off + c * 128: off + (c + 1) * 128],
                                                                rhs=hT[sl][:, k, :], start=(k == 0), stop=(k == 7)) for k in range(8)],
                                 R=[B_win, B_hT[sl]], W=[B_ps[pb]])
                            S.op("act", lambda e: e.activation(out=stF[n][sl][:, c, :], in_=ps[pb][:], func=AF.Copy, scale=scale),
                                 R=[B_ps[pb]], W=[B_stF[n][sl]])
                            if c == nch - 1:
                                S.dma("sp", dst[:, sb * 512:(sb + 1) * 512].rearrange("(c p) t -> p c t", p=128), stF[n][sl][:],
                                      R=[B_stF[n][sl]], W=[B_scr[n]])
                        items.append(it)
                for tt in range(4):
                    def it(tt=tt):
                        pb = next_ps()
                        S.op("pe", [lambda e, k=k: e.matmul(ps[pb][:, 0:264], lhsT=hT[sl][:, k, tt * 128:(tt + 1) * 128],
                                                            rhs=win[:, k, OFF_O:OFF_O + 264], start=(k == 0), stop=(k == 7))
                                    for k in range(8)], R=[B_win, B_hT[sl]], W=[B_ps[pb]])
                        S.op("act", lambda e: e.activation(out=st_og[sl][:, tt, :], in_=ps[pb][:, 0:256], func=AF.Copy),
                             R=[B_ps[pb]], W=[B_og[sl]])
                        S.op("dve", lambda e: e.tensor_copy(out=st_g[sl][:, tt, :], in_=ps[pb][:, 256:264]), R=[B_ps[pb]], W=[B_g[sl]])
                    items.append(it)
                    for (stt, Bst, off) in ((st_dv, B_dv, OFF_DIL + 768), (st_sv, B_sv, OFF_SB + 768)):
                        def it(tt=tt, stt=stt, Bst=Bst, off=off):
                            pb = next_ps()
                            S.op("pe", [lambda e, k=k: e.matmul(ps[pb][:, 0:384], lhsT=hT[sl][:, k, tt * 128:(tt + 1) * 128],
                                                                rhs=win[:, k, off:off + 384], start=(k == 0), stop=(k == 7))
                                        for k in range(8)], R=[B_win, B_hT[sl]], W=[B_ps[pb]])
                            S.op("dve", lambda e: e.tensor_copy(out=stt[sl][:, tt, :], in_=ps[pb][:, 0:384]), R=[B_ps[pb]], W=[Bst[sl]])
                        items.append(it)

                def fin():
                    r0, r1 = sb * 512, (sb + 1) * 512
                    S.dma("sp", og_d[r0:r1, :].rearrange("(t p) c -> p t c", p=128), st_og[sl][:], R=[B_og[sl]], W=[B_scr["og"]])
                    S.dma("sp", gates_d[r0:r1, :].rearrange("(t p) c -> p t c", p=128), st_g[sl][:], R=[B_g[sl]], W=[B_scr["gates"]])
                    S.dma("sp", dv_d[r0:r1, :].rearrange("(t p) c -> p t c", p=128), st_dv[sl][:], R=[B_dv[sl]], W=[B_scr["dv"]])
                    S.dma("sp", sv_d[r0:r1, :].rearrange("(t p) c -> p t c", p=128), st_sv[sl][:], R=[B_sv[sl]], W=[B_scr["sv"]])
                return items, fin

            for tt in range(4):
                NA(0, tt)
                NB(0, tt)
            for sb in range(NSB):
                items, fin = M_items(sb)
                nper = (len(items) + 3) // 4
                for part in range(4):
                    if sb + 1 < NSB:
                        NA(sb + 1, part)
                    for it in items[part * nper:(part + 1) * nper]:
                        it()
                    if sb + 1 < NSB:
                        NB(sb + 1, part)
                fin()
            S.barrier()


    Y = {}
    B_ymix = Buf("ymixT")
    ghp = sbt("ghp", [128, DEPTH, 8], F32)
    B_ghp = Buf("ghp")
    for l_ in range(DEPTH):
        S.dma("sp", ghp[:, l_, :], g_head_p[l_, :, :], W=[B_ghp])

    def head_norm_fm(l, src_ap, B_src, chunk, col0, T, tag):
        sq, B_sq, rs, B_rs, pstat, B_pstat = T
        S.op("act", lambda e: e.activation(out=sq[:], in_=src_ap, func=AF.Square), R=[B_src], W=[B_sq])
        S.op("pe", lambda e: e.matmul(pstat[:], lhsT=cfs("blk64"), rhs=sq[:], start=True, stop=True),
             R=[B_sq, B_cf], W=[B_pstat])
        S.op("act", lambda e: e.activation(out=rs[:], in_=pstat[:], func=AF.Ln, scale=1.0 / 64, bias=EPS),
             R=[B_pstat], W=[B_rs])
        S.op("act", lambda e: e.activation(out=rs[:], in_=rs[:], func=AF.Exp, scale=-0.5), R=[B_rs], W=[B_rs])
        S.op("dve", lambda e: e.scalar_tensor_tensor(out=Y["t"][:, chunk, col0:col0 + 512], in0=src_ap,
                                                     scalar=ghp[:, l, chunk:chunk + 1], in1=rs[:],
                                                     op0=ALU.mult, op1=ALU.mult),
             R=[B_src, B_rs, B_ghp], W=[B_ymix])

    def phase_sb(l, co=None):
        with ExitStack() as st:
            def T(name, shape, dt):
                return st.enter_context(nc.sbuf_tensor(name + "_L%d" % l, list(shape), dt))
            qT = T("sb_qT", [128, SEQ], BF16)
            kT = T("sb_kT", [128, SEQ], BF16)
            V = [T("sb_V%d" % i, [128, NT, 128], BF16) for i in range(2)]
            B_qT, B_kT, B_V = Buf("sb_qT"), Buf("sb_kT"), [Buf("sb_V0"), Buf("sb_V1")]
            C32 = [T("sb_C32_%d" % p, [128, 2, 512], F32) for p in range(2)]
            Cb = [T("sb_Cb_%d" % p, [128, 2, 512], BF16) for p in range(2)]
            B_C32 = [Buf("c32_%d" % p) for p in range(2)]
            B_Cb = [Buf("cb_%d" % p) for p in range(2)]
            e_sb = T("sb_e", [128, 2, 512], F32)
            B_e = Buf("sb_e")
            NSP, NA = 3, 2
            sp_sb = [T("sb_sp%d" % i, [128, 2, 512], BF16) for i in range(NSP)]
            B_sp = [Buf("sb_sp%d" % i) for i in range(NSP)]
            A_sb = [T("sb_A%d" % i, [128, 2, 512], BF16) for i in range(NA)]
            B_A = [Buf("sb_A%d" % i) for i in range(NA)]
            sq = T("sb_sq", [128, 512], F32)
            rs = T("sb_rs", [128, 512], F32)
            normT = (sq, Buf("sb_sq"), rs, Buf("sb_rs"), ps[0], B_ps[0])
            zP = pp[0][:].rearrange("p (h q) -> p h q", h=2)
            xP = pp[1][:].rearrange("p (h q) -> p h q", h=2)
            mstr2 = cbs("mstrict").unsqueeze(1).to_broadcast([128, 2, 128])
            for i in range(2):
                S.op("dve", lambda e, i=i: e.memset(V[i][:], 0.0), W=[B_V[i]])
            for c in range(3):
                S.dma("sp", qT[:], sqT_d[c * 128:(c + 1) * 128, :], R=[B_scr["sqT"]], W=[B_qT])
                S.dma("sp", kT[:], skT_d[c * 128:(c + 1) * 128, :], R=[B_scr["skT"]], W=[B_kT])
                for h in range(2):
                    S.dma("sp", V[h][:, :, h * 64:(h + 1) * 64],
                          sv_d[:, c * 128 + h * 64: c * 128 + (h + 1) * 64].rearrange("(t p) e -> p t e", p=128),
                          R=[B_scr["sv"]], W=[B_V[h]])
                tiles = []
                for sb in range(NSB):
                    for j in range(4 * sb + 3, -1, -1):
                        tiles.append((sb, j))
                n = len(tiles)

                def geom(t):
                    sb, j = tiles[t]
                    c0 = (j - 4 * sb) * 128 if j >= 4 * sb else 0
                    return sb, j, c0, (j >= 4 * sb), (j == 4 * sb + 3), (j == 0)

                def S1(t):
                    sb, j, c0, diag, first, last = geom(t)
                    par = sb % 2
                    if first:
                        S.op("dve", lambda e: e.memset(C32[par][:], 0.0), W=[B_C32[par]])
                        ob = 6 + par
                        S.op("pe", lambda e: e.matmul(ps[ob][:], lhsT=cbs("zeros"), rhs=qT[:, 0:512], start=True, stop=False),
                             R=[B_cb, B_qT], W=[B_ps[ob]])
                    S.op("pe", [lambda e, h=h: e.matmul(ps[h][:, c0:512], lhsT=kT[h * 64:(h + 1) * 64, j * 128:(j + 1) * 128],
                                                        rhs=qT[h * 64:(h + 1) * 64, sb * 512 + c0:(sb + 1) * 512], start=True, stop=True)
                                for h in range(2)], R=[B_kT, B_qT], W=[B_ps[0], B_ps[1]])

                def S2a(t):
                    sb, j, c0, diag, first, last = geom(t)
                    S.op("act", lambda e: e.activation(out=e_sb[:, :, c0:512], in_=zP[:, :, c0:512], func=AF.Exp),
                         R=[B_ps[0], B_ps[1]], W=[B_e])

                def S2(t):
                    sb, j, c0, diag, first, last = geom(t)
                    k = t % NSP
                    S.op("act", lambda e: e.activation(out=sp_sb[k][:, :, c0:512], in_=e_sb[:, :, c0:512], func=AF.Ln, bias=1.0),
                         R=[B_e], W=[B_sp[k]])
                    if diag:
                        S.op("dve", lambda e: e.tensor_tensor(out=sp_sb[k][:, :, c0:c0 + 128], in0=sp_sb[k][:, :, c0:c0 + 128],
                                                              in1=mstr2, op=ALU.mult), R=[B_sp[k], B_cb], W=[B_sp[k]])

                def S3(t):
                    sb, j, c0, diag, first, last = geom(t)
                    k = t % NSP
                    par = sb % 2
                    fns = []
                    for h in range(2):
                        r = slice(h * 64, (h + 1) * 64)
                        pb = 2 + h
                        fns.append(lambda e, r=r, pb=pb: e.matmul(ps[pb][:, c0:512], lhsT=kT[r, j * 128:(j + 1) * 128],
                                                                  rhs=qT[r, sb * 512 + c0:(sb + 1) * 512], start=True, stop=False))
                        fns.append(lambda e, h=h, pb=pb: e.matmul(ps[pb][:, c0:512], lhsT=cbs("negtri"), rhs=sp_sb[k][:, h, c0:512],
                                                                  start=False, stop=first))
                        if not first:
                            chi = C32[par][:, h, :].bitcast(BF16)[:, 2 * c0 + 1:1024:2]
                            fns.append(lambda e, chi=chi, pb=pb: e.matmul(ps[pb][:, c0:512], lhsT=cbs("negones"), rhs=chi,
                                                                          start=False, stop=True))
                    S.op("pe", fns, R=[B_kT, B_qT, B_cb, B_sp[k], B_C32[par]], W=[B_ps[2], B_ps[3]])

                def S4(t):
                    sb, j, c0, diag, first, last = geom(t)
                    a = t % NA
                    S.op("act", lambda e: e.activation(out=A_sb[a][:, :, c0:512], in_=xP[:, :, c0:512], func=AF.Exp),
                         R=[B_ps[2], B_ps[3]], W=[B_A[a]])
                    if diag:
                        S.op("dve", lambda e: e.tensor_tensor(out=A_sb[a][:, :, c0:c0 + 128], in0=A_sb[a][:, :, c0:c0 + 128],
                                                              in1=mstr2, op=ALU.mult), R=[B_A[a], B_cb], W=[B_A[a]])

                def S6(t):
                    sb, j, c0, diag, first, last = geom(t)
                    if last:
                        return
                    k = t % NSP
                    par = sb % 2
                    S.op("dve", lambda e: e.tensor_tensor(out=C32[par][:, :, c0:512], in0=C32[par][:, :, c0:512],
                                                          in1=sp_sb[k][:, :, c0:512], op=ALU.add),
                         R=[B_sp[k]], W=[B_C32[par]])


                def S5(t):
                    sb, j, c0, diag, first, last = geom(t)
                    a = t % NA
                    ob = 6 + (sb % 2)
                    S.op("pe", [lambda e, h=h: e.matmul(ps[ob][:, c0:512], lhsT=V[h][:, j, :], rhs=A_sb[a][:, h, c0:512],
                                                        start=False, stop=False) for h in range(2)],
                         R=[B_V[0], B_V[1], B_A[a]], W=[B_ps[ob]])
                    if last:
                        head_norm_fm(l, ps[ob][:], B_ps[ob], 5 + c, sb * 512, normT, "sb")

                for tau in range(n + 2):
                    if tau < n:
                        S1(tau)
                        S2a(tau)
                        S2(tau)
                    if 0 <= tau - 1 < n:
                        S3(tau - 1)
                        S4(tau - 1)
                        S6(tau - 1)
                    if 0 <= tau - 2 < n:
                        S5(tau - 2)
                    if co is not None and co[0] is not None and (tau % CO_SKIP[0] != CO_SKIP[0] - 1):
                        if next(co[0], "end") in ("hold", "end"):
                            co[0] = None
            if co is not None:
                while co[0] is not None:
                    if next(co[0], "end") in ("hold", "end"):
                        co[0] = None
            S.barrier()

    def phase_dil(l):
        with ExitStack() as st:
            def T(name, shape, dt):
                return st.enter_context(nc.sbuf_tensor(name + "_L%d" % l, list(shape), dt))
            qT = T("dl_qT", [128, SEQ], BF16)
            kT = T("dl_kT", [128, SEQ], BF16)
            Vs = [[T("dl_V%d_%d" % (i, pp), [128, NT, 128], BF16) for i in range(2)] for pp in range(2)]
            B_Vs = [[Buf("dl_V%d_%d" % (i, pp)) for i in range(2)] for pp in range(2)]
            B_qT, B_kT = Buf("dl_qT"), Buf("dl_kT")
            accn = T("dl_accn", [128, SEQ], F32)
            accd = T("dl_accd", [128, SEQ], F32)
            B_accn, B_accd = Buf("accn"), Buf("accd")
            NP_ = 8
            P_sb = [T("dl_P%d" % i, [128, 2, 128], BF16) for i in range(NP_)]
            B_P = [Buf("dl_P%d" % i) for i in range(NP_)]
            rsA = [T("dl_rsA%d" % i, [128, 512], F32) for i in range(4)]
            B_rsA = [Buf("dl_rsA%d" % i) for i in range(4)]
            sq2 = [T("dl_sq%d" % i, [128, 512], F32) for i in range(2)]
            B_sq2 = [Buf("dl_sq%d" % i) for i in range(2)]
            rsB = [T("dl_rsB%d" % i, [128, 512], F32) for i in range(2)]
            B_rsB = [Buf("dl_rsB%d" % i) for i in range(2)]
            for pp in range(2):
                for i in range(2):
                    S.op("dve", lambda e, i=i, pp=pp: e.memset(Vs[pp][i][:], 0.0), W=[B_Vs[pp][i]])
                S.op("dve", lambda e, pp=pp: e.memset(Vs[pp][0][:, :, 64:65], 1.0), W=[B_Vs[pp][0]])
                S.op("dve", lambda e, pp=pp: e.memset(Vs[pp][1][:, :, 0:1], 1.0), W=[B_Vs[pp][1]])
            eo, _ = CB["dilE"]
            vcnt = [0]

            def load_V(c, pi):
                win_, d_ = DIL_PATTERNS[pi]
                nb_ = (SEQ // d_) // 128
                pp = vcnt[0] % 2
                vcnt[0] += 1
                for h in range(2):
                    src = dv_d[:, c * 128 + h * 64: c * 128 + (h + 1) * 64].rearrange("(j i r) e -> i r j e", i=128, r=d_)
                    dst = Vs[pp][h][:, :, h * 64:(h + 1) * 64].rearrange("p (r j) e -> p r j e", r=d_)
                    if d_ <= nb_:
                        for r0 in range(d_):
                            S.dma("sp", dst[:, r0, :, :], src[:, r0, :, :], R=[B_scr["dv"]], W=[B_Vs[pp][h]])
                    else:
                        for j0 in range(nb_):
                            S.dma("sp", dst[:, :, j0, :], src[:, :, j0, :], R=[B_scr["dv"]], W=[B_Vs[pp][h]])
                return pp
            pending = {}
            pending[(0, 0)] = load_V(0, 0)
            for c in range(3):
                S.dma("sp", qT[:], dqT_d[c * 128:(c + 1) * 128, :], R=[B_scr["dqT"]], W=[B_qT])
                S.dma("sp", kT[:], dkT_d[c * 128:(c + 1) * 128, :], R=[B_scr["dkT"]], W=[B_kT])
                grp = [0]
                for pi, (win, d) in enumerate(DIL_PATTERNS):
                    L = SEQ // d
                    nblk = L // 128
                    pp_cur = pending.pop((c, pi))
                    V = Vs[pp_cur]
                    B_V = B_Vs[pp_cur]
                    nxt = (c, pi + 1) if pi + 1 < 3 else ((c + 1, 0) if c + 1 < 3 else None)
                    if nxt is not None:
                        pending[nxt] = load_V(*nxt)
                    gsz = min(4, nblk)
                    tiles = []
                    for r in range(d):
                        for g in range(nblk // gsz):
                            for nloc in range(gsz):
                                nq = g * gsz + nloc
                                for j in (nq - 1, nq):
                                    if j >= 0:
                                        tiles.append((r, g, nloc, nq, j))
                    n = len(tiles)
                    gpar = {}

                    def tok(r, blk):
                        a = r + d * 128 * blk
                        return slice(a, a + d * 127 + 1, d) if d > 1 else slice(a, a + 128)

                    def S1(t):
                        r, g, nloc, nq, j = tiles[t]
                        first = (nloc == 0 and j == max(nq - 1, 0))
                        if first:
                            gp = (grp[0] % 2)
                            pn, pd = 4 + 2 * gp, 5 + 2 * gp
                            grp[0] += 1
                            N = gsz * 128
                            S.op("dve", lambda e: e.memset(ps[pn][:, 0:N], 0.0), W=[B_ps[pn]])
                            S.op("dve", lambda e: e.memset(ps[pd][:, 0:N], 0.0), W=[B_ps[pd]])
                        gpar[t] = (grp[0] - 1) % 2
                        zb = 2 * (t % 2)
                        S.op("pe", [lambda e, h=h: e.matmul(ps[zb + h][:, 0:128], lhsT=kT[h * 64:(h + 1) * 64, tok(r, j)],
                                                            rhs=qT[h * 64:(h + 1) * 64, tok(r, nq)], start=True, stop=True)
                                    for h in range(2)], R=[B_kT, B_qT], W=[B_ps[zb], B_ps[zb + 1]])

                    def S2(t):
                        r, g, nloc, nq, j = tiles[t]
                        zb = 2 * (t % 2)
                        pq = t % NP_
                        zv = PP[t % 2][:].rearrange("p (h q) -> p h q", h=2)[:, :, 0:128]
                        S.op("act", lambda e: e.activation(out=P_sb[pq][:], in_=zv, func=AF.Exp),
                             R=[B_ps[zb], B_ps[zb + 1]], W=[B_P[pq]])
                        e0 = eo + ((c * 3 + pi) * 2) * 256
                        off = 0 if j == nq else 128
                        ev = cb[:, e0:e0 + 512].rearrange("p (h x) -> p h x", h=2)[:, :, off:off + 128]
                        S.op("dve", lambda e: e.tensor_tensor(out=P_sb[pq][:], in0=P_sb[pq][:], in1=ev, op=ALU.mult),
                             R=[B_P[pq], B_cb], W=[B_P[pq]])

                    def S3(t):
                        r, g, nloc, nq, j = tiles[t]
                        gp = gpar[t]
                        pa = t % NP_
                        pn, pd = 4 + 2 * gp, 5 + 2 * gp
                        cs = slice(nloc * 128, (nloc + 1) * 128)
                        S.op("pe", [lambda e, h=h: e.matmul(ps[(pn, pd)[h]][:, cs], lhsT=V[h][:, r * nblk + j, :], rhs=P_sb[pa][:, h, :],
                                                            start=False, stop=False) for h in range(2)],
                             R=[B_V[0], B_V[1], B_P[pa]], W=[B_ps[pn], B_ps[pd]])
                        last = (nloc == gsz - 1 and j == nq)
                        if last:
                            N = gsz * 128
                            a = r + d * 128 * gsz * g
                            dst = slice(a, a + d * (N - 1) + 1, d) if d > 1 else slice(a, a + N)
                            if pi == 0:
                                S.op("dve", lambda e: e.tensor_copy(out=accn[:, dst], in_=ps[pn][:, 0:N]), R=[B_ps[pn]], W=[B_accn])
                                S.op("dve", lambda e: e.tensor_copy(out=accd[:, dst], in_=ps[pd][:, 0:N]), R=[B_ps[pd]], W=[B_accd])
                            else:
                                S.op("dve", lambda e: e.tensor_tensor(out=accn[:, dst], in0=accn[:, dst], in1=ps[pn][:, 0:N], op=ALU.add),
                                     R=[B_ps[pn]], W=[B_accn])
                                S.op("dve", lambda e: e.tensor_tensor(out=accd[:, dst], in0=accd[:, dst], in1=ps[pd][:, 0:N], op=ALU.add),
                                     R=[B_ps[pd]], W=[B_accd])

                    DEP = 3
                    for tau in range(n + DEP):
                        if tau < n:
                            S1(tau)
                            S2(tau)
                        if 0 <= tau - DEP < n:
                            S3(tau - DEP)
                def NA(sb):
                    cs = slice(sb * 512, (sb + 1) * 512)
                    pb = sb % 4
                    S.op("pe", [lambda e: e.matmul(ps[pb][:], lhsT=cfs("selA"), rhs=accn[:, cs], start=True, stop=False),
                                lambda e: e.matmul(ps[pb][:], lhsT=cfs("selB"), rhs=accd[:, cs], start=False, stop=True)],
                         R=[B_accn, B_accd, B_cf], W=[B_ps[pb]])
                    S.op("act", lambda e: e.activation(out=rsA[pb][:], in_=ps[pb][:], func=AF.Ln), R=[B_ps[pb]], W=[B_rsA[pb]])
                    S.op("act", lambda e: e.activation(out=rsA[pb][:], in_=rsA[pb][:], func=AF.Exp, scale=-1.0), R=[B_rsA[pb]], W=[B_rsA[pb]])

                def NB_(sb):
                    cs = slice(sb * 512, (sb + 1) * 512)
                    pb = sb % 4
                    S.op("dve", lambda e: e.tensor_tensor(out=accn[0:64, cs], in0=accn[0:64, cs], in1=rsA[pb][0:64, :], op=ALU.mult),
                         R=[B_rsA[pb]], W=[B_accn])
                    S.op("dve", lambda e: e.tensor_tensor(out=accn[64:128, cs], in0=accd[64:128, cs], in1=rsA[pb][64:128, :], op=ALU.mult),
                         R=[B_rsA[pb], B_accd], W=[B_accn])

                def NC(sb):
                    cs = slice(sb * 512, (sb + 1) * 512)
                    i_ = sb % 2
                    pb = 4 + i_
                    S.op("act", lambda e: e.activation(out=sq2[i_][:], in_=accn[:, cs], func=AF.Square), R=[B_accn], W=[B_sq2[i_]])
                    S.op("pe", lambda e: e.matmul(ps[pb][:], lhsT=cfs("blk64"), rhs=sq2[i_][:], start=True, stop=True),
                         R=[B_sq2[i_], B_cf], W=[B_ps[pb]])
                    S.op("act", lambda e: e.activation(out=rsB[i_][:], in_=ps[pb][:], func=AF.Ln, scale=1.0 / 64, bias=EPS),
                         R=[B_ps[pb]], W=[B_rsB[i_]])
                    S.op("act", lambda e: e.activation(out=rsB[i_][:], in_=rsB[i_][:], func=AF.Exp, scale=-0.5), R=[B_rsB[i_]], W=[B_rsB[i_]])

                def ND(sb):
                    cs = slice(sb * 512, (sb + 1) * 512)
                    i_ = sb % 2
                    S.op("dve", lambda e: e.scalar_tensor_tensor(out=Y["t"][:, 2 + c, cs], in0=accn[:, cs], scalar=ghp[:, l, 2 + c:3 + c],
                                                                 in1=rsB[i_][:], op0=ALU.mult, op1=ALU.mult),
                         R=[B_accn, B_rsB[i_], B_ghp], W=[B_ymix])

                for st_ in range(NSB + 3):
                    if st_ < NSB:
                        NA(st_)
                    if 0 <= st_ - 1 < NSB:
                        NB_(st_ - 1)
                    if 0 <= st_ - 2 < NSB:
                        NC(st_ - 2)
                    if 0 <= st_ - 3 < NSB:
                        ND(st_ - 3)
            S.barrier()


    def gen_mlstm(l, bk):
        with ExitStack() as st:
            def T(name, shape, dt):
                return st.enter_context(nc.sbuf_tensor(name + "_L%d" % l, list(shape), dt))
            cw = T("ml_cw", [128, 2, 4], F32)
            cbi = T("ml_cb", [128, 2], F32)
            ncbi = T("ml_ncb", [128, 2], F32)
            gb = T("ml_gb", [128, 8], F32)
            ghr = T("ml_ghr", [128, 256], F32)
            B_small = Buf("ml_small")
            S.dma("sp", cw[:], conv_w[l, :, :, :], W=[B_small])
            S.dma("sp", cbi[:], conv_b[l, :, :], W=[B_small])
            S.dma("sp", gb[:], gbias[l, :, :], W=[B_small])
            S.dma("sp", ghr[:], g_head_r[l, :, :], W=[B_small])
            S.op("dve", lambda e: e.tensor_scalar(out=ncbi[:], in0=cbi[:], scalar1=-1.0, scalar2=None, op0=ALU.mult),
                 R=[B_small], W=[B_small])
            Wbd = {}
            B_W = Buf("ml_W")
            for nm, src in (("q", w_mq), ("k", w_mk), ("v", w_mv)):
                Wbd[nm] = T("ml_W" + nm, [128, 2, 128], BF16)
                S.op("dve", lambda e, nm=nm: e.memset(Wbd[nm][:], 0.0), W=[B_W])
                for c in range(2):
                    for hh in range(2):
                        S.dma("pool", Wbd[nm][hh * 64:(hh + 1) * 64, c, hh * 64:(hh + 1) * 64], src[l, 2 * c + hh, :, :], W=[B_W])
            graw = T("ml_graw", [128, NT, 8], F32)
            B_g = Buf("ml_graw")
            S.dma("sp", graw[:], gates_d[:, :].rearrange("(t p) c -> p t c", p=128), R=[B_scr["gates"]], W=[B_g])
            S.op("dve", lambda e: e.tensor_tensor(out=graw[:], in0=graw[:], in1=gb[:].unsqueeze(1).to_broadcast([128, NT, 8]),
                                                  op=ALU.add), R=[B_small], W=[B_g])
            nl = T("ml_nl", [128, NT, 4], F32)
            a_t = T("ml_a", [128, NT, 4], F32)
            b_t = T("ml_b", [128, NT, 4], F32)
            bl_t = T("ml_bl", [128, NT, 4], F32)
            B_nl, B_a, B_b, B_bl = Buf("nl"), Buf("a"), Buf("b"), Buf("bl")
            S.op("act", lambda e: e.activation(out=nl[:], in_=graw[:, :, 4:8], func=AF.Exp, scale=-1.0), R=[B_g], W=[B_nl])
            S.op("act", lambda e: e.activation(out=nl[:], in_=nl[:], func=AF.Ln, bias=1.0), R=[B_nl], W=[B_nl])
            nlf = nl[:].rearrange("p t h -> p (t h)")
            S.op("pe", lambda e: e.matmul(ps[bk["c0"]][:, 0:128], lhsT=cfs("triu"), rhs=nlf, start=True, stop=True),
                 R=[B_nl, B_cf], W=[B_ps[bk["c0"]]])
            S.op("pe", lambda e: e.matmul(ps[bk["c1"]][:, 0:128], lhsT=cfs("ones"), rhs=nlf, start=True, stop=True),
                 R=[B_nl, B_cf], W=[B_ps[bk["c1"]]])
            pc = ps[bk["c0"]][:, 0:128].rearrange("p (t h) -> p t h", h=4)
            S.op("dve", lambda e: e.tensor_tensor(out=a_t[:], in0=graw[:, :, 0:4], in1=pc, op=ALU.add), R=[B_g, B_ps[bk["c0"]]], W=[B_a])
            S.op("act", lambda e: e.activation(out=a_t[:], in_=a_t[:], func=AF.Exp), R=[B_a], W=[B_a])
            S.op("act", lambda e: e.activation(out=b_t[:], in_=pc, func=AF.Exp, scale=-1.0), R=[B_ps[bk["c0"]]], W=[B_b])
            S.op("act", lambda e: e.activation(out=bl_t[:], in_=ps[bk["c1"]][:, 0:128].rearrange("p (t h) -> p t h", h=4), func=AF.Exp,
                                               scale=-1.0), R=[B_ps[bk["c1"]]], W=[B_bl])
            C32 = T("ml_C32", [128, 2, 65], F32)
            Cbf = T("ml_Cbf", [128, 2, 65], BF16)
            B_C32, B_Cbf = Buf("ml_C32"), Buf("ml_Cbf")
            S.op("dve", lambda e: e.memset(C32[:], 0.0), W=[B_C32])
            S.op("dve", lambda e: e.memset(Cbf[:], 0.0), W=[B_Cbf])
            xmp = [T("ml_xmp%d" % i, [128, 2, 515], BF16) for i in range(2)]
            B_xmp = [Buf("ml_xmp%d" % i) for i in range(2)]
            ogt = [T("ml_og%d" % i, [128, 4, 256], BF16) for i in range(2)]
            B_ogt = [Buf("ml_og%d" % i) for i in range(2)]
            acc = T("ml_acc", [128, 2, 512], F32)
            ez = T("ml_ez", [128, 2, 512], F32)
            xc = T("ml_xc", [128, 2, 512], BF16)
            B_acc, B_ez, B_xc = Buf("ml_acc"), Buf("ml_ez"), Buf("ml_xc")
            qTs = T("ml_qT", [128, 2, 512], BF16)
            kTs = T("ml_kT", [128, 2, 512], BF16)
            B_qTs, B_kTs = Buf("ml_qT"), Buf("ml_kT")
            ktok = T("ml_ktok", [128, 256], BF16)
            Vaug = T("ml_Vaug", [128, 4, 65], BF16)
            swm = T("ml_swm", [128, 4, 128], BF16)
            B_ktok, B_Vaug, B_swm = Buf("ml_ktok"), Buf("ml_Vaug"), Buf("ml_swm")
            sm = T("ml_sm", [128, 16], F32)
            B_sm = Buf("ml_sm")
            eo = T("ml_eo", [128, 256], F32)
            t1 = T("ml_t1", [128, 256], F32)
            ysq = T("ml_ysq", [128, 256], F32)
            yn = T("ml_yn", [128, 256], BF16)
            B_eo, B_t1, B_ysq, B_yn = Buf("ml_eo"), Buf("ml_t1"), Buf("ml_ysq"), Buf("ml_yn")
            ctmp = T("ml_ctmp", [128, 2, 65], F32)
            B_ctmp = Buf("ml_ctmp")

            yield
            for sb in range(NSB if ML_CUT[0] > 1 else 0):
                i2 = sb % 2
                xm = xmp[i2]
                if sb == 0:
                    S.op("dve", lambda e: e.memset(xm[:, :, 0:3], 0.0), W=[B_xmp[i2]])
                    S.dma("sp", xm[:, :, 3:515], xmT_d[:, 0:512].rearrange("(c p) t -> p c t", p=128), R=[B_scr["xmT"]], W=[B_xmp[i2]])
                else:
                    S.dma("sp", xm[:, :, 0:515], xmT_d[:, sb * 512 - 3:(sb + 1) * 512].rearrange("(c p) t -> p c t", p=128),
                          R=[B_scr["xmT"]], W=[B_xmp[i2]])
                S.dma("sp", ogt[i2][:], og_d[sb * 512:(sb + 1) * 512, :].rearrange("(t p) c -> p t c", p=128), R=[B_scr["og"]],
                      W=[B_ogt[i2]])
                yield
                for c in range(2):
                    S.op("dve", lambda e, c=c: e.tensor_scalar(out=acc[:, c, :], in0=xm[:, c, 0:512], scalar1=cw[:, c, 0:1], scalar2=None,
                                                               op0=ALU.mult), R=[B_xmp[i2], B_small], W=[B_acc])
                    for j in range(1, 4):
                        S.op("dve", lambda e, c=c, j=j: e.scalar_tensor_tensor(out=acc[:, c, :], in0=xm[:, c, j:j + 512],
                                                                               scalar=cw[:, c, j:j + 1], in1=acc[:, c, :],
                                                                               op0=ALU.mult, op1=ALU.add),
                             R=[B_xmp[i2], B_small], W=[B_acc])
                    S.op("act", lambda e, c=c: e.activation(out=ez[:, c, :], in_=acc[:, c, :], func=AF.Exp, scale=-1.0,
                                                            bias=ncbi[:, c:c + 1]), R=[B_acc, B_small], W=[B_ez])
                    S.op("dve", lambda e, c=c: e.tensor_scalar(out=ez[:, c, :], in0=ez[:, c, :], scalar1=1.0, scalar2=None, op0=ALU.add),
                         R=[B_ez], W=[B_ez])
                    S.op("dve", lambda e, c=c: e.reciprocal(out=ez[:, c, :], in_=ez[:, c, :]), R=[B_ez], W=[B_ez])
                    S.op("dve", lambda e, c=c: e.scalar_tensor_tensor(out=xc[:, c, :], in0=acc[:, c, :], scalar=cbi[:, c:c + 1],
                                                                      in1=ez[:, c, :], op0=ALU.add, op1=ALU.mult),
                         R=[B_acc, B_ez, B_small], W=[B_xc])
                yield
                for c in range(2):
                    S.op("pe", lambda e, c=c: e.matmul(ps[bk["q"]][:], lhsT=Wbd["q"][:, c, :], rhs=xc[:, c, :], start=True, stop=True),
                         R=[B_W, B_xc], W=[B_ps[bk["q"]]])
                    S.op("act", lambda e, c=c: e.copy(out=qTs[:, c, :], in_=ps[bk["q"]][:]), R=[B_ps[bk["q"]]], W=[B_qTs])
                    S.op("pe", lambda e, c=c: e.matmul(ps[bk["k"]][:], lhsT=Wbd["k"][:, c, :], rhs=xc[:, c, :], start=True, stop=True),
                         R=[B_W, B_xc], W=[B_ps[bk["k"]]])
                    S.op("act", lambda e, c=c: e.activation(out=kTs[:, c, :], in_=ps[bk["k"]][:], func=AF.Copy, scale=0.125),
                         R=[B_ps[bk["k"]]], W=[B_kTs])
                for i in range(4 if ML_CUT[0] > 2 else 0):
                    cut = ML_CUT[0]
                    ci = sb * 4 + i
                    ts = slice(i * 128, (i + 1) * 128)
                    yield
                    S.op("pe", [lambda e, c=c: e.matmul(ps[bk["kt"]][:, c * 128:(c + 1) * 128], lhsT=xc[:, c, ts], rhs=Wbd["k"][:, c, :],
                                                        start=True, stop=True) for c in range(2)],
                         R=[B_xc, B_W], W=[B_ps[bk["kt"]]])
                    S.op("act", lambda e: e.activation(out=ktok[:], in_=ps[bk["kt"]][:, 0:256], func=AF.Copy, scale=0.125),
                         R=[B_ps[bk["kt"]]], W=[B_ktok])
                    S.op("pe", [lambda e, c=c: e.matmul(ps[bk["vt"]][:, c * 128:(c + 1) * 128], lhsT=xm[:, c, 3 + i * 128:3 + (i + 1) * 128],
                                                        rhs=Wbd["v"][:, c, :], start=True, stop=True) for c in range(2)],
                         R=[B_xmp[i2], B_W], W=[B_ps[bk["vt"]]])
                    S.op("dve", lambda e: e.tensor_tensor(out=Vaug[:, :, 0:64], in0=ps[bk["vt"]][:, 0:256].rearrange("p (h e) -> p h e", h=4),
                                                          in1=a_t[:, ci, :].unsqueeze(2).to_broadcast([128, 4, 64]), op=ALU.mult),
                         R=[B_ps[bk["vt"]], B_a], W=[B_Vaug])
                    S.op("dve", lambda e: e.tensor_copy(out=Vaug[:, :, 64], in_=a_t[:, ci, :]), R=[B_a], W=[B_Vaug])
                    if cut <= 3:
                        continue
                    yield
                    sbank = (bk["S0"], bk["S1"])
                    S.op("pe", [lambda e, h=h: e.matmul(ps[sbank[h % 2]][:, (h // 2) * 128:(h // 2 + 1) * 128],
                                                        lhsT=kTs[(h % 2) * 64:(h % 2 + 1) * 64, h // 2, ts],
                                                        rhs=qTs[(h % 2) * 64:(h % 2 + 1) * 64, h // 2, ts], start=True, stop=True)
                                for h in range(4)], R=[B_kTs, B_qTs], W=[B_ps[bk["S0"]], B_ps[bk["S1"]]])
                    for hh in range(2):
                        S.op("dve", lambda e, hh=hh: e.tensor_tensor(
                            out=swm[:, hh::2, :], in0=ps[sbank[hh]][:, 0:256].rearrange("p (c t) -> p c t", c=2),
                            in1=cbs("triu").unsqueeze(1).to_broadcast([128, 2, 128]), op=ALU.mult),
                            R=[B_ps[sbank[hh]], B_cb], W=[B_swm])
                    if cut <= 4:
                        continue
                    yield
                    fns = []
                    for h in range(4):
                        rows = slice((h % 2) * 64, (h % 2 + 1) * 64)
                        fns.append(lambda e, h=h, rows=rows: e.matmul(ps[bk["H"]][:, h * 65:(h + 1) * 65], lhsT=qTs[rows, h // 2, ts],
                                                                      rhs=Cbf[rows, h // 2, :], start=True, stop=False))
                        fns.append(lambda e, h=h: e.matmul(ps[bk["H"]][:, h * 65:(h + 1) * 65], lhsT=swm[:, h, :], rhs=Vaug[:, h, :],
                                                           start=False, stop=True))
                    S.op("pe", fns, R=[B_qTs, B_Cbf, B_swm, B_Vaug], W=[B_ps[bk["H"]]])
                    if cut <= 5:
                        continue
                    yield
                    S.op("pe", [lambda e, c=c: e.matmul(ps[bk["dC"]][:, c * 130:(c + 1) * 130], lhsT=ktok[:, c * 128:(c + 1) * 128],
                                                        rhs=Vaug[:, 2 * c:2 * c + 2, :].rearrange("p h e -> p (h e)"),
                                                        start=True, stop=True) for c in range(2)],
                         R=[B_ktok, B_Vaug], W=[B_ps[bk["dC"]]])
                    for c in range(2):
                        for hh in range(2):
                            rows = slice(hh * 64, (hh + 1) * 64)
                            S.op("dve", lambda e, c=c, hh=hh, rows=rows: e.tensor_tensor(
                                out=ctmp[rows, c, :], in0=C32[rows, c, :], in1=ps[bk["dC"]][rows, c * 130 + hh * 65:c * 130 + (hh + 1) * 65],
                                op=ALU.add), R=[B_C32, B_ps[bk["dC"]]], W=[B_ctmp])
                            S.op("dve", lambda e, c=c, hh=hh, rows=rows: e.tensor_scalar(
                                out=C32[rows, c, :], in0=ctmp[rows, c, :], scalar1=bl_t[rows, ci, 2 * c + hh:2 * c + hh + 1], scalar2=None,
                                op0=ALU.mult), R=[B_ctmp, B_bl], W=[B_C32])
                    S.op("dve", lambda e: e.tensor_copy(out=Cbf[:], in_=C32[:]), R=[B_C32], W=[B_Cbf])
                    if cut <= 6:
                        continue
                    yield
                    pH = ps[bk["H"]][:, 0:260].rearrange("p (h e) -> p h e", h=4)
                    S.op("dve", lambda e: e.tensor_tensor(out=sm[:, 0:4], in0=pH[:, :, 64], in1=b_t[:, ci, :], op=ALU.mult),
                         R=[B_ps[bk["H"]], B_b], W=[B_sm])
                    S.op("dve", lambda e: e.tensor_scalar(out=sm[:, 4:8], in0=sm[:, 0:4], scalar1=-1.0, scalar2=None, op0=ALU.mult),
                         R=[B_sm], W=[B_sm])
                    S.op("dve", lambda e: e.tensor_tensor(out=sm[:, 4:8], in0=sm[:, 4:8], in1=sm[:, 0:4], op=ALU.max),
                         R=[B_sm], W=[B_sm])
                    S.op("dve", lambda e: e.tensor_scalar(out=sm[:, 4:8], in0=sm[:, 4:8], scalar1=1.0, scalar2=None, op0=ALU.max),
                         R=[B_sm], W=[B_sm])
                    S.op("dve", lambda e: e.reciprocal(out=sm[:, 4:8], in_=sm[:, 4:8]), R=[B_sm], W=[B_sm])
                    S.op("dve", lambda e: e.tensor_tensor(out=sm[:, 8:12], in0=b_t[:, ci, :], in1=sm[:, 4:8], op=ALU.mult),
                         R=[B_sm, B_b], W=[B_sm])
                    yield
                    S.op("act", lambda e: e.activation(out=eo[:], in_=ogt[i2][:, i, :], func=AF.Exp, scale=-1.0), R=[B_ogt[i2]], W=[B_eo])
                    S.op("dve", lambda e: e.tensor_scalar(out=eo[:], in0=eo[:], scalar1=1.0, scalar2=None, op0=ALU.add), R=[B_eo], W=[B_eo])
                    S.op("dve", lambda e: e.tensor_tensor(out=t1[:].rearrange("p (h e) -> p h e", h=4), in0=pH[:, :, 0:64],
                                                          in1=sm[:, 8:12].unsqueeze(2).to_broadcast([128, 4, 64]), op=ALU.mult),
                         R=[B_ps[bk["H"]], B_sm], W=[B_t1])
                    S.op("dve", lambda e: e.reciprocal(out=eo[:], in_=eo[:]), R=[B_eo], W=[B_eo])
                    S.op("dve", lambda e: e.tensor_tensor(out=t1[:], in0=t1[:], in1=eo[:], op=ALU.mult), R=[B_t1, B_eo], W=[B_t1])
                    yield
                    S.op("dve", lambda e: e.tensor_tensor(out=ysq[:], in0=t1[:], in1=t1[:], op=ALU.mult), R=[B_t1], W=[B_ysq])
                    S.op("dve", lambda e: e.tensor_reduce(out=sm[:, 12:16], in_=ysq[:].rearrange("p (h e) -> p h e", h=4), axis=AX.X,
                                                          op=ALU.add), R=[B_ysq], W=[B_sm])
                    S.op("act", lambda e: e.activation(out=sm[:, 12:16], in_=sm[:, 12:16], func=AF.Ln, scale=1.0 / 64, bias=EPS),
                         R=[B_sm], W=[B_sm])
                    S.op("act", lambda e: e.activation(out=sm[:, 12:16], in_=sm[:, 12:16], func=AF.Exp, scale=-0.5), R=[B_sm], W=[B_sm])
                    S.op("dve", lambda e: e.tensor_tensor(out=t1[:].rearrange("p (h e) -> p h e", h=4),
                                                          in0=t1[:].rearrange("p (h e) -> p h e", h=4),
                                                          in1=sm[:, 12:16].unsqueeze(2).to_broadcast([128, 4, 64]), op=ALU.mult),
                         R=[B_sm], W=[B_t1])
                    S.op("dve", lambda e: e.tensor_tensor(out=yn[:], in0=t1[:], in1=ghr[:], op=ALU.mult), R=[B_t1, B_small], W=[B_yn])
                    if cut <= 7:
                        continue
                    yield
                    pT = ps[bk["T"]][:].bitcast(BF16)
                    S.op("pe", [lambda e, c=c: e.transpose(out=pT[:, c * 128:(c + 1) * 128], in_=yn[:, c * 128:(c + 1) * 128],
                                                           identity=cbs("ident")) for c in range(2)], R=[B_yn, B_cb], W=[B_ps[bk["T"]]])
                    S.op("act", lambda e: e.copy(out=Y["t"][:, 0:2, ci * 128:(ci + 1) * 128],
                                                 in_=pT[:, 0:256].rearrange("p (c t) -> p c t", c=2)), R=[B_ps[bk["T"]]], W=[B_ymix])
            yield "hold"


    ML_BANKS_ALONE = {"c0": 0, "c1": 1, "q": 0, "k": 1, "kt": 2, "vt": 3, "S0": 4, "S1": 0, "H": 5, "dC": 6, "T": 7}
    ML_BANKS_CO = {"c0": 4, "c1": 5, "q": 4, "k": 4, "kt": 4, "vt": 4, "S0": 4, "S1": 5, "H": 5, "dC": 4, "T": 4}

    def phase_mlstm(l):
        g = gen_mlstm(l, ML_BANKS_ALONE)
        for _ in g:
            pass
        S.barrier()

    def phase_outproj(l, x_src, B_xsrc, x_dst, B_xdst):
        with ExitStack() as st:
            def T(name, shape, dt):
                return st.enter_context(nc.sbuf_tensor(name + "_L%d" % l, list(shape), dt))
            wo = T("op_w", [128, 8, D], BF16)
            B_wo = Buf("op_w")
            for k in range(8):
                S.dma("pool", wo[:, k, :], w_out[l, k * 128:(k + 1) * 128, :], W=[B_wo])
            xt = [T("op_xt%d" % i, [128, D], F32) for i in range(4)]
            xo = [T("op_xo%d" % i, [128, D], F32) for i in range(4)]
            B_xt = [Buf("op_xt%d" % i) for i in range(4)]
            B_xo = [Buf("op_xo%d" % i) for i in range(4)]
            for t0 in range(2):
                S.dma("sp", xt[t0][:], x_src[t0 * 128:(t0 + 1) * 128, :], R=[B_xsrc], W=[B_xt[t0]])
            for ti in range(NT):
                i = ti % 4
                if ti + 2 < NT:
                    S.dma("sp", xt[(ti + 2) % 4][:], x_src[(ti + 2) * 128:(ti + 3) * 128, :], R=[B_xsrc], W=[B_xt[(ti + 2) % 4]])
                for hf in range(2):
                    pb = (ti * 2 + hf) % 8
                    cs = slice(hf * 512, (hf + 1) * 512)
                    S.op("pe", [lambda e, k=k: e.matmul(ps[pb][:], lhsT=Y["t"][:, k, ti * 128:(ti + 1) * 128], rhs=wo[:, k, cs],
                                                        start=(k == 0), stop=(k == 7)) for k in range(8)],
                         R=[B_ymix, B_wo], W=[B_ps[pb]])
                    S.op("dve", lambda e: e.tensor_tensor(out=xo[i][:, cs], in0=ps[pb][:], in1=mod[:, 2, cs], op=ALU.mult),
                         R=[B_ps[pb], B_mod], W=[B_xo[i]])
                    S.op("dve", lambda e: e.tensor_tensor(out=xo[i][:, cs], in0=xo[i][:, cs], in1=xt[i][:, cs], op=ALU.add),
                         R=[B_xt[i]], W=[B_xo[i]])
                S.dma("sp", x_dst[ti * 128:(ti + 1) * 128, :], xo[i][:], R=[B_xo[i]], W=[B_xdst])
            S.barrier()

    def phase_moe(l, x_src, B_xsrc, x_dst, B_xdst, final):
        with ExitStack() as st:
            def T(name, shape, dt):
                return st.enter_context(nc.sbuf_tensor(name + "_L%d" % l, list(shape), dt))
            wr = T("mo_wr", [128, 8, NEXP], F32)
            rb = T("mo_rb", [128, NEXP], F32)
            B_wr = Buf("mo_wr")
            S.dma("sp", wr[:], w_router.rearrange("(k p) e -> p k e", p=128), W=[B_wr])
            S.dma("sp", rb[:], rbias[:, :], W=[B_wr])
            gfin = None
            if final:
                gfin = mod[:, 0, :]
                S.dma("sp", gfin, g_final[:, :], W=[B_mod])
            h2T = [T("mo_h2T%d" % i, [128, 8, 1024], BF16) for i in range(2)]
            B_h2T = [Buf("mo_h2T%d" % i) for i in range(2)]
            yaccA = T("mo_yacc", [128, 8, D], F32)
            yaccB = T("mo_yaccB", [128, 4, D], F32)
            B_yaccA = [Buf("mo_yacc%d" % i) for i in range(8)]
            B_yaccB = [Buf("mo_yaccB%d" % i) for i in range(4)]

            def ysel(qt, tl):
                if tl < 4 and qt % 2 == 1:
                    return yaccB[:, tl, :], B_yaccB[tl]
                return yaccA[:, tl, :], B_yaccA[tl]
            combTok = [T("mo_combTok%d" % i, [128, 8, NEXP], F32) for i in range(2)]
            B_combT = [Buf("mo_combTok%d" % i) for i in range(2)]
            Wg = [T("mo_Wg%d" % i, [128, 8, DEXP], BF16) for i in range(2)]
            Wu = [T("mo_Wu%d" % i, [128, 8, DEXP], BF16) for i in range(2)]
            Wd = [T("mo_Wd%d" % i, [128, 4, D], BF16) for i in range(2)]
            B_Wg = [Buf("mo_Wg%d" % i) for i in range(2)]
            B_Wu = [Buf("mo_Wu%d" % i) for i in range(2)]
            B_Wd = [Buf("mo_Wd%d" % i) for i in range(2)]
            he = [T("mo_he%d" % i, [128, 4, 512], BF16) for i in range(2)]
            B_he = [Buf("mo_he%d" % i) for i in range(2)]
            sg = [T("mo_sg%d" % i, [128, 512], BF16) for i in range(2)]
            B_sg = [Buf("mo_sg%d" % i) for i in range(2)]
            xt = [T("mo_xt%d" % i, [128, D], F32) for i in range(2)]
            B_xt = [Buf("mo_xt%d" % i) for i in range(2)]
            tmp = T("mo_tmp", [128, D], F32)
            B_tmp = Buf("mo_tmp")
            h2f = [T("mo_h2f%d" % i, [128, D], F32) for i in range(2)]
            B_h2f = [Buf("mo_h2f%d" % i) for i in range(2)]
            h2Tf1 = T("mo_h2Tf", [128, 8, 128], F32)
            h2Tf = [h2Tf1, h2Tf1]
            B_h2Tf1 = Buf("mo_h2Tf")
            B_h2Tf = [B_h2Tf1, B_h2Tf1]
            ss = [T("mo_ss%d" % i, [128, 4], F32) for i in range(2)]
            B_ss = [Buf("mo_ss%d" % i) for i in range(2)]
            ssf = T("mo_ssf", [128, 8, 4], F32)
            B_ssf = [Buf("mo_ssf%d" % i) for i in range(8)]
            rt = [T("mo_rt%d" % i, [128, 8, 64], F32) for i in range(2)]
            B_rt = [Buf("mo_rt%d" % i) for i in range(2)]
            PR = 7

            def load_gu(e):
                i = e % 2
                S.dma("pool", Wg[i][:], w_gate[l, e, :, :].rearrange("(k p) f -> p k f", p=128), W=[B_Wg[i]])
                S.dma("pool", Wu[i][:], w_up[l, e, :, :].rearrange("(k p) f -> p k f", p=128), W=[B_Wu[i]])

            def load_d(e):
                i = e % 2
                S.dma("pool", Wd[i][:], w_down[l, e, :, :].rearrange("(k p) d -> p k d", p=128), W=[B_Wd[i]])

            def load_w(e):
                load_gu(e)
                load_d(e)

            def Ra(qt, tt):
                ti = qt * 8 + tt
                i = ti % 2
                S.dma("sp", xt[i][:], x_src[ti * 128:(ti + 1) * 128, :], R=[B_xsrc], W=[B_xt[i]])
                norm_tile(xt[i], B_xt[i], 3, tmp, B_tmp, h2f[i], B_h2f[i], ss[i], B_ss[i])

            def Rb(qt, tt, halves=(0, 1)):
                ti = qt * 8 + tt
                i = ti % 2
                qb = qt % 2
                for half in halves:
                    S.op("pe", [lambda e, c=c: e.transpose(out=ps[PR][:, (c % 4) * 128:(c % 4 + 1) * 128],
                                                           in_=h2f[i][:, c * 128:(c + 1) * 128], identity=cfs("ident"))
                                for c in range(half * 4, half * 4 + 4)], R=[B_h2f[i], B_cf], W=[B_ps[PR]])
                    S.op("act", lambda e: e.copy(out=h2T[qb][:, half * 4:half * 4 + 4, tt * 128:(tt + 1) * 128],
                                                 in_=ps[PR][:].rearrange("p (c t) -> p c t", c=4)), R=[B_ps[PR]], W=[B_h2T[qb]])
                    S.op("dve", lambda e: e.tensor_copy(out=h2Tf[i][:, half * 4:half * 4 + 4, :],
                                                        in_=ps[PR][:].rearrange("p (c t) -> p c t", c=4)), R=[B_ps[PR]], W=[B_h2Tf[i]])

            def Rb2(qt, tt):
                ti = qt * 8 + tt
                i = ti % 2
                bi = (ti // 4) % 2
                S.op("pe", [lambda e, k=k: e.matmul(ps[PR][:, 0:16], lhsT=h2Tf[i][:, k, :], rhs=wr[:, k, :], start=(k == 0), stop=(k == 7))
                            for k in range(8)], R=[B_h2Tf[i], B_wr], W=[B_ps[PR]])
                S.op("act", lambda e: e.activation(out=rt[bi][:, 0, (tt % 4) * 16:(tt % 4 + 1) * 16], in_=ps[PR][:, 0:16], func=AF.Exp,
                                                   scale=-1.0), R=[B_ps[PR]], W=[B_rt[bi]])
                if tt % 4 == 3:
                    RT(qt, tt // 4, bi)

            def RT(qt, b, bi):
                qb = qt % 2
                r_ = rt[bi]
                sc, g, eq, g2, sel, w_ = [r_[:, k_, :] for k_ in range(6)]
                m1, m2, gs, gmk = r_[:, 6, 0:16], r_[:, 6, 16:32], r_[:, 6, 32:48], r_[:, 6, 48:64]
                gmx, wsum = r_[:, 7, 0:4], r_[:, 7, 4:8]

                def vg(a):
                    return a.rearrange("p (g e) -> p g e", e=4)

                def vt(a):
                    return a.rearrange("p (t e) -> p t e", e=16)

                def bg(a):
                    return a.unsqueeze(2).to_broadcast([128, 16, 4])

                def t4(a):
                    return a.rearrange("p (t g) -> p t g", g=4)
                ops = [
                    lambda e: e.tensor_scalar(out=sc, in0=sc, scalar1=1.0, scalar2=None, op0=ALU.add),
                    lambda e: e.reciprocal(out=sc, in_=sc),
                    lambda e: e.tensor_tensor(out=vt(g), in0=vt(sc), in1=rb[:].unsqueeze(1).to_broadcast([128, 4, 16]), op=ALU.add),
                    lambda e: e.tensor_reduce(out=m1, in_=vg(g), axis=AX.X, op=ALU.max),
                    lambda e: e.tensor_tensor(out=vg(eq), in0=vg(g), in1=bg(m1), op=ALU.is_equal),
                    lambda e: e.scalar_tensor_tensor(out=g2, in0=eq, scalar=-1.0e9, in1=g, op0=ALU.mult, op1=ALU.add),
                    lambda e: e.tensor_reduce(out=m2, in_=vg(g2), axis=AX.X, op=ALU.max),
                    lambda e: e.tensor_tensor(out=gs, in0=m1, in1=m2, op=ALU.add),
                    lambda e: e.tensor_reduce(out=gmx, in_=t4(gs), axis=AX.X, op=ALU.max),
                    lambda e: e.tensor_tensor(out=t4(gmk), in0=t4(gs), in1=gmx.unsqueeze(2).to_broadcast([128, 4, 4]), op=ALU.is_ge),
                    lambda e: e.tensor_tensor(out=vg(sel), in0=vg(g), in1=bg(m2), op=ALU.is_ge),
                    lambda e: e.tensor_tensor(out=vg(sel), in0=vg(sel), in1=bg(gmk), op=ALU.mult),
                    lambda e: e.tensor_tensor(out=w_, in0=sc, in1=sel, op=ALU.mult),
                    lambda e: e.tensor_reduce(out=wsum, in_=vt(w_), axis=AX.X, op=ALU.add),
                    lambda e: e.reciprocal(out=wsum, in_=wsum),
                ]
                for f_ in ops:
                    S.op("dve", f_, R=[B_rt[bi], B_wr], W=[B_rt[bi]])
                S.op("dve", lambda e: e.tensor_tensor(out=combTok[qb][:, 4 * b:4 * b + 4, :], in0=vt(w_),
                                                      in1=wsum.unsqueeze(2).to_broadcast([128, 4, 16]), op=ALU.mult),
                     R=[B_rt[bi]], W=[B_combT[qb]])

            units = [(e_, s2) for e_ in range(NEXP) for s2 in range(2)]

            def GU(qt, u):
                e_, s2 = units[u]
                wi = e_ % 2
                qb = qt % 2
                ci_ = u % 2
                for f in range(4):
                    pg, pu = (0, 1) if f % 2 == 0 else (2, 3)
                    fs = slice(f * 128, (f + 1) * 128)
                    S.op("pe", [lambda e, k=k: e.matmul(ps[pg][:], lhsT=Wg[wi][:, k, fs], rhs=h2T[qb][:, k, s2 * 512:(s2 + 1) * 512],
                                                        start=(k == 0), stop=(k == 7)) for k in range(8)],
                         R=[B_Wg[wi], B_h2T[qb]], W=[B_ps[pg]])
                    S.op("pe", [lambda e, k=k: e.matmul(ps[pu][:], lhsT=Wu[wi][:, k, fs], rhs=h2T[qb][:, k, s2 * 512:(s2 + 1) * 512],
                                                        start=(k == 0), stop=(k == 7)) for k in range(8)],
                         R=[B_Wu[wi], B_h2T[qb]], W=[B_ps[pu]])
                    j = f % 2
                    S.op("act", lambda e: e.activation(out=sg[j][:], in_=ps[pg][:], func=AF.Silu), R=[B_ps[pg]], W=[B_sg[j]])
                    S.op("dve", lambda e: e.tensor_tensor(out=he[ci_][:, f, :], in0=ps[pu][:], in1=sg[j][:], op=ALU.mult),
                         R=[B_ps[pu], B_sg[j]], W=[B_he[ci_]])

            def DOWN(qt, u):
                e_, s2 = units[u]
                wi = e_ % 2
                ci_ = u % 2
                qb = qt % 2
                for t4 in range(4):
                    tl = s2 * 4 + t4
                    for dh in range(2):
                        py = 4 + ((t4 * 2 + dh) % 3)
                        ds_ = slice(dh * 512, (dh + 1) * 512)
                        S.op("pe", [lambda e, f=f: e.matmul(ps[py][:], lhsT=he[ci_][:, f, t4 * 128:(t4 + 1) * 128], rhs=Wd[wi][:, f, ds_],
                                                            start=(f == 0), stop=(f == 3)) for f in range(4)],
                             R=[B_he[ci_], B_Wd[wi]], W=[B_ps[py]])
                        cw_ = combTok[qb][:, tl, e_:e_ + 1]
                        ya_, B_ya = ysel(qt, tl)
                        if e_ == 0:
                            S.op("dve", lambda e: e.tensor_scalar(out=ya_[:, ds_], in0=ps[py][:], scalar1=cw_, scalar2=None, op0=ALU.mult),
                                 R=[B_ps[py], B_combT[qb]], W=[B_ya])
                        else:
                            S.op("dve", lambda e: e.scalar_tensor_tensor(out=ya_[:, ds_], in0=ps[py][:], scalar=cw_, in1=ya_[:, ds_],
                                                                         op0=ALU.mult, op1=ALU.add),
                                 R=[B_ps[py], B_combT[qb]], W=[B_ya])

            def EPIa(qt, tt):
                ti = qt * 8 + tt
                ya_, B_ya = ysel(qt, tt)
                xe, B_xe = xt[tt % 2], B_xt[tt % 2]
                S.dma("sp", xe[:], x_src[ti * 128:(ti + 1) * 128, :], R=[B_xsrc], W=[B_xe])
                S.op("pool", lambda e: e.tensor_tensor(out=ya_, in0=ya_, in1=mod[:, 5, :], op=ALU.mult), R=[B_mod], W=[B_ya])
                S.op("pool", lambda e: e.tensor_tensor(out=ya_, in0=ya_, in1=xe[:], op=ALU.add), R=[B_xe], W=[B_ya])
                if final:
                    s_ = ssf[:, tt, :]
                    S.op("act", lambda e: e.activation(out=xe[:], in_=ya_, func=AF.Square, accum_out=s_[:, 0:1]),
                         R=[B_ya], W=[B_xe, B_ssf[tt]])
                    S.op("act", lambda e: e.activation(out=s_[:, 1:2], in_=s_[:, 0:1], func=AF.Ln, scale=1.0 / D, bias=EPS),
                         R=[B_ssf[tt]], W=[B_ssf[tt]])
                    S.op("act", lambda e: e.activation(out=s_[:, 2:3], in_=s_[:, 1:2], func=AF.Exp, scale=-0.5), R=[B_ssf[tt]], W=[B_ssf[tt]])
                else:
                    S.dma("sp", x_dst[ti * 128:(ti + 1) * 128, :], ya_, R=[B_ya], W=[B_xdst])

            def EPIb(qt, tt):
                if not final:
                    return
                ti = qt * 8 + tt
                ya_, B_ya = ysel(qt, tt)
                s_ = ssf[:, tt, :]
                S.op("dve", lambda e: e.scalar_tensor_tensor(out=ya_, in0=ya_, scalar=s_[:, 2:3], in1=gfin, op0=ALU.mult, op1=ALU.mult),
                     R=[B_ssf[tt], B_mod], W=[B_ya])
                S.dma("sp", x_dst[ti * 128:(ti + 1) * 128, :], ya_, R=[B_ya], W=[B_xdst])

            load_w(0)
            load_w(1)
            for tt in range(9):
                if tt < 8:
                    Ra(0, tt)
                if tt >= 1:
                    Rb2(0, tt - 1)
                if tt < 8:
                    Rb(0, tt)
            for qt in range(4):
                sched = {}
                if qt + 1 < 4:
                    for tt in range(8):
                        sched.setdefault(3 + 3 * tt, []).append(("a", tt))
                        sched.setdefault(4 + 3 * tt, []).append(("b", tt))
                        sched.setdefault(5 + 3 * tt, []).append(("b2", tt))
                GU(qt, 0)
                if qt > 0:
                    for tt in range(4, 8):
                        EPIa(qt - 1, tt)
                for u in range(len(units)):
                    if u + 1 < len(units):
                        GU(qt, u + 1)
                    e_u, s_u = units[u]
                    if s_u == 0 and (e_u + 2 < NEXP or qt + 1 < 4):
                        load_gu((e_u + 2) % NEXP)
                    if qt > 0 and u == 1:
                        for tt in range(4, 8):
                            EPIb(qt - 1, tt)
                    if qt > 0 and 1 <= u <= 4:
                        EPIa(qt - 1, u - 1)
                    if qt > 0 and 2 <= u <= 5:
                        EPIb(qt - 1, u - 2)
                    post = []
                    for kind, tt in sched.get(u, []):
                        if kind == "b":
                            Rb(qt + 1, tt, halves=(0,))
                            post.append(tt)
                        else:
                            {"a": Ra, "b2": Rb2}[kind](qt + 1, tt)
                    DOWN(qt, u)
                    for tt in post:
                        Rb(qt + 1, tt, halves=(1,))
                    if s_u == 1 and (e_u + 2 < NEXP or qt + 1 < 4):
                        load_d((e_u + 2) % NEXP)
                    if qt == 3 and e_u == NEXP - 1 and s_u == 0:
                        for tt in range(4):
                            EPIa(qt, tt)
                if qt == 3:
                    for tt in range(4):
                        EPIb(qt, tt)
                    for tt in range(4, 8):
                        EPIa(qt, tt)
                    for tt in range(4, 8):
                        EPIb(qt, tt)
            S.barrier()

    def dump_dram(name, src, B_src, shape, dt):
        t = dbg_out(name, shape, dt)
        b = Buf("dbg_" + name)
        S.dma("sp", t, src, R=[B_src], W=[b])
        fin_bufs.append(b)

    def dump_sbuf(name, src_ap, B_src, shape, dt):
        t = dbg_out(name, shape, dt)
        b = Buf("dbg_" + name)
        S.dma("sp", t, src_ap, R=[B_src], W=[b])
        fin_bufs.append(b)

    B_xin, B_y = Buf("x_in"), Buf("y_out")
    x_cur, B_xcur = x_in, B_xin
    if isinstance(stop_after, str) and stop_after.startswith("only:"):
        ph = stop_after[5:]
        l = 0
        if ph == "mod":
            phase_mod(0)
        elif ph == "inproj":
            with ExitStack() as wst:
                win_, B_win_ = load_win(0, wst)
                phase_inproj(0, x_in, B_xin, win_, B_win_)
        elif ph == "modin":
            with ExitStack() as wst:
                win_, B_win_ = load_win(0, wst)
                phase_mod(0)
                phase_inproj(0, x_in, B_xin, win_, B_win_)
        elif ph == "sbml":
            with ExitStack() as lay:
                Y["t"] = lay.enter_context(nc.sbuf_tensor("ymixT_L%d" % l, [128, 8, SEQ], BF16))
                g_ml = gen_mlstm(0, ML_BANKS_CO)
                next(g_ml)
                phase_sb(0, co=[g_ml])
                for _ in g_ml:
                    pass
        elif ph in ("ml", "dil", "sb", "outproj"):
            with ExitStack() as lay:
                Y["t"] = lay.enter_context(nc.sbuf_tensor("ymixT_L%d" % l, [128, 8, SEQ], BF16))
                {"ml": phase_mlstm, "dil": phase_dil, "sb": phase_sb}.get(ph, lambda l_: phase_outproj(0, x_in, B_xin, xA, B_xA))(0)
        elif ph == "moe":
            phase_moe(0, x_in, B_xin, xB, B_xB, False)
        S.barrier()
        return nc, out_tensors
    for l in range(DEPTH):
        if stop_after == "mlonly%d" % l:
            with ExitStack() as lay:
                Y["t"] = lay.enter_context(nc.sbuf_tensor("ymixT_L%d" % l, [128, 8, SEQ], BF16))
                phase_mlstm(l)
            break
        with ExitStack() as wst:
            win_, B_win_ = load_win(l, wst)
            phase_mod(l)
            phase_inproj(l, x_cur, B_xcur, win_, B_win_)
        with ExitStack() as lay:
            Y["t"] = lay.enter_context(nc.sbuf_tensor("ymixT_L%d" % l, [128, 8, SEQ], BF16))
            phase_dil(l)
            g_ml = gen_mlstm(l, ML_BANKS_CO)
            next(g_ml)
            phase_sb(l, co=[g_ml])
            for _ in g_ml:
                pass
            if stop_after == "mix%d" % l:
                dump_sbuf("ymixT", Y["t"][:].rearrange("p c t -> p (c t)"), B_ymix, [128, 8 * SEQ], BF16)
                S.wait_all("sp", fin_bufs)
                S.barrier()
                break
            phase_outproj(l, x_cur, B_xcur, xA, B_xA)
        if stop_after == "x1_%d" % l:
            dump_dram("x1", xA, B_xA, [SEQ, D], F32)
            break
        last = (l == DEPTH - 1)
        if last:
            phase_moe(l, xA, B_xA, y_out, B_y, True)
        else:
            phase_moe(l, xA, B_xA, xB, B_xB, False)
            x_cur, B_xcur = xB, B_xB
        if stop_after == "x2_%d" % l:
            dump_dram("x2", xB, B_xB, [SEQ, D], F32)
            break

    S.wait_all("sp", fin_bufs + [B_y])
    S.barrier()
    return nc, out_tensors


def prep_inputs(b, inp, consts):
    cbn, cfn = consts
    f = np.float32
    d = {
        "x": np.ascontiguousarray(inp["x"][b]),
        "c_lay": np.ascontiguousarray(inp["c"][b].reshape(8, 128).T),
        "w_in": inp["w_in"],
        "conv_w": np.ascontiguousarray(inp["conv_w"].reshape(DEPTH, 4, 2, 128).transpose(0, 3, 2, 1)),
        "conv_b": np.ascontiguousarray(inp["conv_b"].reshape(DEPTH, 2, 128).transpose(0, 2, 1)),
        "w_mq": inp["w_mq"], "w_mk": inp["w_mk"], "w_mv": inp["w_mv"],
        "gbias": np.ascontiguousarray(np.broadcast_to(inp["gate_bias"].reshape(DEPTH, 1, 8), (DEPTH, 128, 8))),
        "g_head_p": np.ascontiguousarray(inp["g_head"].reshape(DEPTH, 8, 128).transpose(0, 2, 1)),
        "g_head_r": np.ascontiguousarray(np.broadcast_to(inp["g_head"][:, None, :256], (DEPTH, 128, 256))),
        "w_out": inp["w_out"],
        "w_ada": inp["w_ada"],
        "b_ada": np.ascontiguousarray(inp["b_ada"].reshape(DEPTH, 1, 6 * D)),
        "w_router": inp["w_router"],
        "rbias": np.ascontiguousarray(np.broadcast_to(inp["router_bias"][None, :], (128, NEXP))),
        "w_gate_e": inp["w_gate_e"], "w_up_e": inp["w_up_e"], "w_down_e": inp["w_down_e"],
        "g_final_r": np.ascontiguousarray(np.broadcast_to(inp["g_final"][None, :], (128, D))),
        "cb": cbn, "cf": cfn,
    }
    return {k: np.ascontiguousarray(v, dtype=f) for k, v in d.items()}


def kernel(**inputs):
    inp = {k: np.asarray(v) for k, v in inputs.items()}
    nc, _ = build_program()
    consts = make_consts()
    in_maps = [prep_inputs(b, inp, consts) for b in range(8)]
    res = run_bass_kernel_spmd(nc, in_maps, core_ids=list(range(8)))
    return np.stack([np.asarray(r["y"]) for r in res.results], axis=0).astype(np.float32)
```

```python
import math
import numpy as np
from contextlib import ExitStack
import concourse.bass as bass
import concourse.mybir as mybir
from concourse.bass_utils import run_bass_kernel_spmd

F32 = mybir.dt.float32
BF16 = mybir.dt.bfloat16
AF = mybir.ActivationFunctionType
ALU = mybir.AluOpType
AX = mybir.AxisListType

SEQ = 4096
D = 1024
DEPTH = 2
NT = SEQ // 128
NSB = SEQ // 512
DIN = 2824
NEXP = 16
DEXP = 512
EPS = 1e-6
OFF_O = 256
OFF_DIL = 520
OFF_SB = 520 + 1152
DIL_PATTERNS = ((128, 1), (512, 4), (2048, 16))


class Buf:
    __slots__ = ("name", "w", "r", "excl")

    def __init__(self, name, excl=False):
        self.name = name
        self.w = None
        self.r = []
        self.excl = excl


class _Eng:
    def __init__(self, name, e, sem):
        self.name = name
        self.e = e
        self.sem = sem
        self.cnt = 0
        self.known = {}


class Sched:
    def __init__(self, nc, n_dma_sems=32):
        self.nc = nc
        self.sems = []
        self.eng = {}
        for name, e in (("pe", nc.tensor), ("act", nc.scalar), ("dve", nc.vector),
                        ("pool", nc.gpsimd), ("sp", nc.sync)):
            sem = nc.semaphore("s_" + name).__enter__()
            self.sems.append(sem)
            self.eng[name] = _Eng(name, e, len(self.sems) - 1)
        self.dma_sems = []
        for i in range(n_dma_sems):
            sem = nc.semaphore("s_dma%d" % i).__enter__()
            self.sems.append(sem)
            self.dma_sems.append([len(self.sems) - 1, 0])
        self.dma_rr = 0
        self.ninstr = 0

    def _deps(self, R, W):
        deps = {}

        def add(t):
            if t is None:
                return
            s, v = t
            if deps.get(s, 0) < v:
                deps[s] = v
        for b in R:
            add(b.w)
            if b.excl:
                for t in b.r:
                    add(t)
        for b in W:
            add(b.w)
            for t in b.r:
                add(t)
        return deps

    def _wait(self, E, deps):
        for s, v in deps.items():
            if E.known.get(s, 0) >= v:
                continue
            E.e.wait_ge(self.sems[s], v)
            E.known[s] = v
            self.ninstr += 1

    def _mark(self, R, W, ticket):
        for b in R:
            if b.excl:
                b.w = ticket
                b.r = []
            else:
                b.r.append(ticket)
                if len(b.r) > 32:
                    m = {}
                    for s, v in b.r:
                        if m.get(s, 0) < v:
                            m[s] = v
                    b.r = list(m.items())
        for b in W:
            b.w = ticket
            b.r = []

    def op(self, eng, fns, R=(), W=()):
        E = self.eng[eng]
        if not isinstance(fns, (list, tuple)):
            fns = [fns]
        self._wait(E, self._deps(R, W))
        ins = None
        for f in fns:
            ins = f(E.e)
            self.ninstr += 1
        E.cnt += 1
        ins.then_inc(self.sems[E.sem], 1)
        t = (E.sem, E.cnt)
        self._mark(R, W, t)
        return t

    def dma(self, eng, out, in_, R=(), W=()):
        E = self.eng[eng]
        slot = self.dma_sems[self.dma_rr]
        self.dma_rr = (self.dma_rr + 1) % len(self.dma_sems)
        s, v = slot
        deps = self._deps(R, W)
        if v > 0 and deps.get(s, 0) < v:
            deps[s] = v
        self._wait(E, deps)
        ins = E.e.dma_start(out=out, in_=in_)
        slot[1] = v + 16
        ins.then_inc(self.sems[s], 16)
        self.ninstr += 1
        t = (s, v + 16)
        self._mark(R, W, t)
        return t

    def wait_all(self, eng, bufs):
        E = self.eng[eng]
        self._wait(E, self._deps(bufs, bufs))

    def barrier(self):
        tot = {}
        for E in self.eng.values():
            if E.cnt:
                tot[E.sem] = E.cnt
        for s, v in self.dma_sems:
            if v:
                tot[s] = v
        for E in self.eng.values():
            self._wait(E, dict(tot))


CB = {}
CF = {}


def _layout(table, items):
    off = 0
    for name, w in items:
        table[name] = (off, w)
        off += w
    return off


NCB = _layout(CB, [("ident", 128), ("ones", 128), ("negtri", 128), ("negones", 128),
                   ("mstrict", 128), ("triu", 128), ("onesA", 128), ("onesB", 128),
                   ("blk64", 128), ("zeros", 128), ("dilE", 18 * 256)])
NCF = _layout(CF, [("ident", 128), ("ones", 128), ("triu", 128), ("blk64", 128), ("selA", 128), ("selB", 128)])


def make_consts():
    p = np.arange(128)[:, None].astype(np.float64)
    f = np.arange(128)[None, :].astype(np.float64)
    cb = np.zeros((128, NCB), np.float32)
    cf = np.zeros((128, NCF), np.float32)

    def put(tab, lay, name, val):
        o, w = lay[name]
        tab[:, o:o + w] = val
    ident = (p == f).astype(np.float32)
    ones = np.ones((128, 128), np.float32)
    triu = (p <= f).astype(np.float32)
    blk64 = ((p // 64) == (f // 64)).astype(np.float32)
    put(cb, CB, "ident", ident)
    put(cb, CB, "ones", ones)
    put(cb, CB, "negtri", -(p >= f).astype(np.float32))
    put(cb, CB, "negones", -ones)
    put(cb, CB, "mstrict", (p < f).astype(np.float32))
    put(cb, CB, "triu", triu)
    put(cb, CB, "onesA", (f < 64).astype(np.float32) * ones)
    put(cb, CB, "onesB", (f >= 64).astype(np.float32) * ones)
    put(cb, CB, "blk64", blk64)
    mk = np.arange(128)[:, None].astype(np.float64)
    mq = np.arange(256)[None, :].astype(np.float64)
    dlt = mq - mk
    valid = (dlt >= 0) & (dlt <= 128)
    o, _ = CB["dilE"]
    for h in range(6):
        slope = 2.0 ** (-8.0 * (h + 1) / 6.0)
        for pi, (win, dil) in enumerate(DIL_PATTERNS):
            e = np.where(valid, np.exp(-slope * dil * dlt), 0.0)
            ix = ((h // 2) * 3 + pi) * 2 + (h % 2)
            cb[:, o + ix * 256: o + (ix + 1) * 256] = e
    put(cf, CF, "ident", ident)
    put(cf, CF, "ones", ones)
    put(cf, CF, "triu", triu)
    put(cf, CF, "blk64", blk64)
    selA = np.zeros((128, 128), np.float32); selA[64, 0:64] = 1.0
    selB = np.zeros((128, 128), np.float32); selB[0, 64:128] = 1.0
    put(cf, CF, "selA", selA)
    put(cf, CF, "selB", selB)
    return cb, cf


ML_CUT = [99]
MOE_DBG = [0]
CO_EVERY = [1]
CO_SKIP = [10 ** 9]
SB_DEPTH = [2, 3]


def build_program(stop_after=None, debug=()):
    nc = bass.Bass("TRN2", target_bir_lowering=False)
    S = Sched(nc)
    dbg = {}

    def din(name, shape, dt=F32):
        return nc.dram_tensor(name, list(shape), dt, kind="ExternalInput").ap()

    def dscr(name, shape, dt):
        return nc.dram_tensor(name, list(shape), dt, kind="Internal").ap()

    x_in = din("x", [SEQ, D])
    c_lay = din("c_lay", [128, 8])
    w_in = din("w_in", [DEPTH, D, DIN])
    conv_w = din("conv_w", [DEPTH, 128, 2, 4])
    conv_b = din("conv_b", [DEPTH, 128, 2])
    w_mq = din("w_mq", [DEPTH, 4, 64, 64])
    w_mk = din("w_mk", [DEPTH, 4, 64, 64])
    w_mv = din("w_mv", [DEPTH, 4, 64, 64])
    gbias = din("gbias", [DEPTH, 128, 8])
    g_head_p = din("g_head_p", [DEPTH, 128, 8])
    g_head_r = din("g_head_r", [DEPTH, 128, 256])
    w_out = din("w_out", [DEPTH, D, D])
    w_ada = din("w_ada", [DEPTH, D, 6 * D])
    b_ada = din("b_ada", [DEPTH, 1, 6 * D])
    w_router = din("w_router", [D, NEXP])
    rbias = din("rbias", [128, NEXP])
    w_gate = din("w_gate_e", [DEPTH, NEXP, D, DEXP])
    w_up = din("w_up_e", [DEPTH, NEXP, D, DEXP])
    w_down = din("w_down_e", [DEPTH, NEXP, DEXP, D])
    g_final = din("g_final_r", [128, D])
    cb_in = din("cb", [128, NCB])
    cf_in = din("cf", [128, NCF])
    y_out = nc.dram_tensor("y", [SEQ, D], F32, kind="ExternalOutput").ap()

    xA = dscr("xA", [SEQ, D], F32)
    xB = dscr("xB", [SEQ, D], F32)
    xmT_d = dscr("xmT", [256, SEQ], BF16)
    og_d = dscr("og", [SEQ, 256], BF16)
    gates_d = dscr("gates", [SEQ, 8], F32)
    dqT_d = dscr("dqT", [384, SEQ], BF16)
    dkT_d = dscr("dkT", [384, SEQ], BF16)
    dv_d = dscr("dv", [SEQ, 384], BF16)
    sqT_d = dscr("sqT", [384, SEQ], BF16)
    skT_d = dscr("skT", [384, SEQ], BF16)
    sv_d = dscr("sv", [SEQ, 384], BF16)
    B_xA, B_xB = Buf("xA"), Buf("xB")
    B_scr = {n: Buf(n) for n in ("xmT", "og", "gates", "dqT", "dkT", "dv", "sqT", "skT", "sv")}

    for name in debug:
        pass

    def sbt(name, shape, dt):
        return nc.alloc_sbuf_tensor(name, list(shape), dt)

    cb = sbt("cb_sb", [128, NCB], BF16)
    cf = sbt("cf_sb", [128, NCF], F32)
    B_cb, B_cf = Buf("cb"), Buf("cf")
    S.dma("pool", cb[:], cb_in[:, :], W=[B_cb])
    S.dma("sp", cf[:], cf_in[:, :], W=[B_cf])

    def cbs(name, rows=slice(0, 128)):
        o, w = CB[name]
        return cb[rows, o:o + w]

    def cfs(name, rows=slice(0, 128)):
        o, w = CF[name]
        return cf[rows, o:o + w]

    pp = [nc.alloc_psum_tensor("pp%d" % i, [128, 1024], F32) for i in range(4)]
    ps = [pp[i // 2][:, (i % 2) * 512:(i % 2 + 1) * 512] for i in range(8)]
    PP = pp
    B_ps = [Buf("ps%d" % i, excl=True) for i in range(8)]

    mod = sbt("mod", [128, 6, D], F32)
    B_mod = Buf("mod")
    B_crep = Buf("c_rep")
    c_sb = sbt("c_sb", [128, 8], F32)
    B_c = Buf("c_sb")
    S.dma("sp", c_sb[:], c_lay[:, :], W=[B_c])
    S.op("act", lambda e: e.activation(out=c_sb[:], in_=c_sb[:], func=AF.Silu), R=[B_c], W=[B_c])

    out_tensors = {}

    def dbg_out(name, shape, dt=F32):
        t = nc.dram_tensor("dbg_" + name, list(shape), dt, kind="ExternalOutput").ap()
        out_tensors[name] = t
        return t

    fin_bufs = []

    def phase_mod(l):
        with ExitStack() as st:
            c_rep = st.enter_context(nc.sbuf_tensor("c_rep_L%d" % l, [128, 8, 128], F32))
            S.op("dve", lambda e: e.tensor_copy(out=c_rep[:], in_=c_sb[:].unsqueeze(2).to_broadcast([128, 8, 128])),
                 R=[B_c], W=[B_crep])
            wt = [st.enter_context(nc.sbuf_tensor("wada%d_L%d" % (i, l), [128, 8, 512], F32)) for i in range(2)]
            br = [st.enter_context(nc.sbuf_tensor("brow%d_L%d" % (i, l), [1, 512], F32)) for i in range(2)]
            B_wt = [Buf("wada%d" % i) for i in range(2)]
            B_br = [Buf("brow%d" % i) for i in range(2)]
            for blk in range(12):
                i = blk % 2
                S.dma("sp", wt[i][:], w_ada[l, :, blk * 512:(blk + 1) * 512].rearrange("(k p) n -> p k n", p=128),
                      W=[B_wt[i]])
                S.dma("sp", br[i][:], b_ada[l, :, blk * 512:(blk + 1) * 512], W=[B_br[i]])
                pb = blk % 2
                fns = []
                for k in range(8):
                    fns.append(lambda e, k=k, i=i, pb=pb: e.matmul(ps[pb][:], lhsT=c_rep[:, k, :], rhs=wt[i][:, k, :],
                                                                   start=(k == 0), stop=False))
                fns.append(lambda e, i=i, pb=pb: e.matmul(ps[pb][:], lhsT=cfs("ones", slice(0, 1)), rhs=br[i][:],
                                                          start=False, stop=True))
                S.op("pe", fns, R=[B_wt[i], B_br[i], B_crep, B_cf], W=[B_ps[pb]])
                m = blk // 2
                dst = mod[:, m, (blk % 2) * 512:(blk % 2 + 1) * 512]
                if m in (1, 4):
                    S.op("dve", lambda e, dst=dst, pb=pb: e.tensor_scalar(out=dst, in0=ps[pb][:], scalar1=1.0, scalar2=None,
                                                                         op0=ALU.add), R=[B_ps[pb]], W=[B_mod])
                else:
                    S.op("dve", lambda e, dst=dst, pb=pb: e.tensor_copy(out=dst, in_=ps[pb][:]), R=[B_ps[pb]], W=[B_mod])
            S.barrier()

    def norm_tile(xt, B_xt, sidx, tmp, B_tmp, hout, B_hout, ss, B_ss):
        S.op("act", lambda e: e.activation(out=tmp[:], in_=xt[:], func=AF.Square, accum_out=ss[:, 0:1]),
             R=[B_xt], W=[B_tmp, B_ss])
        S.op("act", lambda e: e.activation(out=ss[:, 1:2], in_=ss[:, 0:1], func=AF.Ln, scale=1.0 / D, bias=EPS),
             R=[B_ss], W=[B_ss])
        S.op("act", lambda e: e.activation(out=ss[:, 2:3], in_=ss[:, 1:2], func=AF.Exp, scale=-0.5),
             R=[B_ss], W=[B_ss])
        S.op("dve", lambda e: e.scalar_tensor_tensor(out=tmp[:], in0=xt[:], scalar=ss[:, 2:3], in1=mod[:, sidx + 1, :],
                                                     op0=ALU.mult, op1=ALU.mult), R=[B_xt, B_ss, B_mod], W=[B_tmp])
        S.op("dve", lambda e: e.tensor_tensor(out=hout[:], in0=tmp[:], in1=mod[:, sidx, :], op=ALU.add),
             R=[B_tmp, B_mod], W=[B_hout])

    def load_win(l, st):
        win = st.enter_context(nc.sbuf_tensor("win_L%d" % l, [128, 8, DIN], BF16))
        B_win = Buf("win")
        for k in range(8):
            S.dma("pool", win[:, k, :], w_in[l, k * 128:(k + 1) * 128, :], W=[B_win])
        return win, B_win

    def phase_inproj(l, x_src, B_xsrc, win, B_win):
        with ExitStack() as st:
            def T(name, shape, dt):
                return st.enter_context(nc.sbuf_tensor(name + "_L%d" % l, list(shape), dt))
            xt = [T("xt%d" % i, [128, D], F32) for i in range(2)]
            B_xt = [Buf("xt%d" % i) for i in range(2)]
            tmp = [T("ntmp%d" % i, [128, D], F32) for i in range(2)]
            B_tmp = [Buf("ntmp%d" % i) for i in range(2)]
            hb = [T("hb%d" % i, [128, D], BF16) for i in range(2)]
            B_hb = [Buf("hb%d" % i) for i in range(2)]
            ss = [T("ss%d" % i, [128, 4], F32) for i in range(2)]
            B_ss = [Buf("ss%d" % i) for i in range(2)]
            hT = [T("hT%d" % i, [128, 8, 512], BF16) for i in range(2)]
            B_hT = [Buf("hT%d" % i) for i in range(2)]
            stF = {n: [T("st_%s%d" % (n, i), [128, w, 512], BF16) for i in range(2)]
                   for n, w in (("xmT", 2), ("dqT", 3), ("dkT", 3), ("sqT", 3), ("skT", 3))}
            B_stF = {n: [Buf("st_%s%d" % (n, i)) for i in range(2)] for n in stF}
            st_og = [T("st_og%d" % i, [128, 4, 256], BF16) for i in range(2)]
            st_g = [T("st_g%d" % i, [128, 4, 8], F32) for i in range(2)]
            st_dv = [T("st_dv%d" % i, [128, 4, 384], BF16) for i in range(2)]
            st_sv = [T("st_sv%d" % i, [128, 4, 384], BF16) for i in range(2)]
            B_og = [Buf("st_og%d" % i) for i in range(2)]
            B_g = [Buf("st_g%d" % i) for i in range(2)]
            B_dv = [Buf("st_dv%d" % i) for i in range(2)]
            B_sv = [Buf("st_sv%d" % i) for i in range(2)]
            fm_specs = [("xmT", 0, 2, 1.0, xmT_d), ("dqT", OFF_DIL, 3, 1.0, dqT_d), ("dkT", OFF_DIL + 384, 3, 0.125, dkT_d),
                        ("sqT", OFF_SB, 3, 1.0, sqT_d), ("skT", OFF_SB + 384, 3, 0.125, skT_d)]
            pcount = [0]

            def next_ps():
                pcount[0] += 1
                return 2 + (pcount[0] % 6)

            def NA(sb, tt):
                ti = sb * 4 + tt
                i = ti % 2
                S.dma("sp", xt[i][:], x_src[ti * 128:(ti + 1) * 128, :], R=[B_xsrc], W=[B_xt[i]])
                norm_tile(xt[i], B_xt[i], 0, tmp[i], B_tmp[i], hb[i], B_hb[i], ss[i], B_ss[i])

            def NB(sb, tt):
                ti = sb * 4 + tt
                i = ti % 2
                sl = sb % 2
                pb = ti % 2
                pT = ps[pb][:].bitcast(BF16)
                S.op("pe", [lambda e, c=c: e.transpose(out=pT[:, c * 128:(c + 1) * 128], in_=hb[i][:, c * 128:(c + 1) * 128],
                                                       identity=cbs("ident")) for c in range(8)],
                     R=[B_hb[i], B_cb], W=[B_ps[pb]])
                S.op("act", lambda e: e.copy(out=hT[sl][:, :, tt * 128:(tt + 1) * 128], in_=pT.rearrange("p (c t) -> p c t", c=8)),
                     R=[B_ps[pb]], W=[B_hT[sl]])

            def M_items(sb):
                sl = sb % 2
                items = []
                for (n, off, nch, scale, dst) in fm_specs:
                    for c in range(nch):
                        def it(n=n, off=off, nch=nch, scale=scale, dst=dst, c=c):
                            pb = next_ps()
                            S.op("pe", [lambda e, k=k: e.matmul(ps[pb][:], lhsT=win[:, k, off + c * 128: off + (c + 1) * 128],
                                                                rhs=hT[sl][:, k, :], start=(k == 0), stop=(k == 7)) for k in range(8)],
                                 R=[B_win, B_hT[sl]], W=[B_ps[pb]])
                            S.op("act", lambda e: e.activation(out=stF[n][sl][:, c, :], in_=ps[pb][:], func=AF.Copy, scale=scale),
                                 R=[B_ps[pb]], W=[B_stF[n][sl]])
                            if c == nch - 1:
                                S.dma("sp", dst[:, sb * 512:(sb + 1) * 512].rearrange("(c p) t -> p c t", p=128), stF[n][sl][:],
                                      R=[B_stF[n][sl]], W=[B_scr[n]])
                        items.append(it)
                for tt in range(4):
                    def it(tt=tt):
                        pb = next_ps()
                        S.op("pe", [lambda e, k=k: e.matmul(ps[pb][:, 0:264], lhsT=hT[sl][:, k, tt * 128:(tt + 1) * 128],
                                                            rhs=win[:, k, OFF_O:OFF_O + 264], start=(k == 0), stop=(k == 7))
                                    for k in range(8)], R=[B_win, B_hT[sl]], W=[B_ps[pb]])
                        S.op("act", lambda e: e.activation(out=st_og[sl][:, tt, :], in_=ps[pb][:, 0:256], func=AF.Copy),
                             R=[B_ps[pb]], W=[B_og[sl]])
                        S.op("dve", lambda e: e.tensor_copy(out=st_g[sl][:, tt, :], in_=ps[pb][:, 256:264]), R=[B_ps[pb]], W=[B_g[sl]])
                    items.append(it)
                    for (stt, Bst, off) in ((st_dv, B_dv, OFF_DIL + 768), (st_sv, B_sv, OFF_SB + 768)):
                        def it(tt=tt, stt=stt, Bst=Bst, off=off):
                            pb = next_ps()
                            S.op("pe", [lambda e, k=k: e.matmul(ps[pb][:, 0:384], lhsT=hT[sl][:, k, tt * 128:(tt + 1) * 128],
                                                                rhs=win[:, k, off:off + 384], start=(k == 0), stop=(k == 7))
                                        for k in range(8)], R=[B_win, B_hT[sl]], W=[B_ps[pb]])
                            S.op("dve", lambda e: e.tensor_copy(out=stt[sl][:, tt, :], in_=ps[pb][:, 0:384]), R=[B_ps[pb]], W=[Bst[sl]])
                        items.append(it)

                def fin():
                    r0, r1 = sb * 512, (sb + 1) * 512
                    S.dma("sp", og_d[r0:r1, :].rearrange("(t p) c -> p t c", p=128), st_og[sl][:], R=[B_og[sl]], W=[B_scr["og"]])
                    S.dma("sp", gates_d[r0:r1, :].rearrange("(t p) c -> p t c", p=128), st_g[sl][:], R=[B_g[sl]], W=[B_scr["gates"]])
                    S.dma("sp", dv_d[r0:r1, :].rearrange("(t p) c -> p t c", p=128), st_dv[sl][:], R=[B_dv[sl]], W=[B_scr["dv"]])
                    S.dma("sp", sv_d[r0:r1, :].rearrange("(t p) c -> p t c", p=128), st_sv[sl][:], R=[B_sv[sl]], W=[B_scr["sv"]])
                return items, fin

            for tt in range(4):
                NA(0, tt)
                NB(0, tt)
            for sb in range(NSB):
                items, fin = M_items(sb)
                nper = (len(items) + 3) // 4
                for part in range(4):
                    if sb + 1 < NSB:
                        NA(sb + 1, part)
                    for it in items[part * nper:(part + 1) * nper]:
                        it()
                    if sb + 1 < NSB:
                        NB(sb + 1, part)
                fin()
            S.barrier()


    Y = {}
    B_ymix = Buf("ymixT")
    ghp = sbt("ghp", [128, DEPTH, 8], F32)
    B_ghp = Buf("ghp")
    for l_ in range(DEPTH):
        S.dma("sp", ghp[:, l_, :], g_head_p[l_, :, :], W=[B_ghp])

    def head_norm_fm(l, src_ap, B_src, chunk, col0, T, tag):
        sq, B_sq, rs, B_rs, pstat, B_pstat = T
        S.op("act", lambda e: e.activation(out=sq[:], in_=src_ap, func=AF.Square), R=[B_src], W=[B_sq])
        S.op("pe", lambda e: e.matmul(pstat[:], lhsT=cfs("blk64"), rhs=sq[:], start=True, stop=True),
             R=[B_sq, B_cf], W=[B_pstat])
        S.op("act", lambda e: e.activation(out=rs[:], in_=pstat[:], func=AF.Ln, scale=1.0 / 64, bias=EPS),
             R=[B_pstat], W=[B_rs])
        S.op("act", lambda e: e.activation(out=rs[:], in_=rs[:], func=AF.Exp, scale=-0.5), R=[B_rs], W=[B_rs])
        S.op("dve", lambda e: e.scalar_tensor_tensor(out=Y["t"][:, chunk, col0:col0 + 512], in0=src_ap,
                                                     scalar=ghp[:, l, chunk:chunk + 1], in1=rs[:],
                                                     op0=ALU.mult, op1=ALU.mult),
             R=[B_src, B_rs, B_ghp], W=[B_ymix])

    def phase_sb(l, co=None):
        with ExitStack() as st:
            def T(name, shape, dt):
                return st.enter_context(nc.sbuf_tensor(name + "_L%d" % l, list(shape), dt))
            qT = T("sb_qT", [128, SEQ], BF16)
            kT = T("sb_kT", [128, SEQ], BF16)
            V = [T("sb_V%d" % i, [128, NT, 128], BF16) for i in range(2)]
            B_qT, B_kT, B_V = Buf("sb_qT"), Buf("sb_kT"), [Buf("sb_V0"), Buf("sb_V1")]
            C32 = [T("sb_C32_%d" % p, [128, 2, 512], F32) for p in range(2)]
            Cb = [T("sb_Cb_%d" % p, [128, 2, 512], BF16) for p in range(2)]
            B_C32 = [Buf("c32_%d" % p) for p in range(2)]
            B_Cb = [Buf("cb_%d" % p) for p in range(2)]
            e_sb = T("sb_e", [128, 2, 512], F32)
            B_e = Buf("sb_e")
            NSP, NA = 3, 2
            sp_sb = [T("sb_sp%d" % i, [128, 2, 512], BF16) for i in range(NSP)]
            B_sp = [Buf("sb_sp%d" % i) for i in range(NSP)]
            A_sb = [T("sb_A%d" % i, [128, 2, 512], BF16) for i in range(NA)]
            B_A = [Buf("sb_A%d" % i) for i in range(NA)]
            sq = T("sb_sq", [128, 512], F32)
            rs = T("sb_rs", [128, 512], F32)
            normT = (sq, Buf("sb_sq"), rs, Buf("sb_rs"), ps[0], B_ps[0])
            zP = pp[0][:].rearrange("p (h q) -> p h q", h=2)
            xP = pp[1][:].rearrange("p (h q) -> p h q", h=2)
            mstr2 = cbs("mstrict").unsqueeze(1).to_broadcast([128, 2, 128])
            for i in range(2):
                S.op("dve", lambda e, i=i: e.memset(V[i][:], 0.0), W=[B_V[i]])
            for c in range(3):
                S.dma("sp", qT[:], sqT_d[c * 128:(c + 1) * 128, :], R=[B_scr["sqT"]], W=[B_qT])
                S.dma("sp", kT[:], skT_d[c * 128:(c + 1) * 128, :], R=[B_scr["skT"]], W=[B_kT])
                for h in range(2):
                    S.dma("sp", V[h][:, :, h * 64:(h + 1) * 64],
                          sv_d[:, c * 128 + h * 64: c * 128 + (h + 1) * 64].rearrange("(t p) e -> p t e", p=128),
                          R=[B_scr["sv"]], W=[B_V[h]])
                tiles = []
                for sb in range(NSB):
                    for j in range(4 * sb + 3, -1, -1):
                        tiles.append((sb, j))
                n = len(tiles)

                def geom(t):
                    sb, j = tiles[t]
                    c0 = (j - 4 * sb) * 128 if j >= 4 * sb else 0
                    return sb, j, c0, (j >= 4 * sb), (j == 4 * sb + 3), (j == 0)

                def S1(t):
                    sb, j, c0, diag, first, last = geom(t)
                    par = sb % 2
                    if first:
                        S.op("dve", lambda e: e.memset(C32[par][:], 0.0), W=[B_C32[par]])
                        ob = 6 + par
                        S.op("dve", lambda e: e.memset(ps[ob][:], 0.0), W=[B_ps[ob]])
                    S.op("pe", [lambda e, h=h: e.matmul(ps[h][:, c0:512], lhsT=kT[h * 64:(h + 1) * 64, j * 128:(j + 1) * 128],
                                                        rhs=qT[h * 64:(h + 1) * 64, sb * 512 + c0:(sb + 1) * 512], start=True, stop=True)
                                for h in range(2)], R=[B_kT, B_qT], W=[B_ps[0], B_ps[1]])

                def S2a(t):
                    sb, j, c0, diag, first, last = geom(t)
                    S.op("act", lambda e: e.activation(out=e_sb[:, :, c0:512], in_=zP[:, :, c0:512], func=AF.Exp),
                         R=[B_ps[0], B_ps[1]], W=[B_e])

                def S2(t):
                    sb, j, c0, diag, first, last = geom(t)
                    k = t % NSP
                    S.op("act", lambda e: e.activation(out=sp_sb[k][:, :, c0:512], in_=e_sb[:, :, c0:512], func=AF.Ln, bias=1.0),
                         R=[B_e], W=[B_sp[k]])
                    if diag:
                        S.op("dve", lambda e: e.tensor_tensor(out=sp_sb[k][:, :, c0:c0 + 128], in0=sp_sb[k][:, :, c0:c0 + 128],
                                                              in1=mstr2, op=ALU.mult), R=[B_sp[k], B_cb], W=[B_sp[k]])

                def S3(t):
                    sb, j, c0, diag, first, last = geom(t)
                    k = t % NSP
                    par = sb % 2
                    fns = []
                    for h in range(2):
                        r = slice(h * 64, (h + 1) * 64)
                        pb = 2 + h
                        fns.append(lambda e, r=r, pb=pb: e.matmul(ps[pb][:, c0:512], lhsT=kT[r, j * 128:(j + 1) * 128],
                                                                  rhs=qT[r, sb * 512 + c0:(sb + 1) * 512], start=True, stop=False))
                        fns.append(lambda e, h=h, pb=pb: e.matmul(ps[pb][:, c0:512], lhsT=cbs("negtri"), rhs=sp_sb[k][:, h, c0:512],
                                                                  start=False, stop=first))
                        if not first:
                            chi = C32[par][:, h, :].bitcast(BF16)[:, 2 * c0 + 1:1024:2]
                            fns.append(lambda e, chi=chi, pb=pb: e.matmul(ps[pb][:, c0:512], lhsT=cbs("negones"), rhs=chi,
                                                                          start=False, stop=True))
                    S.op("pe", fns, R=[B_kT, B_qT, B_cb, B_sp[k], B_C32[par]], W=[B_ps[2], B_ps[3]])

                def S4(t):
                    sb, j, c0, diag, first, last = geom(t)
                    a = t % NA
                    S.op("act", lambda e: e.activation(out=A_sb[a][:, :, c0:512], in_=xP[:, :, c0:512], func=AF.Exp),
                         R=[B_ps[2], B_ps[3]], W=[B_A[a]])
                    if diag:
                        S.op("dve", lambda e: e.tensor_tensor(out=A_sb[a][:, :, c0:c0 + 128], in0=A_sb[a][:, :, c0:c0 + 128],
                                                              in1=mstr2, op=ALU.mult), R=[B_A[a], B_cb], W=[B_A[a]])

                def S6(t):
                    sb, j, c0, diag, first, last = geom(t)
                    if last:
                        return
                    k = t % NSP
                    par = sb % 2
                    S.op("dve", lambda e: e.tensor_tensor(out=C32[par][:, :, c0:512], in0=C32[par][:, :, c0:512],
                                                          in1=sp_sb[k][:, :, c0:512], op=ALU.add),
                         R=[B_sp[k]], W=[B_C32[par]])


                def S5(t):
                    sb, j, c0, diag, first, last = geom(t)
                    a = t % NA
                    ob = 6 + (sb % 2)
                    S.op("pe", [lambda e, h=h: e.matmul(ps[ob][:, c0:512], lhsT=V[h][:, j, :], rhs=A_sb[a][:, h, c0:512],
                                                        start=False, stop=False) for h in range(2)],
                         R=[B_V[0], B_V[1], B_A[a]], W=[B_ps[ob]])
                    if last:
                        head_norm_fm(l, ps[ob][:], B_ps[ob], 5 + c, sb * 512, normT, "sb")

                for tau in range(n + 2):
                    if tau < n:
                        S1(tau)
                        S2a(tau)
                        S2(tau)
                    if 0 <= tau - 1 < n:
                        S3(tau - 1)
                        S4(tau - 1)
                        S6(tau - 1)
                    if 0 <= tau - 2 < n:
                        S5(tau - 2)
                    if co is not None and co[0] is not None and (tau % CO_SKIP[0] != CO_SKIP[0] - 1):
                        if next(co[0], "end") in ("hold", "end"):
                            co[0] = None
            if co is not None:
                while co[0] is not None:
                    if next(co[0], "end") in ("hold", "end"):
                        co[0] = None
            S.barrier()

    def phase_dil(l):
        with ExitStack() as st:
            def T(name, shape, dt):
                return st.enter_context(nc.sbuf_tensor(name + "_L%d" % l, list(shape), dt))
            qT = T("dl_qT", [128, SEQ], BF16)
            kT = T("dl_kT", [128, SEQ], BF16)
            Vs = [[T("dl_V%d_%d" % (i, pp), [128, NT, 128], BF16) for i in range(2)] for pp in range(2)]
            B_Vs = [[Buf("dl_V%d_%d" % (i, pp)) for i in range(2)] for pp in range(2)]
            B_qT, B_kT = Buf("dl_qT"), Buf("dl_kT")
            accn = T("dl_accn", [128, SEQ], F32)
            accd = T("dl_accd", [128, SEQ], F32)
            B_accn, B_accd = Buf("accn"), Buf("accd")
            NP_ = 8
            P_sb = [T("dl_P%d" % i, [128, 2, 128], BF16) for i in range(NP_)]
            B_P = [Buf("dl_P%d" % i) for i in range(NP_)]
            rsA = [T("dl_rsA%d" % i, [128, 512], F32) for i in range(4)]
            B_rsA = [Buf("dl_rsA%d" % i) for i in range(4)]
            sq2 = [T("dl_sq%d" % i, [128, 512], F32) for i in range(2)]
            B_sq2 = [Buf("dl_sq%d" % i) for i in range(2)]
            rsB = [T("dl_rsB%d" % i, [128, 512], F32) for i in range(2)]
            B_rsB = [Buf("dl_rsB%d" % i) for i in range(2)]
            for pp in range(2):
                for i in range(2):
                    S.op("dve", lambda e, i=i, pp=pp: e.memset(Vs[pp][i][:], 0.0), W=[B_Vs[pp][i]])
                S.op("dve", lambda e, pp=pp: e.memset(Vs[pp][0][:, :, 64:65], 1.0), W=[B_Vs[pp][0]])
                S.op("dve", lambda e, pp=pp: e.memset(Vs[pp][1][:, :, 0:1], 1.0), W=[B_Vs[pp][1]])
            eo, _ = CB["dilE"]
            vcnt = [0]

            def load_V(c, pi):
                win_, d_ = DIL_PATTERNS[pi]
                nb_ = (SEQ // d_) // 128
                pp = vcnt[0] % 2
                vcnt[0] += 1
                for h in range(2):
                    src = dv_d[:, c * 128 + h * 64: c * 128 + (h + 1) * 64].rearrange("(j i r) e -> i r j e", i=128, r=d_)
                    dst = Vs[pp][h][:, :, h * 64:(h + 1) * 64].rearrange("p (r j) e -> p r j e", r=d_)
                    if d_ <= nb_:
                        for r0 in range(d_):
                            S.dma("sp", dst[:, r0, :, :], src[:, r0, :, :], R=[B_scr["dv"]], W=[B_Vs[pp][h]])
                    else:
                        for j0 in range(nb_):
                            S.dma("sp", dst[:, :, j0, :], src[:, :, j0, :], R=[B_scr["dv"]], W=[B_Vs[pp][h]])
                return pp
            pending = {}
            pending[(0, 0)] = load_V(0, 0)
            for c in range(3):
                S.dma("sp", qT[:], dqT_d[c * 128:(c + 1) * 128, :], R=[B_scr["dqT"]], W=[B_qT])
                S.dma("sp", kT[:], dkT_d[c * 128:(c + 1) * 128, :], R=[B_scr["dkT"]], W=[B_kT])
                grp = [0]
                for pi, (win, d) in enumerate(DIL_PATTERNS):
                    L = SEQ // d
                    nblk = L // 128
                    pp_cur = pending.pop((c, pi))
                    V = Vs[pp_cur]
                    B_V = B_Vs[pp_cur]
                    nxt = (c, pi + 1) if pi + 1 < 3 else ((c + 1, 0) if c + 1 < 3 else None)
                    if nxt is not None:
                        pending[nxt] = load_V(*nxt)
                    gsz = min(4, nblk)
                    tiles = []
                    for r in range(d):
                        for g in range(nblk // gsz):
                            for nloc in range(gsz):
                                nq = g * gsz + nloc
                                for j in (nq - 1, nq):
                                    if j >= 0:
                                        tiles.append((r, g, nloc, nq, j))
                    n = len(tiles)
                    gpar = {}

                    def tok(r, blk):
                        a = r + d * 128 * blk
                        return slice(a, a + d * 127 + 1, d) if d > 1 else slice(a, a + 128)

                    def S1(t):
                        r, g, nloc, nq, j = tiles[t]
                        first = (nloc == 0 and j == max(nq - 1, 0))
                        if first:
                            gp = (grp[0] % 2)
                            pn, pd = 4 + 2 * gp, 5 + 2 * gp
                            grp[0] += 1
                            N = gsz * 128
                            S.op("dve", lambda e: e.memset(ps[pn][:, 0:N], 0.0), W=[B_ps[pn]])
                            S.op("dve", lambda e: e.memset(ps[pd][:, 0:N], 0.0), W=[B_ps[pd]])
                        gpar[t] = (grp[0] - 1) % 2
                        zb = 2 * (t % 2)
                        S.op("pe", [lambda e, h=h: e.matmul(ps[zb + h][:, 0:128], lhsT=kT[h * 64:(h + 1) * 64, tok(r, j)],
                                                            rhs=qT[h * 64:(h + 1) * 64, tok(r, nq)], start=True, stop=True)
                                    for h in range(2)], R=[B_kT, B_qT], W=[B_ps[zb], B_ps[zb + 1]])

                    def S2(t):
                        r, g, nloc, nq, j = tiles[t]
                        zb = 2 * (t % 2)
                        pq = t % NP_
                        zv = PP[t % 2][:].rearrange("p (h q) -> p h q", h=2)[:, :, 0:128]
                        S.op("act", lambda e: e.activation(out=P_sb[pq][:], in_=zv, func=AF.Exp),
                             R=[B_ps[zb], B_ps[zb + 1]], W=[B_P[pq]])
                        e0 = eo + ((c * 3 + pi) * 2) * 256
                        off = 0 if j == nq else 128
                        ev = cb[:, e0:e0 + 512].rearrange("p (h x) -> p h x", h=2)[:, :, off:off + 128]
                        S.op("dve", lambda e: e.tensor_tensor(out=P_sb[pq][:], in0=P_sb[pq][:], in1=ev, op=ALU.mult),
                             R=[B_P[pq], B_cb], W=[B_P[pq]])

                    def S3(t):
                        r, g, nloc, nq, j = tiles[t]
                        gp = gpar[t]
                        pa = t % NP_
                        pn, pd = 4 + 2 * gp, 5 + 2 * gp
                        cs = slice(nloc * 128, (nloc + 1) * 128)
                        S.op("pe", [lambda e, h=h: e.matmul(ps[(pn, pd)[h]][:, cs], lhsT=V[h][:, r * nblk + j, :], rhs=P_sb[pa][:, h, :],
                                                            start=False, stop=False) for h in range(2)],
                             R=[B_V[0], B_V[1], B_P[pa]], W=[B_ps[pn], B_ps[pd]])
                        last = (nloc == gsz - 1 and j == nq)
                        if last:
                            N = gsz * 128
                            a = r + d * 128 * gsz * g
                            dst = slice(a, a + d * (N - 1) + 1, d) if d > 1 else slice(a, a + N)
                            if pi == 0:
                                S.op("dve", lambda e: e.tensor_copy(out=accn[:, dst], in_=ps[pn][:, 0:N]), R=[B_ps[pn]], W=[B_accn])
                                S.op("dve", lambda e: e.tensor_copy(out=accd[:, dst], in_=ps[pd][:, 0:N]), R=[B_ps[pd]], W=[B_accd])
                            else:
                                S.op("dve", lambda e: e.tensor_tensor(out=accn[:, dst], in0=accn[:, dst], in1=ps[pn][:, 0:N], op=ALU.add),
                                     R=[B_ps[pn]], W=[B_accn])
                                S.op("dve", lambda e: e.tensor_tensor(out=accd[:, dst], in0=accd[:, dst], in1=ps[pd][:, 0:N], op=ALU.add),
                                     R=[B_ps[pd]], W=[B_accd])

                    DEP = 3
                    for tau in range(n + DEP):
                        if tau < n:
                            S1(tau)
                            S2(tau)
                        if 0 <= tau - DEP < n:
                            S3(tau - DEP)
                def NA(sb):
                    cs = slice(sb * 512, (sb + 1) * 512)
                    pb = sb % 4
                    S.op("pe", [lambda e: e.matmul(ps[pb][:], lhsT=cfs("selA"), rhs=accn[:, cs], start=True, stop=False),
                                lambda e: e.matmul(ps[pb][:], lhsT=cfs("selB"), rhs=accd[:, cs], start=False, stop=True)],
                         R=[B_accn, B_accd, B_cf], W=[B_ps[pb]])
                    S.op("act", lambda e: e.activation(out=rsA[pb][:], in_=ps[pb][:], func=AF.Ln), R=[B_ps[pb]], W=[B_rsA[pb]])
                    S.op("act", lambda e: e.activation(out=rsA[pb][:], in_=rsA[pb][:], func=AF.Exp, scale=-1.0), R=[B_rsA[pb]], W=[B_rsA[pb]])

                def NB_(sb):
                    cs = slice(sb * 512, (sb + 1) * 512)
                    pb = sb % 4
                    S.op("dve", lambda e: e.tensor_tensor(out=accn[0:64, cs], in0=accn[0:64, cs], in1=rsA[pb][0:64, :], op=ALU.mult),
                         R=[B_rsA[pb]], W=[B_accn])
                    S.op("dve", lambda e: e.tensor_tensor(out=accn[64:128, cs], in0=accd[64:128, cs], in1=rsA[pb][64:128, :], op=ALU.mult),
                         R=[B_rsA[pb], B_accd], W=[B_accn])

                def NC(sb):
                    cs = slice(sb * 512, (sb + 1) * 512)
                    i_ = sb % 2
                    pb = 4 + i_
                    S.op("act", lambda e: e.activation(out=sq2[i_][:], in_=accn[:, cs], func=AF.Square), R=[B_accn], W=[B_sq2[i_]])
                    S.op("pe", lambda e: e.matmul(ps[pb][:], lhsT=cfs("blk64"), rhs=sq2[i_][:], start=True, stop=True),
                         R=[B_sq2[i_], B_cf], W=[B_ps[pb]])
                    S.op("act", lambda e: e.activation(out=rsB[i_][:], in_=ps[pb][:], func=AF.Ln, scale=1.0 / 64, bias=EPS),
                         R=[B_ps[pb]], W=[B_rsB[i_]])
                    S.op("act", lambda e: e.activation(out=rsB[i_][:], in_=rsB[i_][:], func=AF.Exp, scale=-0.5), R=[B_rsB[i_]], W=[B_rsB[i_]])

                def ND(sb):
                    cs = slice(sb * 512, (sb + 1) * 512)
                    i_ = sb % 2
                    S.op("dve", lambda e: e.scalar_tensor_tensor(out=Y["t"][:, 2 + c, cs], in0=accn[:, cs], scalar=ghp[:, l, 2 + c:3 + c],
                                                                 in1=rsB[i_][:], op0=ALU.mult, op1=ALU.mult),
                         R=[B_accn, B_rsB[i_], B_ghp], W=[B_ymix])

                for st_ in range(NSB + 3):
                    if st_ < NSB:
                        NA(st_)
                    if 0 <= st_ - 1 < NSB:
                        NB_(st_ - 1)
                    if 0 <= st_ - 2 < NSB:
                        NC(st_ - 2)
                    if 0 <= st_ - 3 < NSB:
                        ND(st_ - 3)
            S.barrier()


    def gen_mlstm(l, bk):
        with ExitStack() as st:
            def T(name, shape, dt):
                return st.enter_context(nc.sbuf_tensor(name + "_L%d" % l, list(shape), dt))
            cw = T("ml_cw", [128, 2, 4], F32)
            cbi = T("ml_cb", [128, 2], F32)
            ncbi = T("ml_ncb", [128, 2], F32)
            gb = T("ml_gb", [128, 8], F32)
            ghr = T("ml_ghr", [128, 256], F32)
            B_small = Buf("ml_small")
            S.dma("sp", cw[:], conv_w[l, :, :, :], W=[B_small])
            S.dma("sp", cbi[:], conv_b[l, :, :], W=[B_small])
            S.dma("sp", gb[:], gbias[l, :, :], W=[B_small])
            S.dma("sp", ghr[:], g_head_r[l, :, :], W=[B_small])
            S.op("dve", lambda e: e.tensor_scalar(out=ncbi[:], in0=cbi[:], scalar1=-1.0, scalar2=None, op0=ALU.mult),
                 R=[B_small], W=[B_small])
            Wbd = {}
            B_W = Buf("ml_W")
            for nm, src in (("q", w_mq), ("k", w_mk), ("v", w_mv)):
                Wbd[nm] = T("ml_W" + nm, [128, 2, 128], BF16)
                S.op("dve", lambda e, nm=nm: e.memset(Wbd[nm][:], 0.0), W=[B_W])
                for c in range(2):
                    for hh in range(2):
                        S.dma("pool", Wbd[nm][hh * 64:(hh + 1) * 64, c, hh * 64:(hh + 1) * 64], src[l, 2 * c + hh, :, :], W=[B_W])
            graw = T("ml_graw", [128, NT, 8], F32)
            B_g = Buf("ml_graw")
            S.dma("sp", graw[:], gates_d[:, :].rearrange("(t p) c -> p t c", p=128), R=[B_scr["gates"]], W=[B_g])
            S.op("dve", lambda e: e.tensor_tensor(out=graw[:], in0=graw[:], in1=gb[:].unsqueeze(1).to_broadcast([128, NT, 8]),
                                                  op=ALU.add), R=[B_small], W=[B_g])
            nl = T("ml_nl", [128, NT, 4], F32)
            a_t = T("ml_a", [128, NT, 4], F32)
            b_t = T("ml_b", [128, NT, 4], F32)
            bl_t = T("ml_bl", [128, NT, 4], F32)
            B_nl, B_a, B_b, B_bl = Buf("nl"), Buf("a"), Buf("b"), Buf("bl")
            S.op("act", lambda e: e.activation(out=nl[:], in_=graw[:, :, 4:8], func=AF.Exp, scale=-1.0), R=[B_g], W=[B_nl])
            S.op("act", lambda e: e.activation(out=nl[:], in_=nl[:], func=AF.Ln, bias=1.0), R=[B_nl], W=[B_nl])
            nlf = nl[:].rearrange("p t h -> p (t h)")
            S.op("pe", lambda e: e.matmul(ps[bk["c0"]][:, 0:128], lhsT=cfs("triu"), rhs=nlf, start=True, stop=True),
                 R=[B_nl, B_cf], W=[B_ps[bk["c0"]]])
            S.op("pe", lambda e: e.matmul(ps[bk["c1"]][:, 0:128], lhsT=cfs("ones"), rhs=nlf, start=True, stop=True),
                 R=[B_nl, B_cf], W=[B_ps[bk["c1"]]])
            pc = ps[bk["c0"]][:, 0:128].rearrange("p (t h) -> p t h", h=4)
            S.op("dve", lambda e: e.tensor_tensor(out=a_t[:], in0=graw[:, :, 0:4], in1=pc, op=ALU.add), R=[B_g, B_ps[bk["c0"]]], W=[B_a])
            S.op("act", lambda e: e.activation(out=a_t[:], in_=a_t[:], func=AF.Exp), R=[B_a], W=[B_a])
            S.op("act", lambda e: e.activation(out=b_t[:], in_=pc, func=AF.Exp, scale=-1.0), R=[B_ps[bk["c0"]]], W=[B_b])
            S.op("act", lambda e: e.activation(out=bl_t[:], in_=ps[bk["c1"]][:, 0:128].rearrange("p (t h) -> p t h", h=4), func=AF.Exp,
                                               scale=-1.0), R=[B_ps[bk["c1"]]], W=[B_bl])
            C32 = T("ml_C32", [128, 2, 65], F32)
            Cbf = T("ml_Cbf", [128, 2, 65], BF16)
            B_C32, B_Cbf = Buf("ml_C32"), Buf("ml_Cbf")
            S.op("dve", lambda e: e.memset(C32[:], 0.0), W=[B_C32])
            S.op("dve", lambda e: e.memset(Cbf[:], 0.0), W=[B_Cbf])
            xmp = [T("ml_xmp%d" % i, [128, 2, 515], BF16) for i in range(2)]
            B_xmp = [Buf("ml_xmp%d" % i) for i in range(2)]
            ogt = [T("ml_og%d" % i, [128, 4, 256], BF16) for i in range(2)]
            B_ogt = [Buf("ml_og%d" % i) for i in range(2)]
            acc = T("ml_acc", [128, 2, 512], F32)
            ez = T("ml_ez", [128, 2, 512], F32)
            xc = T("ml_xc", [128, 2, 512], BF16)
            B_acc, B_ez, B_xc = Buf("ml_acc"), Buf("ml_ez"), Buf("ml_xc")
            qTs = T("ml_qT", [128, 2, 512], BF16)
            kTs = T("ml_kT", [128, 2, 512], BF16)
            B_qTs, B_kTs = Buf("ml_qT"), Buf("ml_kT")
            ktok = T("ml_ktok", [128, 256], BF16)
            Vaug = T("ml_Vaug", [128, 4, 65], BF16)
            swm = T("ml_swm", [128, 4, 128], BF16)
            B_ktok, B_Vaug, B_swm = Buf("ml_ktok"), Buf("ml_Vaug"), Buf("ml_swm")
            sm = T("ml_sm", [128, 16], F32)
            B_sm = Buf("ml_sm")
            eo = T("ml_eo", [128, 256], F32)
            t1 = T("ml_t1", [128, 256], F32)
            ysq = T("ml_ysq", [128, 256], F32)
            yn = T("ml_yn", [128, 256], BF16)
            B_eo, B_t1, B_ysq, B_yn = Buf("ml_eo"), Buf("ml_t1"), Buf("ml_ysq"), Buf("ml_yn")
            ctmp = T("ml_ctmp", [128, 2, 65], F32)
            B_ctmp = Buf("ml_ctmp")

            yield
            for sb in range(NSB if ML_CUT[0] > 1 else 0):
                i2 = sb % 2
                xm = xmp[i2]
                if sb == 0:
                    S.op("dve", lambda e: e.memset(xm[:, :, 0:3], 0.0), W=[B_xmp[i2]])
                    S.dma("sp", xm[:, :, 3:515], xmT_d[:, 0:512].rearrange("(c p) t -> p c t", p=128), R=[B_scr["xmT"]], W=[B_xmp[i2]])
                else:
                    S.dma("sp", xm[:, :, 0:515], xmT_d[:, sb * 512 - 3:(sb + 1) * 512].rearrange("(c p) t -> p c t", p=128),
                          R=[B_scr["xmT"]], W=[B_xmp[i2]])
                S.dma("sp", ogt[i2][:], og_d[sb * 512:(sb + 1) * 512, :].rearrange("(t p) c -> p t c", p=128), R=[B_scr["og"]],
                      W=[B_ogt[i2]])
                yield
                for c in range(2):
                    S.op("dve", lambda e, c=c: e.tensor_scalar(out=acc[:, c, :], in0=xm[:, c, 0:512], scalar1=cw[:, c, 0:1], scalar2=None,
                                                               op0=ALU.mult), R=[B_xmp[i2], B_small], W=[B_acc])
                    for j in range(1, 4):
                        S.op("dve", lambda e, c=c, j=j: e.scalar_tensor_tensor(out=acc[:, c, :], in0=xm[:, c, j:j + 512],
                                                                               scalar=cw[:, c, j:j + 1], in1=acc[:, c, :],
                                                                               op0=ALU.mult, op1=ALU.add),
                             R=[B_xmp[i2], B_small], W=[B_acc])
                    S.op("act", lambda e, c=c: e.activation(out=ez[:, c, :], in_=acc[:, c, :], func=AF.Exp, scale=-1.0,
                                                            bias=ncbi[:, c:c + 1]), R=[B_acc, B_small], W=[B_ez])
                    S.op("dve", lambda e, c=c: e.tensor_scalar(out=ez[:, c, :], in0=ez[:, c, :], scalar1=1.0, scalar2=None, op0=ALU.add),
                         R=[B_ez], W=[B_ez])
                    S.op("dve", lambda e, c=c: e.reciprocal(out=ez[:, c, :], in_=ez[:, c, :]), R=[B_ez], W=[B_ez])
                    S.op("dve", lambda e, c=c: e.scalar_tensor_tensor(out=xc[:, c, :], in0=acc[:, c, :], scalar=cbi[:, c:c + 1],
                                                                      in1=ez[:, c, :], op0=ALU.add, op1=ALU.mult),
                         R=[B_acc, B_ez, B_small], W=[B_xc])
                yield
                for c in range(2):
                    S.op("pe", lambda e, c=c: e.matmul(ps[bk["q"]][:], lhsT=Wbd["q"][:, c, :], rhs=xc[:, c, :], start=True, stop=True),
                         R=[B_W, B_xc], W=[B_ps[bk["q"]]])
                    S.op("act", lambda e, c=c: e.copy(out=qTs[:, c, :], in_=ps[bk["q"]][:]), R=[B_ps[bk["q"]]], W=[B_qTs])
                    S.op("pe", lambda e, c=c: e.matmul(ps[bk["k"]][:], lhsT=Wbd["k"][:, c, :], rhs=xc[:, c, :], start=True, stop=True),
                         R=[B_W, B_xc], W=[B_ps[bk["k"]]])
                    S.op("act", lambda e, c=c: e.activation(out=kTs[:, c, :], in_=ps[bk["k"]][:], func=AF.Copy, scale=0.125),
                         R=[B_ps[bk["k"]]], W=[B_kTs])
                for i in range(4 if ML_CUT[0] > 2 else 0):
                    cut = ML_CUT[0]
                    ci = sb * 4 + i
                    ts = slice(i * 128, (i + 1) * 128)
                    yield
                    S.op("pe", [lambda e, c=c: e.matmul(ps[bk["kt"]][:, c * 128:(c + 1) * 128], lhsT=xc[:, c, ts], rhs=Wbd["k"][:, c, :],
                                                        start=True, stop=True) for c in range(2)],
                         R=[B_xc, B_W], W=[B_ps[bk["kt"]]])
                    S.op("act", lambda e: e.activation(out=ktok[:], in_=ps[bk["kt"]][:, 0:256], func=AF.Copy, scale=0.125),
                         R=[B_ps[bk["kt"]]], W=[B_ktok])
                    S.op("pe", [lambda e, c=c: e.matmul(ps[bk["vt"]][:, c * 128:(c + 1) * 128], lhsT=xm[:, c, 3 + i * 128:3 + (i + 1) * 128],
                                                        rhs=Wbd["v"][:, c, :], start=True, stop=True) for c in range(2)],
                         R=[B_xmp[i2], B_W], W=[B_ps[bk["vt"]]])
                    S.op("dve", lambda e: e.tensor_tensor(out=Vaug[:, :, 0:64], in0=ps[bk["vt"]][:, 0:256].rearrange("p (h e) -> p h e", h=4),
                                                          in1=a_t[:, ci, :].unsqueeze(2).to_broadcast([128, 4, 64]), op=ALU.mult),
                         R=[B_ps[bk["vt"]], B_a], W=[B_Vaug])
                    S.op("dve", lambda e: e.tensor_copy(out=Vaug[:, :, 64], in_=a_t[:, ci, :]), R=[B_a], W=[B_Vaug])
                    if cut <= 3:
                        continue
                    yield
                    sbank = (bk["S0"], bk["S1"])
                    S.op("pe", [lambda e, h=h: e.matmul(ps[sbank[h % 2]][:, (h // 2) * 128:(h // 2 + 1) * 128],
                                                        lhsT=kTs[(h % 2) * 64:(h % 2 + 1) * 64, h // 2, ts],
                                                        rhs=qTs[(h % 2) * 64:(h % 2 + 1) * 64, h // 2, ts], start=True, stop=True)
                                for h in range(4)], R=[B_kTs, B_qTs], W=[B_ps[bk["S0"]], B_ps[bk["S1"]]])
                    for hh in range(2):
                        S.op("dve", lambda e, hh=hh: e.tensor_tensor(
                            out=swm[:, hh::2, :], in0=ps[sbank[hh]][:, 0:256].rearrange("p (c t) -> p c t", c=2),
                            in1=cbs("triu").unsqueeze(1).to_broadcast([128, 2, 128]), op=ALU.mult),
                            R=[B_ps[sbank[hh]], B_cb], W=[B_swm])
                    if cut <= 4:
                        continue
                    yield
                    fns = []
                    for h in range(4):
                        rows = slice((h % 2) * 64, (h % 2 + 1) * 64)
                        fns.append(lambda e, h=h, rows=rows: e.matmul(ps[bk["H"]][:, h * 65:(h + 1) * 65], lhsT=qTs[rows, h // 2, ts],
                                                                      rhs=Cbf[rows, h // 2, :], start=True, stop=False))
                        fns.append(lambda e, h=h: e.matmul(ps[bk["H"]][:, h * 65:(h + 1) * 65], lhsT=swm[:, h, :], rhs=Vaug[:, h, :],
                                                           start=False, stop=True))
                    S.op("pe", fns, R=[B_qTs, B_Cbf, B_swm, B_Vaug], W=[B_ps[bk["H"]]])
                    if cut <= 5:
                        continue
                    yield
                    S.op("pe", [lambda e, c=c: e.matmul(ps[bk["dC"]][:, c * 130:(c + 1) * 130], lhsT=ktok[:, c * 128:(c + 1) * 128],
                                                        rhs=Vaug[:, 2 * c:2 * c + 2, :].rearrange("p h e -> p (h e)"),
                                                        start=True, stop=True) for c in range(2)],
                         R=[B_ktok, B_Vaug], W=[B_ps[bk["dC"]]])
                    for c in range(2):
                        for hh in range(2):
                            rows = slice(hh * 64, (hh + 1) * 64)
                            S.op("dve", lambda e, c=c, hh=hh, rows=rows: e.tensor_tensor(
                                out=ctmp[rows, c, :], in0=C32[rows, c, :], in1=ps[bk["dC"]][rows, c * 130 + hh * 65:c * 130 + (hh + 1) * 65],
                                op=ALU.add), R=[B_C32, B_ps[bk["dC"]]], W=[B_ctmp])
                            S.op("dve", lambda e, c=c, hh=hh, rows=rows: e.tensor_scalar(
                                out=C32[rows, c, :], in0=ctmp[rows, c, :], scalar1=bl_t[rows, ci, 2 * c + hh:2 * c + hh + 1], scalar2=None,
                                op0=ALU.mult), R=[B_ctmp, B_bl], W=[B_C32])
                    S.op("dve", lambda e: e.tensor_copy(out=Cbf[:], in_=C32[:]), R=[B_C32], W=[B_Cbf])
                    if cut <= 6:
                        continue
                    yield
                    pH = ps[bk["H"]][:, 0:260].rearrange("p (h e) -> p h e", h=4)
                    S.op("dve", lambda e: e.tensor_tensor(out=sm[:, 0:4], in0=pH[:, :, 64], in1=b_t[:, ci, :], op=ALU.mult),
                         R=[B_ps[bk["H"]], B_b], W=[B_sm])
                    S.op("dve", lambda e: e.tensor_scalar(out=sm[:, 4:8], in0=sm[:, 0:4], scalar1=-1.0, scalar2=None, op0=ALU.mult),
                         R=[B_sm], W=[B_sm])
                    S.op("dve", lambda e: e.tensor_tensor(out=sm[:, 4:8], in0=sm[:, 4:8], in1=sm[:, 0:4], op=ALU.max),
                         R=[B_sm], W=[B_sm])
                    S.op("dve", lambda e: e.tensor_scalar(out=sm[:, 4:8], in0=sm[:, 4:8], scalar1=1.0, scalar2=None, op0=ALU.max),
                         R=[B_sm], W=[B_sm])
                    S.op("dve", lambda e: e.reciprocal(out=sm[:, 4:8], in_=sm[:, 4:8]), R=[B_sm], W=[B_sm])
                    S.op("dve", lambda e: e.tensor_tensor(out=sm[:, 8:12], in0=b_t[:, ci, :], in1=sm[:, 4:8], op=ALU.mult),
                         R=[B_sm, B_b], W=[B_sm])
                    yield
                    S.op("act", lambda e: e.activation(out=eo[:], in_=ogt[i2][:, i, :], func=AF.Exp, scale=-1.0), R=[B_ogt[i2]], W=[B_eo])
                    S.op("dve", lambda e: e.tensor_scalar(out=eo[:], in0=eo[:], scalar1=1.0, scalar2=None, op0=ALU.add), R=[B_eo], W=[B_eo])
                    S.op("dve", lambda e: e.tensor_tensor(out=t1[:].rearrange("p (h e) -> p h e", h=4), in0=pH[:, :, 0:64],
                                                          in1=sm[:, 8:12].unsqueeze(2).to_broadcast([128, 4, 64]), op=ALU.mult),
                         R=[B_ps[bk["H"]], B_sm], W=[B_t1])
                    S.op("dve", lambda e: e.reciprocal(out=eo[:], in_=eo[:]), R=[B_eo], W=[B_eo])
                    S.op("dve", lambda e: e.tensor_tensor(out=t1[:], in0=t1[:], in1=eo[:], op=ALU.mult), R=[B_t1, B_eo], W=[B_t1])
                    yield
                    S.op("dve", lambda e: e.tensor_tensor(out=ysq[:], in0=t1[:], in1=t1[:], op=ALU.mult), R=[B_t1], W=[B_ysq])
                    S.op("dve", lambda e: e.tensor_reduce(out=sm[:, 12:16], in_=ysq[:].rearrange("p (h e) -> p h e", h=4), axis=AX.X,
                                                          op=ALU.add), R=[B_ysq], W=[B_sm])
                    S.op("act", lambda e: e.activation(out=sm[:, 12:16], in_=sm[:, 12:16], func=AF.Ln, scale=1.0 / 64, bias=EPS),
                         R=[B_sm], W=[B_sm])
                    S.op("act", lambda e: e.activation(out=sm[:, 12:16], in_=sm[:, 12:16], func=AF.Exp, scale=-0.5), R=[B_sm], W=[B_sm])
                    S.op("dve", lambda e: e.tensor_tensor(out=t1[:].rearrange("p (h e) -> p h e", h=4),
                                                          in0=t1[:].rearrange("p (h e) -> p h e", h=4),
                                                          in1=sm[:, 12:16].unsqueeze(2).to_broadcast([128, 4, 64]), op=ALU.mult),
                         R=[B_sm], W=[B_t1])
                    S.op("dve", lambda e: e.tensor_tensor(out=yn[:], in0=t1[:], in1=ghr[:], op=ALU.mult), R=[B_t1, B_small], W=[B_yn])
                    if cut <= 7:
                        continue
                    yield
                    pT = ps[bk["T"]][:].bitcast(BF16)
                    S.op("pe", [lambda e, c=c: e.transpose(out=pT[:, c * 128:(c + 1) * 128], in_=yn[:, c * 128:(c + 1) * 128],
                                                           identity=cbs("ident")) for c in range(2)], R=[B_yn, B_cb], W=[B_ps[bk["T"]]])
                    S.op("act", lambda e: e.copy(out=Y["t"][:, 0:2, ci * 128:(ci + 1) * 128],
                                                 in_=pT[:, 0:256].rearrange("p (c t) -> p c t", c=2)), R=[B_ps[bk["T"]]], W=[B_ymix])
            yield "hold"


    ML_BANKS_ALONE = {"c0": 0, "c1": 1, "q": 0, "k": 1, "kt": 2, "vt": 3, "S0": 4, "S1": 0, "H": 5, "dC": 6, "T": 7}
    ML_BANKS_CO = {"c0": 4, "c1": 5, "q": 4, "k": 4, "kt": 4, "vt": 4, "S0": 4, "S1": 5, "H": 5, "dC": 4, "T": 4}

    def phase_mlstm(l):
        g = gen_mlstm(l, ML_BANKS_ALONE)
        for _ in g:
            pass
        S.barrier()

    def phase_outproj(l, x_src, B_xsrc, x_dst, B_xdst):
        with ExitStack() as st:
            def T(name, shape, dt):
                return st.enter_context(nc.sbuf_tensor(name + "_L%d" % l, list(shape), dt))
            wo = T("op_w", [128, 8, D], BF16)
            B_wo = Buf("op_w")
            for k in range(8):
                S.dma("pool", wo[:, k, :], w_out[l, k * 128:(k + 1) * 128, :], W=[B_wo])
            xt = [T("op_xt%d" % i, [128, D], F32) for i in range(4)]
            xo = [T("op_xo%d" % i, [128, D], F32) for i in range(4)]
            B_xt = [Buf("op_xt%d" % i) for i in range(4)]
            B_xo = [Buf("op_xo%d" % i) for i in range(4)]
            for t0 in range(2):
                S.dma("sp", xt[t0][:], x_src[t0 * 128:(t0 + 1) * 128, :], R=[B_xsrc], W=[B_xt[t0]])
            for ti in range(NT):
                i = ti % 4
                if ti + 2 < NT:
                    S.dma("sp", xt[(ti + 2) % 4][:], x_src[(ti + 2) * 128:(ti + 3) * 128, :], R=[B_xsrc], W=[B_xt[(ti + 2) % 4]])
                for hf in range(2):
                    pb = (ti * 2 + hf) % 8
                    cs = slice(hf * 512, (hf + 1) * 512)
                    S.op("pe", [lambda e, k=k: e.matmul(ps[pb][:], lhsT=Y["t"][:, k, ti * 128:(ti + 1) * 128], rhs=wo[:, k, cs],
                                                        start=(k == 0), stop=(k == 7)) for k in range(8)],
                         R=[B_ymix, B_wo], W=[B_ps[pb]])
                    S.op("dve", lambda e: e.tensor_tensor(out=xo[i][:, cs], in0=ps[pb][:], in1=mod[:, 2, cs], op=ALU.mult),
                         R=[B_ps[pb], B_mod], W=[B_xo[i]])
                    S.op("dve", lambda e: e.tensor_tensor(out=xo[i][:, cs], in0=xo[i][:, cs], in1=xt[i][:, cs], op=ALU.add),
                         R=[B_xt[i]], W=[B_xo[i]])
                S.dma("sp", x_dst[ti * 128:(ti + 1) * 128, :], xo[i][:], R=[B_xo[i]], W=[B_xdst])
            S.barrier()

    def phase_moe(l, x_src, B_xsrc, x_dst, B_xdst, final):
        with ExitStack() as st:
            def T(name, shape, dt):
                return st.enter_context(nc.sbuf_tensor(name + "_L%d" % l, list(shape), dt))
            wr = T("mo_wr", [128, 8, NEXP], F32)
            rb = T("mo_rb", [128, NEXP], F32)
            B_wr = Buf("mo_wr")
            S.dma("sp", wr[:], w_router.rearrange("(k p) e -> p k e", p=128), W=[B_wr])
            S.dma("sp", rb[:], rbias[:, :], W=[B_wr])
            gfin = None
            if final:
                gfin = mod[:, 0, :]
                S.dma("sp", gfin, g_final[:, :], W=[B_mod])
            h2T = [T("mo_h2T%d" % i, [128, 8, 1024], BF16) for i in range(2)]
            B_h2T = [Buf("mo_h2T%d" % i) for i in range(2)]
            yaccA = T("mo_yacc", [128, 8, D], F32)
            yaccB = T("mo_yaccB", [128, 4, D], F32)
            B_yaccA = [Buf("mo_yacc%d" % i) for i in range(8)]
            B_yaccB = [Buf("mo_yaccB%d" % i) for i in range(4)]

            def ysel(qt, tl):
                if tl < 4 and qt % 2 == 1:
                    return yaccB[:, tl, :], B_yaccB[tl]
                return yaccA[:, tl, :], B_yaccA[tl]
            combTok = [T("mo_combTok%d" % i, [128, 8, NEXP], F32) for i in range(2)]
            B_combT = [Buf("mo_combTok%d" % i) for i in range(2)]
            Wg = [T("mo_Wg%d" % i, [128, 8, DEXP], BF16) for i in range(2)]
            Wu = [T("mo_Wu%d" % i, [128, 8, DEXP], BF16) for i in range(2)]
            Wd = [T("mo_Wd%d" % i, [128, 4, D], BF16) for i in range(2)]
            B_Wg = [Buf("mo_Wg%d" % i) for i in range(2)]
            B_Wu = [Buf("mo_Wu%d" % i) for i in range(2)]
            B_Wd = [Buf("mo_Wd%d" % i) for i in range(2)]
            he = [T("mo_he%d" % i, [128, 4, 512], BF16) for i in range(2)]
            B_he = [Buf("mo_he%d" % i) for i in range(2)]
            sg = [T("mo_sg%d" % i, [128, 512], BF16) for i in range(2)]
            B_sg = [Buf("mo_sg%d" % i) for i in range(2)]
            xt = [T("mo_xt%d" % i, [128, D], F32) for i in range(2)]
            B_xt = [Buf("mo_xt%d" % i) for i in range(2)]
            tmp = T("mo_tmp", [128, D], F32)
            B_tmp = Buf("mo_tmp")
            h2f = [T("mo_h2f%d" % i, [128, D], F32) for i in range(2)]
            B_h2f = [Buf("mo_h2f%d" % i) for i in range(2)]
            h2Tf1 = T("mo_h2Tf", [128, 8, 128], F32)
            h2Tf = [h2Tf1, h2Tf1]
            B_h2Tf1 = Buf("mo_h2Tf")
            B_h2Tf = [B_h2Tf1, B_h2Tf1]
            ss = [T("mo_ss%d" % i, [128, 4], F32) for i in range(2)]
            B_ss = [Buf("mo_ss%d" % i) for i in range(2)]
            ssf = T("mo_ssf", [128, 8, 4], F32)
            B_ssf = [Buf("mo_ssf%d" % i) for i in range(8)]
            rt = [T("mo_rt%d" % i, [128, 8, 64], F32) for i in range(2)]
            B_rt = [Buf("mo_rt%d" % i) for i in range(2)]
            PR = 7

            def load_gu(e):
                i = e % 2
                S.dma("pool", Wg[i][:], w_gate[l, e, :, :].rearrange("(k p) f -> p k f", p=128), W=[B_Wg[i]])
                S.dma("pool", Wu[i][:], w_up[l, e, :, :].rearrange("(k p) f -> p k f", p=128), W=[B_Wu[i]])

            def load_d(e):
                i = e % 2
                S.dma("pool", Wd[i][:], w_down[l, e, :, :].rearrange("(k p) d -> p k d", p=128), W=[B_Wd[i]])

            def load_w(e):
                load_gu(e)
                load_d(e)

            def Ra(qt, tt):
                ti = qt * 8 + tt
                i = ti % 2
                S.dma("sp", xt[i][:], x_src[ti * 128:(ti + 1) * 128, :], R=[B_xsrc], W=[B_xt[i]])
                norm_tile(xt[i], B_xt[i], 3, tmp, B_tmp, h2f[i], B_h2f[i], ss[i], B_ss[i])

            def Rb(qt, tt, halves=(0, 1)):
                ti = qt * 8 + tt
                i = ti % 2
                qb = qt % 2
                for half in halves:
                    S.op("pe", [lambda e, c=c: e.transpose(out=ps[PR][:, (c % 4) * 128:(c % 4 + 1) * 128],
                                                           in_=h2f[i][:, c * 128:(c + 1) * 128], identity=cfs("ident"))
                                for c in range(half * 4, half * 4 + 4)], R=[B_h2f[i], B_cf], W=[B_ps[PR]])
                    S.op("act", lambda e: e.copy(out=h2T[qb][:, half * 4:half * 4 + 4, tt * 128:(tt + 1) * 128],
                                                 in_=ps[PR][:].rearrange("p (c t) -> p c t", c=4)), R=[B_ps[PR]], W=[B_h2T[qb]])
                    S.op("dve", lambda e: e.tensor_copy(out=h2Tf[i][:, half * 4:half * 4 + 4, :],
                                                        in_=ps[PR][:].rearrange("p (c t) -> p c t", c=4)), R=[B_ps[PR]], W=[B_h2Tf[i]])

            def Rb2(qt, tt):
                ti = qt * 8 + tt
                i = ti % 2
                bi = (ti // 4) % 2
                S.op("pe", [lambda e, k=k: e.matmul(ps[PR][:, 0:16], lhsT=h2Tf[i][:, k, :], rhs=wr[:, k, :], start=(k == 0), stop=(k == 7))
                            for k in range(8)], R=[B_h2Tf[i], B_wr], W=[B_ps[PR]])
                S.op("act", lambda e: e.activation(out=rt[bi][:, 0, (tt % 4) * 16:(tt % 4 + 1) * 16], in_=ps[PR][:, 0:16], func=AF.Exp,
                                                   scale=-1.0), R=[B_ps[PR]], W=[B_rt[bi]])
                if tt % 4 == 3:
                    RT(qt, tt // 4, bi)

            def RT(qt, b, bi):
                qb = qt % 2
                r_ = rt[bi]
                sc, g, eq, g2, sel, w_ = [r_[:, k_, :] for k_ in range(6)]
                m1, m2, gs, gmk = r_[:, 6, 0:16], r_[:, 6, 16:32], r_[:, 6, 32:48], r_[:, 6, 48:64]
                gmx, wsum = r_[:, 7, 0:4], r_[:, 7, 4:8]

                def vg(a):
                    return a.rearrange("p (g e) -> p g e", e=4)

                def vt(a):
                    return a.rearrange("p (t e) -> p t e", e=16)

                def bg(a):
                    return a.unsqueeze(2).to_broadcast([128, 16, 4])

                def t4(a):
                    return a.rearrange("p (t g) -> p t g", g=4)
                ops = [
                    lambda e: e.tensor_scalar(out=sc, in0=sc, scalar1=1.0, scalar2=None, op0=ALU.add),
                    lambda e: e.reciprocal(out=sc, in_=sc),
                    lambda e: e.tensor_tensor(out=vt(g), in0=vt(sc), in1=rb[:].unsqueeze(1).to_broadcast([128, 4, 16]), op=ALU.add),
                    lambda e: e.tensor_reduce(out=m1, in_=vg(g), axis=AX.X, op=ALU.max),
                    lambda e: e.tensor_tensor(out=vg(eq), in0=vg(g), in1=bg(m1), op=ALU.is_equal),
                    lambda e: e.scalar_tensor_tensor(out=g2, in0=eq, scalar=-1.0e9, in1=g, op0=ALU.mult, op1=ALU.add),
                    lambda e: e.tensor_reduce(out=m2, in_=vg(g2), axis=AX.X, op=ALU.max),
                    lambda e: e.tensor_tensor(out=gs, in0=m1, in1=m2, op=ALU.add),
                    lambda e: e.tensor_reduce(out=gmx, in_=t4(gs), axis=AX.X, op=ALU.max),
                    lambda e: e.tensor_tensor(out=t4(gmk), in0=t4(gs), in1=gmx.unsqueeze(2).to_broadcast([128, 4, 4]), op=ALU.is_ge),
                    lambda e: e.tensor_tensor(out=vg(sel), in0=vg(g), in1=bg(m2), op=ALU.is_ge),
                    lambda e: e.tensor_tensor(out=vg(sel), in0=vg(sel), in1=bg(gmk), op=ALU.mult),
                    lambda e: e.tensor_tensor(out=w_, in0=sc, in1=sel, op=ALU.mult),
                    lambda e: e.tensor_reduce(out=wsum, in_=vt(w_), axis=AX.X, op=ALU.add),
                    lambda e: e.reciprocal(out=wsum, in_=wsum),
                ]
                for f_ in ops:
                    S.op("dve", f_, R=[B_rt[bi], B_wr], W=[B_rt[bi]])
                S.op("dve", lambda e: e.tensor_tensor(out=combTok[qb][:, 4 * b:4 * b + 4, :], in0=vt(w_),
                                                      in1=wsum.unsqueeze(2).to_broadcast([128, 4, 16]), op=ALU.mult),
                     R=[B_rt[bi]], W=[B_combT[qb]])

            units = [(e_, s2) for e_ in range(NEXP) for s2 in range(2)]

            def GU(qt, u):
                e_, s2 = units[u]
                wi = e_ % 2
                qb = qt % 2
                ci_ = u % 2
                for f in range(4):
                    pg, pu = (0, 1) if f % 2 == 0 else (2, 3)
                    fs = slice(f * 128, (f + 1) * 128)
                    S.op("pe", [lambda e, k=k: e.matmul(ps[pg][:], lhsT=Wg[wi][:, k, fs], rhs=h2T[qb][:, k, s2 * 512:(s2 + 1) * 512],
                                                        start=(k == 0), stop=(k == 7)) for k in range(8)],
                         R=[B_Wg[wi], B_h2T[qb]], W=[B_ps[pg]])
                    S.op("pe", [lambda e, k=k: e.matmul(ps[pu][:], lhsT=Wu[wi][:, k, fs], rhs=h2T[qb][:, k, s2 * 512:(s2 + 1) * 512],
                                                        start=(k == 0), stop=(k == 7)) for k in range(8)],
                         R=[B_Wu[wi], B_h2T[qb]], W=[B_ps[pu]])
                    j = f % 2
                    S.op("act", lambda e: e.activation(out=sg[j][:], in_=ps[pg][:], func=AF.Silu), R=[B_ps[pg]], W=[B_sg[j]])
                    S.op("dve", lambda e: e.tensor_tensor(out=he[ci_][:, f, :], in0=ps[pu][:], in1=sg[j][:], op=ALU.mult),
                         R=[B_ps[pu], B_sg[j]], W=[B_he[ci_]])

            def DOWN(qt, u):
                e_, s2 = units[u]
                wi = e_ % 2
                ci_ = u % 2
                qb = qt % 2
                for t4 in range(4):
                    tl = s2 * 4 + t4
                    for dh in range(2):
                        py = 4 + ((t4 * 2 + dh) % 3)
                        ds_ = slice(dh * 512, (dh + 1) * 512)
                        S.op("pe", [lambda e, f=f: e.matmul(ps[py][:], lhsT=he[ci_][:, f, t4 * 128:(t4 + 1) * 128], rhs=Wd[wi][:, f, ds_],
                                                            start=(f == 0), stop=(f == 3)) for f in range(4)],
                             R=[B_he[ci_], B_Wd[wi]], W=[B_ps[py]])
                        cw_ = combTok[qb][:, tl, e_:e_ + 1]
                        ya_, B_ya = ysel(qt, tl)
                        if e_ == 0:
                            S.op("dve", lambda e: e.tensor_scalar(out=ya_[:, ds_], in0=ps[py][:], scalar1=cw_, scalar2=None, op0=ALU.mult),
                                 R=[B_ps[py], B_combT[qb]], W=[B_ya])
                        else:
                            S.op("dve", lambda e: e.scalar_tensor_tensor(out=ya_[:, ds_], in0=ps[py][:], scalar=cw_, in1=ya_[:, ds_],
                                                                         op0=ALU.mult, op1=ALU.add),
                                 R=[B_ps[py], B_combT[qb]], W=[B_ya])

            def EPIa(qt, tt):
                ti = qt * 8 + tt
                ya_, B_ya = ysel(qt, tt)
                xe, B_xe = xt[tt % 2], B_xt[tt % 2]
                S.dma("sp", xe[:], x_src[ti * 128:(ti + 1) * 128, :], R=[B_xsrc], W=[B_xe])
                S.op("pool", lambda e: e.tensor_tensor(out=ya_, in0=ya_, in1=mod[:, 5, :], op=ALU.mult), R=[B_mod], W=[B_ya])
                S.op("pool", lambda e: e.tensor_tensor(out=ya_, in0=ya_, in1=xe[:], op=ALU.add), R=[B_xe], W=[B_ya])
                if final:
                    s_ = ssf[:, tt, :]
                    S.op("act", lambda e: e.activation(out=xe[:], in_=ya_, func=AF.Square, accum_out=s_[:, 0:1]),
                         R=[B_ya], W=[B_xe, B_ssf[tt]])
                    S.op("act", lambda e: e.activation(out=s_[:, 1:2], in_=s_[:, 0:1], func=AF.Ln, scale=1.0 / D, bias=EPS),
                         R=[B_ssf[tt]], W=[B_ssf[tt]])
                    S.op("act", lambda e: e.activation(out=s_[:, 2:3], in_=s_[:, 1:2], func=AF.Exp, scale=-0.5), R=[B_ssf[tt]], W=[B_ssf[tt]])
                else:
                    S.dma("sp", x_dst[ti * 128:(ti + 1) * 128, :], ya_, R=[B_ya], W=[B_xdst])

            def EPIb(qt, tt):
                if not final:
                    return
                ti = qt * 8 + tt
                ya_, B_ya = ysel(qt, tt)
                s_ = ssf[:, tt, :]
                S.op("dve", lambda e: e.scalar_tensor_tensor(out=ya_, in0=ya_, scalar=s_[:, 2:3], in1=gfin, op0=ALU.mult, op1=ALU.mult),
                     R=[B_ssf[tt], B_mod], W=[B_ya])
                S.dma("sp", x_dst[ti * 128:(ti + 1) * 128, :], ya_, R=[B_ya], W=[B_xdst])

            load_w(0)
            load_w(1)
            for tt in range(9):
                if tt < 8:
                    Ra(0, tt)
                if tt >= 1:
                    Rb2(0, tt - 1)
                if tt < 8:
                    Rb(0, tt)
            for qt in range(4):
                sched = {}
                if qt + 1 < 4:
                    for tt in range(8):
                        sched.setdefault(3 + 3 * tt, []).append(("a", tt))
                        sched.setdefault(4 + 3 * tt, []).append(("b", tt))
                        sched.setdefault(5 + 3 * tt, []).append(("b2", tt))
                GU(qt, 0)
                if qt > 0:
                    for tt in range(4, 8):
                        EPIa(qt - 1, tt)
                for u in range(len(units)):
                    if u + 1 < len(units):
                        GU(qt, u + 1)
                    e_u, s_u = units[u]
                    if s_u == 0 and (e_u + 2 < NEXP or qt + 1 < 4):
                        load_gu((e_u + 2) % NEXP)
                    if qt > 0 and u == 1:
                        for tt in range(4, 8):
                            EPIb(qt - 1, tt)
                    if qt > 0 and 1 <= u <= 4:
                        EPIa(qt - 1, u - 1)
                    if qt > 0 and 2 <= u <= 5:
                        EPIb(qt - 1, u - 2)
                    post = []
                    for kind, tt in sched.get(u, []):
                        if kind == "b":
                            Rb(qt + 1, tt, halves=(0,))
                            post.append(tt)
                        else:
                            {"a": Ra, "b2": Rb2}[kind](qt + 1, tt)
                    DOWN(qt, u)
                    for tt in post:
                        Rb(qt + 1, tt, halves=(1,))
                    if s_u == 1 and (e_u + 2 < NEXP or qt + 1 < 4):
                        load_d((e_u + 2) % NEXP)
                    if qt == 3 and e_u == NEXP - 1 and s_u == 0:
                        for tt in range(4):
                            EPIa(qt, tt)
                if qt == 3:
                    for tt in range(4):
                        EPIb(qt, tt)
                    for tt in range(4, 8):
                        EPIa(qt, tt)
                    for tt in range(4, 8):
                        EPIb(qt, tt)
            S.barrier()

    def dump_dram(name, src, B_src, shape, dt):
        t = dbg_out(name, shape, dt)
        b = Buf("dbg_" + name)
        S.dma("sp", t, src, R=[B_src], W=[b])
        fin_bufs.append(b)

    def dump_sbuf(name, src_ap, B_src, shape, dt):
        t = dbg_out(name, shape, dt)
        b = Buf("dbg_" + name)
        S.dma("sp", t, src_ap, R=[B_src], W=[b])
        fin_bufs.append(b)

    B_xin, B_y = Buf("x_in"), Buf("y_out")
    x_cur, B_xcur = x_in, B_xin
    if isinstance(stop_after, str) and stop_after.startswith("only:"):
        ph = stop_after[5:]
        l = 0
        if ph == "mod":
            phase_mod(0)
        elif ph == "inproj":
            with ExitStack() as wst:
                win_, B_win_ = load_win(0, wst)
                phase_inproj(0, x_in, B_xin, win_, B_win_)
        elif ph == "modin":
            with ExitStack() as wst:
                win_, B_win_ = load_win(0, wst)
                phase_mod(0)
                phase_inproj(0, x_in, B_xin, win_, B_win_)
        elif ph == "sbml":
            with ExitStack() as lay:
                Y["t"] = lay.enter_context(nc.sbuf_tensor("ymixT_L%d" % l, [128, 8, SEQ], BF16))
                g_ml = gen_mlstm(0, ML_BANKS_CO)
                next(g_ml)
                phase_sb(0, co=[g_ml])
                for _ in g_ml:
                    pass
        elif ph in ("ml", "dil", "sb", "outproj"):
            with ExitStack() as lay:
                Y["t"] = lay.enter_context(nc.sbuf_tensor("ymixT_L%d" % l, [128, 8, SEQ], BF16))
                {"ml": phase_mlstm, "dil": phase_dil, "sb": phase_sb}.get(ph, lambda l_: phase_outproj(0, x_in, B_xin, xA, B_xA))(0)
        elif ph == "moe":
            phase_moe(0, x_in, B_xin, xB, B_xB, False)
        S.barrier()
        return nc, out_tensors
    for l in range(DEPTH):
        if stop_after == "mlonly%d" % l:
            with ExitStack() as lay:
                Y["t"] = lay.enter_context(nc.sbuf_tensor("ymixT_L%d" % l, [128, 8, SEQ], BF16))
                phase_mlstm(l)
            break
        with ExitStack() as wst:
            win_, B_win_ = load_win(l, wst)
            phase_mod(l)
            phase_inproj(l, x_cur, B_xcur, win_, B_win_)
        with ExitStack() as lay:
            Y["t"] = lay.enter_context(nc.sbuf_tensor("ymixT_L%d" % l, [128, 8, SEQ], BF16))
            phase_dil(l)
            g_ml = gen_mlstm(l, ML_BANKS_CO)
            next(g_ml)
            phase_sb(l, co=[g_ml])
            for _ in g_ml:
                pass
            if stop_after == "mix%d" % l:
                dump_sbuf("ymixT", Y["t"][:].rearrange("p c t -> p (c t)"), B_ymix, [128, 8 * SEQ], BF16)
                S.wait_all("sp", fin_bufs)
                S.barrier()
                break
            phase_outproj(l, x_cur, B_xcur, xA, B_xA)
        if stop_after == "x1_%d" % l:
            dump_dram("x1", xA, B_xA, [SEQ, D], F32)
            break
        last = (l == DEPTH - 1)
        if last:
            phase_moe(l, xA, B_xA, y_out, B_y, True)
        else:
            phase_moe(l, xA, B_xA, xB, B_xB, False)
            x_cur, B_xcur = xB, B_xB
        if stop_after == "x2_%d" % l:
            dump_dram("x2", xB, B_xB, [SEQ, D], F32)
            break

    S.wait_all("sp", fin_bufs + [B_y])
    S.barrier()
    return nc, out_tensors


def prep_inputs(b, inp, consts):
    cbn, cfn = consts
    f = np.float32
    d = {
        "x": np.ascontiguousarray(inp["x"][b]),
        "c_lay": np.ascontiguousarray(inp["c"][b].reshape(8, 128).T),
        "w_in": inp["w_in"],
        "conv_w": np.ascontiguousarray(inp["conv_w"].reshape(DEPTH, 4, 2, 128).transpose(0, 3, 2, 1)),
        "conv_b": np.ascontiguousarray(inp["conv_b"].reshape(DEPTH, 2, 128).transpose(0, 2, 1)),
        "w_mq": inp["w_mq"], "w_mk": inp["w_mk"], "w_mv": inp["w_mv"],
        "gbias": np.ascontiguousarray(np.broadcast_to(inp["gate_bias"].reshape(DEPTH, 1, 8), (DEPTH, 128, 8))),
        "g_head_p": np.ascontiguousarray(inp["g_head"].reshape(DEPTH, 8, 128).transpose(0, 2, 1)),
        "g_head_r": np.ascontiguousarray(np.broadcast_to(inp["g_head"][:, None, :256], (DEPTH, 128, 256))),
        "w_out": inp["w_out"],
        "w_ada": inp["w_ada"],
        "b_ada": np.ascontiguousarray(inp["b_ada"].reshape(DEPTH, 1, 6 * D)),
        "w_router": inp["w_router"],
        "rbias": np.ascontiguousarray(np.broadcast_to(inp["router_bias"][None, :], (128, NEXP))),
        "w_gate_e": inp["w_gate_e"], "w_up_e": inp["w_up_e"], "w_down_e": inp["w_down_e"],
        "g_final_r": np.ascontiguousarray(np.broadcast_to(inp["g_final"][None, :], (128, D))),
        "cb": cbn, "cf": cfn,
    }
    return {k: np.ascontiguousarray(v, dtype=f) for k, v in d.items()}


def kernel(**inputs):
    inp = {k: np.asarray(v) for k, v in inputs.items()}
    nc, _ = build_program()
    consts = make_consts()
    in_maps = [prep_inputs(b, inp, consts) for b in range(8)]
    res = run_bass_kernel_spmd(nc, in_maps, core_ids=list(range(8)))
    return np.stack([np.asarray(r["y"]) for r in res.results], axis=0).astype(np.float32)
```

```python
import math
import numpy as np
from contextlib import ExitStack
import concourse.bass as bass
import concourse.mybir as mybir
from concourse.bass_utils import run_bass_kernel_spmd

F32 = mybir.dt.float32
BF16 = mybir.dt.bfloat16
AF = mybir.ActivationFunctionType
ALU = mybir.AluOpType
AX = mybir.AxisListType

SEQ = 4096
D = 1024
DEPTH = 2
NT = SEQ // 128
NSB = SEQ // 512
DIN = 2824
NEXP = 16
DEXP = 512
EPS = 1e-6
OFF_O = 256
OFF_DIL = 520
OFF_SB = 520 + 1152
DIL_PATTERNS = ((128, 1), (512, 4), (2048, 16))


class Buf:
    __slots__ = ("name", "w", "r", "excl")

    def __init__(self, name, excl=False):
        self.name = name
        self.w = None
        self.r = []
        self.excl = excl


class _Eng:
    def __init__(self, name, e, sem):
        self.name = name
        self.e = e
        self.sem = sem
        self.cnt = 0
        self.known = {}


class Sched:
    def __init__(self, nc, n_dma_sems=32):
        self.nc = nc
        self.sems = []
        self.eng = {}
        for name, e in (("pe", nc.tensor), ("act", nc.scalar), ("dve", nc.vector),
                        ("pool", nc.gpsimd), ("sp", nc.sync)):
            sem = nc.semaphore("s_" + name).__enter__()
            self.sems.append(sem)
            self.eng[name] = _Eng(name, e, len(self.sems) - 1)
        self.dma_sems = []
        for i in range(n_dma_sems):
            sem = nc.semaphore("s_dma%d" % i).__enter__()
            self.sems.append(sem)
            self.dma_sems.append([len(self.sems) - 1, 0])
        self.dma_rr = 0
        self.ninstr = 0

    def _deps(self, R, W):
        deps = {}

        def add(t):
            if t is None:
                return
            s, v = t
            if deps.get(s, 0) < v:
                deps[s] = v
        for b in R:
            add(b.w)
            if b.excl:
                for t in b.r:
                    add(t)
        for b in W:
            add(b.w)
            for t in b.r:
                add(t)
        return deps

    def _wait(self, E, deps):
        for s, v in deps.items():
            if E.known.get(s, 0) >= v:
                continue
            E.e.wait_ge(self.sems[s], v)
            E.known[s] = v
            self.ninstr += 1

    def _mark(self, R, W, ticket):
        for b in R:
            if b.excl:
                b.w = ticket
                b.r = []
            else:
                b.r.append(ticket)
                if len(b.r) > 32:
                    m = {}
                    for s, v in b.r:
                        if m.get(s, 0) < v:
                            m[s] = v
                    b.r = list(m.items())
        for b in W:
            b.w = ticket
            b.r = []

    def op(self, eng, fns, R=(), W=()):
        E = self.eng[eng]
        if not isinstance(fns, (list, tuple)):
            fns = [fns]
        self._wait(E, self._deps(R, W))
        ins = None
        for f in fns:
            ins = f(E.e)
            self.ninstr += 1
        E.cnt += 1
        ins.then_inc(self.sems[E.sem], 1)
        t = (E.sem, E.cnt)
        self._mark(R, W, t)
        return t

    def dma(self, eng, out, in_, R=(), W=()):
        E = self.eng[eng]
        slot = self.dma_sems[self.dma_rr]
        self.dma_rr = (self.dma_rr + 1) % len(self.dma_sems)
        s, v = slot
        deps = self._deps(R, W)
        if v > 0 and deps.get(s, 0) < v:
            deps[s] = v
        self._wait(E, deps)
        ins = E.e.dma_start(out=out, in_=in_)
        slot[1] = v + 16
        ins.then_inc(self.sems[s], 16)
        self.ninstr += 1
        t = (s, v + 16)
        self._mark(R, W, t)
        return t

    def wait_all(self, eng, bufs):
        E = self.eng[eng]
        self._wait(E, self._deps(bufs, bufs))

    def barrier(self):
        tot = {}
        for E in self.eng.values():
            if E.cnt:
                tot[E.sem] = E.cnt
        for s, v in self.dma_sems:
            if v:
                tot[s] = v
        for E in self.eng.values():
            self._wait(E, dict(tot))


CB = {}
CF = {}


def _layout(table, items):
    off = 0
    for name, w in items:
        table[name] = (off, w)
        off += w
    return off


NCB = _layout(CB, [("ident", 128), ("ones", 128), ("negtri", 128), ("negones", 128),
                   ("mstrict", 128), ("triu", 128), ("onesA", 128), ("onesB", 128),
                   ("blk64", 128), ("zeros", 128), ("dilE", 18 * 256)])
NCF = _layout(CF, [("ident", 128), ("ones", 128), ("triu", 128), ("blk64", 128), ("selA", 128), ("selB", 128)])


def make_consts():
    p = np.arange(128)[:, None].astype(np.float64)
    f = np.arange(128)[None, :].astype(np.float64)
    cb = np.zeros((128, NCB), np.float32)
    cf = np.zeros((128, NCF), np.float32)

    def put(tab, lay, name, val):
        o, w = lay[name]
        tab[:, o:o + w] = val
    ident = (p == f).astype(np.float32)
    ones = np.ones((128, 128), np.float32)
    triu = (p <= f).astype(np.float32)
    blk64 = ((p // 64) == (f // 64)).astype(np.float32)
    put(cb, CB, "ident", ident)
    put(cb, CB, "ones", ones)
    put(cb, CB, "negtri", -(p >= f).astype(np.float32))
    put(cb, CB, "negones", -ones)
    put(cb, CB, "mstrict", (p < f).astype(np.float32))
    put(cb, CB, "triu", triu)
    put(cb, CB, "onesA", (f < 64).astype(np.float32) * ones)
    put(cb, CB, "onesB", (f >= 64).astype(np.float32) * ones)
    put(cb, CB, "blk64", blk64)
    mk = np.arange(128)[:, None].astype(np.float64)
    mq = np.arange(256)[None, :].astype(np.float64)
    dlt = mq - mk
    valid = (dlt >= 0) & (dlt <= 128)
    o, _ = CB["dilE"]
    for h in range(6):
        slope = 2.0 ** (-8.0 * (h + 1) / 6.0)
        for pi, (win, dil) in enumerate(DIL_PATTERNS):
            e = np.where(valid, np.exp(-slope * dil * dlt), 0.0)
            ix = ((h // 2) * 3 + pi) * 2 + (h % 2)
            cb[:, o + ix * 256: o + (ix + 1) * 256] = e
    put(cf, CF, "ident", ident)
    put(cf, CF, "ones", ones)
    put(cf, CF, "triu", triu)
    put(cf, CF, "blk64", blk64)
    selA = np.zeros((128, 128), np.float32); selA[64, 0:64] = 1.0
    selB = np.zeros((128, 128), np.float32); selB[0, 64:128] = 1.0
    put(cf, CF, "selA", selA)
    put(cf, CF, "selB", selB)
    return cb, cf


ML_CUT = [99]
MOE_DBG = [0]
CO_EVERY = [1]
CO_SKIP = [10 ** 9]
SB_DEPTH = [2, 3]


def build_program(stop_after=None, debug=()):
    nc = bass.Bass("TRN2", target_bir_lowering=False)
    S = Sched(nc)
    dbg = {}

    def din(name, shape, dt=F32):
        return nc.dram_tensor(name, list(shape), dt, kind="ExternalInput").ap()

    def dscr(name, shape, dt):
        return nc.dram_tensor(name, list(shape), dt, kind="Internal").ap()

    x_in = din("x", [SEQ, D])
    c_lay = din("c_lay", [128, 8])
    w_in = din("w_in", [DEPTH, D, DIN])
    conv_w = din("conv_w", [DEPTH, 128, 2, 4])
    conv_b = din("conv_b", [DEPTH, 128, 2])
    w_mq = din("w_mq", [DEPTH, 4, 64, 64])
    w_mk = din("w_mk", [DEPTH, 4, 64, 64])
    w_mv = din("w_mv", [DEPTH, 4, 64, 64])
    gbias = din("gbias", [DEPTH, 128, 8])
    g_head_p = din("g_head_p", [DEPTH, 128, 8])
    g_head_r = din("g_head_r", [DEPTH, 128, 256])
    w_out = din("w_out", [DEPTH, D, D])
    w_ada = din("w_ada", [DEPTH, D, 6 * D])
    b_ada = din("b_ada", [DEPTH, 1, 6 * D])
    w_router = din("w_router", [D, NEXP])
    rbias = din("rbias", [128, NEXP])
    w_gate = din("w_gate_e", [DEPTH, NEXP, D, DEXP])
    w_up = din("w_up_e", [DEPTH, NEXP, D, DEXP])
    w_down = din("w_down_e", [DEPTH, NEXP, DEXP, D])
    g_final = din("g_final_r", [128, D])
    cb_in = din("cb", [128, NCB])
    cf_in = din("cf", [128, NCF])
    y_out = nc.dram_tensor("y", [SEQ, D], F32, kind="ExternalOutput").ap()

    xA = dscr("xA", [SEQ, D], F32)
    xB = dscr("xB", [SEQ, D], F32)
    xmT_d = dscr("xmT", [256, SEQ], BF16)
    og_d = dscr("og", [SEQ, 256], BF16)
    gates_d = dscr("gates", [SEQ, 8], F32)
    dqT_d = dscr("dqT", [384, SEQ], BF16)
    dkT_d = dscr("dkT", [384, SEQ], BF16)
    dv_d = dscr("dv", [SEQ, 384], BF16)
    sqT_d = dscr("sqT", [384, SEQ], BF16)
    skT_d = dscr("skT", [384, SEQ], BF16)
    sv_d = dscr("sv", [SEQ, 384], BF16)
    B_xA, B_xB = Buf("xA"), Buf("xB")
    B_scr = {n: Buf(n) for n in ("xmT", "og", "gates", "dqT", "dkT", "dv", "sqT", "skT", "sv")}

    for name in debug:
        pass

    def sbt(name, shape, dt):
        return nc.alloc_sbuf_tensor(name, list(shape), dt)

    cb = sbt("cb_sb", [128, NCB], BF16)
    cf = sbt("cf_sb", [128, NCF], F32)
    B_cb, B_cf = Buf("cb"), Buf("cf")
    S.dma("pool", cb[:], cb_in[:, :], W=[B_cb])
    S.dma("sp", cf[:], cf_in[:, :], W=[B_cf])

    def cbs(name, rows=slice(0, 128)):
        o, w = CB[name]
        return cb[rows, o:o + w]

    def cfs(name, rows=slice(0, 128)):
        o, w = CF[name]
        return cf[rows, o:o + w]

    pp = [nc.alloc_psum_tensor("pp%d" % i, [128, 1024], F32) for i in range(4)]
    ps = [pp[i // 2][:, (i % 2) * 512:(i % 2 + 1) * 512] for i in range(8)]
    PP = pp
    B_ps = [Buf("ps%d" % i, excl=True) for i in range(8)]

    mod = sbt("mod", [128, 6, D], F32)
    B_mod = Buf("mod")
    B_crep = Buf("c_rep")
    c_sb = sbt("c_sb", [128, 8], F32)
    B_c = Buf("c_sb")
    S.dma("sp", c_sb[:], c_lay[:, :], W=[B_c])
    S.op("act", lambda e: e.activation(out=c_sb[:], in_=c_sb[:], func=AF.Silu), R=[B_c], W=[B_c])

    out_tensors = {}

    def dbg_out(name, shape, dt=F32):
        t = nc.dram_tensor("dbg_" + name, list(shape), dt, kind="ExternalOutput").ap()
        out_tensors[name] = t
        return t

    fin_bufs = []

    def phase_mod(l):
        with ExitStack() as st:
            c_rep = st.enter_context(nc.sbuf_tensor("c_rep_L%d" % l, [128, 8, 128], F32))
            S.op("dve", lambda e: e.tensor_copy(out=c_rep[:], in_=c_sb[:].unsqueeze(2).to_broadcast([128, 8, 128])),
                 R=[B_c], W=[B_crep])
            wt = [st.enter_context(nc.sbuf_tensor("wada%d_L%d" % (i, l), [128, 8, 512], F32)) for i in range(2)]
            br = [st.enter_context(nc.sbuf_tensor("brow%d_L%d" % (i, l), [1, 512], F32)) for i in range(2)]
            B_wt = [Buf("wada%d" % i) for i in range(2)]
            B_br = [Buf("brow%d" % i) for i in range(2)]
            for blk in range(12):
                i = blk % 2
                S.dma("sp", wt[i][:], w_ada[l, :, blk * 512:(blk + 1) * 512].rearrange("(k p) n -> p k n", p=128),
                      W=[B_wt[i]])
                S.dma("sp", br[i][:], b_ada[l, :, blk * 512:(blk + 1) * 512], W=[B_br[i]])
                pb = blk % 2
                fns = []
                for k in range(8):
                    fns.append(lambda e, k=k, i=i, pb=pb: e.matmul(ps[pb][:], lhsT=c_rep[:, k, :], rhs=wt[i][:, k, :],
                                                                   start=(k == 0), stop=False))
                fns.append(lambda e, i=i, pb=pb: e.matmul(ps[pb][:], lhsT=cfs("ones", slice(0, 1)), rhs=br[i][:],
                                                          start=False, stop=True))
                S.op("pe", fns, R=[B_wt[i], B_br[i], B_crep, B_cf], W=[B_ps[pb]])
                m = blk // 2
                dst = mod[:, m, (blk % 2) * 512:(blk % 2 + 1) * 512]
                if m in (1, 4):
                    S.op("dve", lambda e, dst=dst, pb=pb: e.tensor_scalar(out=dst, in0=ps[pb][:], scalar1=1.0, scalar2=None,
                                                                         op0=ALU.add), R=[B_ps[pb]], W=[B_mod])
                else:
                    S.op("dve", lambda e, dst=dst, pb=pb: e.tensor_copy(out=dst, in_=ps[pb][:]), R=[B_ps[pb]], W=[B_mod])
            S.barrier()

    def norm_tile(xt, B_xt, sidx, tmp, B_tmp, hout, B_hout, ss, B_ss):
        S.op("act", lambda e: e.activation(out=tmp[:], in_=xt[:], func=AF.Square, accum_out=ss[:, 0:1]),
             R=[B_xt], W=[B_tmp, B_ss])
        S.op("act", lambda e: e.activation(out=ss[:, 1:2], in_=ss[:, 0:1], func=AF.Ln, scale=1.0 / D, bias=EPS),
             R=[B_ss], W=[B_ss])
        S.op("act", lambda e: e.activation(out=ss[:, 2:3], in_=ss[:, 1:2], func=AF.Exp, scale=-0.5),
             R=[B_ss], W=[B_ss])
        S.op("dve", lambda e: e.scalar_tensor_tensor(out=tmp[:], in0=xt[:], scalar=ss[:, 2:3], in1=mod[:, sidx + 1, :],
                                                     op0=ALU.mult, op1=ALU.mult), R=[B_xt, B_ss, B_mod], W=[B_tmp])
        S.op("dve", lambda e: e.tensor_tensor(out=hout[:], in0=tmp[:], in1=mod[:, sidx, :], op=ALU.add),
             R=[B_tmp, B_mod], W=[B_hout])

    def load_win(l, st):
        win = st.enter_context(nc.sbuf_tensor("win_L%d" % l, [128, 8, DIN], BF16))
        B_win = Buf("win")
        for k in range(8):
            S.dma("pool", win[:, k, :], w_in[l, k * 128:(k + 1) * 128, :], W=[B_win])
        return win, B_win

    def phase_inproj(l, x_src, B_xsrc, win, B_win):
        with ExitStack() as st:
            def T(name, shape, dt):
                return st.enter_context(nc.sbuf_tensor(name + "_L%d" % l, list(shape), dt))
            xt = [T("xt%d" % i, [128, D], F32) for i in range(2)]
            B_xt = [Buf("xt%d" % i) for i in range(2)]
            tmp = [T("ntmp%d" % i, [128, D], F32) for i in range(2)]
            B_tmp = [Buf("ntmp%d" % i) for i in range(2)]
            hb = [T("hb%d" % i, [128, D], BF16) for i in range(2)]
            B_hb = [Buf("hb%d" % i) for i in range(2)]
            ss = [T("ss%d" % i, [128, 4], F32) for i in range(2)]
            B_ss = [Buf("ss%d" % i) for i in range(2)]
            hT = [T("hT%d" % i, [128, 8, 512], BF16) for i in range(2)]
            B_hT = [Buf("hT%d" % i) for i in range(2)]
            stF = {n: [T("st_%s%d" % (n, i), [128, w, 512], BF16) for i in range(2)]
                   for n, w in (("xmT", 2), ("dqT", 3), ("dkT", 3), ("sqT", 3), ("skT", 3))}
            B_stF = {n: [Buf("st_%s%d" % (n, i)) for i in range(2)] for n in stF}
            st_og = [T("st_og%d" % i, [128, 4, 256], BF16) for i in range(2)]
            st_g = [T("st_g%d" % i, [128, 4, 8], F32) for i in range(2)]
            st_dv = [T("st_dv%d" % i, [128, 4, 384], BF16) for i in range(2)]
            st_sv = [T("st_sv%d" % i, [128, 4, 384], BF16) for i in range(2)]
            B_og = [Buf("st_og%d" % i) for i in range(2)]
            B_g = [Buf("st_g%d" % i) for i in range(2)]
            B_dv = [Buf("st_dv%d" % i) for i in range(2)]
            B_sv = [Buf("st_sv%d" % i) for i in range(2)]
            fm_specs = [("xmT", 0, 2, 1.0, xmT_d), ("dqT", OFF_DIL, 3, 1.0, dqT_d), ("dkT", OFF_DIL + 384, 3, 0.125, dkT_d),
                        ("sqT", OFF_SB, 3, 1.0, sqT_d), ("skT", OFF_SB + 384, 3, 0.125, skT_d)]
            pcount = [0]

            def next_ps():
                pcount[0] += 1
                return 2 + (pcount[0] % 6)

            def NA(sb, tt):
                ti = sb * 4 + tt
                i = ti % 2
                S.dma("sp", xt[i][:], x_src[ti * 128:(ti + 1) * 128, :], R=[B_xsrc], W=[B_xt[i]])
                norm_tile(xt[i], B_xt[i], 0, tmp[i], B_tmp[i], hb[i], B_hb[i], ss[i], B_ss[i])

            def NB(sb, tt):
                ti = sb * 4 + tt
                i = ti % 2
                sl = sb % 2
                pb = ti % 2
                pT = ps[pb][:].bitcast(BF16)
                S.op("pe", [lambda e, c=c: e.transpose(out=pT[:, c * 128:(c + 1) * 128], in_=hb[i][:, c * 128:(c + 1) * 128],
                                                       identity=cbs("ident")) for c in range(8)],
                     R=[B_hb[i], B_cb], W=[B_ps[pb]])
                S.op("act", lambda e: e.copy(out=hT[sl][:, :, tt * 128:(tt + 1) * 128], in_=pT.rearrange("p (c t) -> p c t", c=8)),
                     R=[B_ps[pb]], W=[B_hT[sl]])

            def M_items(sb):
                sl = sb % 2
                items = []
                for (n, off, nch, scale, dst) in fm_specs:
                    for c in range(nch):
                        def it(n=n, off=off, nch=nch, scale=scale, dst=dst, c=c):
                            pb = next_ps()
                            S.op("pe", [lambda e, k=k: e.matmul(ps[pb][:], lhsT=win[:, k, off + c * 128: off + (c + 1) * 128],
                                                                rhs=hT[sl][:, k, :], start=(k == 0), stop=(k == 7)) for k in range(8)],
                                 R=[B_win, B_hT[sl]], W=[B_ps[pb]])
                            S.op("act", lambda e: e.activation(out=stF[n][sl][:, c, :], in_=ps[pb][:], func=AF.Copy, scale=scale),
                                 R=[B_ps[pb]], W=[B_stF[n][sl]])
                            if c == nch - 1:
                                S.dma("sp", dst[:, sb * 512:(sb + 1) * 512].rearrange("(c p) t -> p c t", p=128), stF[n][sl][:],
                                      R=[B_stF[n][sl]], W=[B_scr[n]])
                        items.append(it)
                for tt in range(4):
                    def it(tt=tt):
                        pb = next_ps()
                        S.op("pe", [lambda e, k=k: e.matmul(ps[pb][:, 0:264], lhsT=hT[sl][:, k, tt * 128:(tt + 1) * 128],
                                                            rhs=win[:, k, OFF_O:OFF_O + 264], start=(k == 0), stop=(k == 7))
                                    for k in range(8)], R=[B_win, B_hT[sl]], W=[B_ps[pb]])
                        S.op("act", lambda e: e.activation(out=st_og[sl][:, tt, :], in_=ps[pb][:, 0:256], func=AF.Copy),
                             R=[B_ps[pb]], W=[B_og[sl]])
                        S.op("dve", lambda e: e.tensor_copy(out=st_g[sl][:, tt, :], in_=ps[pb][:, 256:264]), R=[B_ps[pb]], W=[B_g[sl]])
                    items.append(it)
                    for (stt, Bst, off) in ((st_dv, B_dv, OFF_DIL + 768), (st_sv, B_sv, OFF_SB + 768)):
                        def it(tt=tt, stt=stt, Bst=Bst, off=off):
                            pb = next_ps()
                            S.op("pe", [lambda e, k=k: e.matmul(ps[pb][:, 0:384], lhsT=hT[sl][:, k, tt * 128:(tt + 1) * 128],
                                                                rhs=win[:, k, off:off + 384], start=(k == 0), stop=(k == 7))
                                        for k in range(8)], R=[B_win, B_hT[sl]], W=[B_ps[pb]])
                            S.op("dve", lambda e: e.tensor_copy(out=stt[sl][:, tt, :], in_=ps[pb][:, 0:384]), R=[B_ps[pb]], W=[Bst[sl]])
                        items.append(it)

                def fin():
                    r0, r1 = sb * 512, (sb + 1) * 512
                    S.dma("sp", og_d[r0:r1, :].rearrange("(t p) c -> p t c", p=128), st_og[sl][:], R=[B_og[sl]], W=[B_scr["og"]])
                    S.dma("sp", gates_d[r0:r1, :].rearrange("(t p) c -> p t c", p=128), st_g[sl][:], R=[B_g[sl]], W=[B_scr["gates"]])
                    S.dma("sp", dv_d[r0:r1, :].rearrange("(t p) c -> p t c", p=128), st_dv[sl][:], R=[B_dv[sl]], W=[B_scr["dv"]])
                    S.dma("sp", sv_d[r0:r1, :].rearrange("(t p) c -> p t c", p=128), st_sv[sl][:], R=[B_sv[sl]], W=[B_scr["sv"]])
                return items, fin

            for tt in range(4):
                NA(0, tt)
                NB(0, tt)
            for sb in range(NSB):
                items, fin = M_items(sb)
                nper = (len(items) + 3) // 4
                for part in range(4):
                    if sb + 1 < NSB:
                        NA(sb + 1, part)
                    for it in items[part * nper:(part + 1) * nper]:
                        it()
                    if sb + 1 < NSB:
                        NB(sb + 1, part)
                fin()
            S.barrier()


    Y = {}
    B_ymix = Buf("ymixT")
    ghp = sbt("ghp", [128, DEPTH, 8], F32)
    B_ghp = Buf("ghp")
    for l_ in range(DEPTH):
        S.dma("sp", ghp[:, l_, :], g_head_p[l_, :, :], W=[B_ghp])

    def head_norm_fm(l, src_ap, B_src, chunk, col0, T, tag):
        sq, B_sq, rs, B_rs, pstat, B_pstat = T
        S.op("act", lambda e: e.activation(out=sq[:], in_=src_ap, func=AF.Square), R=[B_src], W=[B_sq])
        S.op("pe", lambda e: e.matmul(pstat[:], lhsT=cfs("blk64"), rhs=sq[:], start=True, stop=True),
             R=[B_sq, B_cf], W=[B_pstat])
        S.op("act", lambda e: e.activation(out=rs[:], in_=pstat[:], func=AF.Ln, scale=1.0 / 64, bias=EPS),
             R=[B_pstat], W=[B_rs])
        S.op("act", lambda e: e.activation(out=rs[:], in_=rs[:], func=AF.Exp, scale=-0.5), R=[B_rs], W=[B_rs])
        S.op("dve", lambda e: e.scalar_tensor_tensor(out=Y["t"][:, chunk, col0:col0 + 512], in0=src_ap,
                                                     scalar=ghp[:, l, chunk:chunk + 1], in1=rs[:],
                                                     op0=ALU.mult, op1=ALU.mult),
             R=[B_src, B_rs, B_ghp], W=[B_ymix])

    def phase_sb(l, co=None):
        with ExitStack() as st:
            def T(name, shape, dt):
                return st.enter_context(nc.sbuf_tensor(name + "_L%d" % l, list(shape), dt))
            qT = T("sb_qT", [128, SEQ], BF16)
            kT = T("sb_kT", [128, SEQ], BF16)
            V = [T("sb_V%d" % i, [128, NT, 128], BF16) for i in range(2)]
            B_qT, B_kT, B_V = Buf("sb_qT"), Buf("sb_kT"), [Buf("sb_V0"), Buf("sb_V1")]
            C32 = [T("sb_C32_%d" % p, [128, 2, 512], F32) for p in range(2)]
            Cb = [T("sb_Cb_%d" % p, [128, 2, 512], BF16) for p in range(2)]
            B_C32 = [Buf("c32_%d" % p) for p in range(2)]
            B_Cb = [Buf("cb_%d" % p) for p in range(2)]
            e_sb = T("sb_e", [128, 2, 512], F32)
            B_e = Buf("sb_e")
            NSP, NA = 3, 2
            sp_sb = [T("sb_sp%d" % i, [128, 2, 512], BF16) for i in range(NSP)]
            B_sp = [Buf("sb_sp%d" % i) for i in range(NSP)]
            A_sb = [T("sb_A%d" % i, [128, 2, 512], BF16) for i in range(NA)]
            B_A = [Buf("sb_A%d" % i) for i in range(NA)]
            sq = T("sb_sq", [128, 512], F32)
            rs = T("sb_rs", [128, 512], F32)
            normT = (sq, Buf("sb_sq"), rs, Buf("sb_rs"), ps[0], B_ps[0])
            zP = pp[0][:].rearrange("p (h q) -> p h q", h=2)
            xP = pp[1][:].rearrange("p (h q) -> p h q", h=2)
            mstr2 = cbs("mstrict").unsqueeze(1).to_broadcast([128, 2, 128])
            for i in range(2):
                S.op("dve", lambda e, i=i: e.memset(V[i][:], 0.0), W=[B_V[i]])
            for c in range(3):
                S.dma("sp", qT[:], sqT_d[c * 128:(c + 1) * 128, :], R=[B_scr["sqT"]], W=[B_qT])
                S.dma("sp", kT[:], skT_d[c * 128:(c + 1) * 128, :], R=[B_scr["skT"]], W=[B_kT])
                for h in range(2):
                    S.dma("sp", V[h][:, :, h * 64:(h + 1) * 64],
                          sv_d[:, c * 128 + h * 64: c * 128 + (h + 1) * 64].rearrange("(t p) e -> p t e", p=128),
                          R=[B_scr["sv"]], W=[B_V[h]])
                tiles = []
                for sb in range(NSB):
                    for j in range(4 * sb + 3, -1, -1):
                        tiles.append((sb, j))
                n = len(tiles)

                def geom(t):
                    sb, j = tiles[t]
                    c0 = (j - 4 * sb) * 128 if j >= 4 * sb else 0
                    return sb, j, c0, (j >= 4 * sb), (j == 4 * sb + 3), (j == 0)

                def S1(t):
                    sb, j, c0, diag, first, last = geom(t)
                    par = sb % 2
                    if first:
                        S.op("dve", lambda e: e.memset(C32[par][:], 0.0), W=[B_C32[par]])
                        ob = 6 + par
                        S.op("dve", lambda e: e.memset(ps[ob][:], 0.0), W=[B_ps[ob]])
                    S.op("pe", [lambda e, h=h: e.matmul(ps[h][:, c0:512], lhsT=kT[h * 64:(h + 1) * 64, j * 128:(j + 1) * 128],
                                                        rhs=qT[h * 64:(h + 1) * 64, sb * 512 + c0:(sb + 1) * 512], start=True, stop=True)
                                for h in range(2)], R=[B_kT, B_qT], W=[B_ps[0], B_ps[1]])

                def S2a(t):
                    sb, j, c0, diag, first, last = geom(t)
                    S.op("act", lambda e: e.activation(out=e_sb[:, :, c0:512], in_=zP[:, :, c0:512], func=AF.Exp),
                         R=[B_ps[0], B_ps[1]], W=[B_e])

                def S2(t):
                    sb, j, c0, diag, first, last = geom(t)
                    k = t % NSP
                    S.op("act", lambda e: e.activation(out=sp_sb[k][:, :, c0:512], in_=e_sb[:, :, c0:512], func=AF.Ln, bias=1.0),
                         R=[B_e], W=[B_sp[k]])
                    if diag:
                        S.op("dve", lambda e: e.tensor_tensor(out=sp_sb[k][:, :, c0:c0 + 128], in0=sp_sb[k][:, :, c0:c0 + 128],
                                                              in1=mstr2, op=ALU.mult), R=[B_sp[k], B_cb], W=[B_sp[k]])

                def S3(t):
                    sb, j, c0, diag, first, last = geom(t)
                    k = t % NSP
                    par = sb % 2
                    fns = []
                    for h in range(2):
                        r = slice(h * 64, (h + 1) * 64)
                        pb = 2 + h
                        fns.append(lambda e, r=r, pb=pb: e.matmul(ps[pb][:, c0:512], lhsT=kT[r, j * 128:(j + 1) * 128],
                                                                  rhs=qT[r, sb * 512 + c0:(sb + 1) * 512], start=True, stop=False))
                        fns.append(lambda e, h=h, pb=pb: e.matmul(ps[pb][:, c0:512], lhsT=cbs("negtri"), rhs=sp_sb[k][:, h, c0:512],
                                                                  start=False, stop=first))
                        if not first:
                            chi = C32[par][:, h, :].bitcast(BF16)[:, 2 * c0 + 1:1024:2]
                            fns.append(lambda e, chi=chi, pb=pb: e.matmul(ps[pb][:, c0:512], lhsT=cbs("negones"), rhs=chi,
                                                                          start=False, stop=True))
                    S.op("pe", fns, R=[B_kT, B_qT, B_cb, B_sp[k], B_C32[par]], W=[B_ps[2], B_ps[3]])

                def S4(t):
                    sb, j, c0, diag, first, last = geom(t)
                    a = t % NA
                    S.op("act", lambda e: e.activation(out=A_sb[a][:, :, c0:512], in_=xP[:, :, c0:512], func=AF.Exp),
                         R=[B_ps[2], B_ps[3]], W=[B_A[a]])
                    if diag:
                        S.op("dve", lambda e: e.tensor_tensor(out=A_sb[a][:, :, c0:c0 + 128], in0=A_sb[a][:, :, c0:c0 + 128],
                                                              in1=mstr2, op=ALU.mult), R=[B_A[a], B_cb], W=[B_A[a]])

                def S6(t):
                    sb, j, c0, diag, first, last = geom(t)
                    if last:
                        return
                    k = t % NSP
                    par = sb % 2
                    S.op("dve", lambda e: e.tensor_tensor(out=C32[par][:, :, c0:512], in0=C32[par][:, :, c0:512],
                                                          in1=sp_sb[k][:, :, c0:512], op=ALU.add),
                         R=[B_sp[k]], W=[B_C32[par]])


                def S5(t):
                    sb, j, c0, diag, first, last = geom(t)
                    a = t % NA
                    ob = 6 + (sb % 2)
                    S.op("pe", [lambda e, h=h: e.matmul(ps[ob][:, c0:512], lhsT=V[h][:, j, :], rhs=A_sb[a][:, h, c0:512],
                                                        start=False, stop=False) for h in range(2)],
                         R=[B_V[0], B_V[1], B_A[a]], W=[B_ps[ob]])
                    if last:
                        head_norm_fm(l, ps[ob][:], B_ps[ob], 5 + c, sb * 512, normT, "sb")

                for tau in range(n + 2):
                    if tau < n:
                        S1(tau)
                        S2a(tau)
                        S2(tau)
                    if 0 <= tau - 1 < n:
                        S3(tau - 1)
                        S4(tau - 1)
                        S6(tau - 1)
                    if 0 <= tau - 2 < n:
                        S5(tau - 2)
                    if co is not None and co[0] is not None and (tau % CO_SKIP[0] != CO_SKIP[0] - 1):
                        if next(co[0], "end") in ("hold", "end"):
                            co[0] = None
            if co is not None:
                while co[0] is not None:
                    if next(co[0], "end") in ("hold", "end"):
                        co[0] = None
            S.barrier()

    def phase_dil(l):
        with ExitStack() as st:
            def T(name, shape, dt):
                return st.enter_context(nc.sbuf_tensor(name + "_L%d" % l, list(shape), dt))
            qT = T("dl_qT", [128, SEQ], BF16)
            kT = T("dl_kT", [128, SEQ], BF16)
            Vs = [[T("dl_V%d_%d" % (i, pp), [128, NT, 128], BF16) for i in range(2)] for pp in range(2)]
            B_Vs = [[Buf("dl_V%d_%d" % (i, pp)) for i in range(2)] for pp in range(2)]
            B_qT, B_kT = Buf("dl_qT"), Buf("dl_kT")
            accn = T("dl_accn", [128, SEQ], F32)
            accd = T("dl_accd", [128, SEQ], F32)
            B_accn, B_accd = Buf("accn"), Buf("accd")
            NP_ = 8
            P_sb = [T("dl_P%d" % i, [128, 2, 128], BF16) for i in range(NP_)]
            B_P = [Buf("dl_P%d" % i) for i in range(NP_)]
            rsA = [T("dl_rsA%d" % i, [128, 512], F32) for i in range(4)]
            B_rsA = [Buf("dl_rsA%d" % i) for i in range(4)]
            sq2 = [T("dl_sq%d" % i, [128, 512], F32) for i in range(2)]
            B_sq2 = [Buf("dl_sq%d" % i) for i in range(2)]
            rsB = [T("dl_rsB%d" % i, [128, 512], F32) for i in range(2)]
            B_rsB = [Buf("dl_rsB%d" % i) for i in range(2)]
            for pp in range(2):
                for i in range(2):
                    S.op("dve", lambda e, i=i, pp=pp: e.memset(Vs[pp][i][:], 0.0), W=[B_Vs[pp][i]])
                S.op("dve", lambda e, pp=pp: e.memset(Vs[pp][0][:, :, 64:65], 1.0), W=[B_Vs[pp][0]])
                S.op("dve", lambda e, pp=pp: e.memset(Vs[pp][1][:, :, 0:1], 1.0), W=[B_Vs[pp][1]])
            eo, _ = CB["dilE"]
            vcnt = [0]

            def load_V(c, pi):
                win_, d_ = DIL_PATTERNS[pi]
                nb_ = (SEQ // d_) // 128
                pp = vcnt[0] % 2
                vcnt[0] += 1
                for h in range(2):
                    src = dv_d[:, c * 128 + h * 64: c * 128 + (h + 1) * 64].rearrange("(j i r) e -> i r j e", i=128, r=d_)
                    dst = Vs[pp][h][:, :, h * 64:(h + 1) * 64].rearrange("p (r j) e -> p r j e", r=d_)
                    if d_ <= nb_:
                        for r0 in range(d_):
                            S.dma("sp", dst[:, r0, :, :], src[:, r0, :, :], R=[B_scr["dv"]], W=[B_Vs[pp][h]])
                    else:
                        for j0 in range(nb_):
                            S.dma("sp", dst[:, :, j0, :], src[:, :, j0, :], R=[B_scr["dv"]], W=[B_Vs[pp][h]])
                return pp
            pending = {}
            pending[(0, 0)] = load_V(0, 0)
            for c in range(3):
                S.dma("sp", qT[:], dqT_d[c * 128:(c + 1) * 128, :], R=[B_scr["dqT"]], W=[B_qT])
                S.dma("sp", kT[:], dkT_d[c * 128:(c + 1) * 128, :], R=[B_scr["dkT"]], W=[B_kT])
                grp = [0]
                for pi, (win, d) in enumerate(DIL_PATTERNS):
                    L = SEQ // d
                    nblk = L // 128
                    pp_cur = pending.pop((c, pi))
                    V = Vs[pp_cur]
                    B_V = B_Vs[pp_cur]
                    nxt = (c, pi + 1) if pi + 1 < 3 else ((c + 1, 0) if c + 1 < 3 else None)
                    if nxt is not None:
                        pending[nxt] = load_V(*nxt)
                    gsz = min(4, nblk)
                    tiles = []
                    for r in range(d):
                        for g in range(nblk // gsz):
                            for nloc in range(gsz):
                                nq = g * gsz + nloc
                                for j in (nq - 1, nq):
                                    if j >= 0:
                                        tiles.append((r, g, nloc, nq, j))
                    n = len(tiles)
                    gpar = {}

                    def tok(r, blk):
                        a = r + d * 128 * blk
                        return slice(a, a + d * 127 + 1, d) if d > 1 else slice(a, a + 128)

                    def S1(t):
                        r, g, nloc, nq, j = tiles[t]
                        first = (nloc == 0 and j == max(nq - 1, 0))
                        if first:
                            gp = (grp[0] % 2)
                            pn, pd = 4 + 2 * gp, 5 + 2 * gp
                            grp[0] += 1
                            N = gsz * 128
                            S.op("dve", lambda e: e.memset(ps[pn][:, 0:N], 0.0), W=[B_ps[pn]])
                            S.op("dve", lambda e: e.memset(ps[pd][:, 0:N], 0.0), W=[B_ps[pd]])
                        gpar[t] = (grp[0] - 1) % 2
                        zb = 2 * (t % 2)
                        S.op("pe", [lambda e, h=h: e.matmul(ps[zb + h][:, 0:128], lhsT=kT[h * 64:(h + 1) * 64, tok(r, j)],
                                                            rhs=qT[h * 64:(h + 1) * 64, tok(r, nq)], start=True, stop=True)
                                    for h in range(2)], R=[B_kT, B_qT], W=[B_ps[zb], B_ps[zb + 1]])

                    def S2(t):
                        r, g, nloc, nq, j = tiles[t]
                        zb = 2 * (t % 2)
                        pq = t % NP_
                        zv = PP[t % 2][:].rearrange("p (h q) -> p h q", h=2)[:, :, 0:128]
                        S.op("act", lambda e: e.activation(out=P_sb[pq][:], in_=zv, func=AF.Exp),
                             R=[B_ps[zb], B_ps[zb + 1]], W=[B_P[pq]])
                        e0 = eo + ((c * 3 + pi) * 2) * 256
                        off = 0 if j == nq else 128
                        ev = cb[:, e0:e0 + 512].rearrange("p (h x) -> p h x", h=2)[:, :, off:off + 128]
                        S.op("dve", lambda e: e.tensor_tensor(out=P_sb[pq][:], in0=P_sb[pq][:], in1=ev, op=ALU.mult),
                             R=[B_P[pq], B_cb], W=[B_P[pq]])

                    def S3(t):
                        r, g, nloc, nq, j = tiles[t]
                        gp = gpar[t]
                        pa = t % NP_
                        pn, pd = 4 + 2 * gp, 5 + 2 * gp
                        cs = slice(nloc * 128, (nloc + 1) * 128)
                        S.op("pe", [lambda e, h=h: e.matmul(ps[(pn, pd)[h]][:, cs], lhsT=V[h][:, r * nblk + j, :], rhs=P_sb[pa][:, h, :],
                                                            start=False, stop=False) for h in range(2)],
                             R=[B_V[0], B_V[1], B_P[pa]], W=[B_ps[pn], B_ps[pd]])
                        last = (nloc == gsz - 1 and j == nq)
                        if last:
                            N = gsz * 128
                            a = r + d * 128 * gsz * g
                            dst = slice(a, a + d * (N - 1) + 1, d) if d > 1 else slice(a, a + N)
                            if pi == 0:
                                S.op("dve", lambda e: e.tensor_copy(out=accn[:, dst], in_=ps[pn][:, 0:N]), R=[B_ps[pn]], W=[B_accn])
                                S.op("dve", lambda e: e.tensor_copy(out=accd[:, dst], in_=ps[pd][:, 0:N]), R=[B_ps[pd]], W=[B_accd])
                            else:
                                S.op("dve", lambda e: e.tensor_tensor(out=accn[:, dst], in0=accn[:, dst], in1=ps[pn][:, 0:N], op=ALU.add),
                                     R=[B_ps[pn]], W=[B_accn])
                                S.op("dve", lambda e: e.tensor_tensor(out=accd[:, dst], in0=accd[:, dst], in1=ps[pd][:, 0:N], op=ALU.add),
                                     R=[B_ps[pd]], W=[B_accd])

                    DEP = 3
                    for tau in range(n + DEP):
                        if tau < n:
                            S1(tau)
                            S2(tau)
                        if 0 <= tau - DEP < n:
                            S3(tau - DEP)
                def NA(sb):
                    cs = slice(sb * 512, (sb + 1) * 512)
                    pb = sb % 4
                    S.op("pe", [lambda e: e.matmul(ps[pb][:], lhsT=cfs("selA"), rhs=accn[:, cs], start=True, stop=False),
                                lambda e: e.matmul(ps[pb][:], lhsT=cfs("selB"), rhs=accd[:, cs], start=False, stop=True)],
                         R=[B_accn, B_accd, B_cf], W=[B_ps[pb]])
                    S.op("act", lambda e: e.activation(out=rsA[pb][:], in_=ps[pb][:], func=AF.Ln), R=[B_ps[pb]], W=[B_rsA[pb]])
                    S.op("act", lambda e: e.activation(out=rsA[pb][:], in_=rsA[pb][:], func=AF.Exp, scale=-1.0), R=[B_rsA[pb]], W=[B_rsA[pb]])

                def NB_(sb):
                    cs = slice(sb * 512, (sb + 1) * 512)
                    pb = sb % 4
                    S.op("dve", lambda e: e.tensor_tensor(out=accn[0:64, cs], in0=accn[0:64, cs], in1=rsA[pb][0:64, :], op=ALU.mult),
                         R=[B_rsA[pb]], W=[B_accn])
                    S.op("dve", lambda e: e.tensor_tensor(out=accn[64:128, cs], in0=accd[64:128, cs], in1=rsA[pb][64:128, :], op=ALU.mult),
                         R=[B_rsA[pb], B_accd], W=[B_accn])

                def NC(sb):
                    cs = slice(sb * 512, (sb + 1) * 512)
                    i_ = sb % 2
                    pb = 4 + i_
                    S.op("act", lambda e: e.activation(out=sq2[i_][:], in_=accn[:, cs], func=AF.Square), R=[B_accn], W=[B_sq2[i_]])
                    S.op("pe", lambda e: e.matmul(ps[pb][:], lhsT=cfs("blk64"), rhs=sq2[i_][:], start=True, stop=True),
                         R=[B_sq2[i_], B_cf], W=[B_ps[pb]])
                    S.op("act", lambda e: e.activation(out=rsB[i_][:], in_=ps[pb][:], func=AF.Ln, scale=1.0 / 64, bias=EPS),
                         R=[B_ps[pb]], W=[B_rsB[i_]])
                    S.op("act", lambda e: e.activation(out=rsB[i_][:], in_=rsB[i_][:], func=AF.Exp, scale=-0.5), R=[B_rsB[i_]], W=[B_rsB[i_]])

                def ND(sb):
                    cs = slice(sb * 512, (sb + 1) * 512)
                    i_ = sb % 2
                    S.op("dve", lambda e: e.scalar_tensor_tensor(out=Y["t"][:, 2 + c, cs], in0=accn[:, cs], scalar=ghp[:, l, 2 + c:3 + c],
                                                                 in1=rsB[i_][:], op0=ALU.mult, op1=ALU.mult),
                         R=[B_accn, B_rsB[i_], B_ghp], W=[B_ymix])

                for st_ in range(NSB + 3):
                    if st_ < NSB:
                        NA(st_)
                    if 0 <= st_ - 1 < NSB:
                        NB_(st_ - 1)
                    if 0 <= st_ - 2 < NSB:
                        NC(st_ - 2)
                    if 0 <= st_ - 3 < NSB:
                        ND(st_ - 3)
            S.barrier()


    def gen_mlstm(l, bk):
        with ExitStack() as st:
            def T(name, shape, dt):
                return st.enter_context(nc.sbuf_tensor(name + "_L%d" % l, list(shape), dt))
            cw = T("ml_cw", [128, 2, 4], F32)
            cbi = T("ml_cb", [128, 2], F32)
            ncbi = T("ml_ncb", [128, 2], F32)
            gb = T("ml_gb", [128, 8], F32)
            ghr = T("ml_ghr", [128, 256], F32)
            B_small = Buf("ml_small")
            S.dma("sp", cw[:], conv_w[l, :, :, :], W=[B_small])
            S.dma("sp", cbi[:], conv_b[l, :, :], W=[B_small])
            S.dma("sp", gb[:], gbias[l, :, :], W=[B_small])
            S.dma("sp", ghr[:], g_head_r[l, :, :], W=[B_small])
            S.op("dve", lambda e: e.tensor_scalar(out=ncbi[:], in0=cbi[:], scalar1=-1.0, scalar2=None, op0=ALU.mult),
                 R=[B_small], W=[B_small])
            Wbd = {}
            B_W = Buf("ml_W")
            for nm, src in (("q", w_mq), ("k", w_mk), ("v", w_mv)):
                Wbd[nm] = T("ml_W" + nm, [128, 2, 128], BF16)
                S.op("dve", lambda e, nm=nm: e.memset(Wbd[nm][:], 0.0), W=[B_W])
                for c in range(2):
                    for hh in range(2):
                        S.dma("pool", Wbd[nm][hh * 64:(hh + 1) * 64, c, hh * 64:(hh + 1) * 64], src[l, 2 * c + hh, :, :], W=[B_W])
            graw = T("ml_graw", [128, NT, 8], F32)
            B_g = Buf("ml_graw")
            S.dma("sp", graw[:], gates_d[:, :].rearrange("(t p) c -> p t c", p=128), R=[B_scr["gates"]], W=[B_g])
            S.op("dve", lambda e: e.tensor_tensor(out=graw[:], in0=graw[:], in1=gb[:].unsqueeze(1).to_broadcast([128, NT, 8]),
                                                  op=ALU.add), R=[B_small], W=[B_g])
            nl = T("ml_nl", [128, NT, 4], F32)
            a_t = T("ml_a", [128, NT, 4], F32)
            b_t = T("ml_b", [128, NT, 4], F32)
            bl_t = T("ml_bl", [128, NT, 4], F32)
            B_nl, B_a, B_b, B_bl = Buf("nl"), Buf("a"), Buf("b"), Buf("bl")
            S.op("act", lambda e: e.activation(out=nl[:], in_=graw[:, :, 4:8], func=AF.Exp, scale=-1.0), R=[B_g], W=[B_nl])
            S.op("act", lambda e: e.activation(out=nl[:], in_=nl[:], func=AF.Ln, bias=1.0), R=[B_nl], W=[B_nl])
            nlf = nl[:].rearrange("p t h -> p (t h)")
            S.op("pe", lambda e: e.matmul(ps[bk["c0"]][:, 0:128], lhsT=cfs("triu"), rhs=nlf, start=True, stop=True),
                 R=[B_nl, B_cf], W=[B_ps[bk["c0"]]])
            S.op("pe", lambda e: e.matmul(ps[bk["c1"]][:, 0:128], lhsT=cfs("ones"), rhs=nlf, start=True, stop=True),
                 R=[B_nl, B_cf], W=[B_ps[bk["c1"]]])
            pc = ps[bk["c0"]][:, 0:128].rearrange("p (t h) -> p t h", h=4)
            S.op("dve", lambda e: e.tensor_tensor(out=a_t[:], in0=graw[:, :, 0:4], in1=pc, op=ALU.add), R=[B_g, B_ps[bk["c0"]]], W=[B_a])
            S.op("act", lambda e: e.activation(out=a_t[:], in_=a_t[:], func=AF.Exp), R=[B_a], W=[B_a])
            S.op("act", lambda e: e.activation(out=b_t[:], in_=pc, func=AF.Exp, scale=-1.0), R=[B_ps[bk["c0"]]], W=[B_b])
            S.op("act", lambda e: e.activation(out=bl_t[:], in_=ps[bk["c1"]][:, 0:128].rearrange("p (t h) -> p t h", h=4), func=AF.Exp,
                                               scale=-1.0), R=[B_ps[bk["c1"]]], W=[B_bl])
            C32 = T("ml_C32", [128, 2, 65], F32)
            Cbf = T("ml_Cbf", [128, 2, 65], BF16)
            B_C32, B_Cbf = Buf("ml_C32"), Buf("ml_Cbf")
            S.op("dve", lambda e: e.memset(C32[:], 0.0), W=[B_C32])
            S.op("dve", lambda e: e.memset(Cbf[:], 0.0), W=[B_Cbf])
            xmp = [T("ml_xmp%d" % i, [128, 2, 515], BF16) for i in range(2)]
            B_xmp = [Buf("ml_xmp%d" % i) for i in range(2)]
            ogt = [T("ml_og%d" % i, [128, 4, 256], BF16) for i in range(2)]
            B_ogt = [Buf("ml_og%d" % i) for i in range(2)]
            acc = T("ml_acc", [128, 2, 512], F32)
            ez = T("ml_ez", [128, 2, 512], F32)
            xc = T("ml_xc", [128, 2, 512], BF16)
            B_acc, B_ez, B_xc = Buf("ml_acc"), Buf("ml_ez"), Buf("ml_xc")
            qTs = T("ml_qT", [128, 2, 512], BF16)
            kTs = T("ml_kT", [128, 2, 512], BF16)
            B_qTs, B_kTs = Buf("ml_qT"), Buf("ml_kT")
            ktok = T("ml_ktok", [128, 256], BF16)
            Vaug = T("ml_Vaug", [128, 4, 65], BF16)
            swm = T("ml_swm", [128, 4, 128], BF16)
            B_ktok, B_Vaug, B_swm = Buf("ml_ktok"), Buf("ml_Vaug"), Buf("ml_swm")
            sm = T("ml_sm", [128, 16], F32)
            B_sm = Buf("ml_sm")
            eo = T("ml_eo", [128, 256], F32)
            t1 = T("ml_t1", [128, 256], F32)
            ysq = T("ml_ysq", [128, 256], F32)
            yn = T("ml_yn", [128, 256], BF16)
            B_eo, B_t1, B_ysq, B_yn = Buf("ml_eo"), Buf("ml_t1"), Buf("ml_ysq"), Buf("ml_yn")
            ctmp = T("ml_ctmp", [128, 2, 65], F32)
            B_ctmp = Buf("ml_ctmp")

            yield
            for sb in range(NSB if ML_CUT[0] > 1 else 0):
                i2 = sb % 2
                xm = xmp[i2]
                if sb == 0:
                    S.op("dve", lambda e: e.memset(xm[:, :, 0:3], 0.0), W=[B_xmp[i2]])
                    S.dma("sp", xm[:, :, 3:515], xmT_d[:, 0:512].rearrange("(c p) t -> p c t", p=128), R=[B_scr["xmT"]], W=[B_xmp[i2]])
                else:
                    S.dma("sp", xm[:, :, 0:515], xmT_d[:, sb * 512 - 3:(sb + 1) * 512].rearrange("(c p) t -> p c t", p=128),
                          R=[B_scr["xmT"]], W=[B_xmp[i2]])
                S.dma("sp", ogt[i2][:], og_d[sb * 512:(sb + 1) * 512, :].rearrange("(t p) c -> p t c", p=128), R=[B_scr["og"]],
                      W=[B_ogt[i2]])
                yield
                for c in range(2):
                    S.op("dve", lambda e, c=c: e.tensor_scalar(out=acc[:, c, :], in0=xm[:, c, 0:512], scalar1=cw[:, c, 0:1], scalar2=None,
                                                               op0=ALU.mult), R=[B_xmp[i2], B_small], W=[B_acc])
                    for j in range(1, 4):
                        S.op("dve", lambda e, c=c, j=j: e.scalar_tensor_tensor(out=acc[:, c, :], in0=xm[:, c, j:j + 512],
                                                                               scalar=cw[:, c, j:j + 1], in1=acc[:, c, :],
                                                                               op0=ALU.mult, op1=ALU.add),
                             R=[B_xmp[i2], B_small], W=[B_acc])
                    S.op("act", lambda e, c=c: e.activation(out=ez[:, c, :], in_=acc[:, c, :], func=AF.Exp, scale=-1.0,
                                                            bias=ncbi[:, c:c + 1]), R=[B_acc, B_small], W=[B_ez])
                    S.op("dve", lambda e, c=c: e.tensor_scalar(out=ez[:, c, :], in0=ez[:, c, :], scalar1=1.0, scalar2=None, op0=ALU.add),
                         R=[B_ez], W=[B_ez])
                    S.op("dve", lambda e, c=c: e.reciprocal(out=ez[:, c, :], in_=ez[:, c, :]), R=[B_ez], W=[B_ez])
                    S.op("dve", lambda e, c=c: e.scalar_tensor_tensor(out=xc[:, c, :], in0=acc[:, c, :], scalar=cbi[:, c:c + 1],
                                                                      in1=ez[:, c, :], op0=ALU.add, op1=ALU.mult),
                         R=[B_acc, B_ez, B_small], W=[B_xc])
                yield
                for c in range(2):
                    S.op("pe", lambda e, c=c: e.matmul(ps[bk["q"]][:], lhsT=Wbd["q"][:, c, :], rhs=xc[:, c, :], start=True, stop=True),
                         R=[B_W, B_xc], W=[B_ps[bk["q"]]])
                    S.op("act", lambda e, c=c: e.copy(out=qTs[:, c, :], in_=ps[bk["q"]][:]), R=[B_ps[bk["q"]]], W=[B_qTs])
                    S.op("pe", lambda e, c=c: e.matmul(ps[bk["k"]][:], lhsT=Wbd["k"][:, c, :], rhs=xc[:, c, :], start=True, stop=True),
                         R=[B_W, B_xc], W=[B_ps[bk["k"]]])
                    S.op("act", lambda e, c=c: e.activation(out=kTs[:, c, :], in_=ps[bk["k"]][:], func=AF.Copy, scale=0.125),
                         R=[B_ps[bk["k"]]], W=[B_kTs])
                for i in range(4 if ML_CUT[0] > 2 else 0):
                    cut = ML_CUT[0]
                    ci = sb * 4 + i
                    ts = slice(i * 128, (i + 1) * 128)
                    yield
                    S.op("pe", [lambda e, c=c: e.matmul(ps[bk["kt"]][:, c * 128:(c + 1) * 128], lhsT=xc[:, c, ts], rhs=Wbd["k"][:, c, :],
                                                        start=True, stop=True) for c in range(2)],
                         R=[B_xc, B_W], W=[B_ps[bk["kt"]]])
                    S.op("act", lambda e: e.activation(out=ktok[:], in_=ps[bk["kt"]][:, 0:256], func=AF.Copy, scale=0.125),
                         R=[B_ps[bk["kt"]]], W=[B_ktok])
                    S.op("pe", [lambda e, c=c: e.matmul(ps[bk["vt"]][:, c * 128:(c + 1) * 128], lhsT=xm[:, c, 3 + i * 128:3 + (i + 1) * 128],
                                                        rhs=Wbd["v"][:, c, :], start=True, stop=True) for c in range(2)],
                         R=[B_xmp[i2], B_W], W=[B_ps[bk["vt"]]])
                    S.op("dve", lambda e: e.tensor_tensor(out=Vaug[:, :, 0:64], in0=ps[bk["vt"]][:, 0:256].rearrange("p (h e) -> p h e", h=4),
                                                          in1=a_t[:, ci, :].unsqueeze(2).to_broadcast([128, 4, 64]), op=ALU.mult),
                         R=[B_ps[bk["vt"]], B_a], W=[B_Vaug])
                    S.op("dve", lambda e: e.tensor_copy(out=Vaug[:, :, 64], in_=a_t[:, ci, :]), R=[B_a], W=[B_Vaug])
                    if cut <= 3:
                        continue
                    yield
                    sbank = (bk["S0"], bk["S1"])
                    S.op("pe", [lambda e, h=h: e.matmul(ps[sbank[h % 2]][:, (h // 2) * 128:(h // 2 + 1) * 128],
                                                        lhsT=kTs[(h % 2) * 64:(h % 2 + 1) * 64, h // 2, ts],
                                                        rhs=qTs[(h % 2) * 64:(h % 2 + 1) * 64, h // 2, ts], start=True, stop=True)
                                for h in range(4)], R=[B_kTs, B_qTs], W=[B_ps[bk["S0"]], B_ps[bk["S1"]]])
                    for hh in range(2):
                        S.op("dve", lambda e, hh=hh: e.tensor_tensor(
                            out=swm[:, hh::2, :], in0=ps[sbank[hh]][:, 0:256].rearrange("p (c t) -> p c t", c=2),
                            in1=cbs("triu").unsqueeze(1).to_broadcast([128, 2, 128]), op=ALU.mult),
                            R=[B_ps[sbank[hh]], B_cb], W=[B_swm])
                    if cut <= 4:
                        continue
                    yield
                    fns = []
                    for h in range(4):
                        rows = slice((h % 2) * 64, (h % 2 + 1) * 64)
                        fns.append(lambda e, h=h, rows=rows: e.matmul(ps[bk["H"]][:, h * 65:(h + 1) * 65], lhsT=qTs[rows, h // 2, ts],
                                                                      rhs=Cbf[rows, h // 2, :], start=True, stop=False))
                        fns.append(lambda e, h=h: e.matmul(ps[bk["H"]][:, h * 65:(h + 1) * 65], lhsT=swm[:, h, :], rhs=Vaug[:, h, :],
                                                           start=False, stop=True))
                    S.op("pe", fns, R=[B_qTs, B_Cbf, B_swm, B_Vaug], W=[B_ps[bk["H"]]])
                    if cut <= 5:
                        continue
                    yield
                    S.op("pe", [lambda e, c=c: e.matmul(ps[bk["dC"]][:, c * 130:(c + 1) * 130], lhsT=ktok[:, c * 128:(c + 1) * 128],
                                                        rhs=Vaug[:, 2 * c:2 * c + 2, :].rearrange("p h e -> p (h e)"),
                                                        start=True, stop=True) for c in range(2)],
                         R=[B_ktok, B_Vaug], W=[B_ps[bk["dC"]]])
                    for c in range(2):
                        for hh in range(2):
                            rows = slice(hh * 64, (hh + 1) * 64)
                            S.op("dve", lambda e, c=c, hh=hh, rows=rows: e.tensor_tensor(
                                out=ctmp[rows, c, :], in0=C32[rows, c, :], in1=ps[bk["dC"]][rows, c * 130 + hh * 65:c * 130 + (hh + 1) * 65],
                                op=ALU.add), R=[B_C32, B_ps[bk["dC"]]], W=[B_ctmp])
                            S.op("dve", lambda e, c=c, hh=hh, rows=rows: e.tensor_scalar(
                                out=C32[rows, c, :], in0=ctmp[rows, c, :], scalar1=bl_t[rows, ci, 2 * c + hh:2 * c + hh + 1], scalar2=None,
                                op0=ALU.mult), R=[B_ctmp, B_bl], W=[B_C32])
                    S.op("dve", lambda e: e.tensor_copy(out=Cbf[:], in_=C32[:]), R=[B_C32], W=[B_Cbf])
                    if cut <= 6:
                        continue
                    yield
                    pH = ps[bk["H"]][:, 0:260].rearrange("p (h e) -> p h e", h=4)
                    S.op("dve", lambda e: e.tensor_tensor(out=sm[:, 0:4], in0=pH[:, :, 64], in1=b_t[:, ci, :], op=ALU.mult),
                         R=[B_ps[bk["H"]], B_b], W=[B_sm])
                    S.op("dve", lambda e: e.tensor_scalar(out=sm[:, 4:8], in0=sm[:, 0:4], scalar1=-1.0, scalar2=None, op0=ALU.mult),
                         R=[B_sm], W=[B_sm])
                    S.op("dve", lambda e: e.tensor_tensor(out=sm[:, 4:8], in0=sm[:, 4:8], in1=sm[:, 0:4], op=ALU.max),
                         R=[B_sm], W=[B_sm])
                    S.op("dve", lambda e: e.tensor_scalar(out=sm[:, 4:8], in0=sm[:, 4:8], scalar1=1.0, scalar2=None, op0=ALU.max),
                         R=[B_sm], W=[B_sm])
                    S.op("dve", lambda e: e.reciprocal(out=sm[:, 4:8], in_=sm[:, 4:8]), R=[B_sm], W=[B_sm])
                    S.op("dve", lambda e: e.tensor_tensor(out=sm[:, 8:12], in0=b_t[:, ci, :], in1=sm[:, 4:8], op=ALU.mult),
                         R=[B_sm, B_b], W=[B_sm])
                    yield
                    S.op("act", lambda e: e.activation(out=eo[:], in_=ogt[i2][:, i, :], func=AF.Exp, scale=-1.0), R=[B_ogt[i2]], W=[B_eo])
                    S.op("dve", lambda e: e.tensor_scalar(out=eo[:], in0=eo[:], scalar1=1.0, scalar2=None, op0=ALU.add), R=[B_eo], W=[B_eo])
                    S.op("dve", lambda e: e.tensor_tensor(out=t1[:].rearrange("p (h e) -> p h e", h=4), in0=pH[:, :, 0:64],
                                                          in1=sm[:, 8:12].unsqueeze(2).to_broadcast([128, 4, 64]), op=ALU.mult),
                         R=[B_ps[bk["H"]], B_sm], W=[B_t1])
                    S.op("dve", lambda e: e.reciprocal(out=eo[:], in_=eo[:]), R=[B_eo], W=[B_eo])
                    S.op("dve", lambda e: e.tensor_tensor(out=t1[:], in0=t1[:], in1=eo[:], op=ALU.mult), R=[B_t1, B_eo], W=[B_t1])
                    yield
                    S.op("dve", lambda e: e.tensor_tensor(out=ysq[:], in0=t1[:], in1=t1[:], op=ALU.mult), R=[B_t1], W=[B_ysq])
                    S.op("dve", lambda e: e.tensor_reduce(out=sm[:, 12:16], in_=ysq[:].rearrange("p (h e) -> p h e", h=4), axis=AX.X,
                                                          op=ALU.add), R=[B_ysq], W=[B_sm])
                    S.op("act", lambda e: e.activation(out=sm[:, 12:16], in_=sm[:, 12:16], func=AF.Ln, scale=1.0 / 64, bias=EPS),
                         R=[B_sm], W=[B_sm])
                    S.op("act", lambda e: e.activation(out=sm[:, 12:16], in_=sm[:, 12:16], func=AF.Exp, scale=-0.5), R=[B_sm], W=[B_sm])
                    S.op("dve", lambda e: e.tensor_tensor(out=t1[:].rearrange("p (h e) -> p h e", h=4),
                                                          in0=t1[:].rearrange("p (h e) -> p h e", h=4),
                                                          in1=sm[:, 12:16].unsqueeze(2).to_broadcast([128, 4, 64]), op=ALU.mult),
                         R=[B_sm], W=[B_t1])
                    S.op("dve", lambda e: e.tensor_tensor(out=yn[:], in0=t1[:], in1=ghr[:], op=ALU.mult), R=[B_t1, B_small], W=[B_yn])
                    if cut <= 7:
                        continue
                    yield
                    pT = ps[bk["T"]][:].bitcast(BF16)
                    S.op("pe", [lambda e, c=c: e.transpose(out=pT[:, c * 128:(c + 1) * 128], in_=yn[:, c * 128:(c + 1) * 128],
                                                           identity=cbs("ident")) for c in range(2)], R=[B_yn, B_cb], W=[B_ps[bk["T"]]])
                    S.op("act", lambda e: e.copy(out=Y["t"][:, 0:2, ci * 128:(ci + 1) * 128],
                                                 in_=pT[:, 0:256].rearrange("p (c t) -> p c t", c=2)), R=[B_ps[bk["T"]]], W=[B_ymix])
            yield "hold"


    ML_BANKS_ALONE = {"c0": 0, "c1": 1, "q": 0, "k": 1, "kt": 2, "vt": 3, "S0": 4, "S1": 0, "H": 5, "dC": 6, "T": 7}
    ML_BANKS_CO = {"c0": 4, "c1": 5, "q": 4, "k": 4, "kt": 4, "vt": 4, "S0": 4, "S1": 5, "H": 5, "dC": 4, "T": 4}

    def phase_mlstm(l):
        g = gen_mlstm(l, ML_BANKS_ALONE)
        for _ in g:
            pass
        S.barrier()

    def phase_outproj(l, x_src, B_xsrc, x_dst, B_xdst):
        with ExitStack() as st:
            def T(name, shape, dt):
                return st.enter_context(nc.sbuf_tensor(name + "_L%d" % l, list(shape), dt))
            wo = T("op_w", [128, 8, D], BF16)
            B_wo = Buf("op_w")
            for k in range(8):
                S.dma("pool", wo[:, k, :], w_out[l, k * 128:(k + 1) * 128, :], W=[B_wo])
            xt = [T("op_xt%d" % i, [128, D], F32) for i in range(4)]
            xo = [T("op_xo%d" % i, [128, D], F32) for i in range(4)]
            B_xt = [Buf("op_xt%d" % i) for i in range(4)]
            B_xo = [Buf("op_xo%d" % i) for i in range(4)]
            for t0 in range(2):
                S.dma("sp", xt[t0][:], x_src[t0 * 128:(t0 + 1) * 128, :], R=[B_xsrc], W=[B_xt[t0]])
            for ti in range(NT):
                i = ti % 4
                if ti + 2 < NT:
                    S.dma("sp", xt[(ti + 2) % 4][:], x_src[(ti + 2) * 128:(ti + 3) * 128, :], R=[B_xsrc], W=[B_xt[(ti + 2) % 4]])
                for hf in range(2):
                    pb = (ti * 2 + hf) % 8
                    cs = slice(hf * 512, (hf + 1) * 512)
                    S.op("pe", [lambda e, k=k: e.matmul(ps[pb][:], lhsT=Y["t"][:, k, ti * 128:(ti + 1) * 128], rhs=wo[:, k, cs],
                                                        start=(k == 0), stop=(k == 7)) for k in range(8)],
                         R=[B_ymix, B_wo], W=[B_ps[pb]])
                    S.op("dve", lambda e: e.tensor_tensor(out=xo[i][:, cs], in0=ps[pb][:], in1=mod[:, 2, cs], op=ALU.mult),
                         R=[B_ps[pb], B_mod], W=[B_xo[i]])
                    S.op("dve", lambda e: e.tensor_tensor(out=xo[i][:, cs], in0=xo[i][:, cs], in1=xt[i][:, cs], op=ALU.add),
                         R=[B_xt[i]], W=[B_xo[i]])
                S.dma("sp", x_dst[ti * 128:(ti + 1) * 128, :], xo[i][:], R=[B_xo[i]], W=[B_xdst])
            S.barrier()

    def phase_moe(l, x_src, B_xsrc, x_dst, B_xdst, final):
        with ExitStack() as st:
            def T(name, shape, dt):
                return st.enter_context(nc.sbuf_tensor(name + "_L%d" % l, list(shape), dt))
            wr = T("mo_wr", [128, 8, NEXP], F32)
            rb = T("mo_rb", [128, NEXP], F32)
            B_wr = Buf("mo_wr")
            S.dma("sp", wr[:], w_router.rearrange("(k p) e -> p k e", p=128), W=[B_wr])
            S.dma("sp", rb[:], rbias[:, :], W=[B_wr])
            gfin = None
            if final:
                gfin = mod[:, 0, :]
                S.dma("sp", gfin, g_final[:, :], W=[B_mod])
            h2T = [T("mo_h2T%d" % i, [128, 8, 1024], BF16) for i in range(2)]
            B_h2T = [Buf("mo_h2T%d" % i) for i in range(2)]
            yaccA = T("mo_yacc", [128, 8, D], F32)
            yaccB = T("mo_yaccB", [128, 4, D], F32)
            B_yaccA = [Buf("mo_yacc%d" % i) for i in range(8)]
            B_yaccB = [Buf("mo_yaccB%d" % i) for i in range(4)]

            def ysel(qt, tl):
                if tl < 4 and qt % 2 == 1:
                    return yaccB[:, tl, :], B_yaccB[tl]
                return yaccA[:, tl, :], B_yaccA[tl]
            combTok = [T("mo_combTok%d" % i, [128, 8, NEXP], F32) for i in range(2)]
            B_combT = [Buf("mo_combTok%d" % i) for i in range(2)]
            Wg = [T("mo_Wg%d" % i, [128, 8, DEXP], BF16) for i in range(2)]
            Wu = [T("mo_Wu%d" % i, [128, 8, DEXP], BF16) for i in range(2)]
            Wd = [T("mo_Wd%d" % i, [128, 4, D], BF16) for i in range(2)]
            B_Wg = [Buf("mo_Wg%d" % i) for i in range(2)]
            B_Wu = [Buf("mo_Wu%d" % i) for i in range(2)]
            B_Wd = [Buf("mo_Wd%d" % i) for i in range(2)]
            he = [T("mo_he%d" % i, [128, 4, 512], BF16) for i in range(2)]
            B_he = [Buf("mo_he%d" % i) for i in range(2)]
            sg = [T("mo_sg%d" % i, [128, 512], BF16) for i in range(2)]
            B_sg = [Buf("mo_sg%d" % i) for i in range(2)]
            xt = [T("mo_xt%d" % i, [128, D], F32) for i in range(2)]
            B_xt = [Buf("mo_xt%d" % i) for i in range(2)]
            tmp = T("mo_tmp", [128, D], F32)
            B_tmp = Buf("mo_tmp")
            h2f = [T("mo_h2f%d" % i, [128, D], F32) for i in range(2)]
            B_h2f = [Buf("mo_h2f%d" % i) for i in range(2)]
            h2Tf1 = T("mo_h2Tf", [128, 8, 128], F32)
            h2Tf = [h2Tf1, h2Tf1]
            B_h2Tf1 = Buf("mo_h2Tf")
            B_h2Tf = [B_h2Tf1, B_h2Tf1]
            ss = [T("mo_ss%d" % i, [128, 4], F32) for i in range(2)]
            B_ss = [Buf("mo_ss%d" % i) for i in range(2)]
            ssf = T("mo_ssf", [128, 8, 4], F32)
            B_ssf = [Buf("mo_ssf%d" % i) for i in range(8)]
            rt = [T("mo_rt%d" % i, [128, 8, 64], F32) for i in range(2)]
            B_rt = [Buf("mo_rt%d" % i) for i in range(2)]
            PR = 7

            def load_gu(e):
                i = e % 2
                S.dma("pool", Wg[i][:], w_gate[l, e, :, :].rearrange("(k p) f -> p k f", p=128), W=[B_Wg[i]])
                S.dma("pool", Wu[i][:], w_up[l, e, :, :].rearrange("(k p) f -> p k f", p=128), W=[B_Wu[i]])

            def load_d(e):
                i = e % 2
                S.dma("pool", Wd[i][:], w_down[l, e, :, :].rearrange("(k p) d -> p k d", p=128), W=[B_Wd[i]])

            def load_w(e):
                load_gu(e)
                load_d(e)

            def Ra(qt, tt):
                ti = qt * 8 + tt
                i = ti % 2
                S.dma("sp", xt[i][:], x_src[ti * 128:(ti + 1) * 128, :], R=[B_xsrc], W=[B_xt[i]])
                norm_tile(xt[i], B_xt[i], 3, tmp, B_tmp, h2f[i], B_h2f[i], ss[i], B_ss[i])

            def Rb(qt, tt):
                ti = qt * 8 + tt
                i = ti % 2
                qb = qt % 2
                for half in range(2):
                    S.op("pe", [lambda e, c=c: e.transpose(out=ps[PR][:, (c % 4) * 128:(c % 4 + 1) * 128],
                                                           in_=h2f[i][:, c * 128:(c + 1) * 128], identity=cfs("ident"))
                                for c in range(half * 4, half * 4 + 4)], R=[B_h2f[i], B_cf], W=[B_ps[PR]])
                    S.op("act", lambda e: e.copy(out=h2T[qb][:, half * 4:half * 4 + 4, tt * 128:(tt + 1) * 128],
                                                 in_=ps[PR][:].rearrange("p (c t) -> p c t", c=4)), R=[B_ps[PR]], W=[B_h2T[qb]])
                    S.op("dve", lambda e: e.tensor_copy(out=h2Tf[i][:, half * 4:half * 4 + 4, :],
                                                        in_=ps[PR][:].rearrange("p (c t) -> p c t", c=4)), R=[B_ps[PR]], W=[B_h2Tf[i]])

            def Rb2(qt, tt):
                ti = qt * 8 + tt
                i = ti % 2
                bi = (ti // 4) % 2
                S.op("pe", [lambda e, k=k: e.matmul(ps[PR][:, 0:16], lhsT=h2Tf[i][:, k, :], rhs=wr[:, k, :], start=(k == 0), stop=(k == 7))
                            for k in range(8)], R=[B_h2Tf[i], B_wr], W=[B_ps[PR]])
                S.op("act", lambda e: e.activation(out=rt[bi][:, 0, (tt % 4) * 16:(tt % 4 + 1) * 16], in_=ps[PR][:, 0:16], func=AF.Exp,
                                                   scale=-1.0), R=[B_ps[PR]], W=[B_rt[bi]])
                if tt % 4 == 3:
                    RT(qt, tt // 4, bi)

            def RT(qt, b, bi):
                qb = qt % 2
                r_ = rt[bi]
                sc, g, eq, g2, sel, w_ = [r_[:, k_, :] for k_ in range(6)]
                m1, m2, gs, gmk = r_[:, 6, 0:16], r_[:, 6, 16:32], r_[:, 6, 32:48], r_[:, 6, 48:64]
                gmx, wsum = r_[:, 7, 0:4], r_[:, 7, 4:8]

                def vg(a):
                    return a.rearrange("p (g e) -> p g e", e=4)

                def vt(a):
                    return a.rearrange("p (t e) -> p t e", e=16)

                def bg(a):
                    return a.unsqueeze(2).to_broadcast([128, 16, 4])

                def t4(a):
                    return a.rearrange("p (t g) -> p t g", g=4)
                ops = [
                    lambda e: e.tensor_scalar(out=sc, in0=sc, scalar1=1.0, scalar2=None, op0=ALU.add),
                    lambda e: e.reciprocal(out=sc, in_=sc),
                    lambda e: e.tensor_tensor(out=vt(g), in0=vt(sc), in1=rb[:].unsqueeze(1).to_broadcast([128, 4, 16]), op=ALU.add),
                    lambda e: e.tensor_reduce(out=m1, in_=vg(g), axis=AX.X, op=ALU.max),
                    lambda e: e.tensor_tensor(out=vg(eq), in0=vg(g), in1=bg(m1), op=ALU.is_equal),
                    lambda e: e.scalar_tensor_tensor(out=g2, in0=eq, scalar=-1.0e9, in1=g, op0=ALU.mult, op1=ALU.add),
                    lambda e: e.tensor_reduce(out=m2, in_=vg(g2), axis=AX.X, op=ALU.max),
                    lambda e: e.tensor_tensor(out=gs, in0=m1, in1=m2, op=ALU.add),
                    lambda e: e.tensor_reduce(out=gmx, in_=t4(gs), axis=AX.X, op=ALU.max),
                    lambda e: e.tensor_tensor(out=t4(gmk), in0=t4(gs), in1=gmx.unsqueeze(2).to_broadcast([128, 4, 4]), op=ALU.is_ge),
                    lambda e: e.tensor_tensor(out=vg(sel), in0=vg(g), in1=bg(m2), op=ALU.is_ge),
                    lambda e: e.tensor_tensor(out=vg(sel), in0=vg(sel), in1=bg(gmk), op=ALU.mult),
                    lambda e: e.tensor_tensor(out=w_, in0=sc, in1=sel, op=ALU.mult),
                    lambda e: e.tensor_reduce(out=wsum, in_=vt(w_), axis=AX.X, op=ALU.add),
                    lambda e: e.reciprocal(out=wsum, in_=wsum),
                ]
                for f_ in ops:
                    S.op("dve", f_, R=[B_rt[bi], B_wr], W=[B_rt[bi]])
                S.op("dve", lambda e: e.tensor_tensor(out=combTok[qb][:, 4 * b:4 * b + 4, :], in0=vt(w_),
                                                      in1=wsum.unsqueeze(2).to_broadcast([128, 4, 16]), op=ALU.mult),
                     R=[B_rt[bi]], W=[B_combT[qb]])

            units = [(e_, s2) for e_ in range(NEXP) for s2 in range(2)]

            def GU(qt, u):
                e_, s2 = units[u]
                wi = e_ % 2
                qb = qt % 2
                ci_ = u % 2
                for f in range(4):
                    pg, pu = (0, 1) if f % 2 == 0 else (2, 3)
                    fs = slice(f * 128, (f + 1) * 128)
                    S.op("pe", [lambda e, k=k: e.matmul(ps[pg][:], lhsT=Wg[wi][:, k, fs], rhs=h2T[qb][:, k, s2 * 512:(s2 + 1) * 512],
                                                        start=(k == 0), stop=(k == 7)) for k in range(8)],
                         R=[B_Wg[wi], B_h2T[qb]], W=[B_ps[pg]])
                    S.op("pe", [lambda e, k=k: e.matmul(ps[pu][:], lhsT=Wu[wi][:, k, fs], rhs=h2T[qb][:, k, s2 * 512:(s2 + 1) * 512],
                                                        start=(k == 0), stop=(k == 7)) for k in range(8)],
                         R=[B_Wu[wi], B_h2T[qb]], W=[B_ps[pu]])
                    j = f % 2
                    S.op("act", lambda e: e.activation(out=sg[j][:], in_=ps[pg][:], func=AF.Silu), R=[B_ps[pg]], W=[B_sg[j]])
                    S.op("dve", lambda e: e.tensor_tensor(out=he[ci_][:, f, :], in0=ps[pu][:], in1=sg[j][:], op=ALU.mult),
                         R=[B_ps[pu], B_sg[j]], W=[B_he[ci_]])

            def DOWN(qt, u):
                e_, s2 = units[u]
                wi = e_ % 2
                ci_ = u % 2
                qb = qt % 2
                for t4 in range(4):
                    tl = s2 * 4 + t4
                    for dh in range(2):
                        py = 4 + ((t4 * 2 + dh) % 3)
                        ds_ = slice(dh * 512, (dh + 1) * 512)
                        S.op("pe", [lambda e, f=f: e.matmul(ps[py][:], lhsT=he[ci_][:, f, t4 * 128:(t4 + 1) * 128], rhs=Wd[wi][:, f, ds_],
                                                            start=(f == 0), stop=(f == 3)) for f in range(4)],
                             R=[B_he[ci_], B_Wd[wi]], W=[B_ps[py]])
                        cw_ = combTok[qb][:, tl, e_:e_ + 1]
                        ya_, B_ya = ysel(qt, tl)
                        if e_ == 0:
                            S.op("dve", lambda e: e.tensor_scalar(out=ya_[:, ds_], in0=ps[py][:], scalar1=cw_, scalar2=None, op0=ALU.mult),
                                 R=[B_ps[py], B_combT[qb]], W=[B_ya])
                        else:
                            S.op("dve", lambda e: e.scalar_tensor_tensor(out=ya_[:, ds_], in0=ps[py][:], scalar=cw_, in1=ya_[:, ds_],
                                                                         op0=ALU.mult, op1=ALU.add),
                                 R=[B_ps[py], B_combT[qb]], W=[B_ya])

            def EPIa(qt, tt):
                ti = qt * 8 + tt
                ya_, B_ya = ysel(qt, tt)
                xe, B_xe = xt[tt % 2], B_xt[tt % 2]
                S.dma("sp", xe[:], x_src[ti * 128:(ti + 1) * 128, :], R=[B_xsrc], W=[B_xe])
                S.op("pool", lambda e: e.tensor_tensor(out=ya_, in0=ya_, in1=mod[:, 5, :], op=ALU.mult), R=[B_mod], W=[B_ya])
                S.op("pool", lambda e: e.tensor_tensor(out=ya_, in0=ya_, in1=xe[:], op=ALU.add), R=[B_xe], W=[B_ya])
                if final:
                    s_ = ssf[:, tt, :]
                    S.op("act", lambda e: e.activation(out=xe[:], in_=ya_, func=AF.Square, accum_out=s_[:, 0:1]),
                         R=[B_ya], W=[B_xe, B_ssf[tt]])
                    S.op("act", lambda e: e.activation(out=s_[:, 1:2], in_=s_[:, 0:1], func=AF.Ln, scale=1.0 / D, bias=EPS),
                         R=[B_ssf[tt]], W=[B_ssf[tt]])
                    S.op("act", lambda e: e.activation(out=s_[:, 2:3], in_=s_[:, 1:2], func=AF.Exp, scale=-0.5), R=[B_ssf[tt]], W=[B_ssf[tt]])
                else:
                    S.dma("sp", x_dst[ti * 128:(ti + 1) * 128, :], ya_, R=[B_ya], W=[B_xdst])

            def EPIb(qt, tt):
                if not final:
                    return
                ti = qt * 8 + tt
                ya_, B_ya = ysel(qt, tt)
                s_ = ssf[:, tt, :]
                S.op("dve", lambda e: e.scalar_tensor_tensor(out=ya_, in0=ya_, scalar=s_[:, 2:3], in1=gfin, op0=ALU.mult, op1=ALU.mult),
                     R=[B_ssf[tt], B_mod], W=[B_ya])
                S.dma("sp", x_dst[ti * 128:(ti + 1) * 128, :], ya_, R=[B_ya], W=[B_xdst])

            load_w(0)
            load_w(1)
            for tt in range(9):
                if tt < 8:
                    Ra(0, tt)
                if tt >= 1:
                    Rb2(0, tt - 1)
                if tt < 8:
                    Rb(0, tt)
            for qt in range(4):
                sched = {}
                if qt + 1 < 4:
                    for tt in range(8):
                        sched.setdefault(3 + 3 * tt, []).append(("a", tt))
                        sched.setdefault(4 + 3 * tt, []).append(("b", tt))
                        sched.setdefault(5 + 3 * tt, []).append(("b2", tt))
                GU(qt, 0)
                if qt > 0:
                    for tt in range(4, 8):
                        EPIa(qt - 1, tt)
                for u in range(len(units)):
                    if u + 1 < len(units):
                        GU(qt, u + 1)
                    e_u, s_u = units[u]
                    if s_u == 0 and (e_u + 2 < NEXP or qt + 1 < 4):
                        load_gu((e_u + 2) % NEXP)
                    if qt > 0 and u == 1:
                        for tt in range(4, 8):
                            EPIb(qt - 1, tt)
                    if qt > 0 and 1 <= u <= 4:
                        EPIa(qt - 1, u - 1)
                    if qt > 0 and 2 <= u <= 5:
                        EPIb(qt - 1, u - 2)
                    for kind, tt in sched.get(u, []):
                        {"a": Ra, "b": Rb, "b2": Rb2}[kind](qt + 1, tt)
                    DOWN(qt, u)
                    if s_u == 1 and (e_u + 2 < NEXP or qt + 1 < 4):
                        load_d((e_u + 2) % NEXP)
                    if qt == 3 and e_u == NEXP - 1 and s_u == 0:
                        for tt in range(4):
                            EPIa(qt, tt)
                if qt == 3:
                    for tt in range(4):
                        EPIb(qt, tt)
                    for tt in range(4, 8):
                        EPIa(qt, tt)
                    for tt in range(4, 8):
                        EPIb(qt, tt)
            S.barrier()

    def dump_dram(name, src, B_src, shape, dt):
        t = dbg_out(name, shape, dt)
        b = Buf("dbg_" + name)
        S.dma("sp", t, src, R=[B_src], W=[b])
        fin_bufs.append(b)

    def dump_sbuf(name, src_ap, B_src, shape, dt):
        t = dbg_out(name, shape, dt)
        b = Buf("dbg_" + name)
        S.dma("sp", t, src_ap, R=[B_src], W=[b])
        fin_bufs.append(b)

    B_xin, B_y = Buf("x_in"), Buf("y_out")
    x_cur, B_xcur = x_in, B_xin
    if isinstance(stop_after, str) and stop_after.startswith("only:"):
        ph = stop_after[5:]
        l = 0
        if ph == "mod":
            phase_mod(0)
        elif ph == "inproj":
            with ExitStack() as wst:
                win_, B_win_ = load_win(0, wst)
                phase_inproj(0, x_in, B_xin, win_, B_win_)
        elif ph == "modin":
            with ExitStack() as wst:
                win_, B_win_ = load_win(0, wst)
                phase_mod(0)
                phase_inproj(0, x_in, B_xin, win_, B_win_)
        elif ph == "sbml":
            with ExitStack() as lay:
                Y["t"] = lay.enter_context(nc.sbuf_tensor("ymixT_L%d" % l, [128, 8, SEQ], BF16))
                g_ml = gen_mlstm(0, ML_BANKS_CO)
                next(g_ml)
                phase_sb(0, co=[g_ml])
                for _ in g_ml:
                    pass
        elif ph in ("ml", "dil", "sb", "outproj"):
            with ExitStack() as lay:
                Y["t"] = lay.enter_context(nc.sbuf_tensor("ymixT_L%d" % l, [128, 8, SEQ], BF16))
                {"ml": phase_mlstm, "dil": phase_dil, "sb": phase_sb}.get(ph, lambda l_: phase_outproj(0, x_in, B_xin, xA, B_xA))(0)
        elif ph == "moe":
            phase_moe(0, x_in, B_xin, xB, B_xB, False)
        S.barrier()
        return nc, out_tensors
    for l in range(DEPTH):
        if stop_after == "mlonly%d" % l:
            with ExitStack() as lay:
                Y["t"] = lay.enter_context(nc.sbuf_tensor("ymixT_L%d" % l, [128, 8, SEQ], BF16))
                phase_mlstm(l)
            break
        with ExitStack() as wst:
            win_, B_win_ = load_win(l, wst)
            phase_mod(l)
            phase_inproj(l, x_cur, B_xcur, win_, B_win_)
        with ExitStack() as lay:
            Y["t"] = lay.enter_context(nc.sbuf_tensor("ymixT_L%d" % l, [128, 8, SEQ], BF16))
            phase_dil(l)
            g_ml = gen_mlstm(l, ML_BANKS_CO)
            next(g_ml)
            phase_sb(l, co=[g_ml])
            for _ in g_ml:
                pass
            if stop_after == "mix%d" % l:
                dump_sbuf("ymixT", Y["t"][:].rearrange("p c t -> p (c t)"), B_ymix, [128, 8 * SEQ], BF16)
                S.wait_all("sp", fin_bufs)
                S.barrier()
                break
            phase_outproj(l, x_cur, B_xcur, xA, B_xA)
        if stop_after == "x1_%d" % l:
            dump_dram("x1", xA, B_xA, [SEQ, D], F32)
            break
        last = (l == DEPTH - 1)
        if last:
            phase_moe(l, xA, B_xA, y_out, B_y, True)
        else:
            phase_moe(l, xA, B_xA, xB, B_xB, False)
            x_cur, B_xcur = xB, B_xB
        if stop_after == "x2_%d" % l:
            dump_dram("x2", xB, B_xB, [SEQ, D], F32)
            break

    S.wait_all("sp", fin_bufs + [B_y])
    S.barrier()
    return nc, out_tensors


def prep_inputs(b, inp, consts):
    cbn, cfn = consts
    f = np.float32
    d = {
        "x": np.ascontiguousarray(inp["x"][b]),
        "c_lay": np.ascontiguousarray(inp["c"][b].reshape(8, 128).T),
        "w_in": inp["w_in"],
        "conv_w": np.ascontiguousarray(inp["conv_w"].reshape(DEPTH, 4, 2, 128).transpose(0, 3, 2, 1)),
        "conv_b": np.ascontiguousarray(inp["conv_b"].reshape(DEPTH, 2, 128).transpose(0, 2, 1)),
        "w_mq": inp["w_mq"], "w_mk": inp["w_mk"], "w_mv": inp["w_mv"],
        "gbias": np.ascontiguousarray(np.broadcast_to(inp["gate_bias"].reshape(DEPTH, 1, 8), (DEPTH, 128, 8))),
        "g_head_p": np.ascontiguousarray(inp["g_head"].reshape(DEPTH, 8, 128).transpose(0, 2, 1)),
        "g_head_r": np.ascontiguousarray(np.broadcast_to(inp["g_head"][:, None, :256], (DEPTH, 128, 256))),
        "w_out": inp["w_out"],
        "w_ada": inp["w_ada"],
        "b_ada": np.ascontiguousarray(inp["b_ada"].reshape(DEPTH, 1, 6 * D)),
        "w_router": inp["w_router"],
        "rbias": np.ascontiguousarray(np.broadcast_to(inp["router_bias"][None, :], (128, NEXP))),
        "w_gate_e": inp["w_gate_e"], "w_up_e": inp["w_up_e"], "w_down_e": inp["w_down_e"],
        "g_final_r": np.ascontiguousarray(np.broadcast_to(inp["g_final"][None, :], (128, D))),
        "cb": cbn, "cf": cfn,
    }
    return {k: np.ascontiguousarray(v, dtype=f) for k, v in d.items()}


def kernel(**inputs):
    inp = {k: np.asarray(v) for k, v in inputs.items()}
    nc, _ = build_program()
    consts = make_consts()
    in_maps = [prep_inputs(b, inp, consts) for b in range(8)]
    res = run_bass_kernel_spmd(nc, in_maps, core_ids=list(range(8)))
    return np.stack([np.asarray(r["y"]) for r in res.results], axis=0).astype(np.float32)
```

```python
import math
import numpy as np
from contextlib import ExitStack
import concourse.bass as bass
import concourse.mybir as mybir
from concourse.bass_utils import run_bass_kernel_spmd

F32 = mybir.dt.float32
BF16 = mybir.dt.bfloat16
AF = mybir.ActivationFunctionType
ALU = mybir.AluOpType
AX = mybir.AxisListType

SEQ = 4096
D = 1024
DEPTH = 2
NT = SEQ // 128
NSB = SEQ // 512
DIN = 2824
NEXP = 16
DEXP = 512
EPS = 1e-6
OFF_O = 256
OFF_DIL = 520
OFF_SB = 520 + 1152
DIL_PATTERNS = ((128, 1), (512, 4), (2048, 16))


class Buf:
    __slots__ = ("name", "w", "r", "excl")

    def __init__(self, name, excl=False):
        self.name = name
        self.w = None
        self.r = []
        self.excl = excl


class _Eng:
    def __init__(self, name, e, sem):
        self.name = name
        self.e = e
        self.sem = sem
        self.cnt = 0
        self.known = {}


class Sched:
    def __init__(self, nc, n_dma_sems=32):
        self.nc = nc
        self.sems = []
        self.eng = {}
        for name, e in (("pe", nc.tensor), ("act", nc.scalar), ("dve", nc.vector),
                        ("pool", nc.gpsimd), ("sp", nc.sync)):
            sem = nc.semaphore("s_" + name).__enter__()
            self.sems.append(sem)
            self.eng[name] = _Eng(name, e, len(self.sems) - 1)
        self.dma_sems = []
        for i in range(n_dma_sems):
            sem = nc.semaphore("s_dma%d" % i).__enter__()
            self.sems.append(sem)
            self.dma_sems.append([len(self.sems) - 1, 0])
        self.dma_rr = 0
        self.ninstr = 0

    def _deps(self, R, W):
        deps = {}

        def add(t):
            if t is None:
                return
            s, v = t
            if deps.get(s, 0) < v:
                deps[s] = v
        for b in R:
            add(b.w)
            if b.excl:
                for t in b.r:
                    add(t)
        for b in W:
            add(b.w)
            for t in b.r:
                add(t)
        return deps

    def _wait(self, E, deps):
        for s, v in deps.items():
            if E.known.get(s, 0) >= v:
                continue
            E.e.wait_ge(self.sems[s], v)
            E.known[s] = v
            self.ninstr += 1

    def _mark(self, R, W, ticket):
        for b in R:
            if b.excl:
                b.w = ticket
                b.r = []
            else:
                b.r.append(ticket)
                if len(b.r) > 32:
                    m = {}
                    for s, v in b.r:
                        if m.get(s, 0) < v:
                            m[s] = v
                    b.r = list(m.items())
        for b in W:
            b.w = ticket
            b.r = []

    def op(self, eng, fns, R=(), W=()):
        E = self.eng[eng]
        if not isinstance(fns, (list, tuple)):
            fns = [fns]
        self._wait(E, self._deps(R, W))
        ins = None
        for f in fns:
            ins = f(E.e)
            self.ninstr += 1
        E.cnt += 1
        ins.then_inc(self.sems[E.sem], 1)
        t = (E.sem, E.cnt)
        self._mark(R, W, t)
        return t

    def dma(self, eng, out, in_, R=(), W=()):
        E = self.eng[eng]
        slot = self.dma_sems[self.dma_rr]
        self.dma_rr = (self.dma_rr + 1) % len(self.dma_sems)
        s, v = slot
        deps = self._deps(R, W)
        if v > 0 and deps.get(s, 0) < v:
            deps[s] = v
        self._wait(E, deps)
        ins = E.e.dma_start(out=out, in_=in_)
        slot[1] = v + 16
        ins.then_inc(self.sems[s], 16)
        self.ninstr += 1
        t = (s, v + 16)
        self._mark(R, W, t)
        return t

    def wait_all(self, eng, bufs):
        E = self.eng[eng]
        self._wait(E, self._deps(bufs, bufs))

    def barrier(self):
        tot = {}
        for E in self.eng.values():
            if E.cnt:
                tot[E.sem] = E.cnt
        for s, v in self.dma_sems:
            if v:
                tot[s] = v
        for E in self.eng.values():
            self._wait(E, dict(tot))


CB = {}
CF = {}


def _layout(table, items):
    off = 0
    for name, w in items:
        table[name] = (off, w)
        off += w
    return off


NCB = _layout(CB, [("ident", 128), ("ones", 128), ("negtri", 128), ("negones", 128),
                   ("mstrict", 128), ("triu", 128), ("onesA", 128), ("onesB", 128),
                   ("blk64", 128), ("zeros", 128), ("dilE", 18 * 256)])
NCF = _layout(CF, [("ident", 128), ("ones", 128), ("triu", 128), ("blk64", 128), ("selA", 128), ("selB", 128)])


def make_consts():
    p = np.arange(128)[:, None].astype(np.float64)
    f = np.arange(128)[None, :].astype(np.float64)
    cb = np.zeros((128, NCB), np.float32)
    cf = np.zeros((128, NCF), np.float32)

    def put(tab, lay, name, val):
        o, w = lay[name]
        tab[:, o:o + w] = val
    ident = (p == f).astype(np.float32)
    ones = np.ones((128, 128), np.float32)
    triu = (p <= f).astype(np.float32)
    blk64 = ((p // 64) == (f // 64)).astype(np.float32)
    put(cb, CB, "ident", ident)
    put(cb, CB, "ones", ones)
    put(cb, CB, "negtri", -(p >= f).astype(np.float32))
    put(cb, CB, "negones", -ones)
    put(cb, CB, "mstrict", (p < f).astype(np.float32))
    put(cb, CB, "triu", triu)
    put(cb, CB, "onesA", (f < 64).astype(np.float32) * ones)
    put(cb, CB, "onesB", (f >= 64).astype(np.float32) * ones)
    put(cb, CB, "blk64", blk64)
    mk = np.arange(128)[:, None].astype(np.float64)
    mq = np.arange(256)[None, :].astype(np.float64)
    dlt = mq - mk
    valid = (dlt >= 0) & (dlt <= 128)
    o, _ = CB["dilE"]
    for h in range(6):
        slope = 2.0 ** (-8.0 * (h + 1) / 6.0)
        for pi, (win, dil) in enumerate(DIL_PATTERNS):
            e = np.where(valid, np.exp(-slope * dil * dlt), 0.0)
            ix = ((h // 2) * 3 + pi) * 2 + (h % 2)
            cb[:, o + ix * 256: o + (ix + 1) * 256] = e
    put(cf, CF, "ident", ident)
    put(cf, CF, "ones", ones)
    put(cf, CF, "triu", triu)
    put(cf, CF, "blk64", blk64)
    selA = np.zeros((128, 128), np.float32); selA[64, 0:64] = 1.0
    selB = np.zeros((128, 128), np.float32); selB[0, 64:128] = 1.0
    put(cf, CF, "selA", selA)
    put(cf, CF, "selB", selB)
    return cb, cf


ML_CUT = [99]
MOE_DBG = [0]
CO_EVERY = [1]
CO_SKIP = [10 ** 9]
SB_DEPTH = [2, 3]


def build_program(stop_after=None, debug=()):
    nc = bass.Bass("TRN2", target_bir_lowering=False)
    S = Sched(nc)
    dbg = {}

    def din(name, shape, dt=F32):
        return nc.dram_tensor(name, list(shape), dt, kind="ExternalInput").ap()

    def dscr(name, shape, dt):
        return nc.dram_tensor(name, list(shape), dt, kind="Internal").ap()

    x_in = din("x", [SEQ, D])
    c_lay = din("c_lay", [128, 8])
    w_in = din("w_in", [DEPTH, D, DIN])
    conv_w = din("conv_w", [DEPTH, 128, 2, 4])
    conv_b = din("conv_b", [DEPTH, 128, 2])
    w_mq = din("w_mq", [DEPTH, 4, 64, 64])
    w_mk = din("w_mk", [DEPTH, 4, 64, 64])
    w_mv = din("w_mv", [DEPTH, 4, 64, 64])
    gbias = din("gbias", [DEPTH, 128, 8])
    g_head_p = din("g_head_p", [DEPTH, 128, 8])
    g_head_r = din("g_head_r", [DEPTH, 128, 256])
    w_out = din("w_out", [DEPTH, D, D])
    w_ada = din("w_ada", [DEPTH, D, 6 * D])
    b_ada = din("b_ada", [DEPTH, 1, 6 * D])
    w_router = din("w_router", [D, NEXP])
    rbias = din("rbias", [128, NEXP])
    w_gate = din("w_gate_e", [DEPTH, NEXP, D, DEXP])
    w_up = din("w_up_e", [DEPTH, NEXP, D, DEXP])
    w_down = din("w_down_e", [DEPTH, NEXP, DEXP, D])
    g_final = din("g_final_r", [128, D])
    cb_in = din("cb", [128, NCB])
    cf_in = din("cf", [128, NCF])
    y_out = nc.dram_tensor("y", [SEQ, D], F32, kind="ExternalOutput").ap()

    xA = dscr("xA", [SEQ, D], F32)
    xB = dscr("xB", [SEQ, D], F32)
    xmT_d = dscr("xmT", [256, SEQ], BF16)
    og_d = dscr("og", [SEQ, 256], BF16)
    gates_d = dscr("gates", [SEQ, 8], F32)
    dqT_d = dscr("dqT", [384, SEQ], BF16)
    dkT_d = dscr("dkT", [384, SEQ], BF16)
    dv_d = dscr("dv", [SEQ, 384], BF16)
    sqT_d = dscr("sqT", [384, SEQ], BF16)
    skT_d = dscr("skT", [384, SEQ], BF16)
    sv_d = dscr("sv", [SEQ, 384], BF16)
    B_xA, B_xB = Buf("xA"), Buf("xB")
    B_scr = {n: Buf(n) for n in ("xmT", "og", "gates", "dqT", "dkT", "dv", "sqT", "skT", "sv")}

    for name in debug:
        pass

    def sbt(name, shape, dt):
        return nc.alloc_sbuf_tensor(name, list(shape), dt)

    cb = sbt("cb_sb", [128, NCB], BF16)
    cf = sbt("cf_sb", [128, NCF], F32)
    B_cb, B_cf = Buf("cb"), Buf("cf")
    S.dma("pool", cb[:], cb_in[:, :], W=[B_cb])
    S.dma("sp", cf[:], cf_in[:, :], W=[B_cf])

    def cbs(name, rows=slice(0, 128)):
        o, w = CB[name]
        return cb[rows, o:o + w]

    def cfs(name, rows=slice(0, 128)):
        o, w = CF[name]
        return cf[rows, o:o + w]

    pp = [nc.alloc_psum_tensor("pp%d" % i, [128, 1024], F32) for i in range(4)]
    ps = [pp[i // 2][:, (i % 2) * 512:(i % 2 + 1) * 512] for i in range(8)]
    PP = pp
    B_ps = [Buf("ps%d" % i, excl=True) for i in range(8)]

    mod = sbt("mod", [128, 6, D], F32)
    B_mod = Buf("mod")
    B_crep = Buf("c_rep")
    c_sb = sbt("c_sb", [128, 8], F32)
    B_c = Buf("c_sb")
    S.dma("sp", c_sb[:], c_lay[:, :], W=[B_c])
    S.op("act", lambda e: e.activation(out=c_sb[:], in_=c_sb[:], func=AF.Silu), R=[B_c], W=[B_c])

    out_tensors = {}

    def dbg_out(name, shape, dt=F32):
        t = nc.dram_tensor("dbg_" + name, list(shape), dt, kind="ExternalOutput").ap()
        out_tensors[name] = t
        return t

    fin_bufs = []

    def phase_mod(l):
        with ExitStack() as st:
            c_rep = st.enter_context(nc.sbuf_tensor("c_rep_L%d" % l, [128, 8, 128], F32))
            S.op("dve", lambda e: e.tensor_copy(out=c_rep[:], in_=c_sb[:].unsqueeze(2).to_broadcast([128, 8, 128])),
                 R=[B_c], W=[B_crep])
            wt = [st.enter_context(nc.sbuf_tensor("wada%d_L%d" % (i, l), [128, 8, 512], F32)) for i in range(2)]
            br = [st.enter_context(nc.sbuf_tensor("brow%d_L%d" % (i, l), [1, 512], F32)) for i in range(2)]
            B_wt = [Buf("wada%d" % i) for i in range(2)]
            B_br = [Buf("brow%d" % i) for i in range(2)]
            for blk in range(12):
                i = blk % 2
                S.dma("sp", wt[i][:], w_ada[l, :, blk * 512:(blk + 1) * 512].rearrange("(k p) n -> p k n", p=128),
                      W=[B_wt[i]])
                S.dma("sp", br[i][:], b_ada[l, :, blk * 512:(blk + 1) * 512], W=[B_br[i]])
                pb = blk % 2
                fns = []
                for k in range(8):
                    fns.append(lambda e, k=k, i=i, pb=pb: e.matmul(ps[pb][:], lhsT=c_rep[:, k, :], rhs=wt[i][:, k, :],
                                                                   start=(k == 0), stop=False))
                fns.append(lambda e, i=i, pb=pb: e.matmul(ps[pb][:], lhsT=cfs("ones", slice(0, 1)), rhs=br[i][:],
                                                          start=False, stop=True))
                S.op("pe", fns, R=[B_wt[i], B_br[i], B_crep, B_cf], W=[B_ps[pb]])
                m = blk // 2
                dst = mod[:, m, (blk % 2) * 512:(blk % 2 + 1) * 512]
                if m in (1, 4):
                    S.op("dve", lambda e, dst=dst, pb=pb: e.tensor_scalar(out=dst, in0=ps[pb][:], scalar1=1.0, scalar2=None,
                                                                         op0=ALU.add), R=[B_ps[pb]], W=[B_mod])
                else:
                    S.op("dve", lambda e, dst=dst, pb=pb: e.tensor_copy(out=dst, in_=ps[pb][:]), R=[B_ps[pb]], W=[B_mod])
            S.barrier()

    def norm_tile(xt, B_xt, sidx, tmp, B_tmp, hout, B_hout, ss, B_ss):
        S.op("act", lambda e: e.activation(out=tmp[:], in_=xt[:], func=AF.Square, accum_out=ss[:, 0:1]),
             R=[B_xt], W=[B_tmp, B_ss])
        S.op("act", lambda e: e.activation(out=ss[:, 1:2], in_=ss[:, 0:1], func=AF.Ln, scale=1.0 / D, bias=EPS),
             R=[B_ss], W=[B_ss])
        S.op("act", lambda e: e.activation(out=ss[:, 2:3], in_=ss[:, 1:2], func=AF.Exp, scale=-0.5),
             R=[B_ss], W=[B_ss])
        S.op("dve", lambda e: e.scalar_tensor_tensor(out=tmp[:], in0=xt[:], scalar=ss[:, 2:3], in1=mod[:, sidx + 1, :],
                                                     op0=ALU.mult, op1=ALU.mult), R=[B_xt, B_ss, B_mod], W=[B_tmp])
        S.op("dve", lambda e: e.tensor_tensor(out=hout[:], in0=tmp[:], in1=mod[:, sidx, :], op=ALU.add),
             R=[B_tmp, B_mod], W=[B_hout])

    def load_win(l, st):
        win = st.enter_context(nc.sbuf_tensor("win_L%d" % l, [128, 8, DIN], BF16))
        B_win = Buf("win")
        for k in range(8):
            S.dma("pool", win[:, k, :], w_in[l, k * 128:(k + 1) * 128, :], W=[B_win])
        return win, B_win

    def phase_inproj(l, x_src, B_xsrc, win, B_win):
        with ExitStack() as st:
            def T(name, shape, dt):
                return st.enter_context(nc.sbuf_tensor(name + "_L%d" % l, list(shape), dt))
            xt = [T("xt%d" % i, [128, D], F32) for i in range(2)]
            B_xt = [Buf("xt%d" % i) for i in range(2)]
            tmp = [T("ntmp%d" % i, [128, D], F32) for i in range(2)]
            B_tmp = [Buf("ntmp%d" % i) for i in range(2)]
            hb = [T("hb%d" % i, [128, D], BF16) for i in range(2)]
            B_hb = [Buf("hb%d" % i) for i in range(2)]
            ss = [T("ss%d" % i, [128, 4], F32) for i in range(2)]
            B_ss = [Buf("ss%d" % i) for i in range(2)]
            hT = [T("hT%d" % i, [128, 8, 512], BF16) for i in range(2)]
            B_hT = [Buf("hT%d" % i) for i in range(2)]
            stF = {n: [T("st_%s%d" % (n, i), [128, w, 512], BF16) for i in range(2)]
                   for n, w in (("xmT", 2), ("dqT", 3), ("dkT", 3), ("sqT", 3), ("skT", 3))}
            B_stF = {n: [Buf("st_%s%d" % (n, i)) for i in range(2)] for n in stF}
            st_og = [T("st_og%d" % i, [128, 4, 256], BF16) for i in range(2)]
            st_g = [T("st_g%d" % i, [128, 4, 8], F32) for i in range(2)]
            st_dv = [T("st_dv%d" % i, [128, 4, 384], BF16) for i in range(2)]
            st_sv = [T("st_sv%d" % i, [128, 4, 384], BF16) for i in range(2)]
            B_og = [Buf("st_og%d" % i) for i in range(2)]
            B_g = [Buf("st_g%d" % i) for i in range(2)]
            B_dv = [Buf("st_dv%d" % i) for i in range(2)]
            B_sv = [Buf("st_sv%d" % i) for i in range(2)]
            fm_specs = [("xmT", 0, 2, 1.0, xmT_d), ("dqT", OFF_DIL, 3, 1.0, dqT_d), ("dkT", OFF_DIL + 384, 3, 0.125, dkT_d),
                        ("sqT", OFF_SB, 3, 1.0, sqT_d), ("skT", OFF_SB + 384, 3, 0.125, skT_d)]
            pcount = [0]

            def next_ps():
                pcount[0] += 1
                return 2 + (pcount[0] % 6)

            def NA(sb, tt):
                ti = sb * 4 + tt
                i = ti % 2
                S.dma("sp", xt[i][:], x_src[ti * 128:(ti + 1) * 128, :], R=[B_xsrc], W=[B_xt[i]])
                norm_tile(xt[i], B_xt[i], 0, tmp[i], B_tmp[i], hb[i], B_hb[i], ss[i], B_ss[i])

            def NB(sb, tt):
                ti = sb * 4 + tt
                i = ti % 2
                sl = sb % 2
                pb = ti % 2
                pT = ps[pb][:].bitcast(BF16)
                S.op("pe", [lambda e, c=c: e.transpose(out=pT[:, c * 128:(c + 1) * 128], in_=hb[i][:, c * 128:(c + 1) * 128],
                                                       identity=cbs("ident")) for c in range(8)],
                     R=[B_hb[i], B_cb], W=[B_ps[pb]])
                S.op("act", lambda e: e.copy(out=hT[sl][:, :, tt * 128:(tt + 1) * 128], in_=pT.rearrange("p (c t) -> p c t", c=8)),
                     R=[B_ps[pb]], W=[B_hT[sl]])

            def M_items(sb):
                sl = sb % 2
                items = []
                for (n, off, nch, scale, dst) in fm_specs:
                    for c in range(nch):
                        def it(n=n, off=off, nch=nch, scale=scale, dst=dst, c=c):
                            pb = next_ps()
                            S.op("pe", [lambda e, k=k: e.matmul(ps[pb][:], lhsT=win[:, k, off + c * 128: off + (c + 1) * 128],
                                                                rhs=hT[sl][:, k, :], start=(k == 0), stop=(k == 7)) for k in range(8)],
                                 R=[B_win, B_hT[sl]], W=[B_ps[pb]])
                            S.op("act", lambda e: e.activation(out=stF[n][sl][:, c, :], in_=ps[pb][:], func=AF.Copy, scale=scale),
                                 R=[B_ps[pb]], W=[B_stF[n][sl]])
                            if c == nch - 1:
                                S.dma("sp", dst[:, sb * 512:(sb + 1) * 512].rearrange("(c p) t -> p c t", p=128), stF[n][sl][:],
                                      R=[B_stF[n][sl]], W=[B_scr[n]])
                        items.append(it)
                for tt in range(4):
                    def it(tt=tt):
                        pb = next_ps()
                        S.op("pe", [lambda e, k=k: e.matmul(ps[pb][:, 0:264], lhsT=hT[sl][:, k, tt * 128:(tt + 1) * 128],
                                                            rhs=win[:, k, OFF_O:OFF_O + 264], start=(k == 0), stop=(k == 7))
                                    for k in range(8)], R=[B_win, B_hT[sl]], W=[B_ps[pb]])
                        S.op("act", lambda e: e.activation(out=st_og[sl][:, tt, :], in_=ps[pb][:, 0:256], func=AF.Copy),
                             R=[B_ps[pb]], W=[B_og[sl]])
                        S.op("dve", lambda e: e.tensor_copy(out=st_g[sl][:, tt, :], in_=ps[pb][:, 256:264]), R=[B_ps[pb]], W=[B_g[sl]])
                    items.append(it)
                    for (stt, Bst, off) in ((st_dv, B_dv, OFF_DIL + 768), (st_sv, B_sv, OFF_SB + 768)):
                        def it(tt=tt, stt=stt, Bst=Bst, off=off):
                            pb = next_ps()
                            S.op("pe", [lambda e, k=k: e.matmul(ps[pb][:, 0:384], lhsT=hT[sl][:, k, tt * 128:(tt + 1) * 128],
                                                                rhs=win[:, k, off:off + 384], start=(k == 0), stop=(k == 7))
                                        for k in range(8)], R=[B_win, B_hT[sl]], W=[B_ps[pb]])
                            S.op("dve", lambda e: e.tensor_copy(out=stt[sl][:, tt, :], in_=ps[pb][:, 0:384]), R=[B_ps[pb]], W=[Bst[sl]])
                        items.append(it)

                def fin():
                    r0, r1 = sb * 512, (sb + 1) * 512
                    S.dma("sp", og_d[r0:r1, :].rearrange("(t p) c -> p t c", p=128), st_og[sl][:], R=[B_og[sl]], W=[B_scr["og"]])
                    S.dma("sp", gates_d[r0:r1, :].rearrange("(t p) c -> p t c", p=128), st_g[sl][:], R=[B_g[sl]], W=[B_scr["gates"]])
                    S.dma("sp", dv_d[r0:r1, :].rearrange("(t p) c -> p t c", p=128), st_dv[sl][:], R=[B_dv[sl]], W=[B_scr["dv"]])
                    S.dma("sp", sv_d[r0:r1, :].rearrange("(t p) c -> p t c", p=128), st_sv[sl][:], R=[B_sv[sl]], W=[B_scr["sv"]])
                return items, fin

            for tt in range(4):
                NA(0, tt)
                NB(0, tt)
            for sb in range(NSB):
                items, fin = M_items(sb)
                nper = (len(items) + 3) // 4
                for part in range(4):
                    if sb + 1 < NSB:
                        NA(sb + 1, part)
                    for it in items[part * nper:(part + 1) * nper]:
                        it()
                    if sb + 1 < NSB:
                        NB(sb + 1, part)
                fin()
            S.barrier()


    Y = {}
    B_ymix = Buf("ymixT")
    ghp = sbt("ghp", [128, DEPTH, 8], F32)
    B_ghp = Buf("ghp")
    for l_ in range(DEPTH):
        S.dma("sp", ghp[:, l_, :], g_head_p[l_, :, :], W=[B_ghp])

    def head_norm_fm(l, src_ap, B_src, chunk, col0, T, tag):
        sq, B_sq, rs, B_rs, pstat, B_pstat = T
        S.op("act", lambda e: e.activation(out=sq[:], in_=src_ap, func=AF.Square), R=[B_src], W=[B_sq])
        S.op("pe", lambda e: e.matmul(pstat[:], lhsT=cfs("blk64"), rhs=sq[:], start=True, stop=True),
             R=[B_sq, B_cf], W=[B_pstat])
        S.op("act", lambda e: e.activation(out=rs[:], in_=pstat[:], func=AF.Ln, scale=1.0 / 64, bias=EPS),
             R=[B_pstat], W=[B_rs])
        S.op("act", lambda e: e.activation(out=rs[:], in_=rs[:], func=AF.Exp, scale=-0.5), R=[B_rs], W=[B_rs])
        S.op("dve", lambda e: e.scalar_tensor_tensor(out=Y["t"][:, chunk, col0:col0 + 512], in0=src_ap,
                                                     scalar=ghp[:, l, chunk:chunk + 1], in1=rs[:],
                                                     op0=ALU.mult, op1=ALU.mult),
             R=[B_src, B_rs, B_ghp], W=[B_ymix])

    def phase_sb(l, co=None):
        with ExitStack() as st:
            def T(name, shape, dt):
                return st.enter_context(nc.sbuf_tensor(name + "_L%d" % l, list(shape), dt))
            qT = T("sb_qT", [128, SEQ], BF16)
            kT = T("sb_kT", [128, SEQ], BF16)
            V = [T("sb_V%d" % i, [128, NT, 128], BF16) for i in range(2)]
            B_qT, B_kT, B_V = Buf("sb_qT"), Buf("sb_kT"), [Buf("sb_V0"), Buf("sb_V1")]
            C32 = [T("sb_C32_%d" % p, [128, 2, 512], F32) for p in range(2)]
            Cb = [T("sb_Cb_%d" % p, [128, 2, 512], BF16) for p in range(2)]
            B_C32 = [Buf("c32_%d" % p) for p in range(2)]
            B_Cb = [Buf("cb_%d" % p) for p in range(2)]
            e_sb = T("sb_e", [128, 2, 512], F32)
            B_e = Buf("sb_e")
            NSP, NA = 3, 2
            sp_sb = [T("sb_sp%d" % i, [128, 2, 512], BF16) for i in range(NSP)]
            B_sp = [Buf("sb_sp%d" % i) for i in range(NSP)]
            A_sb = [T("sb_A%d" % i, [128, 2, 512], BF16) for i in range(NA)]
            B_A = [Buf("sb_A%d" % i) for i in range(NA)]
            sq = T("sb_sq", [128, 512], F32)
            rs = T("sb_rs", [128, 512], F32)
            normT = (sq, Buf("sb_sq"), rs, Buf("sb_rs"), ps[0], B_ps[0])
            zP = pp[0][:].rearrange("p (h q) -> p h q", h=2)
            xP = pp[1][:].rearrange("p (h q) -> p h q", h=2)
            mstr2 = cbs("mstrict").unsqueeze(1).to_broadcast([128, 2, 128])
            for i in range(2):
                S.op("dve", lambda e, i=i: e.memset(V[i][:], 0.0), W=[B_V[i]])
            for c in range(3):
                S.dma("sp", qT[:], sqT_d[c * 128:(c + 1) * 128, :], R=[B_scr["sqT"]], W=[B_qT])
                S.dma("sp", kT[:], skT_d[c * 128:(c + 1) * 128, :], R=[B_scr["skT"]], W=[B_kT])
                for h in range(2):
                    S.dma("sp", V[h][:, :, h * 64:(h + 1) * 64],
                          sv_d[:, c * 128 + h * 64: c * 128 + (h + 1) * 64].rearrange("(t p) e -> p t e", p=128),
                          R=[B_scr["sv"]], W=[B_V[h]])
                tiles = []
                for sb in range(NSB):
                    for j in range(4 * sb + 3, -1, -1):
                        tiles.append((sb, j))
                n = len(tiles)

                def geom(t):
                    sb, j = tiles[t]
                    c0 = (j - 4 * sb) * 128 if j >= 4 * sb else 0
                    return sb, j, c0, (j >= 4 * sb), (j == 4 * sb + 3), (j == 0)

                def S1(t):
                    sb, j, c0, diag, first, last = geom(t)
                    par = sb % 2
                    if first:
                        S.op("dve", lambda e: e.memset(C32[par][:], 0.0), W=[B_C32[par]])
                        ob = 6 + par
                        S.op("dve", lambda e: e.memset(ps[ob][:], 0.0), W=[B_ps[ob]])
                    S.op("pe", [lambda e, h=h: e.matmul(ps[h][:, c0:512], lhsT=kT[h * 64:(h + 1) * 64, j * 128:(j + 1) * 128],
                                                        rhs=qT[h * 64:(h + 1) * 64, sb * 512 + c0:(sb + 1) * 512], start=True, stop=True)
                                for h in range(2)], R=[B_kT, B_qT], W=[B_ps[0], B_ps[1]])

                def S2a(t):
                    sb, j, c0, diag, first, last = geom(t)
                    S.op("act", lambda e: e.activation(out=e_sb[:, :, c0:512], in_=zP[:, :, c0:512], func=AF.Exp),
                         R=[B_ps[0], B_ps[1]], W=[B_e])

                def S2(t):
                    sb, j, c0, diag, first, last = geom(t)
                    k = t % NSP
                    S.op("act", lambda e: e.activation(out=sp_sb[k][:, :, c0:512], in_=e_sb[:, :, c0:512], func=AF.Ln, bias=1.0),
                         R=[B_e], W=[B_sp[k]])
                    if diag:
                        S.op("dve", lambda e: e.tensor_tensor(out=sp_sb[k][:, :, c0:c0 + 128], in0=sp_sb[k][:, :, c0:c0 + 128],
                                                              in1=mstr2, op=ALU.mult), R=[B_sp[k], B_cb], W=[B_sp[k]])

                def S3(t):
                    sb, j, c0, diag, first, last = geom(t)
                    k = t % NSP
                    par = sb % 2
                    fns = []
                    for h in range(2):
                        r = slice(h * 64, (h + 1) * 64)
                        pb = 2 + h
                        fns.append(lambda e, r=r, pb=pb: e.matmul(ps[pb][:, c0:512], lhsT=kT[r, j * 128:(j + 1) * 128],
                                                                  rhs=qT[r, sb * 512 + c0:(sb + 1) * 512], start=True, stop=False))
                        fns.append(lambda e, h=h, pb=pb: e.matmul(ps[pb][:, c0:512], lhsT=cbs("negtri"), rhs=sp_sb[k][:, h, c0:512],
                                                                  start=False, stop=first))
                        if not first:
                            chi = C32[par][:, h, :].bitcast(BF16)[:, 2 * c0 + 1:1024:2]
                            fns.append(lambda e, chi=chi, pb=pb: e.matmul(ps[pb][:, c0:512], lhsT=cbs("negones"), rhs=chi,
                                                                          start=False, stop=True))
                    S.op("pe", fns, R=[B_kT, B_qT, B_cb, B_sp[k], B_C32[par]], W=[B_ps[2], B_ps[3]])

                def S4(t):
                    sb, j, c0, diag, first, last = geom(t)
                    a = t % NA
                    S.op("act", lambda e: e.activation(out=A_sb[a][:, :, c0:512], in_=xP[:, :, c0:512], func=AF.Exp),
                         R=[B_ps[2], B_ps[3]], W=[B_A[a]])
                    if diag:
                        S.op("dve", lambda e: e.tensor_tensor(out=A_sb[a][:, :, c0:c0 + 128], in0=A_sb[a][:, :, c0:c0 + 128],
                                                              in1=mstr2, op=ALU.mult), R=[B_A[a], B_cb], W=[B_A[a]])

                def S6(t):
                    sb, j, c0, diag, first, last = geom(t)
                    if last:
                        return
                    k = t % NSP
                    par = sb % 2
                    S.op("dve", lambda e: e.tensor_tensor(out=C32[par][:, :, c0:512], in0=C32[par][:, :, c0:512],
                                                          in1=sp_sb[k][:, :, c0:512], op=ALU.add),
                         R=[B_sp[k]], W=[B_C32[par]])


                def S5(t):
                    sb, j, c0, diag, first, last = geom(t)
                    a = t % NA
                    ob = 6 + (sb % 2)
                    S.op("pe", [lambda e, h=h: e.matmul(ps[ob][:, c0:512], lhsT=V[h][:, j, :], rhs=A_sb[a][:, h, c0:512],
                                                        start=False, stop=False) for h in range(2)],
                         R=[B_V[0], B_V[1], B_A[a]], W=[B_ps[ob]])
                    if last:
                        head_norm_fm(l, ps[ob][:], B_ps[ob], 5 + c, sb * 512, normT, "sb")

                for tau in range(n + 2):
                    if tau < n:
                        S1(tau)
                        S2a(tau)
                        S2(tau)
                    if 0 <= tau - 1 < n:
                        S3(tau - 1)
                        S4(tau - 1)
                        S6(tau - 1)
                    if 0 <= tau - 2 < n:
                        S5(tau - 2)
                    if co is not None and co[0] is not None and (tau % CO_SKIP[0] != CO_SKIP[0] - 1):
                        if next(co[0], "end") in ("hold", "end"):
                            co[0] = None
            if co is not None:
                while co[0] is not None:
                    if next(co[0], "end") in ("hold", "end"):
                        co[0] = None
            S.barrier()

    def phase_dil(l):
        with ExitStack() as st:
            def T(name, shape, dt):
                return st.enter_context(nc.sbuf_tensor(name + "_L%d" % l, list(shape), dt))
            qT = T("dl_qT", [128, SEQ], BF16)
            kT = T("dl_kT", [128, SEQ], BF16)
            Vs = [[T("dl_V%d_%d" % (i, pp), [128, NT, 128], BF16) for i in range(2)] for pp in range(2)]
            B_Vs = [[Buf("dl_V%d_%d" % (i, pp)) for i in range(2)] for pp in range(2)]
            B_qT, B_kT = Buf("dl_qT"), Buf("dl_kT")
            accn = T("dl_accn", [128, SEQ], F32)
            accd = T("dl_accd", [128, SEQ], F32)
            B_accn, B_accd = Buf("accn"), Buf("accd")
            NP_ = 8
            P_sb = [T("dl_P%d" % i, [128, 2, 128], BF16) for i in range(NP_)]
            B_P = [Buf("dl_P%d" % i) for i in range(NP_)]
            rsA = [T("dl_rsA%d" % i, [128, 512], F32) for i in range(4)]
            B_rsA = [Buf("dl_rsA%d" % i) for i in range(4)]
            sq2 = [T("dl_sq%d" % i, [128, 512], F32) for i in range(2)]
            B_sq2 = [Buf("dl_sq%d" % i) for i in range(2)]
            rsB = [T("dl_rsB%d" % i, [128, 512], F32) for i in range(2)]
            B_rsB = [Buf("dl_rsB%d" % i) for i in range(2)]
            for pp in range(2):
                for i in range(2):
                    S.op("dve", lambda e, i=i, pp=pp: e.memset(Vs[pp][i][:], 0.0), W=[B_Vs[pp][i]])
                S.op("dve", lambda e, pp=pp: e.memset(Vs[pp][0][:, :, 64:65], 1.0), W=[B_Vs[pp][0]])
                S.op("dve", lambda e, pp=pp: e.memset(Vs[pp][1][:, :, 0:1], 1.0), W=[B_Vs[pp][1]])
            eo, _ = CB["dilE"]
            vcnt = [0]

            def load_V(c, pi):
                win_, d_ = DIL_PATTERNS[pi]
                nb_ = (SEQ // d_) // 128
                pp = vcnt[0] % 2
                vcnt[0] += 1
                for h in range(2):
                    src = dv_d[:, c * 128 + h * 64: c * 128 + (h + 1) * 64].rearrange("(j i r) e -> i r j e", i=128, r=d_)
                    dst = Vs[pp][h][:, :, h * 64:(h + 1) * 64].rearrange("p (r j) e -> p r j e", r=d_)
                    if d_ <= nb_:
                        for r0 in range(d_):
                            S.dma("sp", dst[:, r0, :, :], src[:, r0, :, :], R=[B_scr["dv"]], W=[B_Vs[pp][h]])
                    else:
                        for j0 in range(nb_):
                            S.dma("sp", dst[:, :, j0, :], src[:, :, j0, :], R=[B_scr["dv"]], W=[B_Vs[pp][h]])
                return pp
            pending = {}
            pending[(0, 0)] = load_V(0, 0)
            for c in range(3):
                S.dma("sp", qT[:], dqT_d[c * 128:(c + 1) * 128, :], R=[B_scr["dqT"]], W=[B_qT])
                S.dma("sp", kT[:], dkT_d[c * 128:(c + 1) * 128, :], R=[B_scr["dkT"]], W=[B_kT])
                grp = [0]
                for pi, (win, d) in enumerate(DIL_PATTERNS):
                    L = SEQ // d
                    nblk = L // 128
                    pp_cur = pending.pop((c, pi))
                    V = Vs[pp_cur]
                    B_V = B_Vs[pp_cur]
                    nxt = (c, pi + 1) if pi + 1 < 3 else ((c + 1, 0) if c + 1 < 3 else None)
                    if nxt is not None:
                        pending[nxt] = load_V(*nxt)
                    gsz = min(4, nblk)
                    tiles = []
                    for r in range(d):
                        for g in range(nblk // gsz):
                            for nloc in range(gsz):
                                nq = g * gsz + nloc
                                for j in (nq - 1, nq):
                                    if j >= 0:
                                        tiles.append((r, g, nloc, nq, j))
                    n = len(tiles)
                    gpar = {}

                    def tok(r, blk):
                        a = r + d * 128 * blk
                        return slice(a, a + d * 127 + 1, d) if d > 1 else slice(a, a + 128)

                    def S1(t):
                        r, g, nloc, nq, j = tiles[t]
                        first = (nloc == 0 and j == max(nq - 1, 0))
                        if first:
                            gp = (grp[0] % 2)
                            pn, pd = 4 + 2 * gp, 5 + 2 * gp
                            grp[0] += 1
                            N = gsz * 128
                            S.op("dve", lambda e: e.memset(ps[pn][:, 0:N], 0.0), W=[B_ps[pn]])
                            S.op("dve", lambda e: e.memset(ps[pd][:, 0:N], 0.0), W=[B_ps[pd]])
                        gpar[t] = (grp[0] - 1) % 2
                        zb = 2 * (t % 2)
                        S.op("pe", [lambda e, h=h: e.matmul(ps[zb + h][:, 0:128], lhsT=kT[h * 64:(h + 1) * 64, tok(r, j)],
                                                            rhs=qT[h * 64:(h + 1) * 64, tok(r, nq)], start=True, stop=True)
                                    for h in range(2)], R=[B_kT, B_qT], W=[B_ps[zb], B_ps[zb + 1]])

                    def S2(t):
                        r, g, nloc, nq, j = tiles[t]
                        zb = 2 * (t % 2)
                        pq = t % NP_
                        zv = PP[t % 2][:].rearrange("p (h q) -> p h q", h=2)[:, :, 0:128]
                        S.op("act", lambda e: e.activation(out=P_sb[pq][:], in_=zv, func=AF.Exp),
                             R=[B_ps[zb], B_ps[zb + 1]], W=[B_P[pq]])
                        e0 = eo + ((c * 3 + pi) * 2) * 256
                        off = 0 if j == nq else 128
                        ev = cb[:, e0:e0 + 512].rearrange("p (h x) -> p h x", h=2)[:, :, off:off + 128]
                        S.op("dve", lambda e: e.tensor_tensor(out=P_sb[pq][:], in0=P_sb[pq][:], in1=ev, op=ALU.mult),
                             R=[B_P[pq], B_cb], W=[B_P[pq]])

                    def S3(t):
                        r, g, nloc, nq, j = tiles[t]
                        gp = gpar[t]
                        pa = t % NP_
                        pn, pd = 4 + 2 * gp, 5 + 2 * gp
                        cs = slice(nloc * 128, (nloc + 1) * 128)
                        S.op("pe", [lambda e, h=h: e.matmul(ps[(pn, pd)[h]][:, cs], lhsT=V[h][:, r * nblk + j, :], rhs=P_sb[pa][:, h, :],
                                                            start=False, stop=False) for h in range(2)],
                             R=[B_V[0], B_V[1], B_P[pa]], W=[B_ps[pn], B_ps[pd]])
                        last = (nloc == gsz - 1 and j == nq)
                        if last:
                            N = gsz * 128
                            a = r + d * 128 * gsz * g
                            dst = slice(a, a + d * (N - 1) + 1, d) if d > 1 else slice(a, a + N)
                            if pi == 0:
                                S.op("dve", lambda e: e.tensor_copy(out=accn[:, dst], in_=ps[pn][:, 0:N]), R=[B_ps[pn]], W=[B_accn])
                                S.op("dve", lambda e: e.tensor_copy(out=accd[:, dst], in_=ps[pd][:, 0:N]), R=[B_ps[pd]], W=[B_accd])
                            else:
                                S.op("dve", lambda e: e.tensor_tensor(out=accn[:, dst], in0=accn[:, dst], in1=ps[pn][:, 0:N], op=ALU.add),
                                     R=[B_ps[pn]], W=[B_accn])
                                S.op("dve", lambda e: e.tensor_tensor(out=accd[:, dst], in0=accd[:, dst], in1=ps[pd][:, 0:N], op=ALU.add),
                                     R=[B_ps[pd]], W=[B_accd])

                    DEP = 2
                    for tau in range(n + DEP):
                        if tau < n:
                            S1(tau)
                            S2(tau)
                        if 0 <= tau - DEP < n:
                            S3(tau - DEP)
                def NA(sb):
                    cs = slice(sb * 512, (sb + 1) * 512)
                    pb = sb % 4
                    S.op("pe", [lambda e: e.matmul(ps[pb][:], lhsT=cfs("selA"), rhs=accn[:, cs], start=True, stop=False),
                                lambda e: e.matmul(ps[pb][:], lhsT=cfs("selB"), rhs=accd[:, cs], start=False, stop=True)],
                         R=[B_accn, B_accd, B_cf], W=[B_ps[pb]])
                    S.op("act", lambda e: e.activation(out=rsA[pb][:], in_=ps[pb][:], func=AF.Ln), R=[B_ps[pb]], W=[B_rsA[pb]])
                    S.op("act", lambda e: e.activation(out=rsA[pb][:], in_=rsA[pb][:], func=AF.Exp, scale=-1.0), R=[B_rsA[pb]], W=[B_rsA[pb]])

                def NB_(sb):
                    cs = slice(sb * 512, (sb + 1) * 512)
                    pb = sb % 4
                    S.op("dve", lambda e: e.tensor_tensor(out=accn[0:64, cs], in0=accn[0:64, cs], in1=rsA[pb][0:64, :], op=ALU.mult),
                         R=[B_rsA[pb]], W=[B_accn])
                    S.op("dve", lambda e: e.tensor_tensor(out=accn[64:128, cs], in0=accd[64:128, cs], in1=rsA[pb][64:128, :], op=ALU.mult),
                         R=[B_rsA[pb], B_accd], W=[B_accn])

                def NC(sb):
                    cs = slice(sb * 512, (sb + 1) * 512)
                    i_ = sb % 2
                    pb = 4 + i_
                    S.op("act", lambda e: e.activation(out=sq2[i_][:], in_=accn[:, cs], func=AF.Square), R=[B_accn], W=[B_sq2[i_]])
                    S.op("pe", lambda e: e.matmul(ps[pb][:], lhsT=cfs("blk64"), rhs=sq2[i_][:], start=True, stop=True),
                         R=[B_sq2[i_], B_cf], W=[B_ps[pb]])
                    S.op("act", lambda e: e.activation(out=rsB[i_][:], in_=ps[pb][:], func=AF.Ln, scale=1.0 / 64, bias=EPS),
                         R=[B_ps[pb]], W=[B_rsB[i_]])
                    S.op("act", lambda e: e.activation(out=rsB[i_][:], in_=rsB[i_][:], func=AF.Exp, scale=-0.5), R=[B_rsB[i_]], W=[B_rsB[i_]])

                def ND(sb):
                    cs = slice(sb * 512, (sb + 1) * 512)
                    i_ = sb % 2
                    S.op("dve", lambda e: e.scalar_tensor_tensor(out=Y["t"][:, 2 + c, cs], in0=accn[:, cs], scalar=ghp[:, l, 2 + c:3 + c],
                                                                 in1=rsB[i_][:], op0=ALU.mult, op1=ALU.mult),
                         R=[B_accn, B_rsB[i_], B_ghp], W=[B_ymix])

                for st_ in range(NSB + 3):
                    if st_ < NSB:
                        NA(st_)
                    if 0 <= st_ - 1 < NSB:
                        NB_(st_ - 1)
                    if 0 <= st_ - 2 < NSB:
                        NC(st_ - 2)
                    if 0 <= st_ - 3 < NSB:
                        ND(st_ - 3)
            S.barrier()


    def gen_mlstm(l, bk):
        with ExitStack() as st:
            def T(name, shape, dt):
                return st.enter_context(nc.sbuf_tensor(name + "_L%d" % l, list(shape), dt))
            cw = T("ml_cw", [128, 2, 4], F32)
            cbi = T("ml_cb", [128, 2], F32)
            ncbi = T("ml_ncb", [128, 2], F32)
            gb = T("ml_gb", [128, 8], F32)
            ghr = T("ml_ghr", [128, 256], F32)
            B_small = Buf("ml_small")
            S.dma("sp", cw[:], conv_w[l, :, :, :], W=[B_small])
            S.dma("sp", cbi[:], conv_b[l, :, :], W=[B_small])
            S.dma("sp", gb[:], gbias[l, :, :], W=[B_small])
            S.dma("sp", ghr[:], g_head_r[l, :, :], W=[B_small])
            S.op("dve", lambda e: e.tensor_scalar(out=ncbi[:], in0=cbi[:], scalar1=-1.0, scalar2=None, op0=ALU.mult),
                 R=[B_small], W=[B_small])
            Wbd = {}
            B_W = Buf("ml_W")
            for nm, src in (("q", w_mq), ("k", w_mk), ("v", w_mv)):
                Wbd[nm] = T("ml_W" + nm, [128, 2, 128], BF16)
                S.op("dve", lambda e, nm=nm: e.memset(Wbd[nm][:], 0.0), W=[B_W])
                for c in range(2):
                    for hh in range(2):
                        S.dma("pool", Wbd[nm][hh * 64:(hh + 1) * 64, c, hh * 64:(hh + 1) * 64], src[l, 2 * c + hh, :, :], W=[B_W])
            graw = T("ml_graw", [128, NT, 8], F32)
            B_g = Buf("ml_graw")
            S.dma("sp", graw[:], gates_d[:, :].rearrange("(t p) c -> p t c", p=128), R=[B_scr["gates"]], W=[B_g])
            S.op("dve", lambda e: e.tensor_tensor(out=graw[:], in0=graw[:], in1=gb[:].unsqueeze(1).to_broadcast([128, NT, 8]),
                                                  op=ALU.add), R=[B_small], W=[B_g])
            nl = T("ml_nl", [128, NT, 4], F32)
            a_t = T("ml_a", [128, NT, 4], F32)
            b_t = T("ml_b", [128, NT, 4], F32)
            bl_t = T("ml_bl", [128, NT, 4], F32)
            B_nl, B_a, B_b, B_bl = Buf("nl"), Buf("a"), Buf("b"), Buf("bl")
            S.op("act", lambda e: e.activation(out=nl[:], in_=graw[:, :, 4:8], func=AF.Exp, scale=-1.0), R=[B_g], W=[B_nl])
            S.op("act", lambda e: e.activation(out=nl[:], in_=nl[:], func=AF.Ln, bias=1.0), R=[B_nl], W=[B_nl])
            nlf = nl[:].rearrange("p t h -> p (t h)")
            S.op("pe", lambda e: e.matmul(ps[bk["c0"]][:, 0:128], lhsT=cfs("triu"), rhs=nlf, start=True, stop=True),
                 R=[B_nl, B_cf], W=[B_ps[bk["c0"]]])
            S.op("pe", lambda e: e.matmul(ps[bk["c1"]][:, 0:128], lhsT=cfs("ones"), rhs=nlf, start=True, stop=True),
                 R=[B_nl, B_cf], W=[B_ps[bk["c1"]]])
            pc = ps[bk["c0"]][:, 0:128].rearrange("p (t h) -> p t h", h=4)
            S.op("dve", lambda e: e.tensor_tensor(out=a_t[:], in0=graw[:, :, 0:4], in1=pc, op=ALU.add), R=[B_g, B_ps[bk["c0"]]], W=[B_a])
            S.op("act", lambda e: e.activation(out=a_t[:], in_=a_t[:], func=AF.Exp), R=[B_a], W=[B_a])
            S.op("act", lambda e: e.activation(out=b_t[:], in_=pc, func=AF.Exp, scale=-1.0), R=[B_ps[bk["c0"]]], W=[B_b])
            S.op("act", lambda e: e.activation(out=bl_t[:], in_=ps[bk["c1"]][:, 0:128].rearrange("p (t h) -> p t h", h=4), func=AF.Exp,
                                               scale=-1.0), R=[B_ps[bk["c1"]]], W=[B_bl])
            C32 = T("ml_C32", [128, 2, 65], F32)
            Cbf = T("ml_Cbf", [128, 2, 65], BF16)
            B_C32, B_Cbf = Buf("ml_C32"), Buf("ml_Cbf")
            S.op("dve", lambda e: e.memset(C32[:], 0.0), W=[B_C32])
            S.op("dve", lambda e: e.memset(Cbf[:], 0.0), W=[B_Cbf])
            xmp = [T("ml_xmp%d" % i, [128, 2, 515], BF16) for i in range(2)]
            B_xmp = [Buf("ml_xmp%d" % i) for i in range(2)]
            ogt = [T("ml_og%d" % i, [128, 4, 256], BF16) for i in range(2)]
            B_ogt = [Buf("ml_og%d" % i) for i in range(2)]
            acc = T("ml_acc", [128, 2, 512], F32)
            ez = T("ml_ez", [128, 2, 512], F32)
            xc = T("ml_xc", [128, 2, 512], BF16)
            B_acc, B_ez, B_xc = Buf("ml_acc"), Buf("ml_ez"), Buf("ml_xc")
            qTs = T("ml_qT", [128, 2, 512], BF16)
            kTs = T("ml_kT", [128, 2, 512], BF16)
            B_qTs, B_kTs = Buf("ml_qT"), Buf("ml_kT")
            ktok = T("ml_ktok", [128, 256], BF16)
            Vaug = T("ml_Vaug", [128, 4, 65], BF16)
            swm = T("ml_swm", [128, 4, 128], BF16)
            B_ktok, B_Vaug, B_swm = Buf("ml_ktok"), Buf("ml_Vaug"), Buf("ml_swm")
            sm = T("ml_sm", [128, 16], F32)
            B_sm = Buf("ml_sm")
            eo = T("ml_eo", [128, 256], F32)
            t1 = T("ml_t1", [128, 256], F32)
            ysq = T("ml_ysq", [128, 256], F32)
            yn = T("ml_yn", [128, 256], BF16)
            B_eo, B_t1, B_ysq, B_yn = Buf("ml_eo"), Buf("ml_t1"), Buf("ml_ysq"), Buf("ml_yn")
            ctmp = T("ml_ctmp", [128, 2, 65], F32)
            B_ctmp = Buf("ml_ctmp")

            yield
            for sb in range(NSB if ML_CUT[0] > 1 else 0):
                i2 = sb % 2
                xm = xmp[i2]
                if sb == 0:
                    S.op("dve", lambda e: e.memset(xm[:, :, 0:3], 0.0), W=[B_xmp[i2]])
                    S.dma("sp", xm[:, :, 3:515], xmT_d[:, 0:512].rearrange("(c p) t -> p c t", p=128), R=[B_scr["xmT"]], W=[B_xmp[i2]])
                else:
                    S.dma("sp", xm[:, :, 0:515], xmT_d[:, sb * 512 - 3:(sb + 1) * 512].rearrange("(c p) t -> p c t", p=128),
                          R=[B_scr["xmT"]], W=[B_xmp[i2]])
                S.dma("sp", ogt[i2][:], og_d[sb * 512:(sb + 1) * 512, :].rearrange("(t p) c -> p t c", p=128), R=[B_scr["og"]],
                      W=[B_ogt[i2]])
                yield
                for c in range(2):
                    S.op("dve", lambda e, c=c: e.tensor_scalar(out=acc[:, c, :], in0=xm[:, c, 0:512], scalar1=cw[:, c, 0:1], scalar2=None,
                                                               op0=ALU.mult), R=[B_xmp[i2], B_small], W=[B_acc])
                    for j in range(1, 4):
                        S.op("dve", lambda e, c=c, j=j: e.scalar_tensor_tensor(out=acc[:, c, :], in0=xm[:, c, j:j + 512],
                                                                               scalar=cw[:, c, j:j + 1], in1=acc[:, c, :],
                                                                               op0=ALU.mult, op1=ALU.add),
                             R=[B_xmp[i2], B_small], W=[B_acc])
                    S.op("act", lambda e, c=c: e.activation(out=ez[:, c, :], in_=acc[:, c, :], func=AF.Exp, scale=-1.0,
                                                            bias=ncbi[:, c:c + 1]), R=[B_acc, B_small], W=[B_ez])
                    S.op("dve", lambda e, c=c: e.tensor_scalar(out=ez[:, c, :], in0=ez[:, c, :], scalar1=1.0, scalar2=None, op0=ALU.add),
                         R=[B_ez], W=[B_ez])
                    S.op("dve", lambda e, c=c: e.reciprocal(out=ez[:, c, :], in_=ez[:, c, :]), R=[B_ez], W=[B_ez])
                    S.op("dve", lambda e, c=c: e.scalar_tensor_tensor(out=xc[:, c, :], in0=acc[:, c, :], scalar=cbi[:, c:c + 1],
                                                                      in1=ez[:, c, :], op0=ALU.add, op1=ALU.mult),
                         R=[B_acc, B_ez, B_small], W=[B_xc])
                yield
                for c in range(2):
                    S.op("pe", lambda e, c=c: e.matmul(ps[bk["q"]][:], lhsT=Wbd["q"][:, c, :], rhs=xc[:, c, :], start=True, stop=True),
                         R=[B_W, B_xc], W=[B_ps[bk["q"]]])
                    S.op("act", lambda e, c=c: e.copy(out=qTs[:, c, :], in_=ps[bk["q"]][:]), R=[B_ps[bk["q"]]], W=[B_qTs])
                    S.op("pe", lambda e, c=c: e.matmul(ps[bk["k"]][:], lhsT=Wbd["k"][:, c, :], rhs=xc[:, c, :], start=True, stop=True),
                         R=[B_W, B_xc], W=[B_ps[bk["k"]]])
                    S.op("act", lambda e, c=c: e.activation(out=kTs[:, c, :], in_=ps[bk["k"]][:], func=AF.Copy, scale=0.125),
                         R=[B_ps[bk["k"]]], W=[B_kTs])
                for i in range(4 if ML_CUT[0] > 2 else 0):
                    cut = ML_CUT[0]
                    ci = sb * 4 + i
                    ts = slice(i * 128, (i + 1) * 128)
                    yield
                    S.op("pe", [lambda e, c=c: e.matmul(ps[bk["kt"]][:, c * 128:(c + 1) * 128], lhsT=xc[:, c, ts], rhs=Wbd["k"][:, c, :],
                                                        start=True, stop=True) for c in range(2)],
                         R=[B_xc, B_W], W=[B_ps[bk["kt"]]])
                    S.op("act", lambda e: e.activation(out=ktok[:], in_=ps[bk["kt"]][:, 0:256], func=AF.Copy, scale=0.125),
                         R=[B_ps[bk["kt"]]], W=[B_ktok])
                    S.op("pe", [lambda e, c=c: e.matmul(ps[bk["vt"]][:, c * 128:(c + 1) * 128], lhsT=xm[:, c, 3 + i * 128:3 + (i + 1) * 128],
                                                        rhs=Wbd["v"][:, c, :], start=True, stop=True) for c in range(2)],
                         R=[B_xmp[i2], B_W], W=[B_ps[bk["vt"]]])
                    S.op("dve", lambda e: e.tensor_tensor(out=Vaug[:, :, 0:64], in0=ps[bk["vt"]][:, 0:256].rearrange("p (h e) -> p h e", h=4),
                                                          in1=a_t[:, ci, :].unsqueeze(2).to_broadcast([128, 4, 64]), op=ALU.mult),
                         R=[B_ps[bk["vt"]], B_a], W=[B_Vaug])
                    S.op("dve", lambda e: e.tensor_copy(out=Vaug[:, :, 64], in_=a_t[:, ci, :]), R=[B_a], W=[B_Vaug])
                    if cut <= 3:
                        continue
                    yield
                    sbank = (bk["S0"], bk["S1"])
                    S.op("pe", [lambda e, h=h: e.matmul(ps[sbank[h % 2]][:, (h // 2) * 128:(h // 2 + 1) * 128],
                                                        lhsT=kTs[(h % 2) * 64:(h % 2 + 1) * 64, h // 2, ts],
                                                        rhs=qTs[(h % 2) * 64:(h % 2 + 1) * 64, h // 2, ts], start=True, stop=True)
                                for h in range(4)], R=[B_kTs, B_qTs], W=[B_ps[bk["S0"]], B_ps[bk["S1"]]])
                    for hh in range(2):
                        S.op("dve", lambda e, hh=hh: e.tensor_tensor(
                            out=swm[:, hh::2, :], in0=ps[sbank[hh]][:, 0:256].rearrange("p (c t) -> p c t", c=2),
                            in1=cbs("triu").unsqueeze(1).to_broadcast([128, 2, 128]), op=ALU.mult),
                            R=[B_ps[sbank[hh]], B_cb], W=[B_swm])
                    if cut <= 4:
                        continue
                    yield
                    fns = []
                    for h in range(4):
                        rows = slice((h % 2) * 64, (h % 2 + 1) * 64)
                        fns.append(lambda e, h=h, rows=rows: e.matmul(ps[bk["H"]][:, h * 65:(h + 1) * 65], lhsT=qTs[rows, h // 2, ts],
                                                                      rhs=Cbf[rows, h // 2, :], start=True, stop=False))
                        fns.append(lambda e, h=h: e.matmul(ps[bk["H"]][:, h * 65:(h + 1) * 65], lhsT=swm[:, h, :], rhs=Vaug[:, h, :],
                                                           start=False, stop=True))
                    S.op("pe", fns, R=[B_qTs, B_Cbf, B_swm, B_Vaug], W=[B_ps[bk["H"]]])
                    if cut <= 5:
                        continue
                    yield
                    S.op("pe", [lambda e, c=c: e.matmul(ps[bk["dC"]][:, c * 130:(c + 1) * 130], lhsT=ktok[:, c * 128:(c + 1) * 128],
                                                        rhs=Vaug[:, 2 * c:2 * c + 2, :].rearrange("p h e -> p (h e)"),
                                                        start=True, stop=True) for c in range(2)],
                         R=[B_ktok, B_Vaug], W=[B_ps[bk["dC"]]])
                    for c in range(2):
                        for hh in range(2):
                            rows = slice(hh * 64, (hh + 1) * 64)
                            S.op("dve", lambda e, c=c, hh=hh, rows=rows: e.tensor_tensor(
                                out=ctmp[rows, c, :], in0=C32[rows, c, :], in1=ps[bk["dC"]][rows, c * 130 + hh * 65:c * 130 + (hh + 1) * 65],
                                op=ALU.add), R=[B_C32, B_ps[bk["dC"]]], W=[B_ctmp])
                            S.op("dve", lambda e, c=c, hh=hh, rows=rows: e.tensor_scalar(
                                out=C32[rows, c, :], in0=ctmp[rows, c, :], scalar1=bl_t[rows, ci, 2 * c + hh:2 * c + hh + 1], scalar2=None,
                                op0=ALU.mult), R=[B_ctmp, B_bl], W=[B_C32])
                    S.op("dve", lambda e: e.tensor_copy(out=Cbf[:], in_=C32[:]), R=[B_C32], W=[B_Cbf])
                    if cut <= 6:
                        continue
                    yield
                    pH = ps[bk["H"]][:, 0:260].rearrange("p (h e) -> p h e", h=4)
                    S.op("dve", lambda e: e.tensor_tensor(out=sm[:, 0:4], in0=pH[:, :, 64], in1=b_t[:, ci, :], op=ALU.mult),
                         R=[B_ps[bk["H"]], B_b], W=[B_sm])
                    S.op("dve", lambda e: e.tensor_scalar(out=sm[:, 4:8], in0=sm[:, 0:4], scalar1=-1.0, scalar2=None, op0=ALU.mult),
                         R=[B_sm], W=[B_sm])
                    S.op("dve", lambda e: e.tensor_tensor(out=sm[:, 4:8], in0=sm[:, 4:8], in1=sm[:, 0:4], op=ALU.max),
                         R=[B_sm], W=[B_sm])
                    S.op("dve", lambda e: e.tensor_scalar(out=sm[:, 4:8], in0=sm[:, 4:8], scalar1=1.0, scalar2=None, op0=ALU.max),
                         R=[B_sm], W=[B_sm])
                    S.op("dve", lambda e: e.reciprocal(out=sm[:, 4:8], in_=sm[:, 4:8]), R=[B_sm], W=[B_sm])
                    S.op("dve", lambda e: e.tensor_tensor(out=sm[:, 8:12], in0=b_t[:, ci, :], in1=sm[:, 4:8], op=ALU.mult),
                         R=[B_sm, B_b], W=[B_sm])
                    yield
                    S.op("act", lambda e: e.activation(out=eo[:], in_=ogt[i2][:, i, :], func=AF.Exp, scale=-1.0), R=[B_ogt[i2]], W=[B_eo])
                    S.op("dve", lambda e: e.tensor_scalar(out=eo[:], in0=eo[:], scalar1=1.0, scalar2=None, op0=ALU.add), R=[B_eo], W=[B_eo])
                    S.op("dve", lambda e: e.tensor_tensor(out=t1[:].rearrange("p (h e) -> p h e", h=4), in0=pH[:, :, 0:64],
                                                          in1=sm[:, 8:12].unsqueeze(2).to_broadcast([128, 4, 64]), op=ALU.mult),
                         R=[B_ps[bk["H"]], B_sm], W=[B_t1])
                    S.op("dve", lambda e: e.reciprocal(out=eo[:], in_=eo[:]), R=[B_eo], W=[B_eo])
                    S.op("dve", lambda e: e.tensor_tensor(out=t1[:], in0=t1[:], in1=eo[:], op=ALU.mult), R=[B_t1, B_eo], W=[B_t1])
                    yield
                    S.op("dve", lambda e: e.tensor_tensor(out=ysq[:], in0=t1[:], in1=t1[:], op=ALU.mult), R=[B_t1], W=[B_ysq])
                    S.op("dve", lambda e: e.tensor_reduce(out=sm[:, 12:16], in_=ysq[:].rearrange("p (h e) -> p h e", h=4), axis=AX.X,
                                                          op=ALU.add), R=[B_ysq], W=[B_sm])
                    S.op("act", lambda e: e.activation(out=sm[:, 12:16], in_=sm[:, 12:16], func=AF.Ln, scale=1.0 / 64, bias=EPS),
                         R=[B_sm], W=[B_sm])
                    S.op("act", lambda e: e.activation(out=sm[:, 12:16], in_=sm[:, 12:16], func=AF.Exp, scale=-0.5), R=[B_sm], W=[B_sm])
                    S.op("dve", lambda e: e.tensor_tensor(out=t1[:].rearrange("p (h e) -> p h e", h=4),
                                                          in0=t1[:].rearrange("p (h e) -> p h e", h=4),
                                                          in1=sm[:, 12:16].unsqueeze(2).to_broadcast([128, 4, 64]), op=ALU.mult),
                         R=[B_sm], W=[B_t1])
                    S.op("dve", lambda e: e.tensor_tensor(out=yn[:], in0=t1[:], in1=ghr[:], op=ALU.mult), R=[B_t1, B_small], W=[B_yn])
                    if cut <= 7:
                        continue
                    yield
                    pT = ps[bk["T"]][:].bitcast(BF16)
                    S.op("pe", [lambda e, c=c: e.transpose(out=pT[:, c * 128:(c + 1) * 128], in_=yn[:, c * 128:(c + 1) * 128],
                                                           identity=cbs("ident")) for c in range(2)], R=[B_yn, B_cb], W=[B_ps[bk["T"]]])
                    S.op("act", lambda e: e.copy(out=Y["t"][:, 0:2, ci * 128:(ci + 1) * 128],
                                                 in_=pT[:, 0:256].rearrange("p (c t) -> p c t", c=2)), R=[B_ps[bk["T"]]], W=[B_ymix])
            yield "hold"


    ML_BANKS_ALONE = {"c0": 0, "c1": 1, "q": 0, "k": 1, "kt": 2, "vt": 3, "S0": 4, "S1": 0, "H": 5, "dC": 6, "T": 7}
    ML_BANKS_CO = {"c0": 4, "c1": 5, "q": 4, "k": 4, "kt": 4, "vt": 4, "S0": 4, "S1": 5, "H": 5, "dC": 4, "T": 4}

    def phase_mlstm(l):
        g = gen_mlstm(l, ML_BANKS_ALONE)
        for _ in g:
            pass
        S.barrier()

    def phase_outproj(l, x_src, B_xsrc, x_dst, B_xdst):
        with ExitStack() as st:
            def T(name, shape, dt):
                return st.enter_context(nc.sbuf_tensor(name + "_L%d" % l, list(shape), dt))
            wo = T("op_w", [128, 8, D], BF16)
            B_wo = Buf("op_w")
            for k in range(8):
                S.dma("pool", wo[:, k, :], w_out[l, k * 128:(k + 1) * 128, :], W=[B_wo])
            xt = [T("op_xt%d" % i, [128, D], F32) for i in range(4)]
            xo = [T("op_xo%d" % i, [128, D], F32) for i in range(4)]
            B_xt = [Buf("op_xt%d" % i) for i in range(4)]
            B_xo = [Buf("op_xo%d" % i) for i in range(4)]
            for t0 in range(2):
                S.dma("sp", xt[t0][:], x_src[t0 * 128:(t0 + 1) * 128, :], R=[B_xsrc], W=[B_xt[t0]])
            for ti in range(NT):
                i = ti % 4
                if ti + 2 < NT:
                    S.dma("sp", xt[(ti + 2) % 4][:], x_src[(ti + 2) * 128:(ti + 3) * 128, :], R=[B_xsrc], W=[B_xt[(ti + 2) % 4]])
                for hf in range(2):
                    pb = (ti * 2 + hf) % 8
                    cs = slice(hf * 512, (hf + 1) * 512)
                    S.op("pe", [lambda e, k=k: e.matmul(ps[pb][:], lhsT=Y["t"][:, k, ti * 128:(ti + 1) * 128], rhs=wo[:, k, cs],
                                                        start=(k == 0), stop=(k == 7)) for k in range(8)],
                         R=[B_ymix, B_wo], W=[B_ps[pb]])
                    S.op("dve", lambda e: e.tensor_tensor(out=xo[i][:, cs], in0=ps[pb][:], in1=mod[:, 2, cs], op=ALU.mult),
                         R=[B_ps[pb], B_mod], W=[B_xo[i]])
                    S.op("dve", lambda e: e.tensor_tensor(out=xo[i][:, cs], in0=xo[i][:, cs], in1=xt[i][:, cs], op=ALU.add),
                         R=[B_xt[i]], W=[B_xo[i]])
                S.dma("sp", x_dst[ti * 128:(ti + 1) * 128, :], xo[i][:], R=[B_xo[i]], W=[B_xdst])
            S.barrier()

    def phase_moe(l, x_src, B_xsrc, x_dst, B_xdst, final):
        with ExitStack() as st:
            def T(name, shape, dt):
                return st.enter_context(nc.sbuf_tensor(name + "_L%d" % l, list(shape), dt))
            wr = T("mo_wr", [128, 8, NEXP], F32)
            rb = T("mo_rb", [128, NEXP], F32)
            B_wr = Buf("mo_wr")
            S.dma("sp", wr[:], w_router.rearrange("(k p) e -> p k e", p=128), W=[B_wr])
            S.dma("sp", rb[:], rbias[:, :], W=[B_wr])
            gfin = None
            if final:
                gfin = mod[:, 0, :]
                S.dma("sp", gfin, g_final[:, :], W=[B_mod])
            h2T = [T("mo_h2T%d" % i, [128, 8, 1024], BF16) for i in range(2)]
            B_h2T = [Buf("mo_h2T%d" % i) for i in range(2)]
            yaccA = T("mo_yacc", [128, 8, D], F32)
            yaccB = T("mo_yaccB", [128, 4, D], F32)
            B_yaccA = [Buf("mo_yacc%d" % i) for i in range(8)]
            B_yaccB = [Buf("mo_yaccB%d" % i) for i in range(4)]

            def ysel(qt, tl):
                if tl < 4 and qt % 2 == 1:
                    return yaccB[:, tl, :], B_yaccB[tl]
                return yaccA[:, tl, :], B_yaccA[tl]
            combTok = [T("mo_combTok%d" % i, [128, 8, NEXP], F32) for i in range(2)]
            B_combT = [Buf("mo_combTok%d" % i) for i in range(2)]
            Wg = [T("mo_Wg%d" % i, [128, 8, DEXP], BF16) for i in range(2)]
            Wu = [T("mo_Wu%d" % i, [128, 8, DEXP], BF16) for i in range(2)]
            Wd = [T("mo_Wd%d" % i, [128, 4, D], BF16) for i in range(2)]
            B_Wg = [Buf("mo_Wg%d" % i) for i in range(2)]
            B_Wu = [Buf("mo_Wu%d" % i) for i in range(2)]
            B_Wd = [Buf("mo_Wd%d" % i) for i in range(2)]
            he = [T("mo_he%d" % i, [128, 4, 512], BF16) for i in range(2)]
            B_he = [Buf("mo_he%d" % i) for i in range(2)]
            sg = [T("mo_sg%d" % i, [128, 512], BF16) for i in range(2)]
            B_sg = [Buf("mo_sg%d" % i) for i in range(2)]
            xt = [T("mo_xt%d" % i, [128, D], F32) for i in range(2)]
            B_xt = [Buf("mo_xt%d" % i) for i in range(2)]
            tmp = T("mo_tmp", [128, D], F32)
            B_tmp = Buf("mo_tmp")
            h2f = [T("mo_h2f%d" % i, [128, D], F32) for i in range(2)]
            B_h2f = [Buf("mo_h2f%d" % i) for i in range(2)]
            h2Tf1 = T("mo_h2Tf", [128, 8, 128], F32)
            h2Tf = [h2Tf1, h2Tf1]
            B_h2Tf1 = Buf("mo_h2Tf")
            B_h2Tf = [B_h2Tf1, B_h2Tf1]
            ss = [T("mo_ss%d" % i, [128, 4], F32) for i in range(2)]
            B_ss = [Buf("mo_ss%d" % i) for i in range(2)]
            ssf = T("mo_ssf", [128, 8, 4], F32)
            B_ssf = [Buf("mo_ssf%d" % i) for i in range(8)]
            rt = [T("mo_rt%d" % i, [128, 8, 64], F32) for i in range(2)]
            B_rt = [Buf("mo_rt%d" % i) for i in range(2)]
            PR = 7

            def load_gu(e):
                i = e % 2
                S.dma("pool", Wg[i][:], w_gate[l, e, :, :].rearrange("(k p) f -> p k f", p=128), W=[B_Wg[i]])
                S.dma("pool", Wu[i][:], w_up[l, e, :, :].rearrange("(k p) f -> p k f", p=128), W=[B_Wu[i]])

            def load_d(e):
                i = e % 2
                S.dma("pool", Wd[i][:], w_down[l, e, :, :].rearrange("(k p) d -> p k d", p=128), W=[B_Wd[i]])

            def load_w(e):
                load_gu(e)
                load_d(e)

            def Ra(qt, tt):
                ti = qt * 8 + tt
                i = ti % 2
                S.dma("sp", xt[i][:], x_src[ti * 128:(ti + 1) * 128, :], R=[B_xsrc], W=[B_xt[i]])
                norm_tile(xt[i], B_xt[i], 3, tmp, B_tmp, h2f[i], B_h2f[i], ss[i], B_ss[i])

            def Rb(qt, tt, halves=(0, 1)):
                ti = qt * 8 + tt
                i = ti % 2
                qb = qt % 2
                for half in halves:
                    S.op("pe", [lambda e, c=c: e.transpose(out=ps[PR][:, (c % 4) * 128:(c % 4 + 1) * 128],
                                                           in_=h2f[i][:, c * 128:(c + 1) * 128], identity=cfs("ident"))
                                for c in range(half * 4, half * 4 + 4)], R=[B_h2f[i], B_cf], W=[B_ps[PR]])
                    S.op("act", lambda e: e.copy(out=h2T[qb][:, half * 4:half * 4 + 4, tt * 128:(tt + 1) * 128],
                                                 in_=ps[PR][:].rearrange("p (c t) -> p c t", c=4)), R=[B_ps[PR]], W=[B_h2T[qb]])
                    S.op("dve", lambda e: e.tensor_copy(out=h2Tf[i][:, half * 4:half * 4 + 4, :],
                                                        in_=ps[PR][:].rearrange("p (c t) -> p c t", c=4)), R=[B_ps[PR]], W=[B_h2Tf[i]])

            def Rb2(qt, tt):
                ti = qt * 8 + tt
                i = ti % 2
                bi = (ti // 4) % 2
                S.op("pe", [lambda e, k=k: e.matmul(ps[PR][:, 0:16], lhsT=h2Tf[i][:, k, :], rhs=wr[:, k, :], start=(k == 0), stop=(k == 7))
                            for k in range(8)], R=[B_h2Tf[i], B_wr], W=[B_ps[PR]])
                S.op("act", lambda e: e.activation(out=rt[bi][:, 0, (tt % 4) * 16:(tt % 4 + 1) * 16], in_=ps[PR][:, 0:16], func=AF.Exp,
                                                   scale=-1.0), R=[B_ps[PR]], W=[B_rt[bi]])
                if tt % 4 == 3:
                    RT(qt, tt // 4, bi)

            def RT(qt, b, bi):
                qb = qt % 2
                r_ = rt[bi]
                sc, g, eq, g2, sel, w_ = [r_[:, k_, :] for k_ in range(6)]
                m1, m2, gs, gmk = r_[:, 6, 0:16], r_[:, 6, 16:32], r_[:, 6, 32:48], r_[:, 6, 48:64]
                gmx, wsum = r_[:, 7, 0:4], r_[:, 7, 4:8]

                def vg(a):
                    return a.rearrange("p (g e) -> p g e", e=4)

                def vt(a):
                    return a.rearrange("p (t e) -> p t e", e=16)

                def bg(a):
                    return a.unsqueeze(2).to_broadcast([128, 16, 4])

                def t4(a):
                    return a.rearrange("p (t g) -> p t g", g=4)
                ops = [
                    lambda e: e.tensor_scalar(out=sc, in0=sc, scalar1=1.0, scalar2=None, op0=ALU.add),
                    lambda e: e.reciprocal(out=sc, in_=sc),
                    lambda e: e.tensor_tensor(out=vt(g), in0=vt(sc), in1=rb[:].unsqueeze(1).to_broadcast([128, 4, 16]), op=ALU.add),
                    lambda e: e.tensor_reduce(out=m1, in_=vg(g), axis=AX.X, op=ALU.max),
                    lambda e: e.tensor_tensor(out=vg(eq), in0=vg(g), in1=bg(m1), op=ALU.is_equal),
                    lambda e: e.scalar_tensor_tensor(out=g2, in0=eq, scalar=-1.0e9, in1=g, op0=ALU.mult, op1=ALU.add),
                    lambda e: e.tensor_reduce(out=m2, in_=vg(g2), axis=AX.X, op=ALU.max),
                    lambda e: e.tensor_tensor(out=gs, in0=m1, in1=m2, op=ALU.add),
                    lambda e: e.tensor_reduce(out=gmx, in_=t4(gs), axis=AX.X, op=ALU.max),
                    lambda e: e.tensor_tensor(out=t4(gmk), in0=t4(gs), in1=gmx.unsqueeze(2).to_broadcast([128, 4, 4]), op=ALU.is_ge),
                    lambda e: e.tensor_tensor(out=vg(sel), in0=vg(g), in1=bg(m2), op=ALU.is_ge),
                    lambda e: e.tensor_tensor(out=vg(sel), in0=vg(sel), in1=bg(gmk), op=ALU.mult),
                    lambda e: e.tensor_tensor(out=w_, in0=sc, in1=sel, op=ALU.mult),
                    lambda e: e.tensor_reduce(out=wsum, in_=vt(w_), axis=AX.X, op=ALU.add),
                    lambda e: e.reciprocal(out=wsum, in_=wsum),
                ]
                for f_ in ops:
                    S.op("dve", f_, R=[B_rt[bi], B_wr], W=[B_rt[bi]])
                S.op("dve", lambda e: e.tensor_tensor(out=combTok[qb][:, 4 * b:4 * b + 4, :], in0=vt(w_),
                                                      in1=wsum.unsqueeze(2).to_broadcast([128, 4, 16]), op=ALU.mult),
                     R=[B_rt[bi]], W=[B_combT[qb]])

            units = [(e_, s2) for e_ in range(NEXP) for s2 in range(2)]

            def GU(qt, u):
                e_, s2 = units[u]
                wi = e_ % 2
                qb = qt % 2
                ci_ = u % 2
                for f in range(4):
                    pg, pu = (0, 1) if f % 2 == 0 else (2, 3)
                    fs = slice(f * 128, (f + 1) * 128)
                    S.op("pe", [lambda e, k=k: e.matmul(ps[pg][:], lhsT=Wg[wi][:, k, fs], rhs=h2T[qb][:, k, s2 * 512:(s2 + 1) * 512],
                                                        start=(k == 0), stop=(k == 7)) for k in range(8)],
                         R=[B_Wg[wi], B_h2T[qb]], W=[B_ps[pg]])
                    S.op("pe", [lambda e, k=k: e.matmul(ps[pu][:], lhsT=Wu[wi][:, k, fs], rhs=h2T[qb][:, k, s2 * 512:(s2 + 1) * 512],
                                                        start=(k == 0), stop=(k == 7)) for k in range(8)],
                         R=[B_Wu[wi], B_h2T[qb]], W=[B_ps[pu]])
                    j = f % 2
                    S.op("act", lambda e: e.activation(out=sg[j][:], in_=ps[pg][:], func=AF.Silu), R=[B_ps[pg]], W=[B_sg[j]])
                    S.op("dve", lambda e: e.tensor_tensor(out=he[ci_][:, f, :], in0=ps[pu][:], in1=sg[j][:], op=ALU.mult),
                         R=[B_ps[pu], B_sg[j]], W=[B_he[ci_]])

            def DOWN(qt, u):
                e_, s2 = units[u]
                wi = e_ % 2
                ci_ = u % 2
                qb = qt % 2
                for t4 in range(4):
                    tl = s2 * 4 + t4
                    for dh in range(2):
                        py = 4 + ((t4 * 2 + dh) % 3)
                        ds_ = slice(dh * 512, (dh + 1) * 512)
                        S.op("pe", [lambda e, f=f: e.matmul(ps[py][:], lhsT=he[ci_][:, f, t4 * 128:(t4 + 1) * 128], rhs=Wd[wi][:, f, ds_],
                                                            start=(f == 0), stop=(f == 3)) for f in range(4)],
                             R=[B_he[ci_], B_Wd[wi]], W=[B_ps[py]])
                        cw_ = combTok[qb][:, tl, e_:e_ + 1]
                        ya_, B_ya = ysel(qt, tl)
                        if e_ == 0:
                            S.op("dve", lambda e: e.tensor_scalar(out=ya_[:, ds_], in0=ps[py][:], scalar1=cw_, scalar2=None, op0=ALU.mult),
                                 R=[B_ps[py], B_combT[qb]], W=[B_ya])
                        else:
                            S.op("dve", lambda e: e.scalar_tensor_tensor(out=ya_[:, ds_], in0=ps[py][:], scalar=cw_, in1=ya_[:, ds_],
                                                                         op0=ALU.mult, op1=ALU.add),
                                 R=[B_ps[py], B_combT[qb]], W=[B_ya])

            def EPIa(qt, tt):
                ti = qt * 8 + tt
                ya_, B_ya = ysel(qt, tt)
                xe, B_xe = xt[tt % 2], B_xt[tt % 2]
                S.dma("sp", xe[:], x_src[ti * 128:(ti + 1) * 128, :], R=[B_xsrc], W=[B_xe])
                S.op("pool", lambda e: e.tensor_tensor(out=ya_, in0=ya_, in1=mod[:, 5, :], op=ALU.mult), R=[B_mod], W=[B_ya])
                S.op("pool", lambda e: e.tensor_tensor(out=ya_, in0=ya_, in1=xe[:], op=ALU.add), R=[B_xe], W=[B_ya])
                if final:
                    s_ = ssf[:, tt, :]
                    S.op("act", lambda e: e.activation(out=xe[:], in_=ya_, func=AF.Square, accum_out=s_[:, 0:1]),
                         R=[B_ya], W=[B_xe, B_ssf[tt]])
                    S.op("act", lambda e: e.activation(out=s_[:, 1:2], in_=s_[:, 0:1], func=AF.Ln, scale=1.0 / D, bias=EPS),
                         R=[B_ssf[tt]], W=[B_ssf[tt]])
                    S.op("act", lambda e: e.activation(out=s_[:, 2:3], in_=s_[:, 1:2], func=AF.Exp, scale=-0.5), R=[B_ssf[tt]], W=[B_ssf[tt]])
                else:
                    S.dma("sp", x_dst[ti * 128:(ti + 1) * 128, :], ya_, R=[B_ya], W=[B_xdst])

            def EPIb(qt, tt):
                if not final:
                    return
                ti = qt * 8 + tt
                ya_, B_ya = ysel(qt, tt)
                s_ = ssf[:, tt, :]
                S.op("dve", lambda e: e.scalar_tensor_tensor(out=ya_, in0=ya_, scalar=s_[:, 2:3], in1=gfin, op0=ALU.mult, op1=ALU.mult),
                     R=[B_ssf[tt], B_mod], W=[B_ya])
                S.dma("sp", x_dst[ti * 128:(ti + 1) * 128, :], ya_, R=[B_ya], W=[B_xdst])

            load_w(0)
            load_w(1)
            for tt in range(9):
                if tt < 8:
                    Ra(0, tt)
                if tt >= 1:
                    Rb2(0, tt - 1)
                if tt < 8:
                    Rb(0, tt)
            for qt in range(4):
                sched = {}
                if qt + 1 < 4:
                    for tt in range(8):
                        sched.setdefault(3 + 3 * tt, []).append(("a", tt))
                        sched.setdefault(4 + 3 * tt, []).append(("b", tt))
                        sched.setdefault(5 + 3 * tt, []).append(("b2", tt))
                GU(qt, 0)
                if qt > 0:
                    for tt in range(4, 8):
                        EPIa(qt - 1, tt)
                for u in range(len(units)):
                    if u + 1 < len(units):
                        GU(qt, u + 1)
                    e_u, s_u = units[u]
                    if s_u == 0 and (e_u + 2 < NEXP or qt + 1 < 4):
                        load_gu((e_u + 2) % NEXP)
                    if qt > 0 and u == 1:
                        for tt in range(4, 8):
                            EPIb(qt - 1, tt)
                    if qt > 0 and 1 <= u <= 4:
                        EPIa(qt - 1, u - 1)
                    if qt > 0 and 2 <= u <= 5:
                        EPIb(qt - 1, u - 2)
                    post = []
                    for kind, tt in sched.get(u, []):
                        if kind == "b":
                            Rb(qt + 1, tt, halves=(0,))
                            post.append(tt)
                        else:
                            {"a": Ra, "b2": Rb2}[kind](qt + 1, tt)
                    DOWN(qt, u)
                    for tt in post:
                        Rb(qt + 1, tt, halves=(1,))
                    if s_u == 1 and (e_u + 2 < NEXP or qt + 1 < 4):
                        load_d((e_u + 2) % NEXP)
                    if qt == 3 and e_u == NEXP - 1 and s_u == 0:
                        for tt in range(4):
                            EPIa(qt, tt)
                if qt == 3:
                    for tt in range(4):
                        EPIb(qt, tt)
                    for tt in range(4, 8):
                        EPIa(qt, tt)
                    for tt in range(4, 8):
                        EPIb(qt, tt)
            S.barrier()

    def dump_dram(name, src, B_src, shape, dt):
        t = dbg_out(name, shape, dt)
        b = Buf("dbg_" + name)
        S.dma("sp", t, src, R=[B_src], W=[b])
        fin_bufs.append(b)

    def dump_sbuf(name, src_ap, B_src, shape, dt):
        t = dbg_out(name, shape, dt)
        b = Buf("dbg_" + name)
        S.dma("sp", t, src_ap, R=[B_src], W=[b])
        fin_bufs.append(b)

    B_xin, B_y = Buf("x_in"), Buf("y_out")
    x_cur, B_xcur = x_in, B_xin
    if isinstance(stop_after, str) and stop_after.startswith("only:"):
        ph = stop_after[5:]
        l = 0
        if ph == "mod":
            phase_mod(0)
        elif ph == "inproj":
            with ExitStack() as wst:
                win_, B_win_ = load_win(0, wst)
                phase_inproj(0, x_in, B_xin, win_, B_win_)
        elif ph == "modin":
            with ExitStack() as wst:
                win_, B_win_ = load_win(0, wst)
                phase_mod(0)
                phase_inproj(0, x_in, B_xin, win_, B_win_)
        elif ph == "sbml":
            with ExitStack() as lay:
                Y["t"] = lay.enter_context(nc.sbuf_tensor("ymixT_L%d" % l, [128, 8, SEQ], BF16))
                g_ml = gen_mlstm(0, ML_BANKS_CO)
                next(g_ml)
                phase_sb(0, co=[g_ml])
                for _ in g_ml:
                    pass
        elif ph in ("ml", "dil", "sb", "outproj"):
            with ExitStack() as lay:
                Y["t"] = lay.enter_context(nc.sbuf_tensor("ymixT_L%d" % l, [128, 8, SEQ], BF16))
                {"ml": phase_mlstm, "dil": phase_dil, "sb": phase_sb}.get(ph, lambda l_: phase_outproj(0, x_in, B_xin, xA, B_xA))(0)
        elif ph == "moe":
            phase_moe(0, x_in, B_xin, xB, B_xB, False)
        S.barrier()
        return nc, out_tensors
    for l in range(DEPTH):
        if stop_after == "mlonly%d" % l:
            with ExitStack() as lay:
                Y["t"] = lay.enter_context(nc.sbuf_tensor("ymixT_L%d" % l, [128, 8, SEQ], BF16))
                phase_mlstm(l)
            break
        with ExitStack() as wst:
            win_, B_win_ = load_win(l, wst)
            phase_mod(l)
            phase_inproj(l, x_cur, B_xcur, win_, B_win_)
        with ExitStack() as lay:
            Y["t"] = lay.enter_context(nc.sbuf_tensor("ymixT_L%d" % l, [128, 8, SEQ], BF16))
            phase_dil(l)
            g_ml = gen_mlstm(l, ML_BANKS_CO)
            next(g_ml)
            phase_sb(l, co=[g_ml])
            for _ in g_ml:
                pass
            if stop_after == "mix%d" % l:
                dump_sbuf("ymixT", Y["t"][:].rearrange("p c t -> p (c t)"), B_ymix, [128, 8 * SEQ], BF16)
                S.wait_all("sp", fin_bufs)
                S.barrier()
                break
            phase_outproj(l, x_cur, B_xcur, xA, B_xA)
        if stop_after == "x1_%d" % l:
            dump_dram("x1", xA, B_xA, [SEQ, D], F32)
            break
        last = (l == DEPTH - 1)
        if last:
            phase_moe(l, xA, B_xA, y_out, B_y, True)
        else:
            phase_moe(l, xA, B_xA, xB, B_xB, False)
            x_cur, B_xcur = xB, B_xB
        if stop_after == "x2_%d" % l:
            dump_dram("x2", xB, B_xB, [SEQ, D], F32)
            break

    S.wait_all("sp", fin_bufs + [B_y])
    S.barrier()
    return nc, out_tensors


def prep_inputs(b, inp, consts):
    cbn, cfn = consts
    f = np.float32
    d = {
        "x": np.ascontiguousarray(inp["x"][b]),
        "c_lay": np.ascontiguousarray(inp["c"][b].reshape(8, 128).T),
        "w_in": inp["w_in"],
        "conv_w": np.ascontiguousarray(inp["conv_w"].reshape(DEPTH, 4, 2, 128).transpose(0, 3, 2, 1)),
        "conv_b": np.ascontiguousarray(inp["conv_b"].reshape(DEPTH, 2, 128).transpose(0, 2, 1)),
        "w_mq": inp["w_mq"], "w_mk": inp["w_mk"], "w_mv": inp["w_mv"],
        "gbias": np.ascontiguousarray(np.broadcast_to(inp["gate_bias"].reshape(DEPTH, 1, 8), (DEPTH, 128, 8))),
        "g_head_p": np.ascontiguousarray(inp["g_head"].reshape(DEPTH, 8, 128).transpose(0, 2, 1)),
        "g_head_r": np.ascontiguousarray(np.broadcast_to(inp["g_head"][:, None, :256], (DEPTH, 128, 256))),
        "w_out": inp["w_out"],
        "w_ada": inp["w_ada"],
        "b_ada": np.ascontiguousarray(inp["b_ada"].reshape(DEPTH, 1, 6 * D)),
        "w_router": inp["w_router"],
        "rbias": np.ascontiguousarray(np.broadcast_to(inp["router_bias"][None, :], (128, NEXP))),
        "w_gate_e": inp["w_gate_e"], "w_up_e": inp["w_up_e"], "w_down_e": inp["w_down_e"],
        "g_final_r": np.ascontiguousarray(np.broadcast_to(inp["g_final"][None, :], (128, D))),
        "cb": cbn, "cf": cfn,
    }
    return {k: np.ascontiguousarray(v, dtype=f) for k, v in d.items()}


def kernel(**inputs):
    inp = {k: np.asarray(v) for k, v in inputs.items()}
    nc, _ = build_program()
    consts = make_consts()
    in_maps = [prep_inputs(b, inp, consts) for b in range(8)]
    res = run_bass_kernel_spmd(nc, in_maps, core_ids=list(range(8)))
    return np.stack([np.asarray(r["y"]) for r in res.results], axis=0).astype(np.float32)
```
